# Optimizing a Trainium2 kernel written in Bass

```python
import jax, jax.numpy as jnp
from jax import lax
import numpy as np

D_MODEL = 2048
BATCH = 2
SEQ = 4096
DEPTH = 2

N_A_LAYERS = DEPTH // 2
N_B_LAYERS = DEPTH - N_A_LAYERS
DEEPNORM_ALPHA = (2.0 * DEPTH) ** 0.25
DEEPNORM_BETA = (8.0 * DEPTH) ** -0.25
LN_EPS = 1e-5
CONV_WIDTH = 31
N_HEADS = 16
HEAD_DIM = D_MODEL // N_HEADS
N_KV_GROUPS = 4
HEADS_PER_GROUP = N_HEADS // N_KV_GROUPS
N_KV_SLOTS = 6
CMP_BLOCK = 32
CMP_STRIDE = 16
CMP_HIDDEN = 2 * HEAD_DIM
SEL_BLOCK = 64
SEL_TOPK = 16
SEL_Q_BLOCK = 32
WINDOW = 512
WIN_Q_BLOCK = 128
N_WIN_BLOCKS = WINDOW // WIN_Q_BLOCK + 1
N_BRANCHES = 3
FORCE_SCORE = 1e6
MASK_VALUE = -1e30
N_GROUPS = 4
EXPERTS_PER_GROUP = 8
N_EXPERTS = N_GROUPS * EXPERTS_PER_GROUP
EXPERT_TOPK = 2
EXPERT_DFF = 512
MOE_CHUNK = 128

kernel_name = 'hybrid_conformer_nsa_yoco_hmoe'


def layer_norm(x, g, b):
    x32 = x.astype(jnp.float32)
    mu = jnp.mean(x32, -1, keepdims=True)
    var = jnp.mean(jnp.square(x32 - mu), -1, keepdims=True)
    y = (x32 - mu) * lax.rsqrt(var + LN_EPS) * g.astype(jnp.float32) + b.astype(jnp.float32)
    return y.astype(x.dtype)


def alibi_slopes():
    h = jnp.arange(1, N_HEADS + 1, dtype=jnp.float32)
    return (2.0 ** (-8.0 * h / N_HEADS)).reshape(N_KV_GROUPS, HEADS_PER_GROUP)


def conformer_conv(x, w_pw1, b_pw1, w_dw, b_dw, g, b, w_pw2):
    h = x @ w_pw1 + b_pw1
    a, gate = jnp.split(h, 2, axis=-1)
    h = a * jax.nn.sigmoid(gate)
    h = lax.conv_general_dilated(h, w_dw[:, None, :], window_strides=(1,),
                                 padding=((CONV_WIDTH - 1, 0),),
                                 dimension_numbers=('NWC', 'WIO', 'NWC'),
                                 feature_group_count=D_MODEL) + b_dw
    h = jax.nn.silu(layer_norm(h, g, b))
    return h @ w_pw2


def shared_kv(h, kv_w, cmp_pos, cmp_w1, cmp_w2):
    B, S, _ = h.shape
    kv = (h @ kv_w).reshape(B, S, N_KV_SLOTS, N_KV_GROUPS, HEAD_DIM)
    n_cmp = (S - CMP_BLOCK) // CMP_STRIDE + 1
    idx = jnp.arange(n_cmp)[:, None] * CMP_STRIDE + jnp.arange(CMP_BLOCK)[None, :]
    tok = jnp.stack([kv[:, :, 0], kv[:, :, 1]], 0)
    blocks = tok[:, :, idx] + cmp_pos[:, None, None, :, None, :]
    flat = blocks.transpose(0, 1, 2, 4, 3, 5).reshape(2, B, n_cmp, N_KV_GROUPS, CMP_BLOCK * HEAD_DIM)
    hid = jax.nn.gelu(jnp.einsum('cbngf,cfh->cbngh', flat, cmp_w1))
    comp = jnp.einsum('cbngh,chd->cbngd', hid, cmp_w2)
    return (comp[0], comp[1], kv[:, :, 2], kv[:, :, 3], kv[:, :, 4], kv[:, :, 5])


def compressed_branch(q, k_cmp, v_cmp, slopes):
    S = q.shape[1]
    n_cmp = k_cmp.shape[1]
    pos = jnp.arange(S)
    dist = pos[:, None] - (jnp.arange(n_cmp) * CMP_STRIDE + CMP_BLOCK - 1)[None, :]
    valid = dist >= 0
    s = jnp.einsum('bsghd,bngd->bghsn', q, k_cmp, preferred_element_type=jnp.float32)
    s = s - slopes[None, :, :, None, None] * dist.astype(jnp.float32)
    p = jax.nn.softmax(jnp.where(valid, s, MASK_VALUE), axis=-1)
    p = jnp.where(valid, p, 0.0)
    o = jnp.einsum('bghsn,bngd->bsghd', p.astype(v_cmp.dtype), v_cmp)
    return o, p


def select_blocks(p_cmp, S):
    n_cmp = p_cmp.shape[-1]
    n_sel = S // SEL_BLOCK
    c0 = jnp.arange(n_cmp)[:, None] * CMP_STRIDE
    j0 = jnp.arange(n_sel)[None, :] * SEL_BLOCK
    overlap = jnp.maximum(jnp.minimum(c0 + CMP_BLOCK, j0 + SEL_BLOCK) - jnp.maximum(c0, j0), 0)
    overlap = overlap.astype(jnp.float32) / CMP_BLOCK
    imp = jnp.einsum('bghsn,nj->bgsj', p_cmp, overlap)
    cur = (jnp.arange(S) // SEL_BLOCK)[:, None]
    blk = jnp.arange(n_sel)[None, :]
    forced = (blk == 0) | (blk == cur) | (blk == cur - 1)
    imp = jnp.where(blk > cur, -jnp.inf, imp + jnp.where(forced, FORCE_SCORE, 0.0))
    _, idx = lax.top_k(imp, min(SEL_TOPK, n_sel))
    return idx


def selected_branch(q, k_sel, v_sel, sel_idx, slopes):
    B, S = q.shape[:2]
    n_sel = S // SEL_BLOCK
    topk = sel_idx.shape[-1]
    n_qc = S // SEL_Q_BLOCK
    kb = k_sel.reshape(B, n_sel, SEL_BLOCK, N_KV_GROUPS, HEAD_DIM).transpose(0, 3, 1, 2, 4)
    vb = v_sel.reshape(B, n_sel, SEL_BLOCK, N_KV_GROUPS, HEAD_DIM).transpose(0, 3, 1, 2, 4)
    q_c = q.reshape(B, n_qc, SEL_Q_BLOCK, N_KV_GROUPS, HEADS_PER_GROUP, HEAD_DIM).transpose(1, 0, 3, 4, 2, 5)
    i_c = sel_idx.reshape(B, N_KV_GROUPS, n_qc, SEL_Q_BLOCK, topk).transpose(2, 0, 1, 3, 4)
    p_c = jnp.arange(S).reshape(n_qc, SEL_Q_BLOCK)
    bi = jnp.arange(B)[:, None, None, None]
    gi = jnp.arange(N_KV_GROUPS)[None, :, None, None]
    offs = jnp.arange(SEL_BLOCK)
    n_keys = topk * SEL_BLOCK

    def one_chunk(args):
        qc, ic, pc = args
        kg = kb[bi, gi, ic].reshape(B, N_KV_GROUPS, SEL_Q_BLOCK, n_keys, HEAD_DIM)
        vg = vb[bi, gi, ic].reshape(B, N_KV_GROUPS, SEL_Q_BLOCK, n_keys, HEAD_DIM)
        kpos = (ic[..., None] * SEL_BLOCK + offs).reshape(B, N_KV_GROUPS, SEL_Q_BLOCK, n_keys)
        dist = (pc[None, None, :, None] - kpos)[:, :, None]
        s = jnp.einsum('bghqd,bgqkd->bghqk', qc, kg, preferred_element_type=jnp.float32)
        s = s - slopes[None, :, :, None, None] * dist.astype(jnp.float32)
        p = jax.nn.softmax(jnp.where(dist >= 0, s, MASK_VALUE), axis=-1)
        return jnp.einsum('bghqk,bgqkd->bghqd', p.astype(vg.dtype), vg)

    o = lax.map(one_chunk, (q_c, i_c, p_c))
    return o.transpose(1, 0, 4, 2, 3, 5).reshape(B, S, N_KV_GROUPS, HEADS_PER_GROUP, HEAD_DIM)


def window_branch(q, k_win, v_win, slopes):
    B, S = q.shape[:2]
    nqb = S // WIN_Q_BLOCK

    def band(t):
        tb = t.reshape(B, nqb, WIN_Q_BLOCK, N_KV_GROUPS, HEAD_DIM)
        tp = jnp.pad(tb, ((0, 0), (N_WIN_BLOCKS - 1, 0), (0, 0), (0, 0), (0, 0)))
        return jnp.concatenate([tp[:, j:j + nqb] for j in range(N_WIN_BLOCKS)], axis=2)

    kband = band(k_win)
    vband = band(v_win)
    qpos = jnp.arange(S).reshape(nqb, WIN_Q_BLOCK)
    kpos = (jnp.arange(nqb)[:, None] - (N_WIN_BLOCKS - 1)) * WIN_Q_BLOCK + jnp.arange(N_WIN_BLOCKS * WIN_Q_BLOCK)[None, :]
    dist = qpos[:, :, None] - kpos[:, None, :]
    valid = (dist >= 0) & (dist < WINDOW) & (kpos[:, None, :] >= 0)
    qw = q.reshape(B, nqb, WIN_Q_BLOCK, N_KV_GROUPS, HEADS_PER_GROUP, HEAD_DIM)
    s = jnp.einsum('bnqghd,bnkgd->bnghqk', qw, kband, preferred_element_type=jnp.float32)
    s = s - slopes[None, None, :, :, None, None] * dist[None, :, None, None].astype(jnp.float32)
    p = jax.nn.softmax(jnp.where(valid[None, :, None, None], s, MASK_VALUE), axis=-1)
    o = jnp.einsum('bnghqk,bnkgd->bnqghd', p.astype(vband.dtype), vband)
    return o.reshape(B, S, N_KV_GROUPS, HEADS_PER_GROUP, HEAD_DIM)


def nsa_layer(x, w_qg, w_o, k_cmp, v_cmp, k_sel, v_sel, k_win, v_win):
    B, S, _ = x.shape
    hd = N_HEADS * HEAD_DIM
    slopes = alibi_slopes()
    qg = x @ w_qg
    q = (qg[..., :hd] * HEAD_DIM ** -0.5).reshape(B, S, N_KV_GROUPS, HEADS_PER_GROUP, HEAD_DIM)
    gates = jax.nn.sigmoid(qg[..., hd:].astype(jnp.float32)).reshape(B, S, N_KV_GROUPS, HEADS_PER_GROUP, N_BRANCHES)
    o_cmp, p_cmp = compressed_branch(q, k_cmp, v_cmp, slopes)
    sel_idx = select_blocks(p_cmp, S)
    o_sel = selected_branch(q, k_sel, v_sel, sel_idx, slopes)
    o_win = window_branch(q, k_win, v_win, slopes)
    o = gates[..., 0:1] * o_cmp + gates[..., 1:2] * o_sel + gates[..., 2:3] * o_win
    return o.astype(x.dtype).reshape(B, S, hd) @ w_o


def hierarchical_moe(x, wg, bg, we, be, w_gu, w_down):
    B, S, D = x.shape
    T = B * S
    xt = x.reshape(T, D)
    lg = (xt @ wg).astype(jnp.float32) + bg
    pg = jax.nn.softmax(lg, axis=-1)
    grp = jnp.argmax(lg, axis=-1)
    gw = jnp.take_along_axis(pg, grp[:, None], axis=-1)[:, 0]
    le = ((xt @ we).astype(jnp.float32) + be).reshape(T, N_GROUPS, EXPERTS_PER_GROUP)
    le = jnp.take_along_axis(le, grp[:, None, None], axis=1)[:, 0]
    top_v, top_i = lax.top_k(le, EXPERT_TOPK)
    pw = jax.nn.softmax(top_v, axis=-1) * gw[:, None]
    eid = (grp[:, None] * EXPERTS_PER_GROUP + top_i).reshape(-1)
    tok = jnp.repeat(jnp.arange(T), EXPERT_TOPK)
    wts = pw.reshape(-1)
    A = T * EXPERT_TOPK
    order = jnp.argsort(eid)
    e_s, tok_s, w_s = eid[order], tok[order], wts[order]
    counts = jax.ops.segment_sum(jnp.ones((A,), jnp.int32), eid, num_segments=N_EXPERTS)
    padded = (counts + MOE_CHUNK - 1) // MOE_CHUNK * MOE_CHUNK
    pad_end = jnp.cumsum(padded)
    pad_start = pad_end - padded
    start = jnp.cumsum(counts) - counts
    dest = pad_start[e_s] + jnp.arange(A) - start[e_s]
    n_chunks = (A + N_EXPERTS * (MOE_CHUNK - 1) + MOE_CHUNK - 1) // MOE_CHUNK
    buf = jnp.zeros((n_chunks * MOE_CHUNK, D), x.dtype).at[dest].set(xt[tok_s])
    chunk_e = jnp.clip(jnp.searchsorted(pad_end, jnp.arange(n_chunks) * MOE_CHUNK, side='right'), 0, N_EXPERTS - 1)

    def expert_chunk(args):
        xc, e = args
        g, u = jnp.split(xc @ w_gu[e], 2, axis=-1)
        return (jax.nn.silu(g) * u) @ w_down[e]

    out = lax.map(expert_chunk, (buf.reshape(n_chunks, MOE_CHUNK, D), chunk_e)).reshape(-1, D)
    y = jnp.zeros((T, D), x.dtype).at[tok_s].add(out[dest] * w_s[:, None].astype(x.dtype))
    return y.reshape(B, S, D)


def setup_inputs(seed: int = 0) -> dict:
    key = jax.random.key(seed)
    ks = jax.random.split(key, 24)
    f32 = jnp.float32
    D, G, dh, F, E = D_MODEL, N_KV_GROUPS, HEAD_DIM, EXPERT_DFF, N_EXPERTS
    nrm = lambda k, shape, scale: jax.random.normal(k, shape, f32) * scale
    branch_scale = jnp.array([1.0, DEEPNORM_BETA, 1.0, DEEPNORM_BETA, 1.0, DEEPNORM_BETA], f32)
    kv_w = (nrm(ks[8], (D, N_KV_SLOTS, G * dh), D ** -0.5) * branch_scale[None, :, None]).reshape(D, N_KV_SLOTS * G * dh)
    return {
        'x': nrm(ks[0], (BATCH, SEQ, D), 1.0),
        'conv_w_pw1': nrm(ks[1], (N_A_LAYERS, D, 2 * D), D ** -0.5),
        'conv_b_pw1': nrm(ks[2], (N_A_LAYERS, 2 * D), 0.02),
        'conv_w_dw': nrm(ks[3], (N_A_LAYERS, CONV_WIDTH, D), CONV_WIDTH ** -0.5),
        'conv_b_dw': nrm(ks[4], (N_A_LAYERS, D), 0.02),
        'conv_ln_g': 1.0 + nrm(ks[5], (N_A_LAYERS, D), 0.02),
        'conv_ln_b': nrm(ks[6], (N_A_LAYERS, D), 0.02),
        'conv_w_pw2': nrm(ks[7], (N_A_LAYERS, D, D), D ** -0.5 * DEEPNORM_BETA),
        'kv_w': kv_w,
        'cmp_pos': nrm(ks[9], (2, CMP_BLOCK, dh), 0.02),
        'cmp_w1': nrm(ks[10], (2, CMP_BLOCK * dh, CMP_HIDDEN), (CMP_BLOCK * dh) ** -0.5),
        'cmp_w2': nrm(ks[11], (2, CMP_HIDDEN, dh), CMP_HIDDEN ** -0.5),
        'nsa_w_qg': nrm(ks[12], (N_B_LAYERS, D, N_HEADS * dh + N_BRANCHES * N_HEADS), D ** -0.5),
        'nsa_w_o': nrm(ks[13], (N_B_LAYERS, N_HEADS * dh, D), (N_HEADS * dh) ** -0.5 * DEEPNORM_BETA),
        'moe_wg': nrm(ks[14], (DEPTH, D, N_GROUPS), D ** -0.5),
        'moe_bg': nrm(ks[15], (DEPTH, N_GROUPS), 0.01),
        'moe_we': nrm(ks[16], (DEPTH, D, E), D ** -0.5),
        'moe_be': nrm(ks[17], (DEPTH, E), 0.01),
        'moe_w_gu': nrm(ks[18], (DEPTH, E, D, 2 * F), D ** -0.5),
        'moe_w_down': nrm(ks[19], (DEPTH, E, F, D), F ** -0.5 * DEEPNORM_BETA),
        'ln_g': 1.0 + nrm(ks[20], (DEPTH, 2, D), 0.02),
        'ln_b': nrm(ks[21], (DEPTH, 2, D), 0.02),
    }


def reference(x, conv_w_pw1, conv_b_pw1, conv_w_dw, conv_b_dw, conv_ln_g, conv_ln_b, conv_w_pw2,
              kv_w, cmp_pos, cmp_w1, cmp_w2, nsa_w_qg, nsa_w_o,
              moe_wg, moe_bg, moe_we, moe_be, moe_w_gu, moe_w_down, ln_g, ln_b):
    h = x
    kvs = None
    for l in range(DEPTH):
        if l < N_A_LAYERS:
            mix = conformer_conv(h, conv_w_pw1[l], conv_b_pw1[l], conv_w_dw[l], conv_b_dw[l],
                                 conv_ln_g[l], conv_ln_b[l], conv_w_pw2[l])
        else:
            if l == N_A_LAYERS:
                kvs = shared_kv(h, kv_w, cmp_pos, cmp_w1, cmp_w2)
            j = l - N_A_LAYERS
            mix = nsa_layer(h, nsa_w_qg[j], nsa_w_o[j], *kvs)
        h = layer_norm(DEEPNORM_ALPHA * h + mix, ln_g[l, 0], ln_b[l, 0])
        ffn = hierarchical_moe(h, moe_wg[l], moe_bg[l], moe_we[l], moe_be[l], moe_w_gu[l], moe_w_down[l])
        h = layer_norm(DEEPNORM_ALPHA * h + ffn, ln_g[l, 1], ln_b[l, 1])
    return h
```

```python
import numpy as np
from contextlib import ExitStack, contextmanager
import concourse.bass as bass
import concourse.mybir as mybir
from concourse.bass_utils import run_bass_kernel_spmd

F32, BF16, I32 = mybir.dt.float32, mybir.dt.bfloat16, mybir.dt.int32
AF = mybir.ActivationFunctionType
ALU = mybir.AluOpType
AX = mybir.AxisListType

D = 2048
NCORE = 8
SEQ = 4096
NT = 1024
TT = NT // 128
CT = D // 128
ALPHA = 4.0 ** 0.25
LN_EPS = 1e-5
NEXP = 32
DFF = 512
NEG = -30000.0


class Trk:
    __slots__ = ("w", "r", "dsem", "dcnt", "name")

    def __init__(self, name=""):
        self.w = []
        self.r = {}
        self.dsem = None
        self.dcnt = 0
        self.name = name


class Eng:
    def __init__(self, name, h):
        self.name, self.h = name, h
        self.sem = None
        self.cnt = 0
        self.known = {}
        self.pending = False


class K:
    SEM_ROLL = 30000

    def __init__(self, nc):
        self.nc = nc
        self.es = ExitStack()
        self.stacks = [self.es]
        self.eng = {}
        self.nsem = 0
        for name, h in (("pe", nc.tensor), ("act", nc.scalar), ("dve", nc.vector),
                        ("pool", nc.gpsimd), ("sp", nc.sync)):
            e = Eng(name, h)
            e.sem = self.sem("e_" + name)
            self.eng[name] = e
        self.dmas = []
        self.uid = 0
        self.sem_free = []
        self.scope_trks = [[]]

    def sem(self, name):
        self.nsem += 1
        return self.es.enter_context(self.nc.semaphore(f"{name}_{self.nsem}"))

    def sb(self, name, shape, dt):
        self.uid += 1
        return self.stacks[-1].enter_context(self.nc.sbuf_tensor(f"{name}_{self.uid}", list(shape), dt))

    def ps(self, name, shape, dt=F32):
        self.uid += 1
        return self.stacks[-1].enter_context(self.nc.psum_tensor(f"{name}_{self.uid}", list(shape), dt))

    @contextmanager
    def scope(self):
        st = ExitStack()
        self.stacks.append(st)
        self.scope_trks.append([])
        try:
            yield
        finally:
            self.barrier()
            self.stacks.pop()
            st.close()
            for S in self.scope_trks.pop():
                self.sem_free.append((S.dsem, S.dcnt))
                S.dsem = None
                S.w = []
                S.r = {}

    def _dsem(self, S):
        if S.dsem is None:
            if self.sem_free:
                S.dsem, S.dcnt = self.sem_free.pop()
            else:
                S.dsem = self.sem("d_" + S.name)
                S.dcnt = 0
            self.scope_trks[-1].append(S)

    def close(self):
        self.es.close()

    def _emit_waits(self, E, waits, skip_own=False):
        for sem, val in waits.values():
            if sem is E.sem:
                if skip_own or E.name == "pe":
                    continue
                assert val <= E.cnt, (E.name, val, E.cnt)
            if E.known.get(id(sem), 0) >= val:
                continue
            E.h.wait_ge(sem, val)
            E.known[id(sem)] = val

    @staticmethod
    def _need(waits, mark):
        sem, val = mark
        cur = waits.get(id(sem))
        if cur is None or cur[1] < val:
            waits[id(sem)] = (sem, val)

    def _collect(self, E, R, W):
        waits = {}
        for t in R:
            for m in t.w:
                self._need(waits, m)
        for t in W:
            for m in t.w:
                self._need(waits, m)
            for m in t.r.values():
                if m[0] is E.sem:
                    continue
                self._need(waits, m)
        return waits

    def _roll(self, E):
        if E.cnt >= self.SEM_ROLL and not E.pending:
            E.sem = self.sem("e_" + E.name)
            E.cnt = 0

    def op(self, en, fn, R=(), W=(), inc=True):
        E = self.eng[en]
        self._roll(E)
        self._emit_waits(E, self._collect(E, R, W))
        ins = fn()
        if inc:
            E.cnt += 1
            ins.then_inc(E.sem, 1)
            mark = (E.sem, E.cnt)
            E.pending = False
        else:
            mark = (E.sem, E.cnt + 1)
            E.pending = True
        for t in R:
            cur = t.r.get(id(mark[0]))
            if cur is None or cur[1] < mark[1]:
                t.r[id(mark[0])] = mark
        for t in W:
            t.w = [mark]
            t.r = {}
        return ins

    def dma(self, q, out, in_, S, R=(), W=(), join=False, background=False, **kw):
        E = self.eng[q]
        had = S.dsem is not None
        self._dsem(S)
        if background and not had:
            self.scope_trks[-1].remove(S)
        waits = {}
        for t in R:
            for m in t.w:
                self._need(waits, m)
        for t in W:
            if not join:
                for m in t.w:
                    self._need(waits, m)
            for m in t.r.values():
                self._need(waits, m)
        self._emit_waits(E, waits, skip_own=True)
        ins = E.h.dma_start(out=out, in_=in_, **kw)
        S.dcnt += 16
        ins.then_inc(S.dsem, 16)
        mark = (S.dsem, S.dcnt)
        for t in R:
            t.r[id(mark[0])] = mark
        for t in W:
            if join:
                t.w = [m for m in t.w if m[0] is not S.dsem] + [mark]
            else:
                t.w = [mark]
            t.r = {}
        if not background:
            self.dmas.append(mark)
        return ins

    def release(self, trks):
        for S in trks:
            if S.dsem is not None:
                self.sem_free.append((S.dsem, S.dcnt))
                S.dsem = None
                S.w = []
                S.r = {}

    def gather(self, out, in_, idx, S, R=(), W=(), bounds=None):
        E = self.eng["pool"]
        self._dsem(S)
        waits = {}
        for t in R:
            for m in t.w:
                self._need(waits, m)
        for t in W:
            for m in t.w:
                self._need(waits, m)
            for m in t.r.values():
                self._need(waits, m)
        self._emit_waits(E, waits, skip_own=True)
        kw = {}
        if bounds is not None:
            kw = dict(bounds_check=bounds, oob_is_err=False)
        ins = E.h.indirect_dma_start(out=out, out_offset=None, in_=in_,
                                     in_offset=bass.IndirectOffsetOnAxis(ap=idx, axis=0), **kw)
        S.dcnt += 16
        ins.then_inc(S.dsem, 16)
        mark = (S.dsem, S.dcnt)
        for t in R:
            t.r[id(mark[0])] = mark
        for t in W:
            t.w = [mark]
            t.r = {}
        self.dmas.append(mark)
        return ins

    def collective(self, kind, groups, in_ap, out_ap, S, R=(), W=()):
        E = self.eng["pool"]
        self._dsem(S)
        waits = {}
        for t in R:
            for m in t.w:
                self._need(waits, m)
        for t in W:
            for m in t.w:
                self._need(waits, m)
            for m in t.r.values():
                self._need(waits, m)
        self._emit_waits(E, waits, skip_own=True)
        ins = E.h.collective_compute(kind, ALU.bypass, replica_groups=groups, ins=[in_ap.opt()], outs=[out_ap.opt()])
        S.dcnt += 1
        ins.then_inc(S.dsem, 1)
        mark = (S.dsem, S.dcnt)
        for t in R:
            t.r[id(mark[0])] = mark
        for t in W:
            t.w = [mark]
            t.r = {}
        self.dmas.append(mark)
        return ins

    def barrier(self):
        marks = []
        for F in self.eng.values():
            assert not F.pending, F.name
            if F.cnt:
                marks.append((F.sem, F.cnt))
        seen = {}
        for m in self.dmas:
            self._need(seen, m)
        marks += list(seen.values())
        self.dmas = []
        for E in self.eng.values():
            for sem, val in marks:
                if sem is E.sem:
                    continue
                if E.known.get(id(sem), 0) >= val:
                    continue
                E.h.wait_ge(sem, val)
                E.known[id(sem)] = val

    def mm(self, out, lhsT, rhs, start, stop, R=(), W=(), inc=None, **kw):
        if inc is None:
            inc = stop
        return self.op("pe", lambda: self.nc.tensor.matmul(out, lhsT, rhs, start=start, stop=stop, **kw),
                       R=R, W=W, inc=inc)

    def tr(self, out, in_, ident, R=(), W=(), inc=True):
        return self.op("pe", lambda: self.nc.tensor.transpose(out, in_, ident), R=R, W=W, inc=inc)

    def act(self, out, in_, func, R=(), W=(), **kw):
        return self.op("act", lambda: self.nc.scalar.activation(out=out, in_=in_, func=func, **kw), R=R, W=W)

    def v(self, en, name, *a, R=(), W=(), **kw):
        h = self.eng[en].h
        return self.op(en, lambda: getattr(h, name)(*a, **kw), R=R, W=W)


NC32 = 544


def make_consts():
    c = np.zeros((128, NC32), np.float32)
    c[:, 0:128] = np.eye(128)
    c[:, 128:256] = np.arange(128)[None, :]
    c[:, 256:384] = (np.arange(128)[:, None] < np.arange(128)[None, :])
    c[:, 384:512] = 1.0
    c[:, 512:544] = (np.arange(32) * 128)[None, :]
    return c


def load_consts(k, cst_d):
    c32 = k.sb("c32", [128, NC32], F32)
    tc = Trk("c32")
    k.dma("sp", c32[:], cst_d, tc, W=[tc])
    cbf = k.sb("cbf", [128, 512], BF16)
    tcb = Trk("cbf")
    k.v("dve", "tensor_copy", cbf[:], c32[:, 0:512], R=[tc], W=[tcb])
    return dict(ident=c32[:, 0:128], iota=c32[:, 128:256], ebase=c32[:, 512:544], ones=c32[:, 384:512],
                ident_bf=cbf[:, 0:128], tri_bf=cbf[:, 256:384], ones_bf=cbf[:, 384:512], t32=tc, tbf=tcb)


def pb(ap_row):
    return ap_row.partition_broadcast(128)


def emit_ln_tok(k, x_ap, tx, G, B, tgb, st, tst):
    nc = k.nc
    for i in range(4):
        k.v("dve", "bn_stats", st[:, i * 6:(i + 1) * 6], x_ap[:, i * 512:(i + 1) * 512], R=[tx, tst], W=[tst])
    k.v("dve", "bn_aggr", st[:, 24:26], st[:, 0:24], R=[tst], W=[tst])
    k.act(st[:, 26:27], st[:, 25:26], AF.Sqrt, bias=LN_EPS, scale=1.0, R=[tst], W=[tst])
    k.v("dve", "reciprocal", st[:, 27:28], st[:, 26:27], R=[tst], W=[tst])
    k.v("dve", "scalar_tensor_tensor", st[:, 28:29], st[:, 24:25], -1.0, st[:, 27:28], ALU.mult, ALU.mult,
        R=[tst], W=[tst])
    k.v("dve", "tensor_scalar", x_ap, x_ap, st[:, 27:28], st[:, 28:29], ALU.mult, ALU.add, R=[tx, tst], W=[tx])
    k.v("dve", "tensor_tensor", x_ap, x_ap, G, ALU.mult, R=[tx, tgb], W=[tx])
    k.v("pool", "tensor_tensor", x_ap, x_ap, B, ALU.add, R=[tx, tgb], W=[tx])


def emit_moe(k, C, h_tok, th, wg_d, bg_d, we_d, be_d, wgu_d, wdn_d, ybuf, lng_d, lnb_d, nexp=NEXP, wconv=None):
    nc = k.nc
    rankm = k.sb("rankm", [128, TT, 32], F32)
    t_rank = [Trk(f"rank{i}") for i in range(TT)]
    dest = k.sb("dest", [128, TT, 2], I32)
    wts = k.sb("wts", [128, TT, 2], F32)
    t_dw = [Trk(f"dw{i}") for i in range(TT)]
    h_bf = k.sb("h_bf", [128, TT, D], BF16)
    t_hbf = [Trk(f"hbf{i}") for i in range(TT)]

    with k.scope():
        Wr = k.sb("Wr", [128, CT, 36], F32)
        tWr = Trk("Wr")
        k.dma("sp", Wr[:, :, 0:4], wg_d.rearrange("(ct p) n -> p ct n", p=128), tWr, W=[tWr])
        k.dma("sp", Wr[:, :, 4:36], we_d.rearrange("(ct p) n -> p ct n", p=128), tWr, W=[tWr], join=True)
        rb = k.sb("rb", [128, 36], F32)
        trb = Trk("rb")
        k.dma("sp", rb[:, 0:4], pb(bg_d), trb, W=[trb])
        k.dma("sp", rb[:, 4:36], pb(be_d), trb, W=[trb], join=True)
        hTt = [k.sb("hTt", [128, CT, 128], F32) for _ in range(2)]
        thT = [[Trk(f"hTt{a}{b}") for b in range(4)] for a in range(2)]
        psA = [k.ps("psA", [128, 512]) for _ in range(2)]
        tpsA = [Trk("psA0"), Trk("psA1")]
        psL = k.ps("psL", [128, 512])
        tpsL = Trk("psL")
        psR = k.ps("psR", [128, 512])
        tpsR = Trk("psR")
        L = k.sb("L", [128, TT, 36], F32)
        sm = k.sb("sm", [128, TT, 16], F32)
        ohg = k.sb("ohg", [128, TT, 4], F32)
        r8 = k.sb("r8", [128, TT, 5, 8], F32)
        r32 = k.sb("r32", [128, TT, 5, 32], F32)
        d12 = k.sb("d12", [128, TT, 2], F32)
        M_bf = k.sb("M_bf", [128, TT, 32], BF16)
        tM = [Trk(f"M{i}") for i in range(TT)]
        ts = [Trk(f"rs{i}") for i in range(TT)]
        nq = 0
        for tt in range(TT):
            hb = tt % 2
            k.act(h_bf[:, tt, :], h_tok[:, tt, :], AF.Copy, R=[th[tt]], W=[t_hbf[tt]])
            for q in range(4):
                pa = nq % 2
                nq += 1
                for i in range(4):
                    ct = q * 4 + i
                    k.tr(psA[pa][:, i * 128:(i + 1) * 128], h_tok[:, tt, ct * 128:(ct + 1) * 128], C["ident"],
                         R=[th[tt], C["t32"]], W=[tpsA[pa]], inc=(i == 3))
                dst = hTt[hb][:, q * 4:(q + 1) * 4, :]
                if q % 2 == 0:
                    k.act(dst, psA[pa][:, :].rearrange("p (a b) -> p a b", a=4), AF.Copy, R=[tpsA[pa]], W=[thT[hb][q]])
                else:
                    k.v("dve", "tensor_copy", dst, psA[pa][:, :].rearrange("p (a b) -> p a b", a=4),
                        R=[tpsA[pa]], W=[thT[hb][q]])
            for ct in range(CT):
                k.mm(psL[:, 0:36], hTt[hb][:, ct, :], Wr[:, ct, :], ct == 0, ct == CT - 1,
                     R=[thT[hb][ct // 4], tWr], W=[tpsL])
            S = ts[tt]
            sc = lambda j: sm[:, tt, j:j + 1]
            Lt = L[:, tt, :]
            les, eq1, le2, eq2, e12 = (r8[:, tt, j, :] for j in range(5))
            Mf, E1, E2, rk, tmp = (r32[:, tt, j, :] for j in range(5))
            dv = lambda name, *a, R=(), W=(): k.v("dve", name, *a, R=[S] + list(R), W=[S] + list(W))
            dv("tensor_tensor", Lt, psL[:, 0:36], rb[:], ALU.add, R=[tpsL, trb])
            dv("reduce_max", sc(0), Lt[:, 0:4], AX.X)
            dv("tensor_scalar", ohg[:, tt, :], Lt[:, 0:4], sc(0), None, ALU.is_equal)
            dv("tensor_scalar", sc(1), sc(0), -1.0, None, ALU.mult)
            k.act(le2[:, 0:4], Lt[:, 0:4], AF.Exp,
                  bias=sc(1), scale=1.0, accum_out=sc(2), R=[S], W=[S])
            dv("reciprocal", sc(3), sc(2))
            dv("tensor_scalar", les, Lt[:, 4:12], ohg[:, tt, 0:1], None, ALU.mult)
            for g in range(1, 4):
                dv("scalar_tensor_tensor", les, Lt[:, 4 + 8 * g:12 + 8 * g], ohg[:, tt, g:g + 1], les,
                   ALU.mult, ALU.add)
            dv("reduce_max", sc(4), les, AX.X)
            dv("tensor_scalar", eq1, les, sc(4), None, ALU.is_equal)
            dv("scalar_tensor_tensor", le2, eq1, -1e30, les, ALU.mult, ALU.add)
            dv("reduce_max", sc(5), le2, AX.X)
            dv("tensor_scalar", eq2, le2, sc(5), None, ALU.is_equal)
            dv("tensor_tensor", sc(6), sc(5), sc(4), ALU.subtract)
            k.act(sc(7), sc(6), AF.Exp, R=[S], W=[S])
            dv("tensor_scalar", sc(8), sc(7), 1.0, None, ALU.add)
            dv("reciprocal", sc(9), sc(8))
            dv("tensor_tensor", wts[:, tt, 0:1], sc(9), sc(3), ALU.mult, W=[t_dw[tt]])
            dv("tensor_tensor", wts[:, tt, 1:2], sc(3), wts[:, tt, 0:1], ALU.subtract, W=[t_dw[tt]])
            dv("tensor_tensor", e12, eq1, eq2, ALU.add)
            for g in range(4):
                gs = slice(8 * g, 8 * g + 8)
                dv("tensor_scalar", Mf[:, gs], e12, ohg[:, tt, g:g + 1], None, ALU.mult)
                dv("tensor_scalar", E1[:, gs], eq1, ohg[:, tt, g:g + 1], None, ALU.mult)
                dv("tensor_scalar", E2[:, gs], eq2, ohg[:, tt, g:g + 1], None, ALU.mult)
            dv("tensor_copy", M_bf[:, tt, :], Mf, W=[tM[tt]])
            for t2 in range(tt + 1):
                lhsT = C["ones_bf"] if t2 < tt else C["tri_bf"]
                k.mm(psR[:, 0:32], lhsT, M_bf[:, t2, :], t2 == 0, t2 == tt, R=[tM[t2], C["tbf"]], W=[tpsR])
            dv("scalar_tensor_tensor", rankm[:, tt, :], psR[:, 0:32], 1.0, Mf, ALU.add, ALU.mult,
               R=[tpsR], W=[t_rank[tt]])
            dv("tensor_scalar", rankm[:, tt, :], rankm[:, tt, :], -1.0, None, ALU.add, W=[t_rank[tt]])
            dv("tensor_tensor", rk, psR[:, 0:32], C["ebase"], ALU.add, R=[tpsR, C["t32"]])
            for j, Ej in enumerate((E1, E2)):
                dv("tensor_tensor", tmp, rk, Ej, ALU.mult)
                dv("reduce_sum", d12[:, tt, j:j + 1], tmp, AX.X)
            dv("tensor_copy", dest[:, tt, :], d12[:, tt, :], W=[t_dw[tt]])

    with k.scope():
        NS = 4
        wsl = [k.sb("wsl", [128, 8192], BF16) for _ in range(NS)]
        twsl = [Trk(f"wsl{i}") for i in range(NS)]
        selq = k.sb("selq", [128, TT, 512], BF16)
        tsel = [Trk(f"selq{i}") for i in range(TT)]
        xq = k.sb("xq", [128, CT, 512], BF16)
        txq = [Trk(f"xq{i}") for i in range(CT)]
        sg = k.sb("sg", [128, 512], F32)
        tsg = Trk("sg")
        hT = k.sb("hT", [128, 512], BF16)
        thh = Trk("hT")
        ye = k.sb("ye", [128, D], BF16)
        tye = [Trk(f"ye{i}") for i in range(4)]
        psG = [k.ps("psG", [128, 512]) for _ in range(2)]
        tpsG = [Trk("psG0"), Trk("psG1")]
        psH1 = k.ps("psH1", [128, 512])
        tH1 = Trk("psH1")
        psH2 = k.ps("psH2", [128, 512])
        tH2 = Trk("psH2")
        psY = [k.ps("psY", [128, 512]) for _ in range(4)]
        tpsY = [Trk(f"psY{i}") for i in range(4)]
        tyb = Trk("ybuf")

        chunks = [(e, c) for e in range(nexp) for c in "gud"]

        def issue(ci):
            if ci >= len(chunks):
                return
            e, c = chunks[ci]
            s = ci % NS
            if wconv is not None and e < wconv.nconv:
                if c == "d":
                    k.dma("sp", wsl[s][:, :].rearrange("p (a n) -> p a n", a=4),
                          wconv.wdn_b[e].rearrange("(a p) n -> p a n", p=128), twsl[s], R=[wconv.trk[e]], W=[twsl[s]])
                else:
                    c0 = 0 if c == "g" else 512
                    k.dma("sp", wsl[s][:, :].rearrange("p (a n) -> p a n", a=16),
                          wconv.wgu_b[e].rearrange("(a p) n -> p a n", p=128)[:, :, c0:c0 + 512], twsl[s],
                          R=[wconv.trk[e]], W=[twsl[s]])
                return
            if c == "d":
                k.dma("pool", wsl[s][:, :].rearrange("p (a n) -> p a n", a=4),
                      wdn_d[e].rearrange("(a p) n -> p a n", p=128), twsl[s], W=[twsl[s]])
            else:
                c0 = 0 if c == "g" else 512
                k.dma("pool", wsl[s][:, :].rearrange("p (a n) -> p a n", a=16),
                      wgu_d[e].rearrange("(a p) n -> p a n", p=128)[:, :, c0:c0 + 512], twsl[s], W=[twsl[s]])

        for ci in range(NS - 1):
            issue(ci)
        ci = 0
        nev = 0
        for q0 in range(0, nexp, 4):
            for tt in range(TT):
                for j in range(4):
                    e = q0 + j
                    k.v("dve", "tensor_scalar", selq[:, tt, j * 128:(j + 1) * 128], C["iota"],
                        rankm[:, tt, e:e + 1], None, ALU.is_equal, R=[t_rank[tt], C["t32"]], W=[tsel[tt]])
            for ct in range(CT):
                pg = ct % 2
                for tt in range(TT):
                    k.mm(psG[pg][:, :], h_bf[:, tt, ct * 128:(ct + 1) * 128], selq[:, tt, :], tt == 0, tt == TT - 1,
                         R=[t_hbf[tt], tsel[tt]], W=[tpsG[pg]])
                if ct % 2 == 0:
                    k.act(xq[:, ct, :], psG[pg][:, :], AF.Copy, R=[tpsG[pg]], W=[txq[ct]])
                else:
                    k.v("dve", "tensor_copy", xq[:, ct, :], psG[pg][:, :], R=[tpsG[pg]], W=[txq[ct]])
            for j in range(4):
                e = q0 + j
                for half, (psH, tH) in enumerate(((psH1, tH1), (psH2, tH2))):
                    s = ci % NS
                    issue(ci + NS - 1)
                    wv = wsl[s][:, :].rearrange("p (a n) -> p a n", a=16)
                    for ft in range(4):
                        for ct in range(CT):
                            k.mm(psH[:, ft * 128:(ft + 1) * 128], wv[:, ct, ft * 128:(ft + 1) * 128],
                                 xq[:, ct, j * 128:(j + 1) * 128], ct == 0, ct == CT - 1,
                                 R=[twsl[s], txq[ct]], W=[tH])
                    ci += 1
                k.act(sg[:], psH1[:, :], AF.Silu, R=[tH1], W=[tsg])
                k.v("dve", "tensor_tensor", hT[:], sg[:], psH2[:, :], ALU.mult, R=[tsg, tH2], W=[thh])
                s = ci % NS
                issue(ci + NS - 1)
                wv = wsl[s][:, :].rearrange("p (a n) -> p a n", a=4)
                for cc in range(4):
                    for ft in range(4):
                        k.mm(psY[cc][:, :], hT[:, ft * 128:(ft + 1) * 128], wv[:, ft, cc * 512:(cc + 1) * 512],
                             ft == 0, ft == 3, R=[thh, twsl[s]], W=[tpsY[cc]])
                    if nev % 2 == 0:
                        k.act(ye[:, cc * 512:(cc + 1) * 512], psY[cc][:, :], AF.Copy, R=[tpsY[cc]], W=[tye[cc]])
                    else:
                        k.v("dve", "tensor_copy", ye[:, cc * 512:(cc + 1) * 512], psY[cc][:, :],
                            R=[tpsY[cc]], W=[tye[cc]])
                    nev += 1
                ci += 1
                k.dma("sp", ybuf[e * 128:(e + 1) * 128, :], ye[:], tyb, R=tye, W=[tyb], join=True)

    with k.scope():
        G = k.sb("lnG", [128, D], F32)
        B = k.sb("lnB", [128, D], F32)
        tgb = Trk("lngb")
        k.dma("sp", G[:], pb(lng_d), tgb, W=[tgb])
        k.dma("sp", B[:], pb(lnb_d), tgb, W=[tgb], join=True)
        Y = [[k.sb("Yg", [128, D], BF16) for _ in range(2)] for _ in range(2)]
        tY = [[Trk(f"Y{a}{b}") for b in range(2)] for a in range(2)]
        st = k.sb("lnst", [128, TT, 40], F32)
        tst = [Trk(f"lnst{i}") for i in range(TT)]
        def gat(tt):
            p = tt % 2
            for j in range(2):
                k.gather(Y[p][j][:], ybuf[:, :], dest[:, tt, j:j + 1], tY[p][j], R=[tyb, t_dw[tt]], W=[tY[p][j]])

        gat(0)
        for tt in range(TT):
            p = tt % 2
            if tt + 1 < TT:
                gat(tt + 1)
            x_ap = h_tok[:, tt, :]
            k.op("act", lambda: nc.scalar.mul(x_ap, x_ap, ALPHA), R=[th[tt]], W=[th[tt]])
            k.v("dve", "scalar_tensor_tensor", x_ap, Y[p][0][:], wts[:, tt, 0:1], x_ap, ALU.mult, ALU.add,
                R=[tY[p][0], t_dw[tt], th[tt]], W=[th[tt]])
            k.v("dve", "scalar_tensor_tensor", x_ap, Y[p][1][:], wts[:, tt, 1:2], x_ap, ALU.mult, ALU.add,
                R=[tY[p][1], t_dw[tt], th[tt]], W=[th[tt]])
            emit_ln_tok(k, x_ap, th[tt], G[:], B[:], tgb, st[:, tt, :], tst[tt])


def emit_proj_res_ln(k, aT, taT, W_d, h_tok, th, lng_d, lnb_d, tts=None):
    nc = k.nc
    if tts is None:
        tts = list(range(TT))
    with k.scope():
        Wc = [k.sb("Wc", [128, CT, 512], BF16) for _ in range(2)]
        tWc = [Trk("Wc0"), Trk("Wc1")]
        G = k.sb("lnG", [128, D], F32)
        B = k.sb("lnB", [128, D], F32)
        tgb = Trk("lngb")
        k.dma("sp", G[:], pb(lng_d), tgb, W=[tgb])
        k.dma("sp", B[:], pb(lnb_d), tgb, W=[tgb], join=True)
        st = k.sb("lnst", [128, TT, 40], F32)
        tst = [Trk(f"lnst{i}") for i in range(TT)]
        ps = [k.ps("psP", [128, 512]) for _ in range(4)]
        tps = [Trk(f"psP{i}") for i in range(4)]
        Wv = W_d.rearrange("(ct p) n -> p ct n", p=128)

        def issue(cc):
            if cc < 4:
                k.dma("pool", Wc[cc % 2][:], Wv[:, :, cc * 512:(cc + 1) * 512], tWc[cc % 2], W=[tWc[cc % 2]])

        issue(0)
        n = 0
        for cc in range(4):
            issue(cc + 1)
            for i, tt in enumerate(tts):
                p = n % 4
                n += 1
                for ct in range(CT):
                    k.mm(ps[p][:, :], aT[:, ct, i * 128:(i + 1) * 128], Wc[cc % 2][:, ct, :], ct == 0, ct == CT - 1,
                         R=[taT, tWc[cc % 2]], W=[tps[p]])
                dst = h_tok[:, tt, cc * 512:(cc + 1) * 512]
                k.v("dve", "scalar_tensor_tensor", dst, dst, ALPHA, ps[p][:, :], ALU.mult, ALU.add,
                    R=[tps[p], th[tt]], W=[th[tt]])
        for tt in tts:
            emit_ln_tok(k, h_tok[:, tt, :], th[tt], G[:], B[:], tgb, st[:, tt, :], tst[tt])


def load_tok(k, h_tok, th, src_d):
    hv = src_d.rearrange("(tt p) c -> p tt c", p=128)
    for tt in range(TT):
        k.dma("sp", h_tok[:, tt, :], hv[:, tt, :], th[tt], W=[th[tt]])


def store_tok(k, h_tok, th, dst_d, to):
    ov = dst_d.rearrange("(tt p) c -> p tt c", p=128)
    for tt in range(TT):
        k.dma("sp", ov[:, tt, :], h_tok[:, tt, :], to, R=[th[tt]], W=[to], join=True)


def moe_dram(nc, sfx=""):
    d = {}
    d["wg"] = nc.dram_tensor("moe_wg" + sfx, [D, 4], F32, kind="ExternalInput").ap()
    d["bg"] = nc.dram_tensor("moe_bg" + sfx, [1, 4], F32, kind="ExternalInput").ap()
    d["we"] = nc.dram_tensor("moe_we" + sfx, [D, 32], F32, kind="ExternalInput").ap()
    d["be"] = nc.dram_tensor("moe_be" + sfx, [1, 32], F32, kind="ExternalInput").ap()
    d["wgu"] = nc.dram_tensor("moe_wgu" + sfx, [NEXP, D, 2 * DFF], F32, kind="ExternalInput").ap()
    d["wdn"] = nc.dram_tensor("moe_wdn" + sfx, [NEXP, DFF, D], F32, kind="ExternalInput").ap()
    d["ln"] = nc.dram_tensor("ln_gb" + sfx, [4, D], F32, kind="ExternalInput").ap()
    d["ybuf"] = nc.dram_tensor("ybuf" + sfx, [NEXP * 128, D], BF16).ap()
    return d


def moe_inputs(inp, l, sfx=""):
    return {
        "moe_wg" + sfx: np.ascontiguousarray(inp["moe_wg"][l]),
        "moe_bg" + sfx: np.ascontiguousarray(inp["moe_bg"][l][None]),
        "moe_we" + sfx: np.ascontiguousarray(inp["moe_we"][l]),
        "moe_be" + sfx: np.ascontiguousarray(inp["moe_be"][l][None]),
        "moe_wgu" + sfx: np.ascontiguousarray(inp["moe_w_gu"][l]),
        "moe_wdn" + sfx: np.ascontiguousarray(inp["moe_w_down"][l]),
        "ln_gb" + sfx: np.ascontiguousarray(np.stack([inp["ln_g"][l, 0], inp["ln_b"][l, 0], inp["ln_g"][l, 1], inp["ln_b"][l, 1]])),
    }


class WConv:
    def __init__(self, k, d, sfx, nconv):
        nc = k.nc
        self.k, self.d, self.nconv = k, d, nconv
        self.wgu_b = nc.dram_tensor("wgu_b" + sfx, [max(nconv, 1), D, 2 * DFF], BF16).ap()
        self.wdn_b = nc.dram_tensor("wdn_b" + sfx, [max(nconv, 1), DFF, D], BF16).ap()
        self.trk = [Trk(f"wcv{sfx}_{e}") for e in range(nconv)]
        self.jobs = [(e, c) for e in range(nconv) for c in ("gu", "dn")]
        self.nxt = 0

    def pump(self, n=1):
        k = self.k
        for _ in range(n):
            if self.nxt >= len(self.jobs):
                return
            e, c = self.jobs[self.nxt]
            self.nxt += 1
            if c == "gu":
                k.dma("pool", self.wgu_b[e].rearrange("(a p) n -> p a n", p=128),
                      self.d["wgu"][e].rearrange("(a p) n -> p a n", p=128), self.trk[e], W=[self.trk[e]], join=True,
                      background=True)
            else:
                k.dma("pool", self.wdn_b[e].rearrange("(a p) n -> p a n", p=128),
                      self.d["wdn"][e].rearrange("(a p) n -> p a n", p=128), self.trk[e], W=[self.trk[e]], join=True,
                      background=True)

    def flush(self):
        self.pump(len(self.jobs))


def emit_moe_d(k, C, h_tok, th, d, wconv=None):
    emit_moe(k, C, h_tok, th, d["wg"], d["bg"], d["we"], d["be"], d["wgu"], d["wdn"], d["ybuf"],
             d["ln"][2:3, :], d["ln"][3:4, :], wconv=wconv)


def emit_moe_d_old(k, C, h_tok, th, d):
    emit_moe(k, C, h_tok, th, d["wg"], d["bg"], d["we"], d["be"], d["wgu"], d["wdn"], d["ybuf"],
             d["ln"][2:3, :], d["ln"][3:4, :])


def build_p3():
    nc = bass.Bass("TRN2", target_bir_lowering=False)
    k = K(nc)
    cst_d = nc.dram_tensor("cst", [128, NC32], F32, kind="ExternalInput").ap()
    h_d = nc.dram_tensor("h_in", [NT, D], F32, kind="ExternalInput").ap()
    oT_d = nc.dram_tensor("oT", [D, NT], F32, kind="ExternalInput").ap()
    wo_d = nc.dram_tensor("w_o", [D, D], F32, kind="ExternalInput").ap()
    md = moe_dram(nc)
    out_d = nc.dram_tensor("out", [NT, D], F32, kind="ExternalOutput").ap()
    C = load_consts(k, cst_d)
    h_tok = k.sb("h_tok", [128, TT, D], F32)
    th = [Trk(f"h{i}") for i in range(TT)]
    load_tok(k, h_tok, th, h_d)
    with k.scope():
        aT = k.sb("aT", [128, CT, NT], BF16)
        taT = Trk("aT")
        k.dma("pool", aT[:], oT_d.rearrange("(ct p) t -> p ct t", p=128), taT, W=[taT])
        emit_proj_res_ln(k, aT, taT, wo_d, h_tok, th, md["ln"][0:1, :], md["ln"][1:2, :])
    emit_moe_d(k, C, h_tok, th, md)
    to = Trk("out")
    store_tok(k, h_tok, th, out_d, to)
    k.barrier()
    k.close()
    return nc


HALO = 30
NXC = NT + HALO + 2
NCPAR = 16 * 31 + 16 * 3 + 32


def conv_params(inp):
    wdw = inp["conv_w_dw"][0]
    a = wdw.T.reshape(CT, 128, 31).transpose(1, 0, 2).reshape(128, CT * 31)
    cols = [a]
    for v in (inp["conv_b_dw"][0], inp["conv_ln_g"][0], inp["conv_ln_b"][0]):
        cols.append(v.reshape(CT, 128).T)
    cols.append(inp["conv_b_pw1"][0].reshape(32, 128).T)
    return np.ascontiguousarray(np.concatenate(cols, axis=1).astype(np.float32))


def emit_conv(k, C, xT_d, hm_d, cpar_d, w1_d, w2_d, h_tok, th, lng_d, lnb_d, dbg=None, bg=None):
    nc = k.nc
    with k.scope():
        cp = k.sb("cpar", [128, NCPAR], F32)
        tcp = Trk("cpar")
        k.dma("sp", cp[:], cpar_d, tcp, W=[tcp])
        wdw = cp[:, 0:CT * 31].rearrange("p (j t) -> p j t", j=CT)
        bdw = cp[:, 496:512]
        cg = cp[:, 512:528]
        cb = cp[:, 528:544]
        b1 = cp[:, 544:576]
        hm = k.sb("hm", [128, 1], F32)
        thm = Trk("hm")
        k.dma("sp", hm[:], pb(hm_d), thm, W=[thm])
        gluT = k.sb("gluT", [128, CT, NXC], BF16)
        tglu = [Trk(f"glu{j}") for j in range(CT)]
        with k.scope():
            xT = k.sb("xT", [128, CT, NXC], BF16)
            txT = Trk("xT")
            k.dma("pool", xT[:, :, 0:NT + HALO], xT_d.rearrange("(ct p) t -> p ct t", p=128), txT, W=[txT])
            NS = 3
            wsl = [k.sb("w1s", [128, CT, 512], BF16) for _ in range(NS)]
            twsl = [Trk(f"w1s{i}") for i in range(NS)]
            sig = [k.sb("sig", [128, 512], F32) for _ in range(2)]
            tsig = [Trk("sig0"), Trk("sig1")]
            psA = [k.ps("psA", [128, 512]) for _ in range(2)]
            tpA = [Trk("pA0"), Trk("pA1")]
            psB = [k.ps("psB", [128, 512]) for _ in range(2)]
            tpB = [Trk("pB0"), Trk("pB1")]
            W1v = w1_d.rearrange("(ct p) n -> p ct n", p=128)
            loads = []
            for jg in range(4):
                loads.append(jg * 512)
                loads.append(D + jg * 512)

            nxt = [0]

            def pump(done):
                while nxt[0] < len(loads) and (nxt[0] < NS or (nxt[0] - NS) // 2 < done):
                    i = nxt[0]
                    s_ = i % NS
                    k.dma("pool", wsl[s_][:], W1v[:, :, loads[i]:loads[i] + 512], twsl[s_], W=[twsl[s_]])
                    nxt[0] += 1

            n = 0
            for jg in range(4):
                pump(jg)
                assert nxt[0] >= 2 * jg + 2
                sa, sg_ = (2 * jg) % NS, (2 * jg + 1) % NS
                for jj in range(4):
                    j = jg * 4 + jj
                    for c0, c1 in ((0, 512), (512, 1024), (1024, NT + HALO)):
                        w = c1 - c0
                        p = n % 2
                        n += 1
                        for ct in range(CT):
                            k.mm(psA[p][:, 0:w], wsl[sa][:, ct, jj * 128:(jj + 1) * 128], xT[:, ct, c0:c1],
                                 ct == 0, ct == CT - 1, R=[twsl[sa], txT], W=[tpA[p]])
                        for ct in range(CT):
                            k.mm(psB[p][:, 0:w], wsl[sg_][:, ct, jj * 128:(jj + 1) * 128], xT[:, ct, c0:c1],
                                 ct == 0, ct == CT - 1, R=[twsl[sg_], txT], W=[tpB[p]])
                        k.act(sig[p][:, 0:w], psB[p][:, 0:w], AF.Sigmoid, bias=b1[:, 16 + j:17 + j], scale=1.0,
                              R=[tpB[p], tcp], W=[tsig[p]])
                        k.v("dve", "scalar_tensor_tensor", gluT[:, j, c0:c1], psA[p][:, 0:w], b1[:, j:j + 1],
                            sig[p][:, 0:w], ALU.add, ALU.mult, R=[tpA[p], tsig[p], tcp], W=[tglu[j]])
                    if bg is not None:
                        bg("pw1")
            for j in range(CT):
                k.v("dve", "tensor_scalar", gluT[:, j, 0:HALO], gluT[:, j, 0:HALO], hm[:, 0:1], None, ALU.mult,
                    R=[thm], W=[tglu[j]])
        if dbg is not None:
            tdb = Trk("dbg")
            k.dma("sp", dbg["glu"].rearrange("(j p) t -> p j t", p=128), gluT[:], tdb, R=tglu, W=[tdb], join=True)
        for tc in range(2):
            with k.scope():
                sT = k.sb("sT", [128, CT, 512], BF16)
                tsT = Trk("sT")
                with k.scope():
                    v = k.sb("cv", [128, CT, 512], F32)
                    tv = [Trk(f"cv{j}") for j in range(CT)]
                    dg = [k.sb("dg", [128, 31, 128], BF16) for _ in range(2)]
                    tdg = [Trk("dg0"), Trk("dg1")]
                    vsq = [k.sb("vsq", [128, 512], F32) for _ in range(2)]
                    tvsq = [Trk("vsq0"), Trk("vsq1")]
                    psC = [k.ps("psC", [128, 512]) for _ in range(2)]
                    tpC = [Trk("pC0"), Trk("pC1")]
                    psS = k.ps("psS", [128, 512])
                    tpS = Trk("pS")
                    psQ = k.ps("psQ", [128, 512])
                    tpQ = Trk("pQ")
                    for j in range(CT):
                        p = j % 2
                        for t in range(31):
                            k.v("dve", "tensor_scalar", dg[p][:, t, :], C["ident_bf"],
                                wdw[:, j, t:t + 1], None, ALU.mult, R=[C["tbf"], tcp], W=[tdg[p]])
                        for t in range(31):
                            k.mm(psC[p][:, :], dg[p][:, t, :], gluT[:, j, tc * 512 + t:tc * 512 + t + 512], t == 0, t == 30,
                                 R=[tdg[p], tglu[j]], W=[tpC[p]])
                        k.act(v[:, j, :], psC[p][:, :], AF.Identity, bias=bdw[:, j:j + 1], scale=1.0,
                              R=[tpC[p], tcp], W=[tv[j]])
                        k.act(vsq[p][:], v[:, j, :], AF.Square, R=[tv[j]], W=[tvsq[p]])
                        k.mm(psS[:, :], C["ones"], v[:, j, :], j == 0, j == CT - 1, R=[C["t32"], tv[j]], W=[tpS])
                        k.mm(psQ[:, :], C["ones"], vsq[p][:], j == 0, j == CT - 1, R=[C["t32"], tvsq[p]], W=[tpQ])
                        if bg is not None:
                            bg("conv")
                    if dbg is not None:
                        k.dma("sp", dbg["v"][tc].rearrange("(j p) t -> p j t", p=128), v[:], tdb, R=tv, W=[tdb], join=True)
                    mu = k.sb("mu", [128, 512], F32)
                    rs = k.sb("rs", [128, 512], F32)
                    m2 = k.sb("m2", [128, 512], F32)
                    tm = Trk("murs")
                    k.v("dve", "tensor_scalar", mu[:], psS[:, :], 1.0 / D, None, ALU.mult, R=[tpS], W=[tm])
                    k.v("dve", "tensor_tensor", m2[:], mu[:], mu[:], ALU.mult, R=[tm], W=[tm])
                    k.v("dve", "scalar_tensor_tensor", m2[:], psQ[:, :], 1.0 / D, m2[:], ALU.mult, ALU.subtract,
                        R=[tpQ, tm], W=[tm])
                    k.act(m2[:], m2[:], AF.Sqrt, bias=LN_EPS, scale=1.0, R=[tm], W=[tm])
                    k.v("dve", "reciprocal", rs[:], m2[:], R=[tm], W=[tm])
                    for j in range(CT):
                        k.v("dve", "tensor_tensor", v[:, j, :], v[:, j, :], mu[:], ALU.subtract, R=[tm, tv[j]], W=[tv[j]])
                        k.v("dve", "tensor_tensor", v[:, j, :], v[:, j, :], rs[:], ALU.mult, R=[tm, tv[j]], W=[tv[j]])
                        k.act(sT[:, j, :], v[:, j, :], AF.Silu, bias=cb[:, j:j + 1], scale=cg[:, j:j + 1],
                              R=[tv[j], tcp], W=[tsT])
                if dbg is not None:
                    k.dma("sp", dbg["s"][tc].rearrange("(j p) t -> p j t", p=128), sT[:], tdb, R=[tsT], W=[tdb], join=True)
                emit_proj_res_ln(k, sT, tsT, w2_d, h_tok, th, lng_d, lnb_d, tts=[tc * 4 + i for i in range(4)])


def build_p1():
    nc = bass.Bass("TRN2", target_bir_lowering=False)
    k = K(nc)
    cst_d = nc.dram_tensor("cst", [128, NC32], F32, kind="ExternalInput").ap()
    x_d = nc.dram_tensor("x_tok", [NT, D], F32, kind="ExternalInput").ap()
    xT_d = nc.dram_tensor("xT", [D, NT + HALO], F32, kind="ExternalInput").ap()
    hm_d = nc.dram_tensor("hm", [1, 1], F32, kind="ExternalInput").ap()
    cpar_d = nc.dram_tensor("cpar", [128, NCPAR], F32, kind="ExternalInput").ap()
    w1_d = nc.dram_tensor("w_pw1", [D, 2 * D], F32, kind="ExternalInput").ap()
    w2_d = nc.dram_tensor("w_pw2", [D, D], F32, kind="ExternalInput").ap()
    md = moe_dram(nc)
    out_d = nc.dram_tensor("out", [NT, D], F32, kind="ExternalOutput").ap()
    C = load_consts(k, cst_d)
    h_tok = k.sb("h_tok", [128, TT, D], F32)
    th = [Trk(f"h{i}") for i in range(TT)]
    load_tok(k, h_tok, th, x_d)
    emit_conv(k, C, xT_d, hm_d, cpar_d, w1_d, w2_d, h_tok, th, md["ln"][0:1, :], md["ln"][1:2, :])
    emit_moe_d(k, C, h_tok, th, md)
    to = Trk("out")
    store_tok(k, h_tok, th, out_d, to)
    k.barrier()
    k.close()
    return nc


def p1_inputs(inp, c):
    b, j = divmod(c, 4)
    x = inp["x"][b]
    s0 = j * NT
    xe = np.zeros((NT + HALO, D), np.float32)
    if j > 0:
        xe[:] = x[s0 - HALO:s0 + NT]
    else:
        xe[HALO:] = x[0:NT]
    d = dict(cst=make_consts(), x_tok=np.ascontiguousarray(x[s0:s0 + NT]), xT=np.ascontiguousarray(xe.T),
             hm=np.full((1, 1), 0.0 if j == 0 else 1.0, np.float32), cpar=conv_params(inp),
             w_pw1=np.ascontiguousarray(inp["conv_w_pw1"][0]), w_pw2=np.ascontiguousarray(inp["conv_w_pw2"][0]))
    d.update(moe_inputs(inp, 0))
    return d


NQB = SEQ // 128
NCMP = 255
QSCALE = 128.0 ** -0.5
NCM = 33


def _split3(v):
    import ml_dtypes
    v = v.astype(np.float32)
    a = v.astype(ml_dtypes.bfloat16).astype(np.float32)
    b = (v - a).astype(ml_dtypes.bfloat16).astype(np.float32)
    c = (v - a - b).astype(ml_dtypes.bfloat16).astype(np.float32)
    return a, b, c


def attn_consts(g):
    slopes = np.array([2.0 ** (-8.0 * (4 * g + hh + 1) / 16.0) for hh in range(4)], np.float64)
    kr = np.arange(128)
    LT = np.zeros((128, 32 * 128), np.float32)
    for d in range(32):
        for hh in range(4):
            val = slopes[hh] * (128.0 * (-d) + kr - 64.0)
            for s_, part in enumerate(_split3(val)):
                LT[hh * 3 + s_, d * 128:(d + 1) * 128] = part
    LTc = np.zeros((128, 32 * 128), np.float32)
    for dc in range(32):
        for hh in range(4):
            val = slopes[hh] * (16.0 * kr - 33.0 - 128.0 * dc)
            for s_, part in enumerate(_split3(val)):
                LTc[hh * 3 + s_, dc * 128:(dc + 1) * 128] = part
    HI = np.zeros((128, 512), np.float32)
    for hh in range(4):
        HI[hh * 3:hh * 3 + 3, hh * 128:(hh + 1) * 128] = 1.0
    E = np.zeros((128, SEQ), np.float32)
    for j in range(64):
        E[j, j * 64:(j + 1) * 64] = 1.0
    qr = np.arange(128)
    Mc = np.where(kr[:, None] <= qr[None, :], 0.0, NEG).astype(np.float32)
    Mw = np.where(kr[:, None] > qr[None, :], 0.0, NEG).astype(np.float32)
    Mc4 = np.tile(Mc, (1, 4))
    Mw4 = np.tile(Mw, (1, 4))
    CM = np.zeros((128, NCM * 128), np.float32)
    for di in range(17):
        m = np.where(16 * kr[:, None] + 31 <= 128 * di + qr[None, :], 0.0, NEG)
        CM[:, di * 128:(di + 1) * 128] = m
    for di in range(16):
        m = np.where(16 * kr[:, None] + 31 <= 128 * di + qr[None, :], 0.0, NEG)
        m[127, :] = NEG
        CM[:, (17 + di) * 128:(18 + di) * 128] = m
    n = np.arange(256)[:, None]
    j = np.arange(64)[None, :]
    ov = np.maximum(np.minimum(16 * n + 32, 64 * j + 64) - np.maximum(16 * n, 64 * j), 0) / 32.0
    ov[255] = 0
    ovl = ov.reshape(2, 128, 64).transpose(1, 0, 2).reshape(128, 128)
    ident = np.eye(128, dtype=np.float32)
    t = np.arange(SEQ)
    cur = (t // 64)[:, None]
    blk = np.arange(64)[None, :]
    forced = ((blk == 0) | (blk == cur) | (blk == cur - 1)).astype(np.float32)
    cand = ((blk >= 1) & (blk <= cur - 2)).astype(np.float32)
    f3 = forced.reshape(32, 128, 64).transpose(1, 0, 2).reshape(128, 32 * 64)
    c3 = cand.reshape(32, 128, 64).transpose(1, 0, 2).reshape(128, 32 * 64)
    cb = np.concatenate([LT, LTc, HI, E, Mc4, Mw4, CM, ovl, ident, c3, f3], axis=1).astype(np.float32)
    cf = ident.copy()
    return np.ascontiguousarray(cb), np.ascontiguousarray(cf)


CB_OFF = {}
_o = 0
for _n, _w in (("LT", 32 * 128), ("LTc", 32 * 128), ("HI", 512), ("E", SEQ), ("Mc4", 512), ("Mw4", 512),
               ("CM", NCM * 128), ("ov", 128), ("ident", 128), ("cand", 2048), ("forced", 2048)):
    CB_OFF[_n] = (_o, _o + _w)
    _o += _w
NCB = _o
NCF = 128
NWF = 1024
NWT = 268


def attn_weights(inp, g):
    wqg = inp["nsa_w_qg"][0]
    kvw = inp["kv_w"]
    sl = lambda s_: kvw[:, s_ * 512 + g * 128: s_ * 512 + (g + 1) * 128]
    WF = np.concatenate([wqg[:, g * 512:(g + 1) * 512], sl(2), sl(4), sl(0), sl(1)], axis=1)
    WT = np.concatenate([sl(3), sl(5), wqg[:, 2048 + g * 12: 2048 + (g + 1) * 12]], axis=1)
    posT = np.concatenate([inp["cmp_pos"][0].T, inp["cmp_pos"][1].T], axis=1)
    return dict(WF=np.ascontiguousarray(WF), WT=np.ascontiguousarray(WT), posT=np.ascontiguousarray(posT),
                cw1=np.ascontiguousarray(inp["cmp_w1"]), cw2=np.ascontiguousarray(inp["cmp_w2"]))


def emit_attn(k, hT_d, WF_d, WT_d, posT_d, cw1_d, cw2_d, cb_d, cf_d, o_d, dbg=None, hsrc=None, ob=None, tob=None,
              chunk_order=None, after_qb=None, bg=None):
    nc = k.nc
    cbs = k.sb("cbs", [128, NCB], BF16)
    tcb = Trk("cbs")
    k.dma("pool", cbs[:], cb_d, tcb, W=[tcb])
    cfs = k.sb("cfs", [128, NCF], F32)
    tcf = Trk("cfs")
    k.dma("sp", cfs[:], cf_d, tcf, W=[tcf])
    cbv = lambda n_: cbs[:, CB_OFF[n_][0]:CB_OFF[n_][1]]
    LT, LTc, HI, E_, Mc4, Mw4, CM, ovl, identb = (cbv(n_) for n_ in ("LT", "LTc", "HI", "E", "Mc4", "Mw4", "CM", "ov", "ident"))
    cand = cbv("cand").rearrange("p (a b) -> p a b", a=32)
    forced = cbv("forced").rearrange("p (a b) -> p a b", a=32)
    identf = cfs[:, 0:128]
    QT = k.sb("QT", [128, NQB, 4, 128], BF16)
    tQ = [Trk(f"Q{i}") for i in range(16)]
    KsT = k.sb("KsT", [128, SEQ], BF16)
    KwT = k.sb("KwT", [128, SEQ], BF16)
    tK = [Trk(f"K{i}") for i in range(16)]
    Vs = k.sb("Vs", [128, NQB, 132], BF16)
    Vw = k.sb("Vw", [128, NQB, 132], BF16)
    tV = [Trk(f"V{i}") for i in range(16)]
    gat = k.sb("gat", [128, NQB, 12], F32)
    KcT = k.sb("KcT", [128, 256], BF16)
    Vc = k.sb("Vc", [128, 2, 196], BF16)
    tC = Trk("cmpkv")
    k.v("dve", "memset", Vs[:, :, 128:132], 1.0, W=tV)
    k.v("dve", "memset", Vw[:, :, 128:132], 1.0, W=tV)
    k.v("dve", "memset", Vc[:], 0.0, W=[tC])
    k.v("dve", "memset", KcT[:], 0.0, W=[tC])

    with k.scope():
        rawT = [k.sb("rawT", [128, SEQ], BF16) for _ in range(2)]
        traw = [Trk(f"raw{i}") for i in range(16)]
        with k.scope():
            WF = k.sb("WF", [128, CT, NWF], BF16)
            tWF = Trk("WF")
            WFv = WF_d.rearrange("(ct p) n -> p ct n", p=128)
            k.dma("pool", WF[:, :, 0:512], WFv[:, :, 0:512], tWF, W=[tWF])
            k.dma("pool", WF[:, :, 512:1024], WFv[:, :, 512:1024], tWF, W=[tWF], join=True)
            WT = k.sb("WT", [128, CT, NWT], BF16)
            tWT = Trk("WT")
            k.dma("pool", WT[:], WT_d.rearrange("(ct p) n -> p ct n", p=128), tWT, W=[tWT])
            hc = [k.sb("hc", [128, CT, 256], BF16) for _ in range(2)]
            thc = [Trk("hc0"), Trk("hc1")]
            psF = [k.ps("psF", [128, 512]) for _ in range(3)]
            tpF = [Trk(f"pF{i}") for i in range(3)]
            psT = [k.ps("psT", [128, 512]) for _ in range(2)]
            tpT = [Trk(f"pT{i}") for i in range(2)]
            hv = hT_d.rearrange("(ct p) t -> p ct t", p=128) if hT_d is not None else None

            def issue(c):
                if c < 16:
                    if hsrc is None:
                        k.dma("pool", hc[c % 2][:], hv[:, :, c * 256:(c + 1) * 256], thc[c % 2], W=[thc[c % 2]])
                    else:
                        ap_, Rr = hsrc(c)
                        k.dma("sp", hc[c % 2][:], ap_, thc[c % 2], R=Rr, W=[thc[c % 2]])

            order = list(range(16)) if chunk_order is None else list(chunk_order)

            def issue(i):
                if i < 16:
                    c_ = order[i]
                    if hsrc is None:
                        k.dma("pool", hc[i % 2][:], hv[:, :, c_ * 256:(c_ + 1) * 256], thc[i % 2], W=[thc[i % 2]])
                    else:
                        ap_, Rr = hsrc(c_)
                        k.dma("sp", hc[i % 2][:], ap_, thc[i % 2], R=Rr, W=[thc[i % 2]])

            issue(0)
            nf = 0
            ntk = 0
            for ci_, c in enumerate(order):
                issue(ci_ + 1)
                H = hc[ci_ % 2]
                tH = thc[ci_ % 2]
                for f in range(8):
                    p = nf % 3
                    nf += 1
                    for ct in range(CT):
                        k.mm(psF[p][:, 0:256], WF[:, ct, f * 128:(f + 1) * 128], H[:, ct, :], ct == 0, ct == CT - 1,
                             R=[tWF, tH], W=[tpF[p]])
                    if f < 4:
                        k.act(QT[:, 2 * c:2 * c + 2, f, :], psF[p][:, 0:256].rearrange("p (a b) -> p a b", a=2), AF.Copy,
                              scale=QSCALE, R=[tpF[p]], W=[tQ[c]])
                    else:
                        dst = (KsT, KwT, rawT[0], rawT[1])[f - 4]
                        trk = (tK[c], tK[c], traw[c], traw[c])[f - 4]
                        if f % 2 == 0:
                            k.v("dve", "tensor_copy", dst[:, c * 256:(c + 1) * 256], psF[p][:, 0:256], R=[tpF[p]], W=[trk])
                        else:
                            k.act(dst[:, c * 256:(c + 1) * 256], psF[p][:, 0:256], AF.Copy, R=[tpF[p]], W=[trk])
                for i in range(2):
                    tt = c * 2 + i
                    p = ntk % 2
                    ntk += 1
                    for ct in range(CT):
                        k.mm(psT[p][:, 0:NWT], H[:, ct, i * 128:(i + 1) * 128], WT[:, ct, :], ct == 0, ct == CT - 1,
                             R=[tH, tWT], W=[tpT[p]])
                    k.v("dve", "tensor_copy", Vs[:, tt, 0:128], psT[p][:, 0:128], R=[tpT[p]], W=[tV[c]])
                    k.v("dve", "tensor_copy", Vw[:, tt, 0:128], psT[p][:, 128:256], R=[tpT[p]], W=[tV[c]])
                    k.act(gat[:, tt, :], psT[p][:, 256:268], AF.Sigmoid, R=[tpT[p]], W=[tV[c]])
                if bg is not None:
                    bg("proj")
        with k.scope():
            posT = k.sb("posT", [128, 64], BF16)
            tpos = Trk("posT")
            k.dma("pool", posT[:], posT_d, tpos, W=[tpos])
            W1 = [k.sb("cW1", [128, 32, 256], BF16) for _ in range(2)]
            W2 = [k.sb("cW2", [128, 2, 128], BF16) for _ in range(2)]
            tW = Trk("cW")
            for c in range(2):
                k.dma("pool", W1[c][:], cw1_d[c].rearrange("(cb p) n -> p cb n", p=128), tW, W=[tW], join=True)
                k.dma("pool", W2[c][:], cw2_d[c].rearrange("(a p) n -> p a n", p=128), tW, W=[tW], join=True)
            psH = [k.ps("psH", [128, 512]) for _ in range(2)]
            tpH = [Trk("pH0"), Trk("pH1")]
            psB = k.ps("psB", [128, 512])
            tpB = Trk("pB")
            psO = k.ps("psO", [128, 512])
            tpO = Trk("pO")
            hb = k.sb("hbias", [128, 4], F32)
            thb = Trk("hbias")
            xg = k.sb("xg", [128, 3, 256], F32)
            txg = Trk("xg")
            hid = [[k.sb("hid", [128, 256], BF16) for _ in range(2)] for _ in range(2)]
            thid = [[Trk(f"hid{a}{b}") for b in range(2)] for a in range(2)]
            nh = 0
            for c in range(2):
                rv = rawT[c][:, :].rearrange("p (n s) -> p n s", s=16)
                for hk in range(2):
                    for cb_ in range(32):
                        k.mm(psB[:, hk:hk + 1], W1[c][:, cb_, hk * 128:(hk + 1) * 128], posT[:, c * 32 + cb_:c * 32 + cb_ + 1],
                             cb_ == 0, cb_ == 31, R=[tW, tpos], W=[tpB])
                    k.v("dve", "tensor_copy", hb[:, c * 2 + hk:c * 2 + hk + 1], psB[:, hk:hk + 1], R=[tpB], W=[thb])
                    p = nh % 2
                    nh += 1
                    for cb_ in range(32):
                        k.mm(psH[p][:, 0:NCMP], W1[c][:, cb_, hk * 128:(hk + 1) * 128],
                             rv[:, cb_ // 16:cb_ // 16 + NCMP, cb_ % 16], cb_ == 0, cb_ == 31,
                             R=[tW] + traw, W=[tpH[p]])
                    x_ = xg[:, 0, 0:NCMP]
                    u_ = xg[:, 1, 0:NCMP]
                    s_ = xg[:, 2, 0:NCMP]
                    k.act(x_, psH[p][:, 0:NCMP], AF.Identity, bias=hb[:, c * 2 + hk:c * 2 + hk + 1], scale=1.0,
                          R=[tpH[p], thb], W=[txg])
                    k.v("dve", "tensor_tensor", u_, x_, x_, ALU.mult, R=[txg], W=[txg])
                    k.v("dve", "tensor_scalar", u_, u_, 0.044715, 1.0, ALU.mult, ALU.add, R=[txg], W=[txg])
                    k.v("dve", "tensor_tensor", u_, u_, x_, ALU.mult, R=[txg], W=[txg])
                    k.act(s_, u_, AF.Sigmoid, scale=2.0 * 0.7978845608028654, R=[txg], W=[txg])
                    k.v("dve", "tensor_tensor", hid[c][hk][:, 0:NCMP], x_, s_, ALU.mult, R=[txg], W=[thid[c][hk]])
            for hk in range(2):
                k.mm(psO[:, 0:NCMP], W2[0][:, hk, :], hid[0][hk][:, 0:NCMP], hk == 0, hk == 1,
                     R=[tW, thid[0][hk]], W=[tpO])
            k.v("dve", "tensor_copy", KcT[:, 0:NCMP], psO[:, 0:NCMP], R=[tpO], W=[tC])
            for nt, (n0, n1) in enumerate(((0, 128), (128, NCMP))):
                w = n1 - n0
                for hk in range(2):
                    k.mm(psO[0:w, 256:384], hid[1][hk][:, n0:n1], W2[1][:, hk, :], hk == 0, hk == 1,
                         R=[tW, thid[1][hk]], W=[tpO])
                k.v("dve", "tensor_copy", Vc[0:w, nt, 0:128], psO[0:w, 256:384], R=[tpO], W=[tC])
                k.v("dve", "tensor_copy", Vc[0:w, nt, 128:192], ovl[0:w, nt * 64:(nt + 1) * 64], R=[tcb], W=[tC])
                k.v("dve", "memset", Vc[0:w, nt, 192:193], 1.0, W=[tC])
    if dbg is not None:
        tdb = Trk("dbg")
        k.dma("sp", dbg["KcT"], KcT[:], tdb, R=[tC], W=[tdb], join=True)
        k.dma("sp", dbg["Vc"], Vc[:], tdb, R=[tC], W=[tdb], join=True)
        k.dma("sp", dbg["QT"], QT[:], tdb, R=tQ, W=[tdb], join=True)
        k.dma("sp", dbg["KsT"], KsT[:], tdb, R=tK, W=[tdb], join=True)
        k.dma("sp", dbg["Vs"], Vs[:], tdb, R=tV, W=[tdb], join=True)
        k.dma("sp", dbg["gat"], gat[:], tdb, R=tV, W=[tdb], join=True)

    with k.scope():
        NPS = 3
        psS = [k.ps("psS", [128, 512]) for _ in range(NPS)]
        tpS = [Trk(f"pS{i}") for i in range(NPS)]
        psO = [[k.ps("psOa", [128, 512]) for _ in range(2)] for _ in range(2)]
        tpO = [[Trk(f"pO{a}{b}") for b in range(2)] for a in range(2)]
        psX = k.ps("psX", [128, 512])
        tpX = Trk("pX")
        NPT = 4
        PT = [k.sb("PT", [128, 512], BF16) for _ in range(NPT)]
        tPT = [Trk(f"PT{i}") for i in range(NPT)]
        oacc = [k.sb("oacc", [128, 512], F32) for _ in range(3)]
        toacc = [Trk("oacc0"), Trk("oacc1"), Trk("oacc2")]
        imp = [k.sb("imp", [128, 64], F32) for _ in range(2)]
        timp = [Trk("imp0"), Trk("imp1")]
        sm = k.sb("asm", [128, 6, 16], F32)
        tsm = [Trk(f"asm{i}") for i in range(6)]
        selw = k.sb("selw", [128, 2, 4, 64], F32)
        tselw = [Trk("selw0"), Trk("selw1")]
        NSB = 3
        selb4 = [k.sb("selb4", [64, 512], BF16) for _ in range(NSB)]
        tsb4 = [Trk(f"selb4{i}") for i in range(NSB)]
        tout = Trk("o_out")
        oTt = [k.sb("oTt", [128, 4, 128], BF16) for _ in range(2)]
        toTt = [Trk("oTt0"), Trk("oTt1")]
        st = dict(ns=0, npt=0, no=0, nsm=0)
        Qv = lambda qb: QT[:, qb, :, :].rearrange("p h q -> p (h q)")
        glist = []

        def add_group(qb, lhsK, tKk, extra, Vaug, vw, tVv, first, last, ob, after=None):
            slot = {}

            def scores():
                s_ = st["ns"] % NPS
                st["ns"] += 1
                k.mm(psS[s_][:, :], lhsK, Qv(qb), True, False, R=[tKk, tQ[qb // 2]], W=[tpS[s_]], inc=False)
                for i, (l_, r_, o_, Rr) in enumerate(extra):
                    out_ap = psS[s_][:, :] if o_ is None else psS[s_][:, o_[0]:o_[1]]
                    k.mm(out_ap, l_, r_, False, i == len(extra) - 1, R=Rr, W=[tpS[s_]], inc=(i == len(extra) - 1))
                pt = st["npt"] % NPT
                st["npt"] += 1
                slot["pt"] = pt
                k.act(PT[pt][:], psS[s_][:, :], AF.Exp, R=[tpS[s_]], W=[tPT[pt]])

            def pv():
                pt = slot["pt"]
                for hh in range(4):
                    bank = psO[ob][hh // 2]
                    c0 = (hh % 2) * 256
                    k.mm(bank[:, c0:c0 + vw], PT[pt][:, hh * 128:(hh + 1) * 128], Vaug, first and hh % 2 == 0, last,
                         R=[tPT[pt], tVv], W=[tpO[ob][hh // 2]], inc=last, skip_group_check=True)

            glist.append((scores, pv, after))

        def finish(qb, ob, br, first_branch, vw, with_imp=False):
            oa = oacc[qb % 3]
            to_ = toacc[qb % 3]
            i6 = st["nsm"] % 6
            st["nsm"] += 1
            S_ = tsm[i6]
            for hh in range(4):
                bank = psO[ob][hh // 2]
                tb = tpO[ob][hh // 2]
                c0 = (hh % 2) * 256
                rs = sm[:, i6, hh * 4:hh * 4 + 1]
                sc = sm[:, i6, hh * 4 + 1:hh * 4 + 2]
                k.v("dve", "tensor_scalar", rs, bank[:, c0 + vw - 1:c0 + vw], 1e-30, None, ALU.max, R=[tb, S_], W=[S_])
                k.v("dve", "reciprocal", rs, rs, R=[S_], W=[S_])
                k.v("dve", "tensor_tensor", sc, rs, gat[:, qb, hh * 3 + br:hh * 3 + br + 1], ALU.mult,
                    R=[S_, tV[qb // 2]], W=[S_])
                dst = oa[:, hh * 128:(hh + 1) * 128]
                if first_branch:
                    k.v("dve", "tensor_scalar", dst, bank[:, c0:c0 + 128], sc, None, ALU.mult, R=[tb, S_], W=[to_])
                else:
                    k.v("dve", "scalar_tensor_tensor", dst, bank[:, c0:c0 + 128], sc, dst, ALU.mult, ALU.add,
                        R=[tb, S_], W=[to_])
                if with_imp:
                    im = imp[qb % 2]
                    if hh == 0:
                        k.v("dve", "tensor_scalar", im[:], bank[:, c0 + 128:c0 + 192], rs, None, ALU.mult,
                            R=[tb, S_], W=[timp[qb % 2]])
                    else:
                        k.v("dve", "scalar_tensor_tensor", im[:], bank[:, c0 + 128:c0 + 192], rs, im[:], ALU.mult,
                            ALU.add, R=[tb, S_], W=[timp[qb % 2]])

        deferred = []

        def defer(n, fn):
            deferred.append([n, fn])

        def select_math(qb):
            w = qb % 2
            V_ = selw[:, w, 0, :]
            V2 = selw[:, w, 1, :]
            sl_ = selw[:, w, 2, :]
            m8 = selw[:, w, 3, 0:16]
            T_ = tselw[w]
            k.v("dve", "scalar_tensor_tensor", V_, imp[w][:], 1.0, cand[:, qb, :], ALU.add, ALU.mult,
                R=[timp[w], tcb], W=[T_])
            k.v("dve", "tensor_scalar", V_, V_, -1.0, None, ALU.add, R=[T_], W=[T_])
            k.v("dve", "max", m8[:, 0:8], V_, R=[T_], W=[T_])
            k.v("dve", "match_replace", V2, m8[:, 0:8], V_, -2.0, R=[T_], W=[T_])
            k.v("dve", "max", m8[:, 8:16], V2, R=[T_], W=[T_])
            k.v("dve", "tensor_scalar", sl_, V_, m8[:, 12:13], None, ALU.is_ge, R=[T_], W=[T_])
            k.v("dve", "tensor_tensor", sl_, sl_, cand[:, qb, :], ALU.mult, R=[T_, tcb], W=[T_])
            k.v("dve", "tensor_tensor", sl_, sl_, forced[:, qb, :], ALU.max, R=[T_, tcb], W=[T_])
            k.v("dve", "tensor_scalar", sl_, sl_, -NEG, NEG, ALU.mult, ALU.add, R=[T_], W=[T_])

            def tr_part():
                k.tr(psX[0:64, 0:128], sl_, identf, R=[T_, tcf], W=[tpX])
                sb_ = qb % NSB
                for hh in range(4):
                    if hh % 2 == 0:
                        k.act(selb4[sb_][:, hh * 128:(hh + 1) * 128], psX[0:64, 0:128], AF.Copy, R=[tpX], W=[tsb4[sb_]])
                    else:
                        k.v("dve", "tensor_copy", selb4[sb_][:, hh * 128:(hh + 1) * 128], psX[0:64, 0:128],
                            R=[tpX], W=[tsb4[sb_]])
            defer(2, tr_part)

        def out_part(qb):
            if ob is None:
                k.dma("sp", o_d[qb * 128:(qb + 1) * 128, :], oacc[qb % 3][:], tout, R=[toacc[qb % 3]], W=[tout], join=True)
                return

            def tr_out():
                for hh in range(4):
                    k.tr(psX[:, hh * 128:(hh + 1) * 128], oacc[qb % 3][:, hh * 128:(hh + 1) * 128], identf,
                         R=[toacc[qb % 3], tcf], W=[tpX], inc=(hh == 3))
                k.act(oTt[qb % 2][:], psX[:, :].rearrange("p (h q) -> p h q", h=4), AF.Copy, R=[tpX], W=[toTt[qb % 2]])
                j_, tq = qb // 8, (qb % 8) * 128
                k.dma("sp", ob[j_][:, tq:tq + 128].rearrange("(h p) q -> p h q", p=128), oTt[qb % 2][:],
                      tob[j_], R=[toTt[qb % 2]], W=[tob[j_]], join=True)
                if after_qb is not None:
                    after_qb(qb)
            defer(2, tr_out)

        def cmp_branch(qb):
            ob_ = st["no"] % 2
            st["no"] += 1
            nts = [0] if qb < 16 else [0, 1]
            for ii, nt in enumerate(nts):
                dc = qb - 16 * nt
                extra = [(LTc[0:12, dc * 128:(dc + 1) * 128], HI[0:12, :], None, [tcb])]
                mi = None
                if nt == 0 and qb <= 16:
                    mi = qb
                elif nt == 1:
                    mi = 17 + (qb - 16)
                if mi is not None:
                    for hh in range(4):
                        extra.append((identb, CM[:, mi * 128:(mi + 1) * 128], (hh * 128, (hh + 1) * 128), [tcb]))
                aft = None
                if ii == len(nts) - 1:
                    def aft(qb=qb, ob_=ob_):
                        finish(qb, ob_, 0, True, 193, with_imp=True)
                        select_math(qb)
                add_group(qb, KcT[:, nt * 128:(nt + 1) * 128], tC, extra, Vc[:, nt, 0:193], 193, tC,
                          ii == 0, ii == len(nts) - 1, ob_, after=aft)

        def win_branch(qb):
            ob_ = st["no"] % 2
            st["no"] += 1
            kts = list(range(max(0, qb - 4), qb + 1))
            for ii, kt in enumerate(kts):
                d = qb - kt
                extra = [(LT[0:12, d * 128:(d + 1) * 128], HI[0:12, :], None, [tcb])]
                if kt == qb:
                    extra.append((identb, Mc4, None, [tcb]))
                if kt == qb - 4:
                    extra.append((identb, Mw4, None, [tcb]))
                aft = None
                if ii == len(kts) - 1:
                    def aft(qb=qb, ob_=ob_):
                        finish(qb, ob_, 2, False, 129)
                add_group(qb, KwT[:, kt * 128:(kt + 1) * 128], tK[kt // 2], extra, Vw[:, kt, 0:129], 129, tV[kt // 2],
                          ii == 0, ii == len(kts) - 1, ob_, after=aft)

        def sel_branch(qb):
            ob_ = st["no"] % 2
            st["no"] += 1
            sb_ = qb % NSB
            for kt in range(qb + 1):
                d = qb - kt
                extra = [(LT[0:12, d * 128:(d + 1) * 128], HI[0:12, :], None, [tcb]),
                         (E_[0:64, kt * 128:(kt + 1) * 128], selb4[sb_][:, :], None, [tcb, tsb4[sb_]])]
                if kt == qb:
                    extra.append((identb, Mc4, None, [tcb]))
                aft = None
                if kt == qb:
                    def aft(qb=qb, ob_=ob_):
                        finish(qb, ob_, 1, False, 129)
                        out_part(qb)
                        if bg is not None:
                            bg("attn")
                add_group(qb, KsT[:, kt * 128:(kt + 1) * 128], tK[kt // 2], extra, Vs[:, kt, 0:129], 129, tV[kt // 2],
                          kt == 0, kt == qb, ob_, after=aft)

        cmp_branch(0)
        for qb in range(NQB):
            if qb + 1 < NQB:
                cmp_branch(qb + 1)
            win_branch(qb)
            sel_branch(qb)

        def tick():
            for d_ in list(deferred):
                d_[0] -= 1
                if d_[0] <= 0:
                    deferred.remove(d_)
                    d_[1]()

        prev = None
        for g_ in glist:
            g_[0]()
            if prev is not None:
                prev[1]()
                if prev[2] is not None:
                    prev[2]()
                tick()
            prev = g_
        prev[1]()
        if prev[2] is not None:
            prev[2]()
        while deferred:
            tick()


def build_p2(debug=False):
    nc = bass.Bass("TRN2", target_bir_lowering=False)
    k = K(nc)
    hT_d = nc.dram_tensor("hT", [D, SEQ], F32, kind="ExternalInput").ap()
    WF_d = nc.dram_tensor("WF", [D, NWF], F32, kind="ExternalInput").ap()
    WT_d = nc.dram_tensor("WT", [D, NWT], F32, kind="ExternalInput").ap()
    posT_d = nc.dram_tensor("posT", [128, 64], F32, kind="ExternalInput").ap()
    cw1_d = nc.dram_tensor("cw1", [2, 4096, 256], F32, kind="ExternalInput").ap()
    cw2_d = nc.dram_tensor("cw2", [2, 256, 128], F32, kind="ExternalInput").ap()
    cb_d = nc.dram_tensor("cb", [128, NCB], F32, kind="ExternalInput").ap()
    cf_d = nc.dram_tensor("cf", [128, NCF], F32, kind="ExternalInput").ap()
    o_d = nc.dram_tensor("o", [SEQ, 512], F32, kind="ExternalOutput").ap()
    dbg = None
    if debug:
        dbg = dict(KcT=nc.dram_tensor("d_KcT", [128, 256], BF16, kind="ExternalOutput").ap(),
                   Vc=nc.dram_tensor("d_Vc", [128, 2, 196], BF16, kind="ExternalOutput").ap(),
                   QT=nc.dram_tensor("d_QT", [128, NQB, 4, 128], BF16, kind="ExternalOutput").ap(),
                   KsT=nc.dram_tensor("d_KsT", [128, SEQ], BF16, kind="ExternalOutput").ap(),
                   Vs=nc.dram_tensor("d_Vs", [128, NQB, 132], BF16, kind="ExternalOutput").ap(),
                   gat=nc.dram_tensor("d_gat", [128, NQB, 12], F32, kind="ExternalOutput").ap())
    emit_attn(k, hT_d, WF_d, WT_d, posT_d, cw1_d, cw2_d, cb_d, cf_d, o_d, dbg=dbg)
    k.barrier()
    k.close()
    return nc


def p2_inputs(inp, h2T_b, g):
    cb, cf = attn_consts(g)
    d = dict(hT=h2T_b, cb=cb, cf=cf)
    d.update(attn_weights(inp, g))
    return d


def p3_inputs(inp, h2_c, oT_c):
    d = dict(cst=make_consts(), h_in=h2_c, oT=oT_c, w_o=np.ascontiguousarray(inp["nsa_w_o"][0]))
    d.update(moe_inputs(inp, 1))
    return d


_PROGS = {}


def _prog(name, fn):
    if name not in _PROGS:
        _PROGS[name] = fn()
    return _PROGS[name]


def _kernel3(**inputs):
    inp = {k_: np.asarray(v_) for k_, v_ in inputs.items()}
    cores = list(range(NCORE))
    r1 = run_bass_kernel_spmd(_prog("p1", build_p1), [p1_inputs(inp, c) for c in cores], core_ids=cores)
    h2 = np.stack([r1.results[c]["out"] for c in cores]).reshape(2, SEQ, D)
    h2T = [np.ascontiguousarray(h2[b].T) for b in range(2)]
    r2 = run_bass_kernel_spmd(_prog("p2", build_p2), [p2_inputs(inp, h2T[c // 4], c % 4) for c in cores], core_ids=cores)
    o = np.zeros((2, SEQ, D), np.float32)
    for c in cores:
        o[c // 4, :, (c % 4) * 512:(c % 4 + 1) * 512] = r2.results[c]["o"]
    ins3 = []
    for c in cores:
        b, j = divmod(c, 4)
        ins3.append(p3_inputs(inp, np.ascontiguousarray(h2[b, j * NT:(j + 1) * NT]),
                              np.ascontiguousarray(o[b, j * NT:(j + 1) * NT].T)))
    r3 = run_bass_kernel_spmd(_prog("p3", build_p3), ins3, core_ids=cores)
    out = np.stack([r3.results[c]["out"] for c in cores]).reshape(2, SEQ, D)
    return out.astype(np.float32)


RG4 = [[0, 1, 2, 3], [4, 5, 6, 7]]
NCONV0 = 16
NCONV1 = 0


def build_fused():
    nc = bass.Bass("TRN2", target_bir_lowering=False)
    k = K(nc)
    cst_d = nc.dram_tensor("cst", [128, NC32], F32, kind="ExternalInput").ap()
    x_d = nc.dram_tensor("x_tok", [NT, D], F32, kind="ExternalInput").ap()
    xT_d = nc.dram_tensor("xT", [D, NT + HALO], F32, kind="ExternalInput").ap()
    hm_d = nc.dram_tensor("hm", [1, 1], F32, kind="ExternalInput").ap()
    cpar_d = nc.dram_tensor("cpar", [128, NCPAR], F32, kind="ExternalInput").ap()
    w1_d = nc.dram_tensor("w_pw1", [D, 2 * D], F32, kind="ExternalInput").ap()
    w2_d = nc.dram_tensor("w_pw2", [D, D], F32, kind="ExternalInput").ap()
    md0 = moe_dram(nc, "0")
    WF_d = nc.dram_tensor("WF", [D, NWF], F32, kind="ExternalInput").ap()
    WT_d = nc.dram_tensor("WT", [D, NWT], F32, kind="ExternalInput").ap()
    posT_d = nc.dram_tensor("posT", [128, 64], F32, kind="ExternalInput").ap()
    cw1_d = nc.dram_tensor("cw1", [2, 4096, 256], F32, kind="ExternalInput").ap()
    cw2_d = nc.dram_tensor("cw2", [2, 256, 128], F32, kind="ExternalInput").ap()
    cb_d = nc.dram_tensor("cb", [128, NCB], F32, kind="ExternalInput").ap()
    cf_d = nc.dram_tensor("cf", [128, NCF], F32, kind="ExternalInput").ap()
    idx3_d = nc.dram_tensor("idx3", [128, CT], I32, kind="ExternalInput").ap()
    wo_d = nc.dram_tensor("w_o", [D, D], F32, kind="ExternalInput").ap()
    md1 = moe_dram(nc, "1")
    out_d = nc.dram_tensor("out", [NT, D], F32, kind="ExternalOutput").ap()
    hsp = nc.dram_tensor("hsp", [NT, D], F32).ap()
    xb1 = [nc.dram_tensor(f"xb1_{q}", [D, 256], BF16).ap() for q in range(4)]
    xg1 = [nc.dram_tensor(f"xg1_{q}", [4 * D, 256], BF16).ap() for q in range(4)]
    ob = [nc.dram_tensor(f"ob_{j}", [512, NT], BF16).ap() for j in range(4)]
    og = nc.dram_tensor("og", [4 * D, NT], BF16).ap()
    thsp = Trk("hsp")
    txb1 = [Trk(f"xb1{q}") for q in range(4)]
    txg1 = [Trk(f"xg1{q}") for q in range(4)]
    tob = [Trk(f"ob{j}") for j in range(4)]
    tog = [Trk(f"og{j}") for j in range(4)]

    wc0 = WConv(k, md0, "0", NCONV0)
    wc1 = WConv(k, md1, "1", NCONV1)

    cnt = dict(pw1=0, conv=0, attn=0, proj=0)

    def bg0(where):
        cnt[where] += 1
        if where == "pw1":
            if cnt[where] % 2 == 0:
                wc0.pump(1)
        elif cnt[where] % 4 != 0:
            wc0.pump(1)

    def bg1(where):
        cnt[where] += 1
        if where == "proj":
            wc1.pump(1)
        else:
            wc1.pump(2 if cnt[where] % 2 == 0 else 1)

    C = load_consts(k, cst_d)
    with k.scope():
        h_tok = k.sb("h_tok", [128, TT, D], F32)
        th = [Trk(f"h{i}") for i in range(TT)]
        load_tok(k, h_tok, th, x_d)
        emit_conv(k, C, xT_d, hm_d, cpar_d, w1_d, w2_d, h_tok, th, md0["ln"][0:1, :], md0["ln"][1:2, :], bg=bg0)
        wc0.flush()
        emit_moe_d(k, C, h_tok, th, md0, wconv=wc0)
        store_tok(k, h_tok, th, hsp, thsp)
        with k.scope():
            hTb = k.sb("hTb", [128, CT, NT], BF16)
            thTb = Trk("hTb")
            psA = [k.ps("psTr", [128, 512]) for _ in range(4)]
            tpsA = [Trk(f"psTr{i}") for i in range(4)]
            n = 0
            for tt in range(TT):
                for q in range(4):
                    p = n % 4
                    n += 1
                    for i in range(4):
                        ct = q * 4 + i
                        k.tr(psA[p][:, i * 128:(i + 1) * 128], h_tok[:, tt, ct * 128:(ct + 1) * 128], C["ident"],
                             R=[th[tt], C["t32"]], W=[tpsA[p]], inc=(i == 3))
                    dst = hTb[:, q * 4:(q + 1) * 4, tt * 128:(tt + 1) * 128]
                    src = psA[p][:, :].rearrange("p (a b) -> p a b", a=4)
                    if n % 2 == 0:
                        k.act(dst, src, AF.Copy, R=[tpsA[p]], W=[thTb])
                    else:
                        k.v("dve", "tensor_copy", dst, src, R=[tpsA[p]], W=[thTb])
                if tt % 2 == 1:
                    q_ = tt // 2
                    k.dma("sp", xb1[q_].rearrange("(ct p) t -> p ct t", p=128), hTb[:, :, q_ * 256:(q_ + 1) * 256],
                          txb1[q_], R=[thTb], W=[txb1[q_]])
                    k.collective("AllGather", RG4, xb1[q_], xg1[q_], txg1[q_], R=[txb1[q_]], W=[txg1[q_]])

    k.release(wc0.trk)

    def hsrc(c):
        r, q = divmod(c, 4)
        return xg1[q][r * D:(r + 1) * D, :].rearrange("(ct p) t -> p ct t", p=128), [txg1[q]]

    def after_qb(qb):
        if qb % 8 == 7:
            j_ = qb // 8
            k.collective("AllGather", RG4, ob[j_], og[j_ * D:(j_ + 1) * D, :], tog[j_], R=[tob[j_]], W=[tog[j_]])

    with k.scope():
        emit_attn(k, None, WF_d, WT_d, posT_d, cw1_d, cw2_d, cb_d, cf_d, None, hsrc=hsrc, ob=ob, tob=tob,
                  chunk_order=[r * 4 + q for q in range(4) for r in range(4)], after_qb=after_qb, bg=bg1)
        wc1.flush()

    with k.scope():
        h_tok = k.sb("h_tok", [128, TT, D], F32)
        th = [Trk(f"h{i}") for i in range(TT)]
        hv = hsp.rearrange("(tt p) c -> p tt c", p=128)
        for tt in range(TT):
            k.dma("sp", h_tok[:, tt, :], hv[:, tt, :], th[tt], R=[thsp], W=[th[tt]])
        with k.scope():
            idx3 = k.sb("idx3", [128, CT], I32)
            tidx = Trk("idx3")
            k.dma("sp", idx3[:], idx3_d, tidx, W=[tidx])
            aT = k.sb("aT", [128, CT, NT], BF16)
            taT = Trk("aT")
            tg_ = [Trk(f"aTg{i}") for i in range(CT)]
            for ct in range(CT):
                k.gather(aT[:, ct, :], og[:, :], idx3[:, ct:ct + 1], tg_[ct], R=tog + [tidx], W=[taT] if ct == 0 else [])
            taT.w = [m for t_ in tg_ for m in [(t_.dsem, t_.dcnt)]]
            emit_proj_res_ln(k, aT, taT, wo_d, h_tok, th, md1["ln"][0:1, :], md1["ln"][1:2, :])
        emit_moe_d(k, C, h_tok, th, md1, wconv=wc1)
        to = Trk("out")
        store_tok(k, h_tok, th, out_d, to)
    k.barrier()
    k.close()
    return nc


def fused_inputs(inp, c):
    b, j = divmod(c, 4)
    d = p1_inputs(inp, c)
    m0 = moe_inputs(inp, 0)
    for kk in list(m0):
        del d[kk]
    d.update(moe_inputs(inp, 0, "0"))
    g = j
    cb, cf = attn_consts(g)
    d.update(cb=cb, cf=cf)
    d.update(attn_weights(inp, g))
    ct = np.arange(CT)[None, :]
    p = np.arange(128)[:, None]
    d["idx3"] = np.ascontiguousarray((j * 2048 + ct * 128 + p).astype(np.int32))
    d["w_o"] = np.ascontiguousarray(inp["nsa_w_o"][0])
    d.update(moe_inputs(inp, 1, "1"))
    return d


def kernel_unfused(**inputs):
    return _kernel3(**inputs)


def kernel(**inputs):
    inp = {k_: np.asarray(v_) for k_, v_ in inputs.items()}
    cores = list(range(NCORE))
    r = run_bass_kernel_spmd(_prog("fused", build_fused), [fused_inputs(inp, c) for c in cores], core_ids=cores)
    out = np.stack([r.results[c]["out"] for c in cores]).reshape(2, SEQ, D)
    return out.astype(np.float32)
```

```python
import numpy as np
from contextlib import ExitStack, contextmanager
import concourse.bass as bass
import concourse.mybir as mybir
from concourse.bass_utils import run_bass_kernel_spmd

F32, BF16, I32 = mybir.dt.float32, mybir.dt.bfloat16, mybir.dt.int32
AF = mybir.ActivationFunctionType
ALU = mybir.AluOpType
AX = mybir.AxisListType

D = 2048
NCORE = 8
SEQ = 4096
NT = 1024
TT = NT // 128
CT = D // 128
ALPHA = 4.0 ** 0.25
LN_EPS = 1e-5
NEXP = 32
DFF = 512
NEG = -30000.0


class Trk:
    __slots__ = ("w", "r", "dsem", "dcnt", "name")

    def __init__(self, name=""):
        self.w = []
        self.r = {}
        self.dsem = None
        self.dcnt = 0
        self.name = name


class Eng:
    def __init__(self, name, h):
        self.name, self.h = name, h
        self.sem = None
        self.cnt = 0
        self.known = {}
        self.pending = False


class K:
    SEM_ROLL = 30000

    def __init__(self, nc):
        self.nc = nc
        self.es = ExitStack()
        self.stacks = [self.es]
        self.eng = {}
        self.nsem = 0
        for name, h in (("pe", nc.tensor), ("act", nc.scalar), ("dve", nc.vector),
                        ("pool", nc.gpsimd), ("sp", nc.sync)):
            e = Eng(name, h)
            e.sem = self.sem("e_" + name)
            self.eng[name] = e
        self.dmas = []
        self.uid = 0
        self.sem_free = []
        self.scope_trks = [[]]

    def sem(self, name):
        self.nsem += 1
        return self.es.enter_context(self.nc.semaphore(f"{name}_{self.nsem}"))

    def sb(self, name, shape, dt):
        self.uid += 1
        return self.stacks[-1].enter_context(self.nc.sbuf_tensor(f"{name}_{self.uid}", list(shape), dt))

    def ps(self, name, shape, dt=F32):
        self.uid += 1
        return self.stacks[-1].enter_context(self.nc.psum_tensor(f"{name}_{self.uid}", list(shape), dt))

    @contextmanager
    def scope(self):
        st = ExitStack()
        self.stacks.append(st)
        self.scope_trks.append([])
        try:
            yield
        finally:
            self.barrier()
            self.stacks.pop()
            st.close()
            for S in self.scope_trks.pop():
                self.sem_free.append((S.dsem, S.dcnt))
                S.dsem = None
                S.w = []
                S.r = {}

    def _dsem(self, S):
        if S.dsem is None:
            if self.sem_free:
                S.dsem, S.dcnt = self.sem_free.pop()
            else:
                S.dsem = self.sem("d_" + S.name)
                S.dcnt = 0
            self.scope_trks[-1].append(S)

    def close(self):
        self.es.close()

    def _emit_waits(self, E, waits, skip_own=False):
        for sem, val in waits.values():
            if sem is E.sem:
                if skip_own or E.name == "pe":
                    continue
                assert val <= E.cnt, (E.name, val, E.cnt)
            if E.known.get(id(sem), 0) >= val:
                continue
            E.h.wait_ge(sem, val)
            E.known[id(sem)] = val

    @staticmethod
    def _need(waits, mark):
        sem, val = mark
        cur = waits.get(id(sem))
        if cur is None or cur[1] < val:
            waits[id(sem)] = (sem, val)

    def _collect(self, E, R, W):
        waits = {}
        for t in R:
            for m in t.w:
                self._need(waits, m)
        for t in W:
            for m in t.w:
                self._need(waits, m)
            for m in t.r.values():
                if m[0] is E.sem:
                    continue
                self._need(waits, m)
        return waits

    def _roll(self, E):
        if E.cnt >= self.SEM_ROLL and not E.pending:
            E.sem = self.sem("e_" + E.name)
            E.cnt = 0

    def op(self, en, fn, R=(), W=(), inc=True):
        E = self.eng[en]
        self._roll(E)
        self._emit_waits(E, self._collect(E, R, W))
        ins = fn()
        if inc:
            E.cnt += 1
            ins.then_inc(E.sem, 1)
            mark = (E.sem, E.cnt)
            E.pending = False
        else:
            mark = (E.sem, E.cnt + 1)
            E.pending = True
        for t in R:
            cur = t.r.get(id(mark[0]))
            if cur is None or cur[1] < mark[1]:
                t.r[id(mark[0])] = mark
        for t in W:
            t.w = [mark]
            t.r = {}
        return ins

    def dma(self, q, out, in_, S, R=(), W=(), join=False, background=False, **kw):
        E = self.eng[q]
        had = S.dsem is not None
        self._dsem(S)
        if background and not had:
            self.scope_trks[-1].remove(S)
        waits = {}
        for t in R:
            for m in t.w:
                self._need(waits, m)
        for t in W:
            if not join:
                for m in t.w:
                    self._need(waits, m)
            for m in t.r.values():
                self._need(waits, m)
        self._emit_waits(E, waits, skip_own=True)
        ins = E.h.dma_start(out=out, in_=in_, **kw)
        S.dcnt += 16
        ins.then_inc(S.dsem, 16)
        mark = (S.dsem, S.dcnt)
        for t in R:
            t.r[id(mark[0])] = mark
        for t in W:
            if join:
                t.w = [m for m in t.w if m[0] is not S.dsem] + [mark]
            else:
                t.w = [mark]
            t.r = {}
        if not background:
            self.dmas.append(mark)
        return ins

    def release(self, trks):
        for S in trks:
            if S.dsem is not None:
                self.sem_free.append((S.dsem, S.dcnt))
                S.dsem = None
                S.w = []
                S.r = {}

    def gather(self, out, in_, idx, S, R=(), W=(), bounds=None):
        E = self.eng["pool"]
        self._dsem(S)
        waits = {}
        for t in R:
            for m in t.w:
                self._need(waits, m)
        for t in W:
            for m in t.w:
                self._need(waits, m)
            for m in t.r.values():
                self._need(waits, m)
        self._emit_waits(E, waits, skip_own=True)
        kw = {}
        if bounds is not None:
            kw = dict(bounds_check=bounds, oob_is_err=False)
        ins = E.h.indirect_dma_start(out=out, out_offset=None, in_=in_,
                                     in_offset=bass.IndirectOffsetOnAxis(ap=idx, axis=0), **kw)
        S.dcnt += 16
        ins.then_inc(S.dsem, 16)
        mark = (S.dsem, S.dcnt)
        for t in R:
            t.r[id(mark[0])] = mark
        for t in W:
            t.w = [mark]
            t.r = {}
        self.dmas.append(mark)
        return ins

    def collective(self, kind, groups, in_ap, out_ap, S, R=(), W=()):
        E = self.eng["pool"]
        self._dsem(S)
        waits = {}
        for t in R:
            for m in t.w:
                self._need(waits, m)
        for t in W:
            for m in t.w:
                self._need(waits, m)
            for m in t.r.values():
                self._need(waits, m)
        self._emit_waits(E, waits, skip_own=True)
        ins = E.h.collective_compute(kind, ALU.bypass, replica_groups=groups, ins=[in_ap.opt()], outs=[out_ap.opt()])
        S.dcnt += 1
        ins.then_inc(S.dsem, 1)
        mark = (S.dsem, S.dcnt)
        for t in R:
            t.r[id(mark[0])] = mark
        for t in W:
            t.w = [mark]
            t.r = {}
        self.dmas.append(mark)
        return ins

    def barrier(self):
        marks = []
        for F in self.eng.values():
            assert not F.pending, F.name
            if F.cnt:
                marks.append((F.sem, F.cnt))
        seen = {}
        for m in self.dmas:
            self._need(seen, m)
        marks += list(seen.values())
        self.dmas = []
        for E in self.eng.values():
            for sem, val in marks:
                if sem is E.sem:
                    continue
                if E.known.get(id(sem), 0) >= val:
                    continue
                E.h.wait_ge(sem, val)
                E.known[id(sem)] = val

    def mm(self, out, lhsT, rhs, start, stop, R=(), W=(), inc=None, **kw):
        if inc is None:
            inc = stop
        return self.op("pe", lambda: self.nc.tensor.matmul(out, lhsT, rhs, start=start, stop=stop, **kw),
                       R=R, W=W, inc=inc)

    def tr(self, out, in_, ident, R=(), W=(), inc=True):
        return self.op("pe", lambda: self.nc.tensor.transpose(out, in_, ident), R=R, W=W, inc=inc)

    def act(self, out, in_, func, R=(), W=(), **kw):
        return self.op("act", lambda: self.nc.scalar.activation(out=out, in_=in_, func=func, **kw), R=R, W=W)

    def v(self, en, name, *a, R=(), W=(), **kw):
        h = self.eng[en].h
        return self.op(en, lambda: getattr(h, name)(*a, **kw), R=R, W=W)


NC32 = 544


def make_consts():
    c = np.zeros((128, NC32), np.float32)
    c[:, 0:128] = np.eye(128)
    c[:, 128:256] = np.arange(128)[None, :]
    c[:, 256:384] = (np.arange(128)[:, None] < np.arange(128)[None, :])
    c[:, 384:512] = 1.0
    c[:, 512:544] = (np.arange(32) * 128)[None, :]
    return c


def load_consts(k, cst_d):
    c32 = k.sb("c32", [128, NC32], F32)
    tc = Trk("c32")
    k.dma("sp", c32[:], cst_d, tc, W=[tc])
    cbf = k.sb("cbf", [128, 512], BF16)
    tcb = Trk("cbf")
    k.v("dve", "tensor_copy", cbf[:], c32[:, 0:512], R=[tc], W=[tcb])
    return dict(ident=c32[:, 0:128], iota=c32[:, 128:256], ebase=c32[:, 512:544], ones=c32[:, 384:512],
                ident_bf=cbf[:, 0:128], tri_bf=cbf[:, 256:384], ones_bf=cbf[:, 384:512], t32=tc, tbf=tcb)


def pb(ap_row):
    return ap_row.partition_broadcast(128)


def emit_ln_tok(k, x_ap, tx, G, B, tgb, st, tst):
    nc = k.nc
    for i in range(4):
        k.v("dve", "bn_stats", st[:, i * 6:(i + 1) * 6], x_ap[:, i * 512:(i + 1) * 512], R=[tx, tst], W=[tst])
    k.v("dve", "bn_aggr", st[:, 24:26], st[:, 0:24], R=[tst], W=[tst])
    k.act(st[:, 26:27], st[:, 25:26], AF.Sqrt, bias=LN_EPS, scale=1.0, R=[tst], W=[tst])
    k.v("dve", "reciprocal", st[:, 27:28], st[:, 26:27], R=[tst], W=[tst])
    k.v("dve", "scalar_tensor_tensor", st[:, 28:29], st[:, 24:25], -1.0, st[:, 27:28], ALU.mult, ALU.mult,
        R=[tst], W=[tst])
    k.v("dve", "tensor_scalar", x_ap, x_ap, st[:, 27:28], st[:, 28:29], ALU.mult, ALU.add, R=[tx, tst], W=[tx])
    k.v("dve", "tensor_tensor", x_ap, x_ap, G, ALU.mult, R=[tx, tgb], W=[tx])
    k.v("pool", "tensor_tensor", x_ap, x_ap, B, ALU.add, R=[tx, tgb], W=[tx])


def emit_moe(k, C, h_tok, th, wg_d, bg_d, we_d, be_d, wgu_d, wdn_d, ybuf, lng_d, lnb_d, nexp=NEXP, wconv=None,
             tile_cb=None):
    nc = k.nc
    rankm = k.sb("rankm", [128, TT, 32], F32)
    t_rank = [Trk(f"rank{i}") for i in range(TT)]
    dest = k.sb("dest", [128, TT, 2], I32)
    wts = k.sb("wts", [128, TT, 2], F32)
    t_dw = [Trk(f"dw{i}") for i in range(TT)]
    h_bf = k.sb("h_bf", [128, TT, D], BF16)
    t_hbf = [Trk(f"hbf{i}") for i in range(TT)]

    with k.scope():
        Wr = k.sb("Wr", [128, CT, 36], F32)
        tWr = Trk("Wr")
        k.dma("sp", Wr[:, :, 0:4], wg_d.rearrange("(ct p) n -> p ct n", p=128), tWr, W=[tWr])
        k.dma("sp", Wr[:, :, 4:36], we_d.rearrange("(ct p) n -> p ct n", p=128), tWr, W=[tWr], join=True)
        rb = k.sb("rb", [128, 36], F32)
        trb = Trk("rb")
        k.dma("sp", rb[:, 0:4], pb(bg_d), trb, W=[trb])
        k.dma("sp", rb[:, 4:36], pb(be_d), trb, W=[trb], join=True)
        hTt = [k.sb("hTt", [128, CT, 128], F32) for _ in range(2)]
        thT = [[Trk(f"hTt{a}{b}") for b in range(4)] for a in range(2)]
        psA = [k.ps("psA", [128, 512]) for _ in range(2)]
        tpsA = [Trk("psA0"), Trk("psA1")]
        psL = k.ps("psL", [128, 512])
        tpsL = Trk("psL")
        psR = k.ps("psR", [128, 512])
        tpsR = Trk("psR")
        L = k.sb("L", [128, TT, 36], F32)
        sm = k.sb("sm", [128, TT, 16], F32)
        ohg = k.sb("ohg", [128, TT, 4], F32)
        r8 = k.sb("r8", [128, TT, 5, 8], F32)
        r32 = k.sb("r32", [128, TT, 5, 32], F32)
        d12 = k.sb("d12", [128, TT, 2], F32)
        M_bf = k.sb("M_bf", [128, TT, 32], BF16)
        tM = [Trk(f"M{i}") for i in range(TT)]
        ts = [Trk(f"rs{i}") for i in range(TT)]
        nq = 0
        for tt in range(TT):
            hb = tt % 2
            k.act(h_bf[:, tt, :], h_tok[:, tt, :], AF.Copy, R=[th[tt]], W=[t_hbf[tt]])
            for q in range(4):
                pa = nq % 2
                nq += 1
                for i in range(4):
                    ct = q * 4 + i
                    k.tr(psA[pa][:, i * 128:(i + 1) * 128], h_tok[:, tt, ct * 128:(ct + 1) * 128], C["ident"],
                         R=[th[tt], C["t32"]], W=[tpsA[pa]], inc=(i == 3))
                dst = hTt[hb][:, q * 4:(q + 1) * 4, :]
                if q % 2 == 0:
                    k.act(dst, psA[pa][:, :].rearrange("p (a b) -> p a b", a=4), AF.Copy, R=[tpsA[pa]], W=[thT[hb][q]])
                else:
                    k.v("dve", "tensor_copy", dst, psA[pa][:, :].rearrange("p (a b) -> p a b", a=4),
                        R=[tpsA[pa]], W=[thT[hb][q]])
            for ct in range(CT):
                k.mm(psL[:, 0:36], hTt[hb][:, ct, :], Wr[:, ct, :], ct == 0, ct == CT - 1,
                     R=[thT[hb][ct // 4], tWr], W=[tpsL])
            S = ts[tt]
            sc = lambda j: sm[:, tt, j:j + 1]
            Lt = L[:, tt, :]
            les, eq1, le2, eq2, e12 = (r8[:, tt, j, :] for j in range(5))
            Mf, E1, E2, rk, tmp = (r32[:, tt, j, :] for j in range(5))
            dv = lambda name, *a, R=(), W=(): k.v("dve", name, *a, R=[S] + list(R), W=[S] + list(W))
            dv("tensor_tensor", Lt, psL[:, 0:36], rb[:], ALU.add, R=[tpsL, trb])
            dv("reduce_max", sc(0), Lt[:, 0:4], AX.X)
            dv("tensor_scalar", ohg[:, tt, :], Lt[:, 0:4], sc(0), None, ALU.is_equal)
            dv("tensor_scalar", sc(1), sc(0), -1.0, None, ALU.mult)
            k.act(le2[:, 0:4], Lt[:, 0:4], AF.Exp,
                  bias=sc(1), scale=1.0, accum_out=sc(2), R=[S], W=[S])
            dv("reciprocal", sc(3), sc(2))
            dv("tensor_scalar", les, Lt[:, 4:12], ohg[:, tt, 0:1], None, ALU.mult)
            for g in range(1, 4):
                dv("scalar_tensor_tensor", les, Lt[:, 4 + 8 * g:12 + 8 * g], ohg[:, tt, g:g + 1], les,
                   ALU.mult, ALU.add)
            dv("reduce_max", sc(4), les, AX.X)
            dv("tensor_scalar", eq1, les, sc(4), None, ALU.is_equal)
            dv("scalar_tensor_tensor", le2, eq1, -1e30, les, ALU.mult, ALU.add)
            dv("reduce_max", sc(5), le2, AX.X)
            dv("tensor_scalar", eq2, le2, sc(5), None, ALU.is_equal)
            dv("tensor_tensor", sc(6), sc(5), sc(4), ALU.subtract)
            k.act(sc(7), sc(6), AF.Exp, R=[S], W=[S])
            dv("tensor_scalar", sc(8), sc(7), 1.0, None, ALU.add)
            dv("reciprocal", sc(9), sc(8))
            dv("tensor_tensor", wts[:, tt, 0:1], sc(9), sc(3), ALU.mult, W=[t_dw[tt]])
            dv("tensor_tensor", wts[:, tt, 1:2], sc(3), wts[:, tt, 0:1], ALU.subtract, W=[t_dw[tt]])
            dv("tensor_tensor", e12, eq1, eq2, ALU.add)
            for g in range(4):
                gs = slice(8 * g, 8 * g + 8)
                dv("tensor_scalar", Mf[:, gs], e12, ohg[:, tt, g:g + 1], None, ALU.mult)
                dv("tensor_scalar", E1[:, gs], eq1, ohg[:, tt, g:g + 1], None, ALU.mult)
                dv("tensor_scalar", E2[:, gs], eq2, ohg[:, tt, g:g + 1], None, ALU.mult)
            dv("tensor_copy", M_bf[:, tt, :], Mf, W=[tM[tt]])
            for t2 in range(tt + 1):
                lhsT = C["ones_bf"] if t2 < tt else C["tri_bf"]
                k.mm(psR[:, 0:32], lhsT, M_bf[:, t2, :], t2 == 0, t2 == tt, R=[tM[t2], C["tbf"]], W=[tpsR])
            dv("scalar_tensor_tensor", rankm[:, tt, :], psR[:, 0:32], 1.0, Mf, ALU.add, ALU.mult,
               R=[tpsR], W=[t_rank[tt]])
            dv("tensor_scalar", rankm[:, tt, :], rankm[:, tt, :], -1.0, None, ALU.add, W=[t_rank[tt]])
            dv("tensor_tensor", rk, psR[:, 0:32], C["ebase"], ALU.add, R=[tpsR, C["t32"]])
            for j, Ej in enumerate((E1, E2)):
                dv("tensor_tensor", tmp, rk, Ej, ALU.mult)
                dv("reduce_sum", d12[:, tt, j:j + 1], tmp, AX.X)
            dv("tensor_copy", dest[:, tt, :], d12[:, tt, :], W=[t_dw[tt]])

    with k.scope():
        NS = 4
        wsl = [k.sb("wsl", [128, 8192], BF16) for _ in range(NS)]
        twsl = [Trk(f"wsl{i}") for i in range(NS)]
        selq = k.sb("selq", [128, TT, 512], BF16)
        tsel = [Trk(f"selq{i}") for i in range(TT)]
        xq = k.sb("xq", [128, CT, 512], BF16)
        txq = [Trk(f"xq{i}") for i in range(CT)]
        sg = k.sb("sg", [128, 512], F32)
        tsg = Trk("sg")
        hT = k.sb("hT", [128, 512], BF16)
        thh = Trk("hT")
        ye = k.sb("ye", [128, D], BF16)
        tye = [Trk(f"ye{i}") for i in range(4)]
        psG = [k.ps("psG", [128, 512]) for _ in range(2)]
        tpsG = [Trk("psG0"), Trk("psG1")]
        psH1 = k.ps("psH1", [128, 512])
        tH1 = Trk("psH1")
        psH2 = k.ps("psH2", [128, 512])
        tH2 = Trk("psH2")
        psY = [k.ps("psY", [128, 512]) for _ in range(4)]
        tpsY = [Trk(f"psY{i}") for i in range(4)]
        tyb = Trk("ybuf")

        chunks = [(e, c) for e in range(nexp) for c in "gud"]

        def issue(ci):
            if ci >= len(chunks):
                return
            e, c = chunks[ci]
            s = ci % NS
            if wconv is not None and e < wconv.nconv:
                if c == "d":
                    k.dma("sp", wsl[s][:, :].rearrange("p (a n) -> p a n", a=4),
                          wconv.wdn_b[e].rearrange("(a p) n -> p a n", p=128), twsl[s], R=[wconv.trk[e]], W=[twsl[s]])
                else:
                    c0 = 0 if c == "g" else 512
                    k.dma("sp", wsl[s][:, :].rearrange("p (a n) -> p a n", a=16),
                          wconv.wgu_b[e].rearrange("(a p) n -> p a n", p=128)[:, :, c0:c0 + 512], twsl[s],
                          R=[wconv.trk[e]], W=[twsl[s]])
                return
            if c == "d":
                k.dma("pool", wsl[s][:, :].rearrange("p (a n) -> p a n", a=4),
                      wdn_d[e].rearrange("(a p) n -> p a n", p=128), twsl[s], W=[twsl[s]])
            else:
                c0 = 0 if c == "g" else 512
                k.dma("pool", wsl[s][:, :].rearrange("p (a n) -> p a n", a=16),
                      wgu_d[e].rearrange("(a p) n -> p a n", p=128)[:, :, c0:c0 + 512], twsl[s], W=[twsl[s]])

        for ci in range(NS - 1):
            issue(ci)
        ci = 0
        nev = 0
        for q0 in range(0, nexp, 4):
            for tt in range(TT):
                for j in range(4):
                    e = q0 + j
                    k.v("dve", "tensor_scalar", selq[:, tt, j * 128:(j + 1) * 128], C["iota"],
                        rankm[:, tt, e:e + 1], None, ALU.is_equal, R=[t_rank[tt], C["t32"]], W=[tsel[tt]])
            for ct in range(CT):
                pg = ct % 2
                for tt in range(TT):
                    k.mm(psG[pg][:, :], h_bf[:, tt, ct * 128:(ct + 1) * 128], selq[:, tt, :], tt == 0, tt == TT - 1,
                         R=[t_hbf[tt], tsel[tt]], W=[tpsG[pg]])
                if ct % 2 == 0:
                    k.act(xq[:, ct, :], psG[pg][:, :], AF.Copy, R=[tpsG[pg]], W=[txq[ct]])
                else:
                    k.v("dve", "tensor_copy", xq[:, ct, :], psG[pg][:, :], R=[tpsG[pg]], W=[txq[ct]])
            for j in range(4):
                e = q0 + j
                for half, (psH, tH) in enumerate(((psH1, tH1), (psH2, tH2))):
                    s = ci % NS
                    issue(ci + NS - 1)
                    wv = wsl[s][:, :].rearrange("p (a n) -> p a n", a=16)
                    for ft in range(4):
                        for ct in range(CT):
                            k.mm(psH[:, ft * 128:(ft + 1) * 128], wv[:, ct, ft * 128:(ft + 1) * 128],
                                 xq[:, ct, j * 128:(j + 1) * 128], ct == 0, ct == CT - 1,
                                 R=[twsl[s], txq[ct]], W=[tH])
                    ci += 1
                k.act(sg[:], psH1[:, :], AF.Silu, R=[tH1], W=[tsg])
                k.v("dve", "tensor_tensor", hT[:], sg[:], psH2[:, :], ALU.mult, R=[tsg, tH2], W=[thh])
                s = ci % NS
                issue(ci + NS - 1)
                wv = wsl[s][:, :].rearrange("p (a n) -> p a n", a=4)
                for cc in range(4):
                    for ft in range(4):
                        k.mm(psY[cc][:, :], hT[:, ft * 128:(ft + 1) * 128], wv[:, ft, cc * 512:(cc + 1) * 512],
                             ft == 0, ft == 3, R=[thh, twsl[s]], W=[tpsY[cc]])
                    if nev % 2 == 0:
                        k.act(ye[:, cc * 512:(cc + 1) * 512], psY[cc][:, :], AF.Copy, R=[tpsY[cc]], W=[tye[cc]])
                    else:
                        k.v("dve", "tensor_copy", ye[:, cc * 512:(cc + 1) * 512], psY[cc][:, :],
                            R=[tpsY[cc]], W=[tye[cc]])
                    nev += 1
                ci += 1
                k.dma("sp", ybuf[e * 128:(e + 1) * 128, :], ye[:], tyb, R=tye, W=[tyb], join=True)

    with k.scope():
        G = k.sb("lnG", [128, D], F32)
        B = k.sb("lnB", [128, D], F32)
        tgb = Trk("lngb")
        k.dma("sp", G[:], pb(lng_d), tgb, W=[tgb])
        k.dma("sp", B[:], pb(lnb_d), tgb, W=[tgb], join=True)
        Y = [[k.sb("Yg", [128, D], BF16) for _ in range(2)] for _ in range(2)]
        tY = [[Trk(f"Y{a}{b}") for b in range(2)] for a in range(2)]
        st = k.sb("lnst", [128, TT, 40], F32)
        tst = [Trk(f"lnst{i}") for i in range(TT)]
        def gat(tt):
            p = tt % 2
            for j in range(2):
                k.gather(Y[p][j][:], ybuf[:, :], dest[:, tt, j:j + 1], tY[p][j], R=[tyb, t_dw[tt]], W=[tY[p][j]])

        cb_ctx = tile_cb[0]() if tile_cb is not None else None
        gat(0)
        for tt in range(TT):
            p = tt % 2
            if tt + 1 < TT:
                gat(tt + 1)
            x_ap = h_tok[:, tt, :]
            k.op("act", lambda: nc.scalar.mul(x_ap, x_ap, ALPHA), R=[th[tt]], W=[th[tt]])
            k.v("dve", "scalar_tensor_tensor", x_ap, Y[p][0][:], wts[:, tt, 0:1], x_ap, ALU.mult, ALU.add,
                R=[tY[p][0], t_dw[tt], th[tt]], W=[th[tt]])
            k.v("dve", "scalar_tensor_tensor", x_ap, Y[p][1][:], wts[:, tt, 1:2], x_ap, ALU.mult, ALU.add,
                R=[tY[p][1], t_dw[tt], th[tt]], W=[th[tt]])
            emit_ln_tok(k, x_ap, th[tt], G[:], B[:], tgb, st[:, tt, :], tst[tt])
            if tile_cb is not None:
                tile_cb[1](cb_ctx, tt)


def emit_proj_res_ln(k, aT, taT, W_d, h_tok, th, lng_d, lnb_d, tts=None):
    nc = k.nc
    if tts is None:
        tts = list(range(TT))
    with k.scope():
        Wc = [k.sb("Wc", [128, CT, 512], BF16) for _ in range(2)]
        tWc = [Trk("Wc0"), Trk("Wc1")]
        G = k.sb("lnG", [128, D], F32)
        B = k.sb("lnB", [128, D], F32)
        tgb = Trk("lngb")
        k.dma("sp", G[:], pb(lng_d), tgb, W=[tgb])
        k.dma("sp", B[:], pb(lnb_d), tgb, W=[tgb], join=True)
        st = k.sb("lnst", [128, TT, 40], F32)
        tst = [Trk(f"lnst{i}") for i in range(TT)]
        ps = [k.ps("psP", [128, 512]) for _ in range(4)]
        tps = [Trk(f"psP{i}") for i in range(4)]
        Wv = W_d.rearrange("(ct p) n -> p ct n", p=128)

        def issue(cc):
            if cc < 4:
                k.dma("pool", Wc[cc % 2][:], Wv[:, :, cc * 512:(cc + 1) * 512], tWc[cc % 2], W=[tWc[cc % 2]])

        issue(0)
        n = 0
        for cc in range(4):
            issue(cc + 1)
            for i, tt in enumerate(tts):
                p = n % 4
                n += 1
                for ct in range(CT):
                    k.mm(ps[p][:, :], aT[:, ct, i * 128:(i + 1) * 128], Wc[cc % 2][:, ct, :], ct == 0, ct == CT - 1,
                         R=[taT, tWc[cc % 2]], W=[tps[p]])
                dst = h_tok[:, tt, cc * 512:(cc + 1) * 512]
                k.v("dve", "scalar_tensor_tensor", dst, dst, ALPHA, ps[p][:, :], ALU.mult, ALU.add,
                    R=[tps[p], th[tt]], W=[th[tt]])
        for tt in tts:
            emit_ln_tok(k, h_tok[:, tt, :], th[tt], G[:], B[:], tgb, st[:, tt, :], tst[tt])


def load_tok(k, h_tok, th, src_d):
    hv = src_d.rearrange("(tt p) c -> p tt c", p=128)
    for tt in range(TT):
        k.dma("sp", h_tok[:, tt, :], hv[:, tt, :], th[tt], W=[th[tt]])


def store_tok(k, h_tok, th, dst_d, to):
    ov = dst_d.rearrange("(tt p) c -> p tt c", p=128)
    for tt in range(TT):
        k.dma("sp", ov[:, tt, :], h_tok[:, tt, :], to, R=[th[tt]], W=[to], join=True)


def moe_dram(nc, sfx=""):
    d = {}
    d["wg"] = nc.dram_tensor("moe_wg" + sfx, [D, 4], F32, kind="ExternalInput").ap()
    d["bg"] = nc.dram_tensor("moe_bg" + sfx, [1, 4], F32, kind="ExternalInput").ap()
    d["we"] = nc.dram_tensor("moe_we" + sfx, [D, 32], F32, kind="ExternalInput").ap()
    d["be"] = nc.dram_tensor("moe_be" + sfx, [1, 32], F32, kind="ExternalInput").ap()
    d["wgu"] = nc.dram_tensor("moe_wgu" + sfx, [NEXP, D, 2 * DFF], F32, kind="ExternalInput").ap()
    d["wdn"] = nc.dram_tensor("moe_wdn" + sfx, [NEXP, DFF, D], F32, kind="ExternalInput").ap()
    d["ln"] = nc.dram_tensor("ln_gb" + sfx, [4, D], F32, kind="ExternalInput").ap()
    d["ybuf"] = nc.dram_tensor("ybuf" + sfx, [NEXP * 128, D], BF16).ap()
    return d


def moe_inputs(inp, l, sfx=""):
    return {
        "moe_wg" + sfx: np.ascontiguousarray(inp["moe_wg"][l]),
        "moe_bg" + sfx: np.ascontiguousarray(inp["moe_bg"][l][None]),
        "moe_we" + sfx: np.ascontiguousarray(inp["moe_we"][l]),
        "moe_be" + sfx: np.ascontiguousarray(inp["moe_be"][l][None]),
        "moe_wgu" + sfx: np.ascontiguousarray(inp["moe_w_gu"][l]),
        "moe_wdn" + sfx: np.ascontiguousarray(inp["moe_w_down"][l]),
        "ln_gb" + sfx: np.ascontiguousarray(np.stack([inp["ln_g"][l, 0], inp["ln_b"][l, 0], inp["ln_g"][l, 1], inp["ln_b"][l, 1]])),
    }


class WConv:
    def __init__(self, k, d, sfx, nconv):
        nc = k.nc
        self.k, self.d, self.nconv = k, d, nconv
        self.wgu_b = nc.dram_tensor("wgu_b" + sfx, [max(nconv, 1), D, 2 * DFF], BF16).ap()
        self.wdn_b = nc.dram_tensor("wdn_b" + sfx, [max(nconv, 1), DFF, D], BF16).ap()
        self.trk = [Trk(f"wcv{sfx}_{e}") for e in range(nconv)]
        self.jobs = [(e, c) for e in range(nconv) for c in ("gu", "dn")]
        self.nxt = 0

    def pump(self, n=1):
        k = self.k
        for _ in range(n):
            if self.nxt >= len(self.jobs):
                return
            e, c = self.jobs[self.nxt]
            self.nxt += 1
            if c == "gu":
                k.dma("pool", self.wgu_b[e].rearrange("(a p) n -> p a n", p=128),
                      self.d["wgu"][e].rearrange("(a p) n -> p a n", p=128), self.trk[e], W=[self.trk[e]], join=True,
                      background=True)
            else:
                k.dma("pool", self.wdn_b[e].rearrange("(a p) n -> p a n", p=128),
                      self.d["wdn"][e].rearrange("(a p) n -> p a n", p=128), self.trk[e], W=[self.trk[e]], join=True,
                      background=True)

    def flush(self):
        self.pump(len(self.jobs))


def emit_moe_d(k, C, h_tok, th, d, wconv=None, tile_cb=None):
    emit_moe(k, C, h_tok, th, d["wg"], d["bg"], d["we"], d["be"], d["wgu"], d["wdn"], d["ybuf"],
             d["ln"][2:3, :], d["ln"][3:4, :], wconv=wconv, tile_cb=tile_cb)


def emit_moe_d_old(k, C, h_tok, th, d):
    emit_moe(k, C, h_tok, th, d["wg"], d["bg"], d["we"], d["be"], d["wgu"], d["wdn"], d["ybuf"],
             d["ln"][2:3, :], d["ln"][3:4, :])


def build_p3():
    nc = bass.Bass("TRN2", target_bir_lowering=False)
    k = K(nc)
    cst_d = nc.dram_tensor("cst", [128, NC32], F32, kind="ExternalInput").ap()
    h_d = nc.dram_tensor("h_in", [NT, D], F32, kind="ExternalInput").ap()
    oT_d = nc.dram_tensor("oT", [D, NT], F32, kind="ExternalInput").ap()
    wo_d = nc.dram_tensor("w_o", [D, D], F32, kind="ExternalInput").ap()
    md = moe_dram(nc)
    out_d = nc.dram_tensor("out", [NT, D], F32, kind="ExternalOutput").ap()
    C = load_consts(k, cst_d)
    h_tok = k.sb("h_tok", [128, TT, D], F32)
    th = [Trk(f"h{i}") for i in range(TT)]
    load_tok(k, h_tok, th, h_d)
    with k.scope():
        aT = k.sb("aT", [128, CT, NT], BF16)
        taT = Trk("aT")
        k.dma("pool", aT[:], oT_d.rearrange("(ct p) t -> p ct t", p=128), taT, W=[taT])
        emit_proj_res_ln(k, aT, taT, wo_d, h_tok, th, md["ln"][0:1, :], md["ln"][1:2, :])
    emit_moe_d(k, C, h_tok, th, md)
    to = Trk("out")
    store_tok(k, h_tok, th, out_d, to)
    k.barrier()
    k.close()
    return nc


HALO = 30
NXC = NT + HALO + 2
NCPAR = 16 * 31 + 16 * 3 + 32


def conv_params(inp):
    wdw = inp["conv_w_dw"][0]
    a = wdw.T.reshape(CT, 128, 31).transpose(1, 0, 2).reshape(128, CT * 31)
    cols = [a]
    for v in (inp["conv_b_dw"][0], inp["conv_ln_g"][0], inp["conv_ln_b"][0]):
        cols.append(v.reshape(CT, 128).T)
    cols.append(inp["conv_b_pw1"][0].reshape(32, 128).T)
    return np.ascontiguousarray(np.concatenate(cols, axis=1).astype(np.float32))


def emit_conv(k, C, xT_d, hm_d, cpar_d, w1_d, w2_d, h_tok, th, lng_d, lnb_d, dbg=None, bg=None):
    nc = k.nc
    with k.scope():
        cp = k.sb("cpar", [128, NCPAR], F32)
        tcp = Trk("cpar")
        k.dma("sp", cp[:], cpar_d, tcp, W=[tcp])
        wdw = cp[:, 0:CT * 31].rearrange("p (j t) -> p j t", j=CT)
        bdw = cp[:, 496:512]
        cg = cp[:, 512:528]
        cb = cp[:, 528:544]
        b1 = cp[:, 544:576]
        hm = k.sb("hm", [128, 1], F32)
        thm = Trk("hm")
        k.dma("sp", hm[:], pb(hm_d), thm, W=[thm])
        gluT = k.sb("gluT", [128, CT, NXC], BF16)
        tglu = [Trk(f"glu{j}") for j in range(CT)]
        with k.scope():
            xT = k.sb("xT", [128, CT, NXC], BF16)
            txT = Trk("xT")
            k.dma("pool", xT[:, :, 0:NT + HALO], xT_d.rearrange("(ct p) t -> p ct t", p=128), txT, W=[txT])
            NS = 3
            wsl = [k.sb("w1s", [128, CT, 512], BF16) for _ in range(NS)]
            twsl = [Trk(f"w1s{i}") for i in range(NS)]
            sig = [k.sb("sig", [128, 512], F32) for _ in range(2)]
            tsig = [Trk("sig0"), Trk("sig1")]
            psA = [k.ps("psA", [128, 512]) for _ in range(2)]
            tpA = [Trk("pA0"), Trk("pA1")]
            psB = [k.ps("psB", [128, 512]) for _ in range(2)]
            tpB = [Trk("pB0"), Trk("pB1")]
            W1v = w1_d.rearrange("(ct p) n -> p ct n", p=128)
            loads = []
            for jg in range(4):
                loads.append(jg * 512)
                loads.append(D + jg * 512)

            nxt = [0]

            def pump(done):
                while nxt[0] < len(loads) and (nxt[0] < NS or (nxt[0] - NS) // 2 < done):
                    i = nxt[0]
                    s_ = i % NS
                    k.dma("pool", wsl[s_][:], W1v[:, :, loads[i]:loads[i] + 512], twsl[s_], W=[twsl[s_]])
                    nxt[0] += 1

            n = 0
            for jg in range(4):
                pump(jg)
                assert nxt[0] >= 2 * jg + 2
                sa, sg_ = (2 * jg) % NS, (2 * jg + 1) % NS
                for jj in range(4):
                    j = jg * 4 + jj
                    for c0, c1 in ((0, 512), (512, 1024), (1024, NT + HALO)):
                        w = c1 - c0
                        p = n % 2
                        n += 1
                        for ct in range(CT):
                            k.mm(psA[p][:, 0:w], wsl[sa][:, ct, jj * 128:(jj + 1) * 128], xT[:, ct, c0:c1],
                                 ct == 0, ct == CT - 1, R=[twsl[sa], txT], W=[tpA[p]])
                        for ct in range(CT):
                            k.mm(psB[p][:, 0:w], wsl[sg_][:, ct, jj * 128:(jj + 1) * 128], xT[:, ct, c0:c1],
                                 ct == 0, ct == CT - 1, R=[twsl[sg_], txT], W=[tpB[p]])
                        k.act(sig[p][:, 0:w], psB[p][:, 0:w], AF.Sigmoid, bias=b1[:, 16 + j:17 + j], scale=1.0,
                              R=[tpB[p], tcp], W=[tsig[p]])
                        k.v("dve", "scalar_tensor_tensor", gluT[:, j, c0:c1], psA[p][:, 0:w], b1[:, j:j + 1],
                            sig[p][:, 0:w], ALU.add, ALU.mult, R=[tpA[p], tsig[p], tcp], W=[tglu[j]])
                    if bg is not None:
                        bg("pw1")
            for j in range(CT):
                k.v("dve", "tensor_scalar", gluT[:, j, 0:HALO], gluT[:, j, 0:HALO], hm[:, 0:1], None, ALU.mult,
                    R=[thm], W=[tglu[j]])
        if dbg is not None:
            tdb = Trk("dbg")
            k.dma("sp", dbg["glu"].rearrange("(j p) t -> p j t", p=128), gluT[:], tdb, R=tglu, W=[tdb], join=True)
        for tc in range(2):
            with k.scope():
                sT = k.sb("sT", [128, CT, 512], BF16)
                tsT = Trk("sT")
                with k.scope():
                    v = k.sb("cv", [128, CT, 512], F32)
                    tv = [Trk(f"cv{j}") for j in range(CT)]
                    dg = [k.sb("dg", [128, 31, 128], BF16) for _ in range(2)]
                    tdg = [Trk("dg0"), Trk("dg1")]
                    vsq = [k.sb("vsq", [128, 512], F32) for _ in range(2)]
                    tvsq = [Trk("vsq0"), Trk("vsq1")]
                    psC = [k.ps("psC", [128, 512]) for _ in range(2)]
                    tpC = [Trk("pC0"), Trk("pC1")]
                    psS = k.ps("psS", [128, 512])
                    tpS = Trk("pS")
                    psQ = k.ps("psQ", [128, 512])
                    tpQ = Trk("pQ")
                    for j in range(CT):
                        p = j % 2
                        for t in range(31):
                            k.v("dve", "tensor_scalar", dg[p][:, t, :], C["ident_bf"],
                                wdw[:, j, t:t + 1], None, ALU.mult, R=[C["tbf"], tcp], W=[tdg[p]])
                        for t in range(31):
                            k.mm(psC[p][:, :], dg[p][:, t, :], gluT[:, j, tc * 512 + t:tc * 512 + t + 512], t == 0, t == 30,
                                 R=[tdg[p], tglu[j]], W=[tpC[p]])
                        k.act(v[:, j, :], psC[p][:, :], AF.Identity, bias=bdw[:, j:j + 1], scale=1.0,
                              R=[tpC[p], tcp], W=[tv[j]])
                        k.act(vsq[p][:], v[:, j, :], AF.Square, R=[tv[j]], W=[tvsq[p]])
                        k.mm(psS[:, :], C["ones"], v[:, j, :], j == 0, j == CT - 1, R=[C["t32"], tv[j]], W=[tpS])
                        k.mm(psQ[:, :], C["ones"], vsq[p][:], j == 0, j == CT - 1, R=[C["t32"], tvsq[p]], W=[tpQ])
                        if bg is not None:
                            bg("conv")
                    if dbg is not None:
                        k.dma("sp", dbg["v"][tc].rearrange("(j p) t -> p j t", p=128), v[:], tdb, R=tv, W=[tdb], join=True)
                    mu = k.sb("mu", [128, 512], F32)
                    rs = k.sb("rs", [128, 512], F32)
                    m2 = k.sb("m2", [128, 512], F32)
                    tm = Trk("murs")
                    k.v("dve", "tensor_scalar", mu[:], psS[:, :], 1.0 / D, None, ALU.mult, R=[tpS], W=[tm])
                    k.v("dve", "tensor_tensor", m2[:], mu[:], mu[:], ALU.mult, R=[tm], W=[tm])
                    k.v("dve", "scalar_tensor_tensor", m2[:], psQ[:, :], 1.0 / D, m2[:], ALU.mult, ALU.subtract,
                        R=[tpQ, tm], W=[tm])
                    k.act(m2[:], m2[:], AF.Sqrt, bias=LN_EPS, scale=1.0, R=[tm], W=[tm])
                    k.v("dve", "reciprocal", rs[:], m2[:], R=[tm], W=[tm])
                    for j in range(CT):
                        k.v("dve", "tensor_tensor", v[:, j, :], v[:, j, :], mu[:], ALU.subtract, R=[tm, tv[j]], W=[tv[j]])
                        k.v("dve", "tensor_tensor", v[:, j, :], v[:, j, :], rs[:], ALU.mult, R=[tm, tv[j]], W=[tv[j]])
                        k.act(sT[:, j, :], v[:, j, :], AF.Silu, bias=cb[:, j:j + 1], scale=cg[:, j:j + 1],
                              R=[tv[j], tcp], W=[tsT])
                if dbg is not None:
                    k.dma("sp", dbg["s"][tc].rearrange("(j p) t -> p j t", p=128), sT[:], tdb, R=[tsT], W=[tdb], join=True)
                emit_proj_res_ln(k, sT, tsT, w2_d, h_tok, th, lng_d, lnb_d, tts=[tc * 4 + i for i in range(4)])


def build_p1():
    nc = bass.Bass("TRN2", target_bir_lowering=False)
    k = K(nc)
    cst_d = nc.dram_tensor("cst", [128, NC32], F32, kind="ExternalInput").ap()
    x_d = nc.dram_tensor("x_tok", [NT, D], F32, kind="ExternalInput").ap()
    xT_d = nc.dram_tensor("xT", [D, NT + HALO], F32, kind="ExternalInput").ap()
    hm_d = nc.dram_tensor("hm", [1, 1], F32, kind="ExternalInput").ap()
    cpar_d = nc.dram_tensor("cpar", [128, NCPAR], F32, kind="ExternalInput").ap()
    w1_d = nc.dram_tensor("w_pw1", [D, 2 * D], F32, kind="ExternalInput").ap()
    w2_d = nc.dram_tensor("w_pw2", [D, D], F32, kind="ExternalInput").ap()
    md = moe_dram(nc)
    out_d = nc.dram_tensor("out", [NT, D], F32, kind="ExternalOutput").ap()
    C = load_consts(k, cst_d)
    h_tok = k.sb("h_tok", [128, TT, D], F32)
    th = [Trk(f"h{i}") for i in range(TT)]
    load_tok(k, h_tok, th, x_d)
    emit_conv(k, C, xT_d, hm_d, cpar_d, w1_d, w2_d, h_tok, th, md["ln"][0:1, :], md["ln"][1:2, :])
    emit_moe_d(k, C, h_tok, th, md)
    to = Trk("out")
    store_tok(k, h_tok, th, out_d, to)
    k.barrier()
    k.close()
    return nc


def p1_inputs(inp, c):
    b, j = divmod(c, 4)
    x = inp["x"][b]
    s0 = j * NT
    xe = np.zeros((NT + HALO, D), np.float32)
    if j > 0:
        xe[:] = x[s0 - HALO:s0 + NT]
    else:
        xe[HALO:] = x[0:NT]
    d = dict(cst=make_consts(), x_tok=np.ascontiguousarray(x[s0:s0 + NT]), xT=np.ascontiguousarray(xe.T),
             hm=np.full((1, 1), 0.0 if j == 0 else 1.0, np.float32), cpar=conv_params(inp),
             w_pw1=np.ascontiguousarray(inp["conv_w_pw1"][0]), w_pw2=np.ascontiguousarray(inp["conv_w_pw2"][0]))
    d.update(moe_inputs(inp, 0))
    return d


NQB = SEQ // 128
NCMP = 255
QSCALE = 128.0 ** -0.5
NCM = 33


def _split3(v):
    import ml_dtypes
    v = v.astype(np.float32)
    a = v.astype(ml_dtypes.bfloat16).astype(np.float32)
    b = (v - a).astype(ml_dtypes.bfloat16).astype(np.float32)
    c = (v - a - b).astype(ml_dtypes.bfloat16).astype(np.float32)
    return a, b, c


def attn_consts(g):
    slopes = np.array([2.0 ** (-8.0 * (4 * g + hh + 1) / 16.0) for hh in range(4)], np.float64)
    kr = np.arange(128)
    LT = np.zeros((128, 32 * 128), np.float32)
    for d in range(32):
        for hh in range(4):
            val = slopes[hh] * (128.0 * (-d) + kr - 64.0)
            for s_, part in enumerate(_split3(val)):
                LT[hh * 3 + s_, d * 128:(d + 1) * 128] = part
    LTc = np.zeros((128, 32 * 128), np.float32)
    for dc in range(32):
        for hh in range(4):
            val = slopes[hh] * (16.0 * kr - 33.0 - 128.0 * dc)
            for s_, part in enumerate(_split3(val)):
                LTc[hh * 3 + s_, dc * 128:(dc + 1) * 128] = part
    HI = np.zeros((128, 512), np.float32)
    for hh in range(4):
        HI[hh * 3:hh * 3 + 3, hh * 128:(hh + 1) * 128] = 1.0
    E = np.zeros((128, SEQ), np.float32)
    for j in range(64):
        E[j, j * 64:(j + 1) * 64] = 1.0
    qr = np.arange(128)
    Mc = np.where(kr[:, None] <= qr[None, :], 0.0, NEG).astype(np.float32)
    Mw = np.where(kr[:, None] > qr[None, :], 0.0, NEG).astype(np.float32)
    Mc4 = np.tile(Mc, (1, 4))
    Mw4 = np.tile(Mw, (1, 4))
    CM = np.zeros((128, NCM * 128), np.float32)
    for di in range(17):
        m = np.where(16 * kr[:, None] + 31 <= 128 * di + qr[None, :], 0.0, NEG)
        CM[:, di * 128:(di + 1) * 128] = m
    for di in range(16):
        m = np.where(16 * kr[:, None] + 31 <= 128 * di + qr[None, :], 0.0, NEG)
        m[127, :] = NEG
        CM[:, (17 + di) * 128:(18 + di) * 128] = m
    n = np.arange(256)[:, None]
    j = np.arange(64)[None, :]
    ov = np.maximum(np.minimum(16 * n + 32, 64 * j + 64) - np.maximum(16 * n, 64 * j), 0) / 32.0
    ov[255] = 0
    ovl = ov.reshape(2, 128, 64).transpose(1, 0, 2).reshape(128, 128)
    ident = np.eye(128, dtype=np.float32)
    t = np.arange(SEQ)
    cur = (t // 64)[:, None]
    blk = np.arange(64)[None, :]
    forced = ((blk == 0) | (blk == cur) | (blk == cur - 1)).astype(np.float32)
    cand = ((blk >= 1) & (blk <= cur - 2)).astype(np.float32)
    f3 = forced.reshape(32, 128, 64).transpose(1, 0, 2).reshape(128, 32 * 64)
    c3 = cand.reshape(32, 128, 64).transpose(1, 0, 2).reshape(128, 32 * 64)
    cb = np.concatenate([LT, LTc, HI, E, Mc4, Mw4, CM, ovl, ident, c3, f3], axis=1).astype(np.float32)
    cf = ident.copy()
    return np.ascontiguousarray(cb), np.ascontiguousarray(cf)


CB_OFF = {}
_o = 0
for _n, _w in (("LT", 32 * 128), ("LTc", 32 * 128), ("HI", 512), ("E", SEQ), ("Mc4", 512), ("Mw4", 512),
               ("CM", NCM * 128), ("ov", 128), ("ident", 128), ("cand", 2048), ("forced", 2048)):
    CB_OFF[_n] = (_o, _o + _w)
    _o += _w
NCB = _o
NCF = 128
NWF = 1024
NWT = 268


def attn_weights(inp, g):
    wqg = inp["nsa_w_qg"][0]
    kvw = inp["kv_w"]
    sl = lambda s_: kvw[:, s_ * 512 + g * 128: s_ * 512 + (g + 1) * 128]
    WF = np.concatenate([wqg[:, g * 512:(g + 1) * 512], sl(2), sl(4), sl(0), sl(1)], axis=1)
    WT = np.concatenate([sl(3), sl(5), wqg[:, 2048 + g * 12: 2048 + (g + 1) * 12]], axis=1)
    posT = np.concatenate([inp["cmp_pos"][0].T, inp["cmp_pos"][1].T], axis=1)
    return dict(WF=np.ascontiguousarray(WF), WT=np.ascontiguousarray(WT), posT=np.ascontiguousarray(posT),
                cw1=np.ascontiguousarray(inp["cmp_w1"]), cw2=np.ascontiguousarray(inp["cmp_w2"]))


def emit_attn(k, hT_d, WF_d, WT_d, posT_d, cw1_d, cw2_d, cb_d, cf_d, o_d, dbg=None, hsrc=None, ob=None, tob=None,
              chunk_order=None, after_qb=None, bg=None):
    nc = k.nc
    cbs = k.sb("cbs", [128, NCB], BF16)
    tcb = Trk("cbs")
    k.dma("pool", cbs[:], cb_d, tcb, W=[tcb])
    cfs = k.sb("cfs", [128, NCF], F32)
    tcf = Trk("cfs")
    k.dma("sp", cfs[:], cf_d, tcf, W=[tcf])
    cbv = lambda n_: cbs[:, CB_OFF[n_][0]:CB_OFF[n_][1]]
    LT, LTc, HI, E_, Mc4, Mw4, CM, ovl, identb = (cbv(n_) for n_ in ("LT", "LTc", "HI", "E", "Mc4", "Mw4", "CM", "ov", "ident"))
    cand = cbv("cand").rearrange("p (a b) -> p a b", a=32)
    forced = cbv("forced").rearrange("p (a b) -> p a b", a=32)
    identf = cfs[:, 0:128]
    QT = k.sb("QT", [128, NQB, 4, 128], BF16)
    tQ = [Trk(f"Q{i}") for i in range(16)]
    KsT = k.sb("KsT", [128, SEQ], BF16)
    KwT = k.sb("KwT", [128, SEQ], BF16)
    tK = [Trk(f"K{i}") for i in range(16)]
    Vs = k.sb("Vs", [128, NQB, 132], BF16)
    Vw = k.sb("Vw", [128, NQB, 132], BF16)
    tV = [Trk(f"V{i}") for i in range(16)]
    gat = k.sb("gat", [128, NQB, 12], F32)
    KcT = k.sb("KcT", [128, 256], BF16)
    Vc = k.sb("Vc", [128, 2, 196], BF16)
    tC = Trk("cmpkv")
    k.v("dve", "memset", Vs[:, :, 128:132], 1.0, W=tV)
    k.v("dve", "memset", Vw[:, :, 128:132], 1.0, W=tV)
    k.v("dve", "memset", Vc[:], 0.0, W=[tC])
    k.v("dve", "memset", KcT[:], 0.0, W=[tC])

    with k.scope():
        rawT = [k.sb("rawT", [128, SEQ], BF16) for _ in range(2)]
        traw = [Trk(f"raw{i}") for i in range(16)]
        with k.scope():
            WF = k.sb("WF", [128, CT, NWF], BF16)
            tWF = Trk("WF")
            WFv = WF_d.rearrange("(ct p) n -> p ct n", p=128)
            k.dma("pool", WF[:, :, 0:512], WFv[:, :, 0:512], tWF, W=[tWF])
            k.dma("pool", WF[:, :, 512:1024], WFv[:, :, 512:1024], tWF, W=[tWF], join=True)
            WT = k.sb("WT", [128, CT, NWT], BF16)
            tWT = Trk("WT")
            k.dma("pool", WT[:], WT_d.rearrange("(ct p) n -> p ct n", p=128), tWT, W=[tWT])
            hc = [k.sb("hc", [128, CT, 256], BF16) for _ in range(2)]
            thc = [Trk("hc0"), Trk("hc1")]
            psF = [k.ps("psF", [128, 512]) for _ in range(3)]
            tpF = [Trk(f"pF{i}") for i in range(3)]
            psT = [k.ps("psT", [128, 512]) for _ in range(2)]
            tpT = [Trk(f"pT{i}") for i in range(2)]
            hv = hT_d.rearrange("(ct p) t -> p ct t", p=128) if hT_d is not None else None

            def issue(c):
                if c < 16:
                    if hsrc is None:
                        k.dma("pool", hc[c % 2][:], hv[:, :, c * 256:(c + 1) * 256], thc[c % 2], W=[thc[c % 2]])
                    else:
                        ap_, Rr = hsrc(c)
                        k.dma("sp", hc[c % 2][:], ap_, thc[c % 2], R=Rr, W=[thc[c % 2]])

            order = list(range(16)) if chunk_order is None else list(chunk_order)

            def issue(i):
                if i < 16:
                    c_ = order[i]
                    if hsrc is None:
                        k.dma("pool", hc[i % 2][:], hv[:, :, c_ * 256:(c_ + 1) * 256], thc[i % 2], W=[thc[i % 2]])
                    else:
                        ap_, Rr = hsrc(c_)
                        k.dma("sp", hc[i % 2][:], ap_, thc[i % 2], R=Rr, W=[thc[i % 2]])

            issue(0)
            nf = 0
            ntk = 0
            for ci_, c in enumerate(order):
                issue(ci_ + 1)
                H = hc[ci_ % 2]
                tH = thc[ci_ % 2]
                for f in range(8):
                    p = nf % 3
                    nf += 1
                    for ct in range(CT):
                        k.mm(psF[p][:, 0:256], WF[:, ct, f * 128:(f + 1) * 128], H[:, ct, :], ct == 0, ct == CT - 1,
                             R=[tWF, tH], W=[tpF[p]])
                    if f < 4:
                        k.act(QT[:, 2 * c:2 * c + 2, f, :], psF[p][:, 0:256].rearrange("p (a b) -> p a b", a=2), AF.Copy,
                              scale=QSCALE, R=[tpF[p]], W=[tQ[c]])
                    else:
                        dst = (KsT, KwT, rawT[0], rawT[1])[f - 4]
                        trk = (tK[c], tK[c], traw[c], traw[c])[f - 4]
                        if f % 2 == 0:
                            k.v("dve", "tensor_copy", dst[:, c * 256:(c + 1) * 256], psF[p][:, 0:256], R=[tpF[p]], W=[trk])
                        else:
                            k.act(dst[:, c * 256:(c + 1) * 256], psF[p][:, 0:256], AF.Copy, R=[tpF[p]], W=[trk])
                for i in range(2):
                    tt = c * 2 + i
                    p = ntk % 2
                    ntk += 1
                    for ct in range(CT):
                        k.mm(psT[p][:, 0:NWT], H[:, ct, i * 128:(i + 1) * 128], WT[:, ct, :], ct == 0, ct == CT - 1,
                             R=[tH, tWT], W=[tpT[p]])
                    k.v("dve", "tensor_copy", Vs[:, tt, 0:128], psT[p][:, 0:128], R=[tpT[p]], W=[tV[c]])
                    k.v("dve", "tensor_copy", Vw[:, tt, 0:128], psT[p][:, 128:256], R=[tpT[p]], W=[tV[c]])
                    k.act(gat[:, tt, :], psT[p][:, 256:268], AF.Sigmoid, R=[tpT[p]], W=[tV[c]])
                if bg is not None:
                    bg("proj")
        with k.scope():
            posT = k.sb("posT", [128, 64], BF16)
            tpos = Trk("posT")
            k.dma("pool", posT[:], posT_d, tpos, W=[tpos])
            W1 = [k.sb("cW1", [128, 32, 256], BF16) for _ in range(2)]
            W2 = [k.sb("cW2", [128, 2, 128], BF16) for _ in range(2)]
            tW = Trk("cW")
            for c in range(2):
                k.dma("pool", W1[c][:], cw1_d[c].rearrange("(cb p) n -> p cb n", p=128), tW, W=[tW], join=True)
                k.dma("pool", W2[c][:], cw2_d[c].rearrange("(a p) n -> p a n", p=128), tW, W=[tW], join=True)
            psH = [k.ps("psH", [128, 512]) for _ in range(2)]
            tpH = [Trk("pH0"), Trk("pH1")]
            psB = k.ps("psB", [128, 512])
            tpB = Trk("pB")
            psO = k.ps("psO", [128, 512])
            tpO = Trk("pO")
            hb = k.sb("hbias", [128, 4], F32)
            thb = Trk("hbias")
            xg = k.sb("xg", [128, 3, 256], F32)
            txg = Trk("xg")
            hid = [[k.sb("hid", [128, 256], BF16) for _ in range(2)] for _ in range(2)]
            thid = [[Trk(f"hid{a}{b}") for b in range(2)] for a in range(2)]
            nh = 0
            for c in range(2):
                rv = rawT[c][:, :].rearrange("p (n s) -> p n s", s=16)
                for hk in range(2):
                    for cb_ in range(32):
                        k.mm(psB[:, hk:hk + 1], W1[c][:, cb_, hk * 128:(hk + 1) * 128], posT[:, c * 32 + cb_:c * 32 + cb_ + 1],
                             cb_ == 0, cb_ == 31, R=[tW, tpos], W=[tpB])
                    k.v("dve", "tensor_copy", hb[:, c * 2 + hk:c * 2 + hk + 1], psB[:, hk:hk + 1], R=[tpB], W=[thb])
                    p = nh % 2
                    nh += 1
                    for cb_ in range(32):
                        k.mm(psH[p][:, 0:NCMP], W1[c][:, cb_, hk * 128:(hk + 1) * 128],
                             rv[:, cb_ // 16:cb_ // 16 + NCMP, cb_ % 16], cb_ == 0, cb_ == 31,
                             R=[tW] + traw, W=[tpH[p]])
                    x_ = xg[:, 0, 0:NCMP]
                    u_ = xg[:, 1, 0:NCMP]
                    s_ = xg[:, 2, 0:NCMP]
                    k.act(x_, psH[p][:, 0:NCMP], AF.Identity, bias=hb[:, c * 2 + hk:c * 2 + hk + 1], scale=1.0,
                          R=[tpH[p], thb], W=[txg])
                    k.v("dve", "tensor_tensor", u_, x_, x_, ALU.mult, R=[txg], W=[txg])
                    k.v("dve", "tensor_scalar", u_, u_, 0.044715, 1.0, ALU.mult, ALU.add, R=[txg], W=[txg])
                    k.v("dve", "tensor_tensor", u_, u_, x_, ALU.mult, R=[txg], W=[txg])
                    k.act(s_, u_, AF.Sigmoid, scale=2.0 * 0.7978845608028654, R=[txg], W=[txg])
                    k.v("dve", "tensor_tensor", hid[c][hk][:, 0:NCMP], x_, s_, ALU.mult, R=[txg], W=[thid[c][hk]])
            for hk in range(2):
                k.mm(psO[:, 0:NCMP], W2[0][:, hk, :], hid[0][hk][:, 0:NCMP], hk == 0, hk == 1,
                     R=[tW, thid[0][hk]], W=[tpO])
            k.v("dve", "tensor_copy", KcT[:, 0:NCMP], psO[:, 0:NCMP], R=[tpO], W=[tC])
            for nt, (n0, n1) in enumerate(((0, 128), (128, NCMP))):
                w = n1 - n0
                for hk in range(2):
                    k.mm(psO[0:w, 256:384], hid[1][hk][:, n0:n1], W2[1][:, hk, :], hk == 0, hk == 1,
                         R=[tW, thid[1][hk]], W=[tpO])
                k.v("dve", "tensor_copy", Vc[0:w, nt, 0:128], psO[0:w, 256:384], R=[tpO], W=[tC])
                k.v("dve", "tensor_copy", Vc[0:w, nt, 128:192], ovl[0:w, nt * 64:(nt + 1) * 64], R=[tcb], W=[tC])
                k.v("dve", "memset", Vc[0:w, nt, 192:193], 1.0, W=[tC])
    if dbg is not None:
        tdb = Trk("dbg")
        k.dma("sp", dbg["KcT"], KcT[:], tdb, R=[tC], W=[tdb], join=True)
        k.dma("sp", dbg["Vc"], Vc[:], tdb, R=[tC], W=[tdb], join=True)
        k.dma("sp", dbg["QT"], QT[:], tdb, R=tQ, W=[tdb], join=True)
        k.dma("sp", dbg["KsT"], KsT[:], tdb, R=tK, W=[tdb], join=True)
        k.dma("sp", dbg["Vs"], Vs[:], tdb, R=tV, W=[tdb], join=True)
        k.dma("sp", dbg["gat"], gat[:], tdb, R=tV, W=[tdb], join=True)

    with k.scope():
        NPS = 3
        psS = [k.ps("psS", [128, 512]) for _ in range(NPS)]
        tpS = [Trk(f"pS{i}") for i in range(NPS)]
        psO = [[k.ps("psOa", [128, 512]) for _ in range(2)] for _ in range(2)]
        tpO = [[Trk(f"pO{a}{b}") for b in range(2)] for a in range(2)]
        psX = k.ps("psX", [128, 512])
        tpX = Trk("pX")
        NPT = 4
        PT = [k.sb("PT", [128, 512], BF16) for _ in range(NPT)]
        tPT = [Trk(f"PT{i}") for i in range(NPT)]
        oacc = [k.sb("oacc", [128, 512], F32) for _ in range(3)]
        toacc = [Trk("oacc0"), Trk("oacc1"), Trk("oacc2")]
        imp = [k.sb("imp", [128, 64], F32) for _ in range(2)]
        timp = [Trk("imp0"), Trk("imp1")]
        sm = k.sb("asm", [128, 6, 16], F32)
        tsm = [Trk(f"asm{i}") for i in range(6)]
        selw = k.sb("selw", [128, 2, 4, 64], F32)
        tselw = [Trk("selw0"), Trk("selw1")]
        NSB = 3
        selb4 = [k.sb("selb4", [64, 512], BF16) for _ in range(NSB)]
        tsb4 = [Trk(f"selb4{i}") for i in range(NSB)]
        tout = Trk("o_out")
        oTt = [k.sb("oTt", [128, 4, 128], BF16) for _ in range(2)]
        toTt = [Trk("oTt0"), Trk("oTt1")]
        st = dict(ns=0, npt=0, no=0, nsm=0)
        Qv = lambda qb: QT[:, qb, :, :].rearrange("p h q -> p (h q)")
        glist = []

        def add_group(qb, lhsK, tKk, extra, Vaug, vw, tVv, first, last, ob, after=None):
            slot = {}

            def scores():
                s_ = st["ns"] % NPS
                st["ns"] += 1
                k.mm(psS[s_][:, :], lhsK, Qv(qb), True, False, R=[tKk, tQ[qb // 2]], W=[tpS[s_]], inc=False)
                for i, (l_, r_, o_, Rr) in enumerate(extra):
                    out_ap = psS[s_][:, :] if o_ is None else psS[s_][:, o_[0]:o_[1]]
                    k.mm(out_ap, l_, r_, False, i == len(extra) - 1, R=Rr, W=[tpS[s_]], inc=(i == len(extra) - 1))
                pt = st["npt"] % NPT
                st["npt"] += 1
                slot["pt"] = pt
                k.act(PT[pt][:], psS[s_][:, :], AF.Exp, R=[tpS[s_]], W=[tPT[pt]])

            def pv():
                pt = slot["pt"]
                for hh in range(4):
                    bank = psO[ob][hh // 2]
                    c0 = (hh % 2) * 256
                    k.mm(bank[:, c0:c0 + vw], PT[pt][:, hh * 128:(hh + 1) * 128], Vaug, first and hh % 2 == 0, last,
                         R=[tPT[pt], tVv], W=[tpO[ob][hh // 2]], inc=last, skip_group_check=True)

            glist.append((scores, pv, after))

        def finish(qb, ob, br, first_branch, vw, with_imp=False):
            oa = oacc[qb % 3]
            to_ = toacc[qb % 3]
            i6 = st["nsm"] % 6
            st["nsm"] += 1
            S_ = tsm[i6]
            for hh in range(4):
                bank = psO[ob][hh // 2]
                tb = tpO[ob][hh // 2]
                c0 = (hh % 2) * 256
                rs = sm[:, i6, hh * 4:hh * 4 + 1]
                sc = sm[:, i6, hh * 4 + 1:hh * 4 + 2]
                k.v("dve", "tensor_scalar", rs, bank[:, c0 + vw - 1:c0 + vw], 1e-30, None, ALU.max, R=[tb, S_], W=[S_])
                k.v("dve", "reciprocal", rs, rs, R=[S_], W=[S_])
                k.v("dve", "tensor_tensor", sc, rs, gat[:, qb, hh * 3 + br:hh * 3 + br + 1], ALU.mult,
                    R=[S_, tV[qb // 2]], W=[S_])
                dst = oa[:, hh * 128:(hh + 1) * 128]
                if first_branch:
                    k.v("dve", "tensor_scalar", dst, bank[:, c0:c0 + 128], sc, None, ALU.mult, R=[tb, S_], W=[to_])
                else:
                    k.v("dve", "scalar_tensor_tensor", dst, bank[:, c0:c0 + 128], sc, dst, ALU.mult, ALU.add,
                        R=[tb, S_], W=[to_])
                if with_imp:
                    im = imp[qb % 2]
                    if hh == 0:
                        k.v("dve", "tensor_scalar", im[:], bank[:, c0 + 128:c0 + 192], rs, None, ALU.mult,
                            R=[tb, S_], W=[timp[qb % 2]])
                    else:
                        k.v("dve", "scalar_tensor_tensor", im[:], bank[:, c0 + 128:c0 + 192], rs, im[:], ALU.mult,
                            ALU.add, R=[tb, S_], W=[timp[qb % 2]])

        deferred = []

        def defer(n, fn):
            deferred.append([n, fn])

        def select_math(qb):
            w = qb % 2
            V_ = selw[:, w, 0, :]
            V2 = selw[:, w, 1, :]
            sl_ = selw[:, w, 2, :]
            m8 = selw[:, w, 3, 0:16]
            T_ = tselw[w]
            k.v("dve", "scalar_tensor_tensor", V_, imp[w][:], 1.0, cand[:, qb, :], ALU.add, ALU.mult,
                R=[timp[w], tcb], W=[T_])
            k.v("dve", "tensor_scalar", V_, V_, -1.0, None, ALU.add, R=[T_], W=[T_])
            k.v("dve", "max", m8[:, 0:8], V_, R=[T_], W=[T_])
            k.v("dve", "match_replace", V2, m8[:, 0:8], V_, -2.0, R=[T_], W=[T_])
            k.v("dve", "max", m8[:, 8:16], V2, R=[T_], W=[T_])
            k.v("dve", "tensor_scalar", sl_, V_, m8[:, 12:13], None, ALU.is_ge, R=[T_], W=[T_])
            k.v("dve", "tensor_tensor", sl_, sl_, cand[:, qb, :], ALU.mult, R=[T_, tcb], W=[T_])
            k.v("dve", "tensor_tensor", sl_, sl_, forced[:, qb, :], ALU.max, R=[T_, tcb], W=[T_])
            k.v("dve", "tensor_scalar", sl_, sl_, -NEG, NEG, ALU.mult, ALU.add, R=[T_], W=[T_])

            def tr_part():
                k.tr(psX[0:64, 0:128], sl_, identf, R=[T_, tcf], W=[tpX])
                sb_ = qb % NSB
                for hh in range(4):
                    if hh % 2 == 0:
                        k.act(selb4[sb_][:, hh * 128:(hh + 1) * 128], psX[0:64, 0:128], AF.Copy, R=[tpX], W=[tsb4[sb_]])
                    else:
                        k.v("dve", "tensor_copy", selb4[sb_][:, hh * 128:(hh + 1) * 128], psX[0:64, 0:128],
                            R=[tpX], W=[tsb4[sb_]])
            defer(2, tr_part)

        def out_part(qb):
            if ob is None:
                k.dma("sp", o_d[qb * 128:(qb + 1) * 128, :], oacc[qb % 3][:], tout, R=[toacc[qb % 3]], W=[tout], join=True)
                return

            def tr_out():
                for hh in range(4):
                    k.tr(psX[:, hh * 128:(hh + 1) * 128], oacc[qb % 3][:, hh * 128:(hh + 1) * 128], identf,
                         R=[toacc[qb % 3], tcf], W=[tpX], inc=(hh == 3))
                k.act(oTt[qb % 2][:], psX[:, :].rearrange("p (h q) -> p h q", h=4), AF.Copy, R=[tpX], W=[toTt[qb % 2]])
                j_, tq = qb // 8, (qb % 8) * 128
                k.dma("sp", ob[j_][:, tq:tq + 128].rearrange("(h p) q -> p h q", p=128), oTt[qb % 2][:],
                      tob[j_], R=[toTt[qb % 2]], W=[tob[j_]], join=True)
                if after_qb is not None:
                    after_qb(qb)
            defer(2, tr_out)

        def cmp_branch(qb):
            ob_ = st["no"] % 2
            st["no"] += 1
            nts = [0] if qb < 16 else [0, 1]
            for ii, nt in enumerate(nts):
                dc = qb - 16 * nt
                extra = [(LTc[0:12, dc * 128:(dc + 1) * 128], HI[0:12, :], None, [tcb])]
                mi = None
                if nt == 0 and qb <= 16:
                    mi = qb
                elif nt == 1:
                    mi = 17 + (qb - 16)
                if mi is not None:
                    for hh in range(4):
                        extra.append((identb, CM[:, mi * 128:(mi + 1) * 128], (hh * 128, (hh + 1) * 128), [tcb]))
                aft = None
                if ii == len(nts) - 1:
                    def aft(qb=qb, ob_=ob_):
                        finish(qb, ob_, 0, True, 193, with_imp=True)
                        select_math(qb)
                add_group(qb, KcT[:, nt * 128:(nt + 1) * 128], tC, extra, Vc[:, nt, 0:193], 193, tC,
                          ii == 0, ii == len(nts) - 1, ob_, after=aft)

        def win_branch(qb):
            ob_ = st["no"] % 2
            st["no"] += 1
            kts = list(range(max(0, qb - 4), qb + 1))
            for ii, kt in enumerate(kts):
                d = qb - kt
                extra = [(LT[0:12, d * 128:(d + 1) * 128], HI[0:12, :], None, [tcb])]
                if kt == qb:
                    extra.append((identb, Mc4, None, [tcb]))
                if kt == qb - 4:
                    extra.append((identb, Mw4, None, [tcb]))
                aft = None
                if ii == len(kts) - 1:
                    def aft(qb=qb, ob_=ob_):
                        finish(qb, ob_, 2, False, 129)
                add_group(qb, KwT[:, kt * 128:(kt + 1) * 128], tK[kt // 2], extra, Vw[:, kt, 0:129], 129, tV[kt // 2],
                          ii == 0, ii == len(kts) - 1, ob_, after=aft)

        def sel_branch(qb):
            ob_ = st["no"] % 2
            st["no"] += 1
            sb_ = qb % NSB
            for kt in range(qb + 1):
                d = qb - kt
                extra = [(LT[0:12, d * 128:(d + 1) * 128], HI[0:12, :], None, [tcb]),
                         (E_[0:64, kt * 128:(kt + 1) * 128], selb4[sb_][:, :], None, [tcb, tsb4[sb_]])]
                if kt == qb:
                    extra.append((identb, Mc4, None, [tcb]))
                aft = None
                if kt == qb:
                    def aft(qb=qb, ob_=ob_):
                        finish(qb, ob_, 1, False, 129)
                        out_part(qb)
                        if bg is not None:
                            bg("attn")
                add_group(qb, KsT[:, kt * 128:(kt + 1) * 128], tK[kt // 2], extra, Vs[:, kt, 0:129], 129, tV[kt // 2],
                          kt == 0, kt == qb, ob_, after=aft)

        cmp_branch(0)
        for qb in range(NQB):
            if qb + 1 < NQB:
                cmp_branch(qb + 1)
            win_branch(qb)
            sel_branch(qb)

        def tick():
            for d_ in list(deferred):
                d_[0] -= 1
                if d_[0] <= 0:
                    deferred.remove(d_)
                    d_[1]()

        prev = None
        for g_ in glist:
            g_[0]()
            if prev is not None:
                prev[1]()
                if prev[2] is not None:
                    prev[2]()
                tick()
            prev = g_
        prev[1]()
        if prev[2] is not None:
            prev[2]()
        while deferred:
            tick()


def build_p2(debug=False):
    nc = bass.Bass("TRN2", target_bir_lowering=False)
    k = K(nc)
    hT_d = nc.dram_tensor("hT", [D, SEQ], F32, kind="ExternalInput").ap()
    WF_d = nc.dram_tensor("WF", [D, NWF], F32, kind="ExternalInput").ap()
    WT_d = nc.dram_tensor("WT", [D, NWT], F32, kind="ExternalInput").ap()
    posT_d = nc.dram_tensor("posT", [128, 64], F32, kind="ExternalInput").ap()
    cw1_d = nc.dram_tensor("cw1", [2, 4096, 256], F32, kind="ExternalInput").ap()
    cw2_d = nc.dram_tensor("cw2", [2, 256, 128], F32, kind="ExternalInput").ap()
    cb_d = nc.dram_tensor("cb", [128, NCB], F32, kind="ExternalInput").ap()
    cf_d = nc.dram_tensor("cf", [128, NCF], F32, kind="ExternalInput").ap()
    o_d = nc.dram_tensor("o", [SEQ, 512], F32, kind="ExternalOutput").ap()
    dbg = None
    if debug:
        dbg = dict(KcT=nc.dram_tensor("d_KcT", [128, 256], BF16, kind="ExternalOutput").ap(),
                   Vc=nc.dram_tensor("d_Vc", [128, 2, 196], BF16, kind="ExternalOutput").ap(),
                   QT=nc.dram_tensor("d_QT", [128, NQB, 4, 128], BF16, kind="ExternalOutput").ap(),
                   KsT=nc.dram_tensor("d_KsT", [128, SEQ], BF16, kind="ExternalOutput").ap(),
                   Vs=nc.dram_tensor("d_Vs", [128, NQB, 132], BF16, kind="ExternalOutput").ap(),
                   gat=nc.dram_tensor("d_gat", [128, NQB, 12], F32, kind="ExternalOutput").ap())
    emit_attn(k, hT_d, WF_d, WT_d, posT_d, cw1_d, cw2_d, cb_d, cf_d, o_d, dbg=dbg)
    k.barrier()
    k.close()
    return nc


def p2_inputs(inp, h2T_b, g):
    cb, cf = attn_consts(g)
    d = dict(hT=h2T_b, cb=cb, cf=cf)
    d.update(attn_weights(inp, g))
    return d


def p3_inputs(inp, h2_c, oT_c):
    d = dict(cst=make_consts(), h_in=h2_c, oT=oT_c, w_o=np.ascontiguousarray(inp["nsa_w_o"][0]))
    d.update(moe_inputs(inp, 1))
    return d


_PROGS = {}


def _prog(name, fn):
    if name not in _PROGS:
        _PROGS[name] = fn()
    return _PROGS[name]


def _kernel3(**inputs):
    inp = {k_: np.asarray(v_) for k_, v_ in inputs.items()}
    cores = list(range(NCORE))
    r1 = run_bass_kernel_spmd(_prog("p1", build_p1), [p1_inputs(inp, c) for c in cores], core_ids=cores)
    h2 = np.stack([r1.results[c]["out"] for c in cores]).reshape(2, SEQ, D)
    h2T = [np.ascontiguousarray(h2[b].T) for b in range(2)]
    r2 = run_bass_kernel_spmd(_prog("p2", build_p2), [p2_inputs(inp, h2T[c // 4], c % 4) for c in cores], core_ids=cores)
    o = np.zeros((2, SEQ, D), np.float32)
    for c in cores:
        o[c // 4, :, (c % 4) * 512:(c % 4 + 1) * 512] = r2.results[c]["o"]
    ins3 = []
    for c in cores:
        b, j = divmod(c, 4)
        ins3.append(p3_inputs(inp, np.ascontiguousarray(h2[b, j * NT:(j + 1) * NT]),
                              np.ascontiguousarray(o[b, j * NT:(j + 1) * NT].T)))
    r3 = run_bass_kernel_spmd(_prog("p3", build_p3), ins3, core_ids=cores)
    out = np.stack([r3.results[c]["out"] for c in cores]).reshape(2, SEQ, D)
    return out.astype(np.float32)


RG4 = [[0, 1, 2, 3], [4, 5, 6, 7]]
NCONV0 = 16
NCONV1 = 0


def build_fused():
    nc = bass.Bass("TRN2", target_bir_lowering=False)
    k = K(nc)
    cst_d = nc.dram_tensor("cst", [128, NC32], F32, kind="ExternalInput").ap()
    x_d = nc.dram_tensor("x_tok", [NT, D], F32, kind="ExternalInput").ap()
    xT_d = nc.dram_tensor("xT", [D, NT + HALO], F32, kind="ExternalInput").ap()
    hm_d = nc.dram_tensor("hm", [1, 1], F32, kind="ExternalInput").ap()
    cpar_d = nc.dram_tensor("cpar", [128, NCPAR], F32, kind="ExternalInput").ap()
    w1_d = nc.dram_tensor("w_pw1", [D, 2 * D], F32, kind="ExternalInput").ap()
    w2_d = nc.dram_tensor("w_pw2", [D, D], F32, kind="ExternalInput").ap()
    md0 = moe_dram(nc, "0")
    WF_d = nc.dram_tensor("WF", [D, NWF], F32, kind="ExternalInput").ap()
    WT_d = nc.dram_tensor("WT", [D, NWT], F32, kind="ExternalInput").ap()
    posT_d = nc.dram_tensor("posT", [128, 64], F32, kind="ExternalInput").ap()
    cw1_d = nc.dram_tensor("cw1", [2, 4096, 256], F32, kind="ExternalInput").ap()
    cw2_d = nc.dram_tensor("cw2", [2, 256, 128], F32, kind="ExternalInput").ap()
    cb_d = nc.dram_tensor("cb", [128, NCB], F32, kind="ExternalInput").ap()
    cf_d = nc.dram_tensor("cf", [128, NCF], F32, kind="ExternalInput").ap()
    idx3_d = nc.dram_tensor("idx3", [128, CT], I32, kind="ExternalInput").ap()
    wo_d = nc.dram_tensor("w_o", [D, D], F32, kind="ExternalInput").ap()
    md1 = moe_dram(nc, "1")
    out_d = nc.dram_tensor("out", [NT, D], F32, kind="ExternalOutput").ap()
    hsp = nc.dram_tensor("hsp", [NT, D], F32).ap()
    xb1 = [nc.dram_tensor(f"xb1_{q}", [D, 256], BF16).ap() for q in range(4)]
    xg1 = [nc.dram_tensor(f"xg1_{q}", [4 * D, 256], BF16).ap() for q in range(4)]
    ob = [nc.dram_tensor(f"ob_{j}", [512, NT], BF16).ap() for j in range(4)]
    og = nc.dram_tensor("og", [4 * D, NT], BF16).ap()
    thsp = Trk("hsp")
    txb1 = [Trk(f"xb1{q}") for q in range(4)]
    txg1 = [Trk(f"xg1{q}") for q in range(4)]
    tob = [Trk(f"ob{j}") for j in range(4)]
    tog = [Trk(f"og{j}") for j in range(4)]

    wc0 = WConv(k, md0, "0", NCONV0)
    wc1 = WConv(k, md1, "1", NCONV1)

    cnt = dict(pw1=0, conv=0, attn=0, proj=0)

    def bg0(where):
        cnt[where] += 1
        if where == "pw1":
            if cnt[where] % 2 == 0:
                wc0.pump(1)
        elif cnt[where] % 4 != 0:
            wc0.pump(1)

    def bg1(where):
        cnt[where] += 1
        if where == "proj":
            wc1.pump(1)
        else:
            wc1.pump(2 if cnt[where] % 2 == 0 else 1)

    C = load_consts(k, cst_d)
    with k.scope():
        h_tok = k.sb("h_tok", [128, TT, D], F32)
        th = [Trk(f"h{i}") for i in range(TT)]
        load_tok(k, h_tok, th, x_d)
        emit_conv(k, C, xT_d, hm_d, cpar_d, w1_d, w2_d, h_tok, th, md0["ln"][0:1, :], md0["ln"][1:2, :], bg=bg0)
        wc0.flush()
        def cb_setup():
            ctx = dict(hTb=k.sb("hTb", [128, CT, NT], BF16), thTb=[Trk(f"hTb{i}") for i in range(TT)],
                       psA=[k.ps("psTr", [128, 512]) for _ in range(4)], tpsA=[Trk(f"psTr{i}") for i in range(4)], n=0)
            return ctx

        def cb_tile(ctx, tt):
            hvs = hsp.rearrange("(tt p) c -> p tt c", p=128)
            k.dma("sp", hvs[:, tt, :], h_tok[:, tt, :], thsp, R=[th[tt]], W=[thsp], join=True)
            hTb, psA, tpsA = ctx["hTb"], ctx["psA"], ctx["tpsA"]
            for q in range(4):
                p = ctx["n"] % 4
                ctx["n"] += 1
                for i in range(4):
                    ct = q * 4 + i
                    k.tr(psA[p][:, i * 128:(i + 1) * 128], h_tok[:, tt, ct * 128:(ct + 1) * 128], C["ident"],
                         R=[th[tt], C["t32"]], W=[tpsA[p]], inc=(i == 3))
                dst = hTb[:, q * 4:(q + 1) * 4, tt * 128:(tt + 1) * 128]
                src = psA[p][:, :].rearrange("p (a b) -> p a b", a=4)
                if q % 2 == 0:
                    k.act(dst, src, AF.Copy, R=[tpsA[p]], W=[ctx["thTb"][tt]])
                else:
                    k.v("dve", "tensor_copy", dst, src, R=[tpsA[p]], W=[ctx["thTb"][tt]])
            if tt % 2 == 1:
                q_ = tt // 2
                k.dma("sp", xb1[q_].rearrange("(ct p) t -> p ct t", p=128), hTb[:, :, q_ * 256:(q_ + 1) * 256],
                      txb1[q_], R=[ctx["thTb"][tt - 1], ctx["thTb"][tt]], W=[txb1[q_]])
                k.collective("AllGather", RG4, xb1[q_], xg1[q_], txg1[q_], R=[txb1[q_]], W=[txg1[q_]])

        emit_moe_d(k, C, h_tok, th, md0, wconv=wc0, tile_cb=(cb_setup, cb_tile))
    k.release(wc0.trk)

    def hsrc(c):
        r, q = divmod(c, 4)
        return xg1[q][r * D:(r + 1) * D, :].rearrange("(ct p) t -> p ct t", p=128), [txg1[q]]

    def after_qb(qb):
        if qb % 8 == 7:
            j_ = qb // 8
            k.collective("AllGather", RG4, ob[j_], og[j_ * D:(j_ + 1) * D, :], tog[j_], R=[tob[j_]], W=[tog[j_]])

    with k.scope():
        emit_attn(k, None, WF_d, WT_d, posT_d, cw1_d, cw2_d, cb_d, cf_d, None, hsrc=hsrc, ob=ob, tob=tob,
                  chunk_order=[r * 4 + q for q in range(4) for r in range(4)], after_qb=after_qb, bg=bg1)
        wc1.flush()

    with k.scope():
        h_tok = k.sb("h_tok", [128, TT, D], F32)
        th = [Trk(f"h{i}") for i in range(TT)]
        hv = hsp.rearrange("(tt p) c -> p tt c", p=128)
        for tt in range(TT):
            k.dma("sp", h_tok[:, tt, :], hv[:, tt, :], th[tt], R=[thsp], W=[th[tt]])
        with k.scope():
            idx3 = k.sb("idx3", [128, CT], I32)
            tidx = Trk("idx3")
            k.dma("sp", idx3[:], idx3_d, tidx, W=[tidx])
            aT = k.sb("aT", [128, CT, NT], BF16)
            taT = Trk("aT")
            tg_ = [Trk(f"aTg{i}") for i in range(CT)]
            for ct in range(CT):
                k.gather(aT[:, ct, :], og[:, :], idx3[:, ct:ct + 1], tg_[ct], R=tog + [tidx], W=[taT] if ct == 0 else [])
            taT.w = [m for t_ in tg_ for m in [(t_.dsem, t_.dcnt)]]
            emit_proj_res_ln(k, aT, taT, wo_d, h_tok, th, md1["ln"][0:1, :], md1["ln"][1:2, :])
        emit_moe_d(k, C, h_tok, th, md1, wconv=wc1)
        to = Trk("out")
        store_tok(k, h_tok, th, out_d, to)
    k.barrier()
    k.close()
    return nc


def fused_inputs(inp, c):
    b, j = divmod(c, 4)
    d = p1_inputs(inp, c)
    m0 = moe_inputs(inp, 0)
    for kk in list(m0):
        del d[kk]
    d.update(moe_inputs(inp, 0, "0"))
    g = j
    cb, cf = attn_consts(g)
    d.update(cb=cb, cf=cf)
    d.update(attn_weights(inp, g))
    ct = np.arange(CT)[None, :]
    p = np.arange(128)[:, None]
    d["idx3"] = np.ascontiguousarray((j * 2048 + ct * 128 + p).astype(np.int32))
    d["w_o"] = np.ascontiguousarray(inp["nsa_w_o"][0])
    d.update(moe_inputs(inp, 1, "1"))
    return d


def kernel_unfused(**inputs):
    return _kernel3(**inputs)


def kernel(**inputs):
    inp = {k_: np.asarray(v_) for k_, v_ in inputs.items()}
    cores = list(range(NCORE))
    r = run_bass_kernel_spmd(_prog("fused", build_fused), [fused_inputs(inp, c) for c in cores], core_ids=cores)
    out = np.stack([r.results[c]["out"] for c in cores]).reshape(2, SEQ, D)
    return out.astype(np.float32)
```

```python
import numpy as np
from contextlib import ExitStack, contextmanager
import concourse.bass as bass
import concourse.mybir as mybir
from concourse.bass_utils import run_bass_kernel_spmd

F32, BF16, I32 = mybir.dt.float32, mybir.dt.bfloat16, mybir.dt.int32
AF = mybir.ActivationFunctionType
ALU = mybir.AluOpType
AX = mybir.AxisListType

D = 2048
NCORE = 8
SEQ = 4096
NT = 1024
TT = NT // 128
CT = D // 128
ALPHA = 4.0 ** 0.25
LN_EPS = 1e-5
NEXP = 32
DFF = 512
NEG = -30000.0


class Trk:
    __slots__ = ("w", "r", "dsem", "dcnt", "name")

    def __init__(self, name=""):
        self.w = []
        self.r = {}
        self.dsem = None
        self.dcnt = 0
        self.name = name


class Eng:
    def __init__(self, name, h):
        self.name, self.h = name, h
        self.sem = None
        self.cnt = 0
        self.known = {}
        self.pending = False


class K:
    SEM_ROLL = 30000

    def __init__(self, nc):
        self.nc = nc
        self.es = ExitStack()
        self.stacks = [self.es]
        self.eng = {}
        self.nsem = 0
        for name, h in (("pe", nc.tensor), ("act", nc.scalar), ("dve", nc.vector),
                        ("pool", nc.gpsimd), ("sp", nc.sync)):
            e = Eng(name, h)
            e.sem = self.sem("e_" + name)
            self.eng[name] = e
        self.dmas = []
        self.uid = 0
        self.sem_free = []
        self.scope_trks = [[]]

    def sem(self, name):
        self.nsem += 1
        return self.es.enter_context(self.nc.semaphore(f"{name}_{self.nsem}"))

    def sb(self, name, shape, dt):
        self.uid += 1
        return self.stacks[-1].enter_context(self.nc.sbuf_tensor(f"{name}_{self.uid}", list(shape), dt))

    def ps(self, name, shape, dt=F32):
        self.uid += 1
        return self.stacks[-1].enter_context(self.nc.psum_tensor(f"{name}_{self.uid}", list(shape), dt))

    @contextmanager
    def scope(self):
        st = ExitStack()
        self.stacks.append(st)
        self.scope_trks.append([])
        try:
            yield
        finally:
            self.barrier()
            self.stacks.pop()
            st.close()
            for S in self.scope_trks.pop():
                self.sem_free.append((S.dsem, S.dcnt))
                S.dsem = None
                S.w = []
                S.r = {}

    def _dsem(self, S):
        if S.dsem is None:
            if self.sem_free:
                S.dsem, S.dcnt = self.sem_free.pop()
            else:
                S.dsem = self.sem("d_" + S.name)
                S.dcnt = 0
            self.scope_trks[-1].append(S)

    def close(self):
        self.es.close()

    def _emit_waits(self, E, waits, skip_own=False):
        for sem, val in waits.values():
            if sem is E.sem:
                if skip_own or E.name == "pe":
                    continue
                assert val <= E.cnt, (E.name, val, E.cnt)
            if E.known.get(id(sem), 0) >= val:
                continue
            E.h.wait_ge(sem, val)
            E.known[id(sem)] = val

    @staticmethod
    def _need(waits, mark):
        sem, val = mark
        cur = waits.get(id(sem))
        if cur is None or cur[1] < val:
            waits[id(sem)] = (sem, val)

    def _collect(self, E, R, W):
        waits = {}
        for t in R:
            for m in t.w:
                self._need(waits, m)
        for t in W:
            for m in t.w:
                self._need(waits, m)
            for m in t.r.values():
                if m[0] is E.sem:
                    continue
                self._need(waits, m)
        return waits

    def _roll(self, E):
        if E.cnt >= self.SEM_ROLL and not E.pending:
            E.sem = self.sem("e_" + E.name)
            E.cnt = 0

    def op(self, en, fn, R=(), W=(), inc=True):
        E = self.eng[en]
        self._roll(E)
        self._emit_waits(E, self._collect(E, R, W))
        ins = fn()
        if inc:
            E.cnt += 1
            ins.then_inc(E.sem, 1)
            mark = (E.sem, E.cnt)
            E.pending = False
        else:
            mark = (E.sem, E.cnt + 1)
            E.pending = True
        for t in R:
            cur = t.r.get(id(mark[0]))
            if cur is None or cur[1] < mark[1]:
                t.r[id(mark[0])] = mark
        for t in W:
            t.w = [mark]
            t.r = {}
        return ins

    def dma(self, q, out, in_, S, R=(), W=(), join=False, background=False, **kw):
        E = self.eng[q]
        had = S.dsem is not None
        self._dsem(S)
        if background and not had:
            self.scope_trks[-1].remove(S)
        waits = {}
        for t in R:
            for m in t.w:
                self._need(waits, m)
        for t in W:
            if not join:
                for m in t.w:
                    self._need(waits, m)
            for m in t.r.values():
                self._need(waits, m)
        self._emit_waits(E, waits, skip_own=True)
        ins = E.h.dma_start(out=out, in_=in_, **kw)
        S.dcnt += 16
        ins.then_inc(S.dsem, 16)
        mark = (S.dsem, S.dcnt)
        for t in R:
            t.r[id(mark[0])] = mark
        for t in W:
            if join:
                t.w = [m for m in t.w if m[0] is not S.dsem] + [mark]
            else:
                t.w = [mark]
            t.r = {}
        if not background:
            self.dmas.append(mark)
        return ins

    def release(self, trks):
        for S in trks:
            if S.dsem is not None:
                self.sem_free.append((S.dsem, S.dcnt))
                S.dsem = None
                S.w = []
                S.r = {}

    def gather(self, out, in_, idx, S, R=(), W=(), bounds=None):
        E = self.eng["pool"]
        self._dsem(S)
        waits = {}
        for t in R:
            for m in t.w:
                self._need(waits, m)
        for t in W:
            for m in t.w:
                self._need(waits, m)
            for m in t.r.values():
                self._need(waits, m)
        self._emit_waits(E, waits, skip_own=True)
        kw = {}
        if bounds is not None:
            kw = dict(bounds_check=bounds, oob_is_err=False)
        ins = E.h.indirect_dma_start(out=out, out_offset=None, in_=in_,
                                     in_offset=bass.IndirectOffsetOnAxis(ap=idx, axis=0), **kw)
        S.dcnt += 16
        ins.then_inc(S.dsem, 16)
        mark = (S.dsem, S.dcnt)
        for t in R:
            t.r[id(mark[0])] = mark
        for t in W:
            t.w = [mark]
            t.r = {}
        self.dmas.append(mark)
        return ins

    def collective(self, kind, groups, in_ap, out_ap, S, R=(), W=()):
        E = self.eng["pool"]
        self._dsem(S)
        waits = {}
        for t in R:
            for m in t.w:
                self._need(waits, m)
        for t in W:
            for m in t.w:
                self._need(waits, m)
            for m in t.r.values():
                self._need(waits, m)
        self._emit_waits(E, waits, skip_own=True)
        ins = E.h.collective_compute(kind, ALU.bypass, replica_groups=groups, ins=[in_ap.opt()], outs=[out_ap.opt()])
        S.dcnt += 1
        ins.then_inc(S.dsem, 1)
        mark = (S.dsem, S.dcnt)
        for t in R:
            t.r[id(mark[0])] = mark
        for t in W:
            t.w = [mark]
            t.r = {}
        self.dmas.append(mark)
        return ins

    def barrier(self):
        marks = []
        for F in self.eng.values():
            assert not F.pending, F.name
            if F.cnt:
                marks.append((F.sem, F.cnt))
        seen = {}
        for m in self.dmas:
            self._need(seen, m)
        marks += list(seen.values())
        self.dmas = []
        for E in self.eng.values():
            for sem, val in marks:
                if sem is E.sem:
                    continue
                if E.known.get(id(sem), 0) >= val:
                    continue
                E.h.wait_ge(sem, val)
                E.known[id(sem)] = val

    def mm(self, out, lhsT, rhs, start, stop, R=(), W=(), inc=None, **kw):
        if inc is None:
            inc = stop
        return self.op("pe", lambda: self.nc.tensor.matmul(out, lhsT, rhs, start=start, stop=stop, **kw),
                       R=R, W=W, inc=inc)

    def tr(self, out, in_, ident, R=(), W=(), inc=True):
        return self.op("pe", lambda: self.nc.tensor.transpose(out, in_, ident), R=R, W=W, inc=inc)

    def act(self, out, in_, func, R=(), W=(), **kw):
        return self.op("act", lambda: self.nc.scalar.activation(out=out, in_=in_, func=func, **kw), R=R, W=W)

    def v(self, en, name, *a, R=(), W=(), **kw):
        h = self.eng[en].h
        return self.op(en, lambda: getattr(h, name)(*a, **kw), R=R, W=W)


NC32 = 544


def make_consts():
    c = np.zeros((128, NC32), np.float32)
    c[:, 0:128] = np.eye(128)
    c[:, 128:256] = np.arange(128)[None, :]
    c[:, 256:384] = (np.arange(128)[:, None] < np.arange(128)[None, :])
    c[:, 384:512] = 1.0
    c[:, 512:544] = (np.arange(32) * 128)[None, :]
    return c


def load_consts(k, cst_d):
    c32 = k.sb("c32", [128, NC32], F32)
    tc = Trk("c32")
    k.dma("sp", c32[:], cst_d, tc, W=[tc])
    cbf = k.sb("cbf", [128, 512], BF16)
    tcb = Trk("cbf")
    k.v("dve", "tensor_copy", cbf[:], c32[:, 0:512], R=[tc], W=[tcb])
    return dict(ident=c32[:, 0:128], iota=c32[:, 128:256], ebase=c32[:, 512:544], ones=c32[:, 384:512],
                ident_bf=cbf[:, 0:128], tri_bf=cbf[:, 256:384], ones_bf=cbf[:, 384:512], t32=tc, tbf=tcb)


def pb(ap_row):
    return ap_row.partition_broadcast(128)


def emit_ln_tok(k, x_ap, tx, G, B, tgb, st, tst):
    nc = k.nc
    for i in range(4):
        k.v("dve", "bn_stats", st[:, i * 6:(i + 1) * 6], x_ap[:, i * 512:(i + 1) * 512], R=[tx, tst], W=[tst])
    k.v("dve", "bn_aggr", st[:, 24:26], st[:, 0:24], R=[tst], W=[tst])
    k.act(st[:, 26:27], st[:, 25:26], AF.Sqrt, bias=LN_EPS, scale=1.0, R=[tst], W=[tst])
    k.v("dve", "reciprocal", st[:, 27:28], st[:, 26:27], R=[tst], W=[tst])
    k.v("dve", "scalar_tensor_tensor", st[:, 28:29], st[:, 24:25], -1.0, st[:, 27:28], ALU.mult, ALU.mult,
        R=[tst], W=[tst])
    k.v("dve", "tensor_scalar", x_ap, x_ap, st[:, 27:28], st[:, 28:29], ALU.mult, ALU.add, R=[tx, tst], W=[tx])
    k.v("dve", "tensor_tensor", x_ap, x_ap, G, ALU.mult, R=[tx, tgb], W=[tx])
    k.v("pool", "tensor_tensor", x_ap, x_ap, B, ALU.add, R=[tx, tgb], W=[tx])


def emit_moe(k, C, h_tok, th, wg_d, bg_d, we_d, be_d, wgu_d, wdn_d, ybuf, lng_d, lnb_d, nexp=NEXP, wconv=None,
             tile_cb=None):
    nc = k.nc
    rankm = k.sb("rankm", [128, TT, 32], F32)
    t_rank = [Trk(f"rank{i}") for i in range(TT)]
    dest = k.sb("dest", [128, TT, 2], I32)
    wts = k.sb("wts", [128, TT, 2], F32)
    t_dw = [Trk(f"dw{i}") for i in range(TT)]
    h_bf = k.sb("h_bf", [128, TT, D], BF16)
    t_hbf = [Trk(f"hbf{i}") for i in range(TT)]

    with k.scope():
        Wr = k.sb("Wr", [128, CT, 36], F32)
        tWr = Trk("Wr")
        k.dma("sp", Wr[:, :, 0:4], wg_d.rearrange("(ct p) n -> p ct n", p=128), tWr, W=[tWr])
        k.dma("sp", Wr[:, :, 4:36], we_d.rearrange("(ct p) n -> p ct n", p=128), tWr, W=[tWr], join=True)
        rb = k.sb("rb", [128, 36], F32)
        trb = Trk("rb")
        k.dma("sp", rb[:, 0:4], pb(bg_d), trb, W=[trb])
        k.dma("sp", rb[:, 4:36], pb(be_d), trb, W=[trb], join=True)
        hTt = [k.sb("hTt", [128, CT, 128], F32) for _ in range(2)]
        thT = [[Trk(f"hTt{a}{b}") for b in range(4)] for a in range(2)]
        psA = [k.ps("psA", [128, 512]) for _ in range(2)]
        tpsA = [Trk("psA0"), Trk("psA1")]
        psL = k.ps("psL", [128, 512])
        tpsL = Trk("psL")
        psR = k.ps("psR", [128, 512])
        tpsR = Trk("psR")
        L = k.sb("L", [128, TT, 36], F32)
        sm = k.sb("sm", [128, TT, 16], F32)
        ohg = k.sb("ohg", [128, TT, 4], F32)
        r8 = k.sb("r8", [128, TT, 5, 8], F32)
        r32 = k.sb("r32", [128, TT, 5, 32], F32)
        d12 = k.sb("d12", [128, TT, 2], F32)
        M_bf = k.sb("M_bf", [128, TT, 32], BF16)
        tM = [Trk(f"M{i}") for i in range(TT)]
        ts = [Trk(f"rs{i}") for i in range(TT)]
        nq = 0
        for tt in range(TT):
            hb = tt % 2
            k.act(h_bf[:, tt, :], h_tok[:, tt, :], AF.Copy, R=[th[tt]], W=[t_hbf[tt]])
            for q in range(4):
                pa = nq % 2
                nq += 1
                for i in range(4):
                    ct = q * 4 + i
                    k.tr(psA[pa][:, i * 128:(i + 1) * 128], h_tok[:, tt, ct * 128:(ct + 1) * 128], C["ident"],
                         R=[th[tt], C["t32"]], W=[tpsA[pa]], inc=(i == 3))
                dst = hTt[hb][:, q * 4:(q + 1) * 4, :]
                if q % 2 == 0:
                    k.act(dst, psA[pa][:, :].rearrange("p (a b) -> p a b", a=4), AF.Copy, R=[tpsA[pa]], W=[thT[hb][q]])
                else:
                    k.v("dve", "tensor_copy", dst, psA[pa][:, :].rearrange("p (a b) -> p a b", a=4),
                        R=[tpsA[pa]], W=[thT[hb][q]])
            for ct in range(CT):
                k.mm(psL[:, 0:36], hTt[hb][:, ct, :], Wr[:, ct, :], ct == 0, ct == CT - 1,
                     R=[thT[hb][ct // 4], tWr], W=[tpsL])
            S = ts[tt]
            sc = lambda j: sm[:, tt, j:j + 1]
            Lt = L[:, tt, :]
            les, eq1, le2, eq2, e12 = (r8[:, tt, j, :] for j in range(5))
            Mf, E1, E2, rk, tmp = (r32[:, tt, j, :] for j in range(5))
            dv = lambda name, *a, R=(), W=(): k.v("dve", name, *a, R=[S] + list(R), W=[S] + list(W))
            dv("tensor_tensor", Lt, psL[:, 0:36], rb[:], ALU.add, R=[tpsL, trb])
            dv("reduce_max", sc(0), Lt[:, 0:4], AX.X)
            dv("tensor_scalar", ohg[:, tt, :], Lt[:, 0:4], sc(0), None, ALU.is_equal)
            dv("tensor_scalar", sc(1), sc(0), -1.0, None, ALU.mult)
            k.act(le2[:, 0:4], Lt[:, 0:4], AF.Exp,
                  bias=sc(1), scale=1.0, accum_out=sc(2), R=[S], W=[S])
            dv("reciprocal", sc(3), sc(2))
            dv("tensor_scalar", les, Lt[:, 4:12], ohg[:, tt, 0:1], None, ALU.mult)
            for g in range(1, 4):
                dv("scalar_tensor_tensor", les, Lt[:, 4 + 8 * g:12 + 8 * g], ohg[:, tt, g:g + 1], les,
                   ALU.mult, ALU.add)
            dv("reduce_max", sc(4), les, AX.X)
            dv("tensor_scalar", eq1, les, sc(4), None, ALU.is_equal)
            dv("scalar_tensor_tensor", le2, eq1, -1e30, les, ALU.mult, ALU.add)
            dv("reduce_max", sc(5), le2, AX.X)
            dv("tensor_scalar", eq2, le2, sc(5), None, ALU.is_equal)
            dv("tensor_tensor", sc(6), sc(5), sc(4), ALU.subtract)
            k.act(sc(7), sc(6), AF.Exp, R=[S], W=[S])
            dv("tensor_scalar", sc(8), sc(7), 1.0, None, ALU.add)
            dv("reciprocal", sc(9), sc(8))
            dv("tensor_tensor", wts[:, tt, 0:1], sc(9), sc(3), ALU.mult, W=[t_dw[tt]])
            dv("tensor_tensor", wts[:, tt, 1:2], sc(3), wts[:, tt, 0:1], ALU.subtract, W=[t_dw[tt]])
            dv("tensor_tensor", e12, eq1, eq2, ALU.add)
            for g in range(4):
                gs = slice(8 * g, 8 * g + 8)
                dv("tensor_scalar", Mf[:, gs], e12, ohg[:, tt, g:g + 1], None, ALU.mult)
                dv("tensor_scalar", E1[:, gs], eq1, ohg[:, tt, g:g + 1], None, ALU.mult)
                dv("tensor_scalar", E2[:, gs], eq2, ohg[:, tt, g:g + 1], None, ALU.mult)
            dv("tensor_copy", M_bf[:, tt, :], Mf, W=[tM[tt]])
            for t2 in range(tt + 1):
                lhsT = C["ones_bf"] if t2 < tt else C["tri_bf"]
                k.mm(psR[:, 0:32], lhsT, M_bf[:, t2, :], t2 == 0, t2 == tt, R=[tM[t2], C["tbf"]], W=[tpsR])
            dv("scalar_tensor_tensor", rankm[:, tt, :], psR[:, 0:32], 1.0, Mf, ALU.add, ALU.mult,
               R=[tpsR], W=[t_rank[tt]])
            dv("tensor_scalar", rankm[:, tt, :], rankm[:, tt, :], -1.0, None, ALU.add, W=[t_rank[tt]])
            dv("tensor_tensor", rk, psR[:, 0:32], C["ebase"], ALU.add, R=[tpsR, C["t32"]])
            for j, Ej in enumerate((E1, E2)):
                dv("tensor_tensor", tmp, rk, Ej, ALU.mult)
                dv("reduce_sum", d12[:, tt, j:j + 1], tmp, AX.X)
            dv("tensor_copy", dest[:, tt, :], d12[:, tt, :], W=[t_dw[tt]])

    with k.scope():
        NS = 4
        wsl = [k.sb("wsl", [128, 8192], BF16) for _ in range(NS)]
        twsl = [Trk(f"wsl{i}") for i in range(NS)]
        selq = k.sb("selq", [128, TT, 512], BF16)
        tsel = [Trk(f"selq{i}") for i in range(TT)]
        xq = k.sb("xq", [128, CT, 512], BF16)
        txq = [Trk(f"xq{i}") for i in range(CT)]
        sg = k.sb("sg", [128, 512], F32)
        tsg = Trk("sg")
        hT = k.sb("hT", [128, 512], BF16)
        thh = Trk("hT")
        ye = k.sb("ye", [128, D], BF16)
        tye = [Trk(f"ye{i}") for i in range(4)]
        psG = [k.ps("psG", [128, 512]) for _ in range(2)]
        tpsG = [Trk("psG0"), Trk("psG1")]
        psH1 = k.ps("psH1", [128, 512])
        tH1 = Trk("psH1")
        psH2 = k.ps("psH2", [128, 512])
        tH2 = Trk("psH2")
        psY = [k.ps("psY", [128, 512]) for _ in range(4)]
        tpsY = [Trk(f"psY{i}") for i in range(4)]
        tyb = Trk("ybuf")

        chunks = [(e, c) for e in range(nexp) for c in "gud"]

        def issue(ci):
            if ci >= len(chunks):
                return
            e, c = chunks[ci]
            s = ci % NS
            if wconv is not None and e < wconv.nconv:
                if c == "d":
                    k.dma("sp", wsl[s][:, :].rearrange("p (a n) -> p a n", a=4),
                          wconv.wdn_b[e].rearrange("(a p) n -> p a n", p=128), twsl[s], R=[wconv.trk[e]], W=[twsl[s]])
                else:
                    c0 = 0 if c == "g" else 512
                    k.dma("sp", wsl[s][:, :].rearrange("p (a n) -> p a n", a=16),
                          wconv.wgu_b[e].rearrange("(a p) n -> p a n", p=128)[:, :, c0:c0 + 512], twsl[s],
                          R=[wconv.trk[e]], W=[twsl[s]])
                return
            if c == "d":
                k.dma("pool", wsl[s][:, :].rearrange("p (a n) -> p a n", a=4),
                      wdn_d[e].rearrange("(a p) n -> p a n", p=128), twsl[s], W=[twsl[s]])
            else:
                c0 = 0 if c == "g" else 512
                k.dma("pool", wsl[s][:, :].rearrange("p (a n) -> p a n", a=16),
                      wgu_d[e].rearrange("(a p) n -> p a n", p=128)[:, :, c0:c0 + 512], twsl[s], W=[twsl[s]])

        for ci in range(NS - 1):
            issue(ci)
        ci = 0
        nev = 0
        for q0 in range(0, nexp, 4):
            for tt in range(TT):
                for j in range(4):
                    e = q0 + j
                    k.v("dve", "tensor_scalar", selq[:, tt, j * 128:(j + 1) * 128], C["iota"],
                        rankm[:, tt, e:e + 1], None, ALU.is_equal, R=[t_rank[tt], C["t32"]], W=[tsel[tt]])
            for ct in range(CT):
                pg = ct % 2
                for tt in range(TT):
                    k.mm(psG[pg][:, :], h_bf[:, tt, ct * 128:(ct + 1) * 128], selq[:, tt, :], tt == 0, tt == TT - 1,
                         R=[t_hbf[tt], tsel[tt]], W=[tpsG[pg]])
                if ct % 2 == 0:
                    k.act(xq[:, ct, :], psG[pg][:, :], AF.Copy, R=[tpsG[pg]], W=[txq[ct]])
                else:
                    k.v("dve", "tensor_copy", xq[:, ct, :], psG[pg][:, :], R=[tpsG[pg]], W=[txq[ct]])
            for j in range(4):
                e = q0 + j
                for half, (psH, tH) in enumerate(((psH1, tH1), (psH2, tH2))):
                    s = ci % NS
                    issue(ci + NS - 1)
                    wv = wsl[s][:, :].rearrange("p (a n) -> p a n", a=16)
                    for ft in range(4):
                        for ct in range(CT):
                            k.mm(psH[:, ft * 128:(ft + 1) * 128], wv[:, ct, ft * 128:(ft + 1) * 128],
                                 xq[:, ct, j * 128:(j + 1) * 128], ct == 0, ct == CT - 1,
                                 R=[twsl[s], txq[ct]], W=[tH])
                    ci += 1
                k.act(sg[:], psH1[:, :], AF.Silu, R=[tH1], W=[tsg])
                k.v("dve", "tensor_tensor", hT[:], sg[:], psH2[:, :], ALU.mult, R=[tsg, tH2], W=[thh])
                s = ci % NS
                issue(ci + NS - 1)
                wv = wsl[s][:, :].rearrange("p (a n) -> p a n", a=4)
                for cc in range(4):
                    for ft in range(4):
                        k.mm(psY[cc][:, :], hT[:, ft * 128:(ft + 1) * 128], wv[:, ft, cc * 512:(cc + 1) * 512],
                             ft == 0, ft == 3, R=[thh, twsl[s]], W=[tpsY[cc]])
                    if nev % 2 == 0:
                        k.act(ye[:, cc * 512:(cc + 1) * 512], psY[cc][:, :], AF.Copy, R=[tpsY[cc]], W=[tye[cc]])
                    else:
                        k.v("dve", "tensor_copy", ye[:, cc * 512:(cc + 1) * 512], psY[cc][:, :],
                            R=[tpsY[cc]], W=[tye[cc]])
                    nev += 1
                ci += 1
                k.dma("sp", ybuf[e * 128:(e + 1) * 128, :], ye[:], tyb, R=tye, W=[tyb], join=True)

    with k.scope():
        G = k.sb("lnG", [128, D], F32)
        B = k.sb("lnB", [128, D], F32)
        tgb = Trk("lngb")
        k.dma("sp", G[:], pb(lng_d), tgb, W=[tgb])
        k.dma("sp", B[:], pb(lnb_d), tgb, W=[tgb], join=True)
        Y = [[k.sb("Yg", [128, D], BF16) for _ in range(2)] for _ in range(2)]
        tY = [[Trk(f"Y{a}{b}") for b in range(2)] for a in range(2)]
        st = k.sb("lnst", [128, TT, 40], F32)
        tst = [Trk(f"lnst{i}") for i in range(TT)]
        def gat(tt):
            p = tt % 2
            for j in range(2):
                k.gather(Y[p][j][:], ybuf[:, :], dest[:, tt, j:j + 1], tY[p][j], R=[tyb, t_dw[tt]], W=[tY[p][j]])

        cb_ctx = tile_cb[0]() if tile_cb is not None else None
        gat(0)
        for tt in range(TT):
            p = tt % 2
            if tt + 1 < TT:
                gat(tt + 1)
            x_ap = h_tok[:, tt, :]
            k.op("act", lambda: nc.scalar.mul(x_ap, x_ap, ALPHA), R=[th[tt]], W=[th[tt]])
            k.v("dve", "scalar_tensor_tensor", x_ap, Y[p][0][:], wts[:, tt, 0:1], x_ap, ALU.mult, ALU.add,
                R=[tY[p][0], t_dw[tt], th[tt]], W=[th[tt]])
            k.v("dve", "scalar_tensor_tensor", x_ap, Y[p][1][:], wts[:, tt, 1:2], x_ap, ALU.mult, ALU.add,
                R=[tY[p][1], t_dw[tt], th[tt]], W=[th[tt]])
            emit_ln_tok(k, x_ap, th[tt], G[:], B[:], tgb, st[:, tt, :], tst[tt])
            if tile_cb is not None:
                tile_cb[1](cb_ctx, tt)


def emit_proj_res_ln(k, aT, taT, W_d, h_tok, th, lng_d, lnb_d, tts=None):
    nc = k.nc
    if tts is None:
        tts = list(range(TT))
    with k.scope():
        Wc = [k.sb("Wc", [128, CT, 512], BF16) for _ in range(2)]
        tWc = [Trk("Wc0"), Trk("Wc1")]
        G = k.sb("lnG", [128, D], F32)
        B = k.sb("lnB", [128, D], F32)
        tgb = Trk("lngb")
        k.dma("sp", G[:], pb(lng_d), tgb, W=[tgb])
        k.dma("sp", B[:], pb(lnb_d), tgb, W=[tgb], join=True)
        st = k.sb("lnst", [128, TT, 40], F32)
        tst = [Trk(f"lnst{i}") for i in range(TT)]
        ps = [k.ps("psP", [128, 512]) for _ in range(4)]
        tps = [Trk(f"psP{i}") for i in range(4)]
        Wv = W_d.rearrange("(ct p) n -> p ct n", p=128)

        def issue(cc):
            if cc < 4:
                k.dma("pool", Wc[cc % 2][:], Wv[:, :, cc * 512:(cc + 1) * 512], tWc[cc % 2], W=[tWc[cc % 2]])

        issue(0)
        n = 0
        for cc in range(4):
            issue(cc + 1)
            for i, tt in enumerate(tts):
                p = n % 4
                n += 1
                for ct in range(CT):
                    k.mm(ps[p][:, :], aT[:, ct, i * 128:(i + 1) * 128], Wc[cc % 2][:, ct, :], ct == 0, ct == CT - 1,
                         R=[taT, tWc[cc % 2]], W=[tps[p]])
                dst = h_tok[:, tt, cc * 512:(cc + 1) * 512]
                k.v("dve", "scalar_tensor_tensor", dst, dst, ALPHA, ps[p][:, :], ALU.mult, ALU.add,
                    R=[tps[p], th[tt]], W=[th[tt]])
        for tt in tts:
            emit_ln_tok(k, h_tok[:, tt, :], th[tt], G[:], B[:], tgb, st[:, tt, :], tst[tt])


def load_tok(k, h_tok, th, src_d):
    hv = src_d.rearrange("(tt p) c -> p tt c", p=128)
    for tt in range(TT):
        k.dma("sp", h_tok[:, tt, :], hv[:, tt, :], th[tt], W=[th[tt]])


def store_tok(k, h_tok, th, dst_d, to):
    ov = dst_d.rearrange("(tt p) c -> p tt c", p=128)
    for tt in range(TT):
        k.dma("sp", ov[:, tt, :], h_tok[:, tt, :], to, R=[th[tt]], W=[to], join=True)


def moe_dram(nc, sfx=""):
    d = {}
    d["wg"] = nc.dram_tensor("moe_wg" + sfx, [D, 4], F32, kind="ExternalInput").ap()
    d["bg"] = nc.dram_tensor("moe_bg" + sfx, [1, 4], F32, kind="ExternalInput").ap()
    d["we"] = nc.dram_tensor("moe_we" + sfx, [D, 32], F32, kind="ExternalInput").ap()
    d["be"] = nc.dram_tensor("moe_be" + sfx, [1, 32], F32, kind="ExternalInput").ap()
    d["wgu"] = nc.dram_tensor("moe_wgu" + sfx, [NEXP, D, 2 * DFF], F32, kind="ExternalInput").ap()
    d["wdn"] = nc.dram_tensor("moe_wdn" + sfx, [NEXP, DFF, D], F32, kind="ExternalInput").ap()
    d["ln"] = nc.dram_tensor("ln_gb" + sfx, [4, D], F32, kind="ExternalInput").ap()
    d["ybuf"] = nc.dram_tensor("ybuf" + sfx, [NEXP * 128, D], BF16).ap()
    return d


def moe_inputs(inp, l, sfx=""):
    return {
        "moe_wg" + sfx: np.ascontiguousarray(inp["moe_wg"][l]),
        "moe_bg" + sfx: np.ascontiguousarray(inp["moe_bg"][l][None]),
        "moe_we" + sfx: np.ascontiguousarray(inp["moe_we"][l]),
        "moe_be" + sfx: np.ascontiguousarray(inp["moe_be"][l][None]),
        "moe_wgu" + sfx: np.ascontiguousarray(inp["moe_w_gu"][l]),
        "moe_wdn" + sfx: np.ascontiguousarray(inp["moe_w_down"][l]),
        "ln_gb" + sfx: np.ascontiguousarray(np.stack([inp["ln_g"][l, 0], inp["ln_b"][l, 0], inp["ln_g"][l, 1], inp["ln_b"][l, 1]])),
    }


class WConv:
    def __init__(self, k, d, sfx, nconv):
        nc = k.nc
        self.k, self.d, self.nconv = k, d, nconv
        self.wgu_b = nc.dram_tensor("wgu_b" + sfx, [max(nconv, 1), D, 2 * DFF], BF16).ap()
        self.wdn_b = nc.dram_tensor("wdn_b" + sfx, [max(nconv, 1), DFF, D], BF16).ap()
        self.trk = [Trk(f"wcv{sfx}_{e}") for e in range(nconv)]
        self.jobs = [(e, c) for e in range(nconv) for c in ("gu", "dn")]
        self.nxt = 0

    def pump(self, n=1):
        k = self.k
        for _ in range(n):
            if self.nxt >= len(self.jobs):
                return
            e, c = self.jobs[self.nxt]
            self.nxt += 1
            if c == "gu":
                k.dma("pool", self.wgu_b[e].rearrange("(a p) n -> p a n", p=128),
                      self.d["wgu"][e].rearrange("(a p) n -> p a n", p=128), self.trk[e], W=[self.trk[e]], join=True,
                      background=True)
            else:
                k.dma("pool", self.wdn_b[e].rearrange("(a p) n -> p a n", p=128),
                      self.d["wdn"][e].rearrange("(a p) n -> p a n", p=128), self.trk[e], W=[self.trk[e]], join=True,
                      background=True)

    def flush(self):
        self.pump(len(self.jobs))


def emit_moe_d(k, C, h_tok, th, d, wconv=None, tile_cb=None):
    emit_moe(k, C, h_tok, th, d["wg"], d["bg"], d["we"], d["be"], d["wgu"], d["wdn"], d["ybuf"],
             d["ln"][2:3, :], d["ln"][3:4, :], wconv=wconv, tile_cb=tile_cb)


def emit_moe_d_old(k, C, h_tok, th, d):
    emit_moe(k, C, h_tok, th, d["wg"], d["bg"], d["we"], d["be"], d["wgu"], d["wdn"], d["ybuf"],
             d["ln"][2:3, :], d["ln"][3:4, :])


def build_p3():
    nc = bass.Bass("TRN2", target_bir_lowering=False)
    k = K(nc)
    cst_d = nc.dram_tensor("cst", [128, NC32], F32, kind="ExternalInput").ap()
    h_d = nc.dram_tensor("h_in", [NT, D], F32, kind="ExternalInput").ap()
    oT_d = nc.dram_tensor("oT", [D, NT], F32, kind="ExternalInput").ap()
    wo_d = nc.dram_tensor("w_o", [D, D], F32, kind="ExternalInput").ap()
    md = moe_dram(nc)
    out_d = nc.dram_tensor("out", [NT, D], F32, kind="ExternalOutput").ap()
    C = load_consts(k, cst_d)
    h_tok = k.sb("h_tok", [128, TT, D], F32)
    th = [Trk(f"h{i}") for i in range(TT)]
    load_tok(k, h_tok, th, h_d)
    with k.scope():
        aT = k.sb("aT", [128, CT, NT], BF16)
        taT = Trk("aT")
        k.dma("pool", aT[:], oT_d.rearrange("(ct p) t -> p ct t", p=128), taT, W=[taT])
        emit_proj_res_ln(k, aT, taT, wo_d, h_tok, th, md["ln"][0:1, :], md["ln"][1:2, :])
    emit_moe_d(k, C, h_tok, th, md)
    to = Trk("out")
    store_tok(k, h_tok, th, out_d, to)
    k.barrier()
    k.close()
    return nc


HALO = 30
NXC = NT + HALO + 2
NCPAR = 16 * 31 + 16 * 3 + 32


def conv_params(inp):
    wdw = inp["conv_w_dw"][0]
    a = wdw.T.reshape(CT, 128, 31).transpose(1, 0, 2).reshape(128, CT * 31)
    cols = [a]
    for v in (inp["conv_b_dw"][0], inp["conv_ln_g"][0], inp["conv_ln_b"][0]):
        cols.append(v.reshape(CT, 128).T)
    cols.append(inp["conv_b_pw1"][0].reshape(32, 128).T)
    return np.ascontiguousarray(np.concatenate(cols, axis=1).astype(np.float32))


def emit_conv(k, C, xT_d, hm_d, cpar_d, w1_d, w2_d, h_tok, th, lng_d, lnb_d, dbg=None, bg=None):
    nc = k.nc
    with k.scope():
        cp = k.sb("cpar", [128, NCPAR], F32)
        tcp = Trk("cpar")
        k.dma("sp", cp[:], cpar_d, tcp, W=[tcp])
        wdw = cp[:, 0:CT * 31].rearrange("p (j t) -> p j t", j=CT)
        bdw = cp[:, 496:512]
        cg = cp[:, 512:528]
        cb = cp[:, 528:544]
        b1 = cp[:, 544:576]
        hm = k.sb("hm", [128, 1], F32)
        thm = Trk("hm")
        k.dma("sp", hm[:], pb(hm_d), thm, W=[thm])
        gluT = k.sb("gluT", [128, CT, NXC], BF16)
        tglu = [Trk(f"glu{j}") for j in range(CT)]
        with k.scope():
            xT = k.sb("xT", [128, CT, NXC], BF16)
            txT = Trk("xT")
            k.dma("pool", xT[:, :, 0:NT + HALO], xT_d.rearrange("(ct p) t -> p ct t", p=128), txT, W=[txT])
            NS = 3
            wsl = [k.sb("w1s", [128, CT, 512], BF16) for _ in range(NS)]
            twsl = [Trk(f"w1s{i}") for i in range(NS)]
            sig = [k.sb("sig", [128, 512], F32) for _ in range(2)]
            tsig = [Trk("sig0"), Trk("sig1")]
            psA = [k.ps("psA", [128, 512]) for _ in range(2)]
            tpA = [Trk("pA0"), Trk("pA1")]
            psB = [k.ps("psB", [128, 512]) for _ in range(2)]
            tpB = [Trk("pB0"), Trk("pB1")]
            W1v = w1_d.rearrange("(ct p) n -> p ct n", p=128)
            loads = []
            for jg in range(4):
                loads.append(jg * 512)
                loads.append(D + jg * 512)

            nxt = [0]

            def pump(done):
                while nxt[0] < len(loads) and (nxt[0] < NS or (nxt[0] - NS) // 2 < done):
                    i = nxt[0]
                    s_ = i % NS
                    k.dma("pool", wsl[s_][:], W1v[:, :, loads[i]:loads[i] + 512], twsl[s_], W=[twsl[s_]])
                    nxt[0] += 1

            n = 0
            for jg in range(4):
                pump(jg)
                assert nxt[0] >= 2 * jg + 2
                sa, sg_ = (2 * jg) % NS, (2 * jg + 1) % NS
                for jj in range(4):
                    j = jg * 4 + jj
                    for c0, c1 in ((0, 512), (512, 1024), (1024, NT + HALO)):
                        w = c1 - c0
                        p = n % 2
                        n += 1
                        for ct in range(CT):
                            k.mm(psA[p][:, 0:w], wsl[sa][:, ct, jj * 128:(jj + 1) * 128], xT[:, ct, c0:c1],
                                 ct == 0, ct == CT - 1, R=[twsl[sa], txT], W=[tpA[p]])
                        for ct in range(CT):
                            k.mm(psB[p][:, 0:w], wsl[sg_][:, ct, jj * 128:(jj + 1) * 128], xT[:, ct, c0:c1],
                                 ct == 0, ct == CT - 1, R=[twsl[sg_], txT], W=[tpB[p]])
                        k.act(sig[p][:, 0:w], psB[p][:, 0:w], AF.Sigmoid, bias=b1[:, 16 + j:17 + j], scale=1.0,
                              R=[tpB[p], tcp], W=[tsig[p]])
                        k.v("dve", "scalar_tensor_tensor", gluT[:, j, c0:c1], psA[p][:, 0:w], b1[:, j:j + 1],
                            sig[p][:, 0:w], ALU.add, ALU.mult, R=[tpA[p], tsig[p], tcp], W=[tglu[j]])
                    if bg is not None:
                        bg("pw1")
            for j in range(CT):
                k.v("dve", "tensor_scalar", gluT[:, j, 0:HALO], gluT[:, j, 0:HALO], hm[:, 0:1], None, ALU.mult,
                    R=[thm], W=[tglu[j]])
        if dbg is not None:
            tdb = Trk("dbg")
            k.dma("sp", dbg["glu"].rearrange("(j p) t -> p j t", p=128), gluT[:], tdb, R=tglu, W=[tdb], join=True)
        for tc in range(2):
            with k.scope():
                sT = k.sb("sT", [128, CT, 512], BF16)
                tsT = Trk("sT")
                with k.scope():
                    v = k.sb("cv", [128, CT, 512], F32)
                    tv = [Trk(f"cv{j}") for j in range(CT)]
                    dg = [k.sb("dg", [128, 31, 128], BF16) for _ in range(2)]
                    tdg = [Trk("dg0"), Trk("dg1")]
                    vsq = [k.sb("vsq", [128, 512], F32) for _ in range(2)]
                    tvsq = [Trk("vsq0"), Trk("vsq1")]
                    psC = [k.ps("psC", [128, 512]) for _ in range(2)]
                    tpC = [Trk("pC0"), Trk("pC1")]
                    psS = k.ps("psS", [128, 512])
                    tpS = Trk("pS")
                    psQ = k.ps("psQ", [128, 512])
                    tpQ = Trk("pQ")
                    for j in range(CT):
                        p = j % 2
                        for t in range(31):
                            k.v("dve", "tensor_scalar", dg[p][:, t, :], C["ident_bf"],
                                wdw[:, j, t:t + 1], None, ALU.mult, R=[C["tbf"], tcp], W=[tdg[p]])
                        for t in range(31):
                            k.mm(psC[p][:, :], dg[p][:, t, :], gluT[:, j, tc * 512 + t:tc * 512 + t + 512], t == 0, t == 30,
                                 R=[tdg[p], tglu[j]], W=[tpC[p]])
                        k.act(v[:, j, :], psC[p][:, :], AF.Identity, bias=bdw[:, j:j + 1], scale=1.0,
                              R=[tpC[p], tcp], W=[tv[j]])
                        k.act(vsq[p][:], v[:, j, :], AF.Square, R=[tv[j]], W=[tvsq[p]])
                        k.mm(psS[:, :], C["ones"], v[:, j, :], j == 0, j == CT - 1, R=[C["t32"], tv[j]], W=[tpS])
                        k.mm(psQ[:, :], C["ones"], vsq[p][:], j == 0, j == CT - 1, R=[C["t32"], tvsq[p]], W=[tpQ])
                        if bg is not None:
                            bg("conv")
                    if dbg is not None:
                        k.dma("sp", dbg["v"][tc].rearrange("(j p) t -> p j t", p=128), v[:], tdb, R=tv, W=[tdb], join=True)
                    mu = k.sb("mu", [128, 512], F32)
                    rs = k.sb("rs", [128, 512], F32)
                    m2 = k.sb("m2", [128, 512], F32)
                    tm = Trk("murs")
                    k.v("dve", "tensor_scalar", mu[:], psS[:, :], 1.0 / D, None, ALU.mult, R=[tpS], W=[tm])
                    k.v("dve", "tensor_tensor", m2[:], mu[:], mu[:], ALU.mult, R=[tm], W=[tm])
                    k.v("dve", "scalar_tensor_tensor", m2[:], psQ[:, :], 1.0 / D, m2[:], ALU.mult, ALU.subtract,
                        R=[tpQ, tm], W=[tm])
                    k.act(m2[:], m2[:], AF.Sqrt, bias=LN_EPS, scale=1.0, R=[tm], W=[tm])
                    k.v("dve", "reciprocal", rs[:], m2[:], R=[tm], W=[tm])
                    for j in range(CT):
                        k.v("dve", "tensor_tensor", v[:, j, :], v[:, j, :], mu[:], ALU.subtract, R=[tm, tv[j]], W=[tv[j]])
                        k.v("dve", "tensor_tensor", v[:, j, :], v[:, j, :], rs[:], ALU.mult, R=[tm, tv[j]], W=[tv[j]])
                        k.act(sT[:, j, :], v[:, j, :], AF.Silu, bias=cb[:, j:j + 1], scale=cg[:, j:j + 1],
                              R=[tv[j], tcp], W=[tsT])
                if dbg is not None:
                    k.dma("sp", dbg["s"][tc].rearrange("(j p) t -> p j t", p=128), sT[:], tdb, R=[tsT], W=[tdb], join=True)
                emit_proj_res_ln(k, sT, tsT, w2_d, h_tok, th, lng_d, lnb_d, tts=[tc * 4 + i for i in range(4)])


def build_p1():
    nc = bass.Bass("TRN2", target_bir_lowering=False)
    k = K(nc)
    cst_d = nc.dram_tensor("cst", [128, NC32], F32, kind="ExternalInput").ap()
    x_d = nc.dram_tensor("x_tok", [NT, D], F32, kind="ExternalInput").ap()
    xT_d = nc.dram_tensor("xT", [D, NT + HALO], F32, kind="ExternalInput").ap()
    hm_d = nc.dram_tensor("hm", [1, 1], F32, kind="ExternalInput").ap()
    cpar_d = nc.dram_tensor("cpar", [128, NCPAR], F32, kind="ExternalInput").ap()
    w1_d = nc.dram_tensor("w_pw1", [D, 2 * D], F32, kind="ExternalInput").ap()
    w2_d = nc.dram_tensor("w_pw2", [D, D], F32, kind="ExternalInput").ap()
    md = moe_dram(nc)
    out_d = nc.dram_tensor("out", [NT, D], F32, kind="ExternalOutput").ap()
    C = load_consts(k, cst_d)
    h_tok = k.sb("h_tok", [128, TT, D], F32)
    th = [Trk(f"h{i}") for i in range(TT)]
    load_tok(k, h_tok, th, x_d)
    emit_conv(k, C, xT_d, hm_d, cpar_d, w1_d, w2_d, h_tok, th, md["ln"][0:1, :], md["ln"][1:2, :])
    emit_moe_d(k, C, h_tok, th, md)
    to = Trk("out")
    store_tok(k, h_tok, th, out_d, to)
    k.barrier()
    k.close()
    return nc


def p1_inputs(inp, c):
    b, j = divmod(c, 4)
    x = inp["x"][b]
    s0 = j * NT
    xe = np.zeros((NT + HALO, D), np.float32)
    if j > 0:
        xe[:] = x[s0 - HALO:s0 + NT]
    else:
        xe[HALO:] = x[0:NT]
    d = dict(cst=make_consts(), x_tok=np.ascontiguousarray(x[s0:s0 + NT]), xT=np.ascontiguousarray(xe.T),
             hm=np.full((1, 1), 0.0 if j == 0 else 1.0, np.float32), cpar=conv_params(inp),
             w_pw1=np.ascontiguousarray(inp["conv_w_pw1"][0]), w_pw2=np.ascontiguousarray(inp["conv_w_pw2"][0]))
    d.update(moe_inputs(inp, 0))
    return d


NQB = SEQ // 128
NCMP = 255
QSCALE = 128.0 ** -0.5
NCM = 33


def _split3(v):
    import ml_dtypes
    v = v.astype(np.float32)
    a = v.astype(ml_dtypes.bfloat16).astype(np.float32)
    b = (v - a).astype(ml_dtypes.bfloat16).astype(np.float32)
    c = (v - a - b).astype(ml_dtypes.bfloat16).astype(np.float32)
    return a, b, c


def attn_consts(g):
    slopes = np.array([2.0 ** (-8.0 * (4 * g + hh + 1) / 16.0) for hh in range(4)], np.float64)
    kr = np.arange(128)
    LT = np.zeros((128, 32 * 128), np.float32)
    for d in range(32):
        for hh in range(4):
            val = slopes[hh] * (128.0 * (-d) + kr - 64.0)
            for s_, part in enumerate(_split3(val)):
                LT[hh * 3 + s_, d * 128:(d + 1) * 128] = part
    LTc = np.zeros((128, 32 * 128), np.float32)
    for dc in range(32):
        for hh in range(4):
            val = slopes[hh] * (16.0 * kr - 33.0 - 128.0 * dc)
            for s_, part in enumerate(_split3(val)):
                LTc[hh * 3 + s_, dc * 128:(dc + 1) * 128] = part
    HI = np.zeros((128, 512), np.float32)
    for hh in range(4):
        HI[hh * 3:hh * 3 + 3, hh * 128:(hh + 1) * 128] = 1.0
    E = np.zeros((128, SEQ), np.float32)
    for d in range(32):
        for half in range(2):
            r = 62 - 2 * d + half
            E[r, d * 128 + half * 64:d * 128 + half * 64 + 64] = 1.0
    E[64:76, :] = LT[0:12, :]
    qr = np.arange(128)
    Mc = np.where(kr[:, None] <= qr[None, :], 0.0, NEG).astype(np.float32)
    Mw = np.where(kr[:, None] > qr[None, :], 0.0, NEG).astype(np.float32)
    Mc4 = np.tile(Mc, (1, 4))
    Mw4 = np.tile(Mw, (1, 4))
    CM = np.zeros((128, NCM * 128), np.float32)
    for di in range(17):
        m = np.where(16 * kr[:, None] + 31 <= 128 * di + qr[None, :], 0.0, NEG)
        CM[:, di * 128:(di + 1) * 128] = m
    for di in range(16):
        m = np.where(16 * kr[:, None] + 31 <= 128 * di + qr[None, :], 0.0, NEG)
        m[127, :] = NEG
        CM[:, (17 + di) * 128:(18 + di) * 128] = m
    n = np.arange(256)[:, None]
    j = np.arange(64)[None, :]
    ov = np.maximum(np.minimum(16 * n + 32, 64 * j + 64) - np.maximum(16 * n, 64 * j), 0) / 32.0
    ov[255] = 0
    ovl = ov.reshape(2, 128, 64).transpose(1, 0, 2).reshape(128, 128)
    ident = np.eye(128, dtype=np.float32)
    t = np.arange(SEQ)
    cur = (t // 64)[:, None]
    blk = np.arange(64)[None, :]
    forced = ((blk == 0) | (blk == cur) | (blk == cur - 1)).astype(np.float32)
    cand = ((blk >= 1) & (blk <= cur - 2)).astype(np.float32)
    f3 = forced.reshape(32, 128, 64).transpose(1, 0, 2).reshape(128, 32 * 64)
    c3 = cand.reshape(32, 128, 64).transpose(1, 0, 2).reshape(128, 32 * 64)
    cb = np.concatenate([LT, LTc, HI, E, Mc4, Mw4, CM, ovl, ident, c3, f3], axis=1).astype(np.float32)
    cf = ident.copy()
    return np.ascontiguousarray(cb), np.ascontiguousarray(cf)


CB_OFF = {}
_o = 0
for _n, _w in (("LT", 32 * 128), ("LTc", 32 * 128), ("HI", 512), ("E", SEQ), ("Mc4", 512), ("Mw4", 512),
               ("CM", NCM * 128), ("ov", 128), ("ident", 128), ("cand", 2048), ("forced", 2048)):
    CB_OFF[_n] = (_o, _o + _w)
    _o += _w
NCB = _o
NCF = 128
NWF = 1024
NWT = 268


def attn_weights(inp, g):
    wqg = inp["nsa_w_qg"][0]
    kvw = inp["kv_w"]
    sl = lambda s_: kvw[:, s_ * 512 + g * 128: s_ * 512 + (g + 1) * 128]
    WF = np.concatenate([wqg[:, g * 512:(g + 1) * 512], sl(2), sl(4), sl(0), sl(1)], axis=1)
    WT = np.concatenate([sl(3), sl(5), wqg[:, 2048 + g * 12: 2048 + (g + 1) * 12]], axis=1)
    posT = np.concatenate([inp["cmp_pos"][0].T, inp["cmp_pos"][1].T], axis=1)
    return dict(WF=np.ascontiguousarray(WF), WT=np.ascontiguousarray(WT), posT=np.ascontiguousarray(posT),
                cw1=np.ascontiguousarray(inp["cmp_w1"]), cw2=np.ascontiguousarray(inp["cmp_w2"]))


def emit_attn(k, hT_d, WF_d, WT_d, posT_d, cw1_d, cw2_d, cb_d, cf_d, o_d, dbg=None, hsrc=None, ob=None, tob=None,
              chunk_order=None, after_qb=None, bg=None):
    nc = k.nc
    cbs = k.sb("cbs", [128, NCB], BF16)
    tcb = Trk("cbs")
    k.dma("pool", cbs[:], cb_d, tcb, W=[tcb])
    cfs = k.sb("cfs", [128, NCF], F32)
    tcf = Trk("cfs")
    k.dma("sp", cfs[:], cf_d, tcf, W=[tcf])
    cbv = lambda n_: cbs[:, CB_OFF[n_][0]:CB_OFF[n_][1]]
    LT, LTc, HI, E_, Mc4, Mw4, CM, ovl, identb = (cbv(n_) for n_ in ("LT", "LTc", "HI", "E", "Mc4", "Mw4", "CM", "ov", "ident"))
    cand = cbv("cand").rearrange("p (a b) -> p a b", a=32)
    forced = cbv("forced").rearrange("p (a b) -> p a b", a=32)
    identf = cfs[:, 0:128]
    QT = k.sb("QT", [128, NQB, 4, 128], BF16)
    tQ = [Trk(f"Q{i}") for i in range(16)]
    KsT = k.sb("KsT", [128, SEQ], BF16)
    KwT = k.sb("KwT", [128, SEQ], BF16)
    tK = [Trk(f"K{i}") for i in range(16)]
    Vs = k.sb("Vs", [128, NQB, 132], BF16)
    Vw = k.sb("Vw", [128, NQB, 132], BF16)
    tV = [Trk(f"V{i}") for i in range(16)]
    gat = k.sb("gat", [128, NQB, 12], F32)
    KcT = k.sb("KcT", [128, 256], BF16)
    Vc = k.sb("Vc", [128, 2, 196], BF16)
    tC = Trk("cmpkv")
    k.v("dve", "memset", Vs[:, :, 128:132], 1.0, W=tV)
    k.v("dve", "memset", Vw[:, :, 128:132], 1.0, W=tV)
    k.v("dve", "memset", Vc[:], 0.0, W=[tC])
    k.v("dve", "memset", KcT[:], 0.0, W=[tC])

    with k.scope():
        rawT = [k.sb("rawT", [128, SEQ], BF16) for _ in range(2)]
        traw = [Trk(f"raw{i}") for i in range(16)]
        with k.scope():
            WF = k.sb("WF", [128, CT, NWF], BF16)
            tWF = Trk("WF")
            WFv = WF_d.rearrange("(ct p) n -> p ct n", p=128)
            k.dma("pool", WF[:, :, 0:512], WFv[:, :, 0:512], tWF, W=[tWF])
            k.dma("pool", WF[:, :, 512:1024], WFv[:, :, 512:1024], tWF, W=[tWF], join=True)
            WT = k.sb("WT", [128, CT, NWT], BF16)
            tWT = Trk("WT")
            k.dma("pool", WT[:], WT_d.rearrange("(ct p) n -> p ct n", p=128), tWT, W=[tWT])
            hc = [k.sb("hc", [128, CT, 256], BF16) for _ in range(2)]
            thc = [Trk("hc0"), Trk("hc1")]
            psF = [k.ps("psF", [128, 512]) for _ in range(3)]
            tpF = [Trk(f"pF{i}") for i in range(3)]
            psT = [k.ps("psT", [128, 512]) for _ in range(2)]
            tpT = [Trk(f"pT{i}") for i in range(2)]
            hv = hT_d.rearrange("(ct p) t -> p ct t", p=128) if hT_d is not None else None

            def issue(c):
                if c < 16:
                    if hsrc is None:
                        k.dma("pool", hc[c % 2][:], hv[:, :, c * 256:(c + 1) * 256], thc[c % 2], W=[thc[c % 2]])
                    else:
                        ap_, Rr = hsrc(c)
                        k.dma("sp", hc[c % 2][:], ap_, thc[c % 2], R=Rr, W=[thc[c % 2]])

            order = list(range(16)) if chunk_order is None else list(chunk_order)

            def issue(i):
                if i < 16:
                    c_ = order[i]
                    if hsrc is None:
                        k.dma("pool", hc[i % 2][:], hv[:, :, c_ * 256:(c_ + 1) * 256], thc[i % 2], W=[thc[i % 2]])
                    else:
                        ap_, Rr = hsrc(c_)
                        k.dma("sp", hc[i % 2][:], ap_, thc[i % 2], R=Rr, W=[thc[i % 2]])

            issue(0)
            nf = 0
            ntk = 0
            for ci_, c in enumerate(order):
                issue(ci_ + 1)
                H = hc[ci_ % 2]
                tH = thc[ci_ % 2]
                for f in range(8):
                    p = nf % 3
                    nf += 1
                    for ct in range(CT):
                        k.mm(psF[p][:, 0:256], WF[:, ct, f * 128:(f + 1) * 128], H[:, ct, :], ct == 0, ct == CT - 1,
                             R=[tWF, tH], W=[tpF[p]])
                    if f < 4:
                        k.act(QT[:, 2 * c:2 * c + 2, f, :], psF[p][:, 0:256].rearrange("p (a b) -> p a b", a=2), AF.Copy,
                              scale=QSCALE, R=[tpF[p]], W=[tQ[c]])
                    else:
                        dst = (KsT, KwT, rawT[0], rawT[1])[f - 4]
                        trk = (tK[c], tK[c], traw[c], traw[c])[f - 4]
                        if f % 2 == 0:
                            k.v("dve", "tensor_copy", dst[:, c * 256:(c + 1) * 256], psF[p][:, 0:256], R=[tpF[p]], W=[trk])
                        else:
                            k.act(dst[:, c * 256:(c + 1) * 256], psF[p][:, 0:256], AF.Copy, R=[tpF[p]], W=[trk])
                for i in range(2):
                    tt = c * 2 + i
                    p = ntk % 2
                    ntk += 1
                    for ct in range(CT):
                        k.mm(psT[p][:, 0:NWT], H[:, ct, i * 128:(i + 1) * 128], WT[:, ct, :], ct == 0, ct == CT - 1,
                             R=[tH, tWT], W=[tpT[p]])
                    k.v("dve", "tensor_copy", Vs[:, tt, 0:128], psT[p][:, 0:128], R=[tpT[p]], W=[tV[c]])
                    k.v("dve", "tensor_copy", Vw[:, tt, 0:128], psT[p][:, 128:256], R=[tpT[p]], W=[tV[c]])
                    k.act(gat[:, tt, :], psT[p][:, 256:268], AF.Sigmoid, R=[tpT[p]], W=[tV[c]])
                if bg is not None:
                    bg("proj")
        with k.scope():
            posT = k.sb("posT", [128, 64], BF16)
            tpos = Trk("posT")
            k.dma("pool", posT[:], posT_d, tpos, W=[tpos])
            W1 = [k.sb("cW1", [128, 32, 256], BF16) for _ in range(2)]
            W2 = [k.sb("cW2", [128, 2, 128], BF16) for _ in range(2)]
            tW = Trk("cW")
            for c in range(2):
                k.dma("pool", W1[c][:], cw1_d[c].rearrange("(cb p) n -> p cb n", p=128), tW, W=[tW], join=True)
                k.dma("pool", W2[c][:], cw2_d[c].rearrange("(a p) n -> p a n", p=128), tW, W=[tW], join=True)
            psH = [k.ps("psH", [128, 512]) for _ in range(2)]
            tpH = [Trk("pH0"), Trk("pH1")]
            psB = k.ps("psB", [128, 512])
            tpB = Trk("pB")
            psO = k.ps("psO", [128, 512])
            tpO = Trk("pO")
            hb = k.sb("hbias", [128, 4], F32)
            thb = Trk("hbias")
            xg = k.sb("xg", [128, 3, 256], F32)
            txg = Trk("xg")
            hid = [[k.sb("hid", [128, 256], BF16) for _ in range(2)] for _ in range(2)]
            thid = [[Trk(f"hid{a}{b}") for b in range(2)] for a in range(2)]
            nh = 0
            for c in range(2):
                rv = rawT[c][:, :].rearrange("p (n s) -> p n s", s=16)
                for hk in range(2):
                    for cb_ in range(32):
                        k.mm(psB[:, hk:hk + 1], W1[c][:, cb_, hk * 128:(hk + 1) * 128], posT[:, c * 32 + cb_:c * 32 + cb_ + 1],
                             cb_ == 0, cb_ == 31, R=[tW, tpos], W=[tpB])
                    k.v("dve", "tensor_copy", hb[:, c * 2 + hk:c * 2 + hk + 1], psB[:, hk:hk + 1], R=[tpB], W=[thb])
                    p = nh % 2
                    nh += 1
                    for cb_ in range(32):
                        k.mm(psH[p][:, 0:NCMP], W1[c][:, cb_, hk * 128:(hk + 1) * 128],
                             rv[:, cb_ // 16:cb_ // 16 + NCMP, cb_ % 16], cb_ == 0, cb_ == 31,
                             R=[tW] + traw, W=[tpH[p]])
                    x_ = xg[:, 0, 0:NCMP]
                    u_ = xg[:, 1, 0:NCMP]
                    s_ = xg[:, 2, 0:NCMP]
                    k.act(x_, psH[p][:, 0:NCMP], AF.Identity, bias=hb[:, c * 2 + hk:c * 2 + hk + 1], scale=1.0,
                          R=[tpH[p], thb], W=[txg])
                    k.v("dve", "tensor_tensor", u_, x_, x_, ALU.mult, R=[txg], W=[txg])
                    k.v("dve", "tensor_scalar", u_, u_, 0.044715, 1.0, ALU.mult, ALU.add, R=[txg], W=[txg])
                    k.v("dve", "tensor_tensor", u_, u_, x_, ALU.mult, R=[txg], W=[txg])
                    k.act(s_, u_, AF.Sigmoid, scale=2.0 * 0.7978845608028654, R=[txg], W=[txg])
                    k.v("dve", "tensor_tensor", hid[c][hk][:, 0:NCMP], x_, s_, ALU.mult, R=[txg], W=[thid[c][hk]])
            for hk in range(2):
                k.mm(psO[:, 0:NCMP], W2[0][:, hk, :], hid[0][hk][:, 0:NCMP], hk == 0, hk == 1,
                     R=[tW, thid[0][hk]], W=[tpO])
            k.v("dve", "tensor_copy", KcT[:, 0:NCMP], psO[:, 0:NCMP], R=[tpO], W=[tC])
            for nt, (n0, n1) in enumerate(((0, 128), (128, NCMP))):
                w = n1 - n0
                for hk in range(2):
                    k.mm(psO[0:w, 256:384], hid[1][hk][:, n0:n1], W2[1][:, hk, :], hk == 0, hk == 1,
                         R=[tW, thid[1][hk]], W=[tpO])
                k.v("dve", "tensor_copy", Vc[0:w, nt, 0:128], psO[0:w, 256:384], R=[tpO], W=[tC])
                k.v("dve", "tensor_copy", Vc[0:w, nt, 128:192], ovl[0:w, nt * 64:(nt + 1) * 64], R=[tcb], W=[tC])
                k.v("dve", "memset", Vc[0:w, nt, 192:193], 1.0, W=[tC])
    if dbg is not None:
        tdb = Trk("dbg")
        k.dma("sp", dbg["KcT"], KcT[:], tdb, R=[tC], W=[tdb], join=True)
        k.dma("sp", dbg["Vc"], Vc[:], tdb, R=[tC], W=[tdb], join=True)
        k.dma("sp", dbg["QT"], QT[:], tdb, R=tQ, W=[tdb], join=True)
        k.dma("sp", dbg["KsT"], KsT[:], tdb, R=tK, W=[tdb], join=True)
        k.dma("sp", dbg["Vs"], Vs[:], tdb, R=tV, W=[tdb], join=True)
        k.dma("sp", dbg["gat"], gat[:], tdb, R=tV, W=[tdb], join=True)

    with k.scope():
        NPS = 3
        psS = [k.ps("psS", [128, 512]) for _ in range(NPS)]
        tpS = [Trk(f"pS{i}") for i in range(NPS)]
        psO = [[k.ps("psOa", [128, 512]) for _ in range(2)] for _ in range(2)]
        tpO = [[Trk(f"pO{a}{b}") for b in range(2)] for a in range(2)]
        psX = k.ps("psX", [128, 512])
        tpX = Trk("pX")
        NPT = 4
        PT = [k.sb("PT", [128, 512], BF16) for _ in range(NPT)]
        tPT = [Trk(f"PT{i}") for i in range(NPT)]
        oacc = [k.sb("oacc", [128, 512], F32) for _ in range(3)]
        toacc = [Trk("oacc0"), Trk("oacc1"), Trk("oacc2")]
        imp = [k.sb("imp", [128, 64], F32) for _ in range(2)]
        timp = [Trk("imp0"), Trk("imp1")]
        sm = k.sb("asm", [128, 6, 16], F32)
        tsm = [Trk(f"asm{i}") for i in range(6)]
        selw = k.sb("selw", [128, 2, 4, 64], F32)
        tselw = [Trk("selw0"), Trk("selw1")]
        NSB = 3
        selb4 = [k.sb("selb4", [76, 512], BF16) for _ in range(NSB)]
        tsb4 = [Trk(f"selb4{i}") for i in range(NSB)]
        for i_ in range(NSB):
            k.dma("pool", selb4[i_][64:76, :], cb_d[0:12, CB_OFF["HI"][0]:CB_OFF["HI"][1]], tsb4[i_], W=[tsb4[i_]])
        tout = Trk("o_out")
        oTt = [k.sb("oTt", [128, 4, 128], BF16) for _ in range(2)]
        toTt = [Trk("oTt0"), Trk("oTt1")]
        st = dict(ns=0, npt=0, no=0, nsm=0)
        Qv = lambda qb: QT[:, qb, :, :].rearrange("p h q -> p (h q)")
        glist = []

        def add_group(qb, lhsK, tKk, extra, Vaug, vw, tVv, first, last, ob, after=None):
            slot = {}

            def scores():
                s_ = st["ns"] % NPS
                st["ns"] += 1
                k.mm(psS[s_][:, :], lhsK, Qv(qb), True, False, R=[tKk, tQ[qb // 2]], W=[tpS[s_]], inc=False)
                for i, (l_, r_, o_, Rr) in enumerate(extra):
                    out_ap = psS[s_][:, :] if o_ is None else psS[s_][:, o_[0]:o_[1]]
                    k.mm(out_ap, l_, r_, False, i == len(extra) - 1, R=Rr, W=[tpS[s_]], inc=(i == len(extra) - 1))
                pt = st["npt"] % NPT
                st["npt"] += 1
                slot["pt"] = pt
                k.act(PT[pt][:], psS[s_][:, :], AF.Exp, R=[tpS[s_]], W=[tPT[pt]])

            def pv():
                pt = slot["pt"]
                for hh in range(4):
                    bank = psO[ob][hh // 2]
                    c0 = (hh % 2) * 256
                    k.mm(bank[:, c0:c0 + vw], PT[pt][:, hh * 128:(hh + 1) * 128], Vaug, first and hh % 2 == 0, last,
                         R=[tPT[pt], tVv], W=[tpO[ob][hh // 2]], inc=last, skip_group_check=True)

            glist.append((scores, pv, after))

        def finish(qb, ob, br, first_branch, vw, with_imp=False):
            oa = oacc[qb % 3]
            to_ = toacc[qb % 3]
            i6 = st["nsm"] % 6
            st["nsm"] += 1
            S_ = tsm[i6]
            for hh in range(4):
                bank = psO[ob][hh // 2]
                tb = tpO[ob][hh // 2]
                c0 = (hh % 2) * 256
                rs = sm[:, i6, hh * 4:hh * 4 + 1]
                sc = sm[:, i6, hh * 4 + 1:hh * 4 + 2]
                k.v("dve", "tensor_scalar", rs, bank[:, c0 + vw - 1:c0 + vw], 1e-30, None, ALU.max, R=[tb, S_], W=[S_])
                k.v("dve", "reciprocal", rs, rs, R=[S_], W=[S_])
                k.v("dve", "tensor_tensor", sc, rs, gat[:, qb, hh * 3 + br:hh * 3 + br + 1], ALU.mult,
                    R=[S_, tV[qb // 2]], W=[S_])
                dst = oa[:, hh * 128:(hh + 1) * 128]
                if first_branch:
                    k.v("dve", "tensor_scalar", dst, bank[:, c0:c0 + 128], sc, None, ALU.mult, R=[tb, S_], W=[to_])
                else:
                    k.v("dve", "scalar_tensor_tensor", dst, bank[:, c0:c0 + 128], sc, dst, ALU.mult, ALU.add,
                        R=[tb, S_], W=[to_])
                if with_imp:
                    im = imp[qb % 2]
                    if hh == 0:
                        k.v("dve", "tensor_scalar", im[:], bank[:, c0 + 128:c0 + 192], rs, None, ALU.mult,
                            R=[tb, S_], W=[timp[qb % 2]])
                    else:
                        k.v("dve", "scalar_tensor_tensor", im[:], bank[:, c0 + 128:c0 + 192], rs, im[:], ALU.mult,
                            ALU.add, R=[tb, S_], W=[timp[qb % 2]])

        deferred = []

        def defer(n, fn):
            deferred.append([n, fn])

        def select_math(qb):
            w = qb % 2
            V_ = selw[:, w, 0, :]
            V2 = selw[:, w, 1, :]
            sl_ = selw[:, w, 2, :]
            m8 = selw[:, w, 3, 0:16]
            T_ = tselw[w]
            k.v("dve", "scalar_tensor_tensor", V_, imp[w][:], 1.0, cand[:, qb, :], ALU.add, ALU.mult,
                R=[timp[w], tcb], W=[T_])
            k.v("dve", "tensor_scalar", V_, V_, -1.0, None, ALU.add, R=[T_], W=[T_])
            k.v("dve", "max", m8[:, 0:8], V_, R=[T_], W=[T_])
            k.v("dve", "match_replace", V2, m8[:, 0:8], V_, -2.0, R=[T_], W=[T_])
            k.v("dve", "max", m8[:, 8:16], V2, R=[T_], W=[T_])
            k.v("dve", "tensor_scalar", sl_, V_, m8[:, 12:13], None, ALU.is_ge, R=[T_], W=[T_])
            k.v("dve", "tensor_tensor", sl_, sl_, cand[:, qb, :], ALU.mult, R=[T_, tcb], W=[T_])
            k.v("dve", "tensor_tensor", sl_, sl_, forced[:, qb, :], ALU.max, R=[T_, tcb], W=[T_])
            k.v("dve", "tensor_scalar", sl_, sl_, -NEG, NEG, ALU.mult, ALU.add, R=[T_], W=[T_])
            rlo = max(0, 62 - 2 * qb)
            jlo = rlo - 62 + 2 * qb
            k.v("dve", "memset", V2, 0.0, R=[T_], W=[T_])
            k.v("dve", "tensor_copy", V2[:, rlo:64], sl_[:, jlo:jlo + 64 - rlo], R=[T_], W=[T_])

            def tr_part():
                k.tr(psX[0:64, 0:128], V2, identf, R=[T_, tcf], W=[tpX])
                sb_ = qb % NSB
                for hh in range(4):
                    if hh % 2 == 0:
                        k.act(selb4[sb_][0:64, hh * 128:(hh + 1) * 128], psX[0:64, 0:128], AF.Copy, R=[tpX], W=[tsb4[sb_]])
                    else:
                        k.v("dve", "tensor_copy", selb4[sb_][0:64, hh * 128:(hh + 1) * 128], psX[0:64, 0:128],
                            R=[tpX], W=[tsb4[sb_]])
            defer(2, tr_part)

        def out_part(qb):
            if ob is None:
                k.dma("sp", o_d[qb * 128:(qb + 1) * 128, :], oacc[qb % 3][:], tout, R=[toacc[qb % 3]], W=[tout], join=True)
                return

            def tr_out():
                for hh in range(4):
                    k.tr(psX[:, hh * 128:(hh + 1) * 128], oacc[qb % 3][:, hh * 128:(hh + 1) * 128], identf,
                         R=[toacc[qb % 3], tcf], W=[tpX], inc=(hh == 3))
                k.act(oTt[qb % 2][:], psX[:, :].rearrange("p (h q) -> p h q", h=4), AF.Copy, R=[tpX], W=[toTt[qb % 2]])
                j_, tq = qb // 8, (qb % 8) * 128
                k.dma("sp", ob[j_][:, tq:tq + 128].rearrange("(h p) q -> p h q", p=128), oTt[qb % 2][:],
                      tob[j_], R=[toTt[qb % 2]], W=[tob[j_]], join=True)
                if after_qb is not None:
                    after_qb(qb)
            defer(2, tr_out)

        def cmp_branch(qb):
            ob_ = st["no"] % 2
            st["no"] += 1
            nts = [0] if qb < 16 else [0, 1]
            for ii, nt in enumerate(nts):
                dc = qb - 16 * nt
                extra = [(LTc[0:12, dc * 128:(dc + 1) * 128], HI[0:12, :], None, [tcb])]
                mi = None
                if nt == 0 and qb <= 16:
                    mi = qb
                elif nt == 1:
                    mi = 17 + (qb - 16)
                if mi is not None:
                    for hh in range(4):
                        extra.append((identb, CM[:, mi * 128:(mi + 1) * 128], (hh * 128, (hh + 1) * 128), [tcb]))
                aft = None
                if ii == len(nts) - 1:
                    def aft(qb=qb, ob_=ob_):
                        finish(qb, ob_, 0, True, 193, with_imp=True)
                        select_math(qb)
                add_group(qb, KcT[:, nt * 128:(nt + 1) * 128], tC, extra, Vc[:, nt, 0:193], 193, tC,
                          ii == 0, ii == len(nts) - 1, ob_, after=aft)

        def win_branch(qb):
            ob_ = st["no"] % 2
            st["no"] += 1
            kts = list(range(max(0, qb - 4), qb + 1))
            for ii, kt in enumerate(kts):
                d = qb - kt
                extra = [(LT[0:12, d * 128:(d + 1) * 128], HI[0:12, :], None, [tcb])]
                if kt == qb:
                    extra.append((identb, Mc4, None, [tcb]))
                if kt == qb - 4:
                    extra.append((identb, Mw4, None, [tcb]))
                aft = None
                if ii == len(kts) - 1:
                    def aft(qb=qb, ob_=ob_):
                        finish(qb, ob_, 2, False, 129)
                add_group(qb, KwT[:, kt * 128:(kt + 1) * 128], tK[kt // 2], extra, Vw[:, kt, 0:129], 129, tV[kt // 2],
                          ii == 0, ii == len(kts) - 1, ob_, after=aft)

        def sel_branch(qb):
            ob_ = st["no"] % 2
            st["no"] += 1
            sb_ = qb % NSB
            for kt in range(qb + 1):
                d = qb - kt
                extra = [(E_[0:76, d * 128:(d + 1) * 128], selb4[sb_][0:76, :], None, [tcb, tsb4[sb_]])]
                if kt == qb:
                    extra.append((identb, Mc4, None, [tcb]))
                aft = None
                if kt == qb:
                    def aft(qb=qb, ob_=ob_):
                        finish(qb, ob_, 1, False, 129)
                        out_part(qb)
                        if bg is not None:
                            bg("attn")
                add_group(qb, KsT[:, kt * 128:(kt + 1) * 128], tK[kt // 2], extra, Vs[:, kt, 0:129], 129, tV[kt // 2],
                          kt == 0, kt == qb, ob_, after=aft)

        cmp_branch(0)
        for qb in range(NQB):
            if qb + 1 < NQB:
                cmp_branch(qb + 1)
            win_branch(qb)
            sel_branch(qb)

        def tick():
            for d_ in list(deferred):
                d_[0] -= 1
                if d_[0] <= 0:
                    deferred.remove(d_)
                    d_[1]()

        prev = None
        for g_ in glist:
            g_[0]()
            if prev is not None:
                prev[1]()
                if prev[2] is not None:
                    prev[2]()
                tick()
            prev = g_
        prev[1]()
        if prev[2] is not None:
            prev[2]()
        while deferred:
            tick()


def build_p2(debug=False):
    nc = bass.Bass("TRN2", target_bir_lowering=False)
    k = K(nc)
    hT_d = nc.dram_tensor("hT", [D, SEQ], F32, kind="ExternalInput").ap()
    WF_d = nc.dram_tensor("WF", [D, NWF], F32, kind="ExternalInput").ap()
    WT_d = nc.dram_tensor("WT", [D, NWT], F32, kind="ExternalInput").ap()
    posT_d = nc.dram_tensor("posT", [128, 64], F32, kind="ExternalInput").ap()
    cw1_d = nc.dram_tensor("cw1", [2, 4096, 256], F32, kind="ExternalInput").ap()
    cw2_d = nc.dram_tensor("cw2", [2, 256, 128], F32, kind="ExternalInput").ap()
    cb_d = nc.dram_tensor("cb", [128, NCB], F32, kind="ExternalInput").ap()
    cf_d = nc.dram_tensor("cf", [128, NCF], F32, kind="ExternalInput").ap()
    o_d = nc.dram_tensor("o", [SEQ, 512], F32, kind="ExternalOutput").ap()
    dbg = None
    if debug:
        dbg = dict(KcT=nc.dram_tensor("d_KcT", [128, 256], BF16, kind="ExternalOutput").ap(),
                   Vc=nc.dram_tensor("d_Vc", [128, 2, 196], BF16, kind="ExternalOutput").ap(),
                   QT=nc.dram_tensor("d_QT", [128, NQB, 4, 128], BF16, kind="ExternalOutput").ap(),
                   KsT=nc.dram_tensor("d_KsT", [128, SEQ], BF16, kind="ExternalOutput").ap(),
                   Vs=nc.dram_tensor("d_Vs", [128, NQB, 132], BF16, kind="ExternalOutput").ap(),
                   gat=nc.dram_tensor("d_gat", [128, NQB, 12], F32, kind="ExternalOutput").ap())
    emit_attn(k, hT_d, WF_d, WT_d, posT_d, cw1_d, cw2_d, cb_d, cf_d, o_d, dbg=dbg)
    k.barrier()
    k.close()
    return nc


def p2_inputs(inp, h2T_b, g):
    cb, cf = attn_consts(g)
    d = dict(hT=h2T_b, cb=cb, cf=cf)
    d.update(attn_weights(inp, g))
    return d


def p3_inputs(inp, h2_c, oT_c):
    d = dict(cst=make_consts(), h_in=h2_c, oT=oT_c, w_o=np.ascontiguousarray(inp["nsa_w_o"][0]))
    d.update(moe_inputs(inp, 1))
    return d


_PROGS = {}


def _prog(name, fn):
    if name not in _PROGS:
        _PROGS[name] = fn()
    return _PROGS[name]


def _kernel3(**inputs):
    inp = {k_: np.asarray(v_) for k_, v_ in inputs.items()}
    cores = list(range(NCORE))
    r1 = run_bass_kernel_spmd(_prog("p1", build_p1), [p1_inputs(inp, c) for c in cores], core_ids=cores)
    h2 = np.stack([r1.results[c]["out"] for c in cores]).reshape(2, SEQ, D)
    h2T = [np.ascontiguousarray(h2[b].T) for b in range(2)]
    r2 = run_bass_kernel_spmd(_prog("p2", build_p2), [p2_inputs(inp, h2T[c // 4], c % 4) for c in cores], core_ids=cores)
    o = np.zeros((2, SEQ, D), np.float32)
    for c in cores:
        o[c // 4, :, (c % 4) * 512:(c % 4 + 1) * 512] = r2.results[c]["o"]
    ins3 = []
    for c in cores:
        b, j = divmod(c, 4)
        ins3.append(p3_inputs(inp, np.ascontiguousarray(h2[b, j * NT:(j + 1) * NT]),
                              np.ascontiguousarray(o[b, j * NT:(j + 1) * NT].T)))
    r3 = run_bass_kernel_spmd(_prog("p3", build_p3), ins3, core_ids=cores)
    out = np.stack([r3.results[c]["out"] for c in cores]).reshape(2, SEQ, D)
    return out.astype(np.float32)


RG4 = [[0, 1, 2, 3], [4, 5, 6, 7]]
NCONV0 = 8
NCONV1 = 0


def build_fused():
    nc = bass.Bass("TRN2", target_bir_lowering=False)
    k = K(nc)
    cst_d = nc.dram_tensor("cst", [128, NC32], F32, kind="ExternalInput").ap()
    x_d = nc.dram_tensor("x_tok", [NT, D], F32, kind="ExternalInput").ap()
    xT_d = nc.dram_tensor("xT", [D, NT + HALO], F32, kind="ExternalInput").ap()
    hm_d = nc.dram_tensor("hm", [1, 1], F32, kind="ExternalInput").ap()
    cpar_d = nc.dram_tensor("cpar", [128, NCPAR], F32, kind="ExternalInput").ap()
    w1_d = nc.dram_tensor("w_pw1", [D, 2 * D], F32, kind="ExternalInput").ap()
    w2_d = nc.dram_tensor("w_pw2", [D, D], F32, kind="ExternalInput").ap()
    md0 = moe_dram(nc, "0")
    WF_d = nc.dram_tensor("WF", [D, NWF], F32, kind="ExternalInput").ap()
    WT_d = nc.dram_tensor("WT", [D, NWT], F32, kind="ExternalInput").ap()
    posT_d = nc.dram_tensor("posT", [128, 64], F32, kind="ExternalInput").ap()
    cw1_d = nc.dram_tensor("cw1", [2, 4096, 256], F32, kind="ExternalInput").ap()
    cw2_d = nc.dram_tensor("cw2", [2, 256, 128], F32, kind="ExternalInput").ap()
    cb_d = nc.dram_tensor("cb", [128, NCB], F32, kind="ExternalInput").ap()
    cf_d = nc.dram_tensor("cf", [128, NCF], F32, kind="ExternalInput").ap()
    idx3_d = nc.dram_tensor("idx3", [128, CT], I32, kind="ExternalInput").ap()
    wo_d = nc.dram_tensor("w_o", [D, D], F32, kind="ExternalInput").ap()
    md1 = moe_dram(nc, "1")
    out_d = nc.dram_tensor("out", [NT, D], F32, kind="ExternalOutput").ap()
    hsp = nc.dram_tensor("hsp", [NT, D], F32).ap()
    xb1 = [nc.dram_tensor(f"xb1_{q}", [D, 256], BF16).ap() for q in range(4)]
    xg1 = [nc.dram_tensor(f"xg1_{q}", [4 * D, 256], BF16).ap() for q in range(4)]
    ob = [nc.dram_tensor(f"ob_{j}", [512, NT], BF16).ap() for j in range(4)]
    og = nc.dram_tensor("og", [4 * D, NT], BF16).ap()
    thsp = Trk("hsp")
    txb1 = [Trk(f"xb1{q}") for q in range(4)]
    txg1 = [Trk(f"xg1{q}") for q in range(4)]
    tob = [Trk(f"ob{j}") for j in range(4)]
    tog = [Trk(f"og{j}") for j in range(4)]

    wc0 = WConv(k, md0, "0", NCONV0)
    wc1 = WConv(k, md1, "1", NCONV1)

    cnt = dict(pw1=0, conv=0, attn=0, proj=0)

    def bg0(where):
        cnt[where] += 1
        if where == "conv" and cnt[where] % 2 == 0:
            wc0.pump(1)

    def bg1(where):
        cnt[where] += 1
        if where == "proj":
            wc1.pump(1)
        else:
            wc1.pump(2 if cnt[where] % 2 == 0 else 1)

    C = load_consts(k, cst_d)
    with k.scope():
        h_tok = k.sb("h_tok", [128, TT, D], F32)
        th = [Trk(f"h{i}") for i in range(TT)]
        load_tok(k, h_tok, th, x_d)
        emit_conv(k, C, xT_d, hm_d, cpar_d, w1_d, w2_d, h_tok, th, md0["ln"][0:1, :], md0["ln"][1:2, :], bg=bg0)
        wc0.flush()
        def cb_setup():
            ctx = dict(hTb=k.sb("hTb", [128, CT, NT], BF16), thTb=[Trk(f"hTb{i}") for i in range(TT)],
                       psA=[k.ps("psTr", [128, 512]) for _ in range(4)], tpsA=[Trk(f"psTr{i}") for i in range(4)], n=0)
            return ctx

        def cb_tile(ctx, tt):
            hvs = hsp.rearrange("(tt p) c -> p tt c", p=128)
            k.dma("sp", hvs[:, tt, :], h_tok[:, tt, :], thsp, R=[th[tt]], W=[thsp], join=True)
            hTb, psA, tpsA = ctx["hTb"], ctx["psA"], ctx["tpsA"]
            for q in range(4):
                p = ctx["n"] % 4
                ctx["n"] += 1
                for i in range(4):
                    ct = q * 4 + i
                    k.tr(psA[p][:, i * 128:(i + 1) * 128], h_tok[:, tt, ct * 128:(ct + 1) * 128], C["ident"],
                         R=[th[tt], C["t32"]], W=[tpsA[p]], inc=(i == 3))
                dst = hTb[:, q * 4:(q + 1) * 4, tt * 128:(tt + 1) * 128]
                src = psA[p][:, :].rearrange("p (a b) -> p a b", a=4)
                if q % 2 == 0:
                    k.act(dst, src, AF.Copy, R=[tpsA[p]], W=[ctx["thTb"][tt]])
                else:
                    k.v("dve", "tensor_copy", dst, src, R=[tpsA[p]], W=[ctx["thTb"][tt]])
            if tt % 2 == 1:
                q_ = tt // 2
                k.dma("sp", xb1[q_].rearrange("(ct p) t -> p ct t", p=128), hTb[:, :, q_ * 256:(q_ + 1) * 256],
                      txb1[q_], R=[ctx["thTb"][tt - 1], ctx["thTb"][tt]], W=[txb1[q_]])
                k.collective("AllGather", RG4, xb1[q_], xg1[q_], txg1[q_], R=[txb1[q_]], W=[txg1[q_]])

        emit_moe_d(k, C, h_tok, th, md0, wconv=wc0, tile_cb=(cb_setup, cb_tile))
    k.release(wc0.trk)

    def hsrc(c):
        r, q = divmod(c, 4)
        return xg1[q][r * D:(r + 1) * D, :].rearrange("(ct p) t -> p ct t", p=128), [txg1[q]]

    def after_qb(qb):
        if qb % 8 == 7:
            j_ = qb // 8
            k.collective("AllGather", RG4, ob[j_], og[j_ * D:(j_ + 1) * D, :], tog[j_], R=[tob[j_]], W=[tog[j_]])

    with k.scope():
        emit_attn(k, None, WF_d, WT_d, posT_d, cw1_d, cw2_d, cb_d, cf_d, None, hsrc=hsrc, ob=ob, tob=tob,
                  chunk_order=[r * 4 + q for q in range(4) for r in range(4)], after_qb=after_qb, bg=bg1)
        wc1.flush()

    with k.scope():
        h_tok = k.sb("h_tok", [128, TT, D], F32)
        th = [Trk(f"h{i}") for i in range(TT)]
        hv = hsp.rearrange("(tt p) c -> p tt c", p=128)
        for tt in range(TT):
            k.dma("sp", h_tok[:, tt, :], hv[:, tt, :], th[tt], R=[thsp], W=[th[tt]])
        with k.scope():
            idx3 = k.sb("idx3", [128, CT], I32)
            tidx = Trk("idx3")
            k.dma("sp", idx3[:], idx3_d, tidx, W=[tidx])
            aT = k.sb("aT", [128, CT, NT], BF16)
            taT = Trk("aT")
            tg_ = [Trk(f"aTg{i}") for i in range(CT)]
            for ct in range(CT):
                k.gather(aT[:, ct, :], og[:, :], idx3[:, ct:ct + 1], tg_[ct], R=tog + [tidx], W=[taT] if ct == 0 else [])
            taT.w = [m for t_ in tg_ for m in [(t_.dsem, t_.dcnt)]]
            emit_proj_res_ln(k, aT, taT, wo_d, h_tok, th, md1["ln"][0:1, :], md1["ln"][1:2, :])
        emit_moe_d(k, C, h_tok, th, md1, wconv=wc1)
        to = Trk("out")
        store_tok(k, h_tok, th, out_d, to)
    k.barrier()
    k.close()
    return nc


def fused_inputs(inp, c):
    b, j = divmod(c, 4)
    d = p1_inputs(inp, c)
    m0 = moe_inputs(inp, 0)
    for kk in list(m0):
        del d[kk]
    d.update(moe_inputs(inp, 0, "0"))
    g = j
    cb, cf = attn_consts(g)
    d.update(cb=cb, cf=cf)
    d.update(attn_weights(inp, g))
    ct = np.arange(CT)[None, :]
    p = np.arange(128)[:, None]
    d["idx3"] = np.ascontiguousarray((j * 2048 + ct * 128 + p).astype(np.int32))
    d["w_o"] = np.ascontiguousarray(inp["nsa_w_o"][0])
    d.update(moe_inputs(inp, 1, "1"))
    return d


def kernel_unfused(**inputs):
    return _kernel3(**inputs)


def kernel(**inputs):
    inp = {k_: np.asarray(v_) for k_, v_ in inputs.items()}
    cores = list(range(NCORE))
    r = run_bass_kernel_spmd(_prog("fused", build_fused), [fused_inputs(inp, c) for c in cores], core_ids=cores)
    out = np.stack([r.results[c]["out"] for c in cores]).reshape(2, SEQ, D)
    return out.astype(np.float32)
```

```python
import numpy as np
from contextlib import ExitStack, contextmanager
import concourse.bass as bass
import concourse.mybir as mybir
from concourse.bass_utils import run_bass_kernel_spmd

F32, BF16, I32 = mybir.dt.float32, mybir.dt.bfloat16, mybir.dt.int32
AF = mybir.ActivationFunctionType
ALU = mybir.AluOpType
AX = mybir.AxisListType

D = 2048
NCORE = 8
SEQ = 4096
NT = 1024
TT = NT // 128
CT = D // 128
ALPHA = 4.0 ** 0.25
LN_EPS = 1e-5
NEXP = 32
DFF = 512
NEG = -30000.0


class Trk:
    __slots__ = ("w", "r", "dsem", "dcnt", "name")

    def __init__(self, name=""):
        self.w = []
        self.r = {}
        self.dsem = None
        self.dcnt = 0
        self.name = name


class Eng:
    def __init__(self, name, h):
        self.name, self.h = name, h
        self.sem = None
        self.cnt = 0
        self.known = {}
        self.pending = False


class K:
    SEM_ROLL = 30000

    def __init__(self, nc):
        self.nc = nc
        self.es = ExitStack()
        self.stacks = [self.es]
        self.eng = {}
        self.nsem = 0
        for name, h in (("pe", nc.tensor), ("act", nc.scalar), ("dve", nc.vector),
                        ("pool", nc.gpsimd), ("sp", nc.sync)):
            e = Eng(name, h)
            e.sem = self.sem("e_" + name)
            self.eng[name] = e
        self.dmas = []
        self.uid = 0
        self.sem_free = []
        self.scope_trks = [[]]

    def sem(self, name):
        self.nsem += 1
        return self.es.enter_context(self.nc.semaphore(f"{name}_{self.nsem}"))

    def sb(self, name, shape, dt):
        self.uid += 1
        return self.stacks[-1].enter_context(self.nc.sbuf_tensor(f"{name}_{self.uid}", list(shape), dt))

    def ps(self, name, shape, dt=F32):
        self.uid += 1
        return self.stacks[-1].enter_context(self.nc.psum_tensor(f"{name}_{self.uid}", list(shape), dt))

    @contextmanager
    def scope(self):
        st = ExitStack()
        self.stacks.append(st)
        self.scope_trks.append([])
        try:
            yield
        finally:
            self.barrier()
            self.stacks.pop()
            st.close()
            for S in self.scope_trks.pop():
                self.sem_free.append((S.dsem, S.dcnt))
                S.dsem = None
                S.w = []
                S.r = {}

    def _dsem(self, S):
        if S.dsem is None:
            if self.sem_free:
                S.dsem, S.dcnt = self.sem_free.pop()
            else:
                S.dsem = self.sem("d_" + S.name)
                S.dcnt = 0
            self.scope_trks[-1].append(S)

    def close(self):
        self.es.close()

    def _emit_waits(self, E, waits, skip_own=False):
        for sem, val in waits.values():
            if sem is E.sem:
                if skip_own or E.name == "pe":
                    continue
                assert val <= E.cnt, (E.name, val, E.cnt)
            if E.known.get(id(sem), 0) >= val:
                continue
            E.h.wait_ge(sem, val)
            E.known[id(sem)] = val

    @staticmethod
    def _need(waits, mark):
        sem, val = mark
        cur = waits.get(id(sem))
        if cur is None or cur[1] < val:
            waits[id(sem)] = (sem, val)

    def _collect(self, E, R, W):
        waits = {}
        for t in R:
            for m in t.w:
                self._need(waits, m)
        for t in W:
            for m in t.w:
                self._need(waits, m)
            for m in t.r.values():
                if m[0] is E.sem:
                    continue
                self._need(waits, m)
        return waits

    def _roll(self, E):
        if E.cnt >= self.SEM_ROLL and not E.pending:
            E.sem = self.sem("e_" + E.name)
            E.cnt = 0

    def op(self, en, fn, R=(), W=(), inc=True):
        E = self.eng[en]
        self._roll(E)
        self._emit_waits(E, self._collect(E, R, W))
        ins = fn()
        if inc:
            E.cnt += 1
            ins.then_inc(E.sem, 1)
            mark = (E.sem, E.cnt)
            E.pending = False
        else:
            mark = (E.sem, E.cnt + 1)
            E.pending = True
        for t in R:
            cur = t.r.get(id(mark[0]))
            if cur is None or cur[1] < mark[1]:
                t.r[id(mark[0])] = mark
        for t in W:
            t.w = [mark]
            t.r = {}
        return ins

    def dma(self, q, out, in_, S, R=(), W=(), join=False, background=False, **kw):
        E = self.eng[q]
        had = S.dsem is not None
        self._dsem(S)
        if background and not had:
            self.scope_trks[-1].remove(S)
        waits = {}
        for t in R:
            for m in t.w:
                self._need(waits, m)
        for t in W:
            if not join:
                for m in t.w:
                    self._need(waits, m)
            for m in t.r.values():
                self._need(waits, m)
        self._emit_waits(E, waits, skip_own=True)
        ins = E.h.dma_start(out=out, in_=in_, **kw)
        S.dcnt += 16
        ins.then_inc(S.dsem, 16)
        mark = (S.dsem, S.dcnt)
        for t in R:
            t.r[id(mark[0])] = mark
        for t in W:
            if join:
                t.w = [m for m in t.w if m[0] is not S.dsem] + [mark]
            else:
                t.w = [mark]
            t.r = {}
        if not background:
            self.dmas.append(mark)
        return ins

    def release(self, trks):
        for S in trks:
            if S.dsem is not None:
                self.sem_free.append((S.dsem, S.dcnt))
                S.dsem = None
                S.w = []
                S.r = {}

    def gather(self, out, in_, idx, S, R=(), W=(), bounds=None):
        E = self.eng["pool"]
        self._dsem(S)
        waits = {}
        for t in R:
            for m in t.w:
                self._need(waits, m)
        for t in W:
            for m in t.w:
                self._need(waits, m)
            for m in t.r.values():
                self._need(waits, m)
        self._emit_waits(E, waits, skip_own=True)
        kw = {}
        if bounds is not None:
            kw = dict(bounds_check=bounds, oob_is_err=False)
        ins = E.h.indirect_dma_start(out=out, out_offset=None, in_=in_,
                                     in_offset=bass.IndirectOffsetOnAxis(ap=idx, axis=0), **kw)
        S.dcnt += 16
        ins.then_inc(S.dsem, 16)
        mark = (S.dsem, S.dcnt)
        for t in R:
            t.r[id(mark[0])] = mark
        for t in W:
            t.w = [mark]
            t.r = {}
        self.dmas.append(mark)
        return ins

    def collective(self, kind, groups, in_ap, out_ap, S, R=(), W=()):
        E = self.eng["pool"]
        self._dsem(S)
        waits = {}
        for t in R:
            for m in t.w:
                self._need(waits, m)
        for t in W:
            for m in t.w:
                self._need(waits, m)
            for m in t.r.values():
                self._need(waits, m)
        self._emit_waits(E, waits, skip_own=True)
        ins = E.h.collective_compute(kind, ALU.bypass, replica_groups=groups, ins=[in_ap.opt()], outs=[out_ap.opt()])
        S.dcnt += 1
        ins.then_inc(S.dsem, 1)
        mark = (S.dsem, S.dcnt)
        for t in R:
            t.r[id(mark[0])] = mark
        for t in W:
            t.w = [mark]
            t.r = {}
        self.dmas.append(mark)
        return ins

    def barrier(self):
        marks = []
        for F in self.eng.values():
            assert not F.pending, F.name
            if F.cnt:
                marks.append((F.sem, F.cnt))
        seen = {}
        for m in self.dmas:
            self._need(seen, m)
        marks += list(seen.values())
        self.dmas = []
        for E in self.eng.values():
            for sem, val in marks:
                if sem is E.sem:
                    continue
                if E.known.get(id(sem), 0) >= val:
                    continue
                E.h.wait_ge(sem, val)
                E.known[id(sem)] = val

    def mm(self, out, lhsT, rhs, start, stop, R=(), W=(), inc=None, **kw):
        if inc is None:
            inc = stop
        return self.op("pe", lambda: self.nc.tensor.matmul(out, lhsT, rhs, start=start, stop=stop, **kw),
                       R=R, W=W, inc=inc)

    def tr(self, out, in_, ident, R=(), W=(), inc=True):
        return self.op("pe", lambda: self.nc.tensor.transpose(out, in_, ident), R=R, W=W, inc=inc)

    def act(self, out, in_, func, R=(), W=(), **kw):
        return self.op("act", lambda: self.nc.scalar.activation(out=out, in_=in_, func=func, **kw), R=R, W=W)

    def v(self, en, name, *a, R=(), W=(), **kw):
        h = self.eng[en].h
        return self.op(en, lambda: getattr(h, name)(*a, **kw), R=R, W=W)


NC32 = 544


def make_consts():
    c = np.zeros((128, NC32), np.float32)
    c[:, 0:128] = np.eye(128)
    c[:, 128:256] = np.arange(128)[None, :]
    c[:, 256:384] = (np.arange(128)[:, None] < np.arange(128)[None, :])
    c[:, 384:512] = 1.0
    c[:, 512:544] = (np.arange(32) * 128)[None, :]
    return c


def load_consts(k, cst_d):
    c32 = k.sb("c32", [128, NC32], F32)
    tc = Trk("c32")
    k.dma("sp", c32[:], cst_d, tc, W=[tc])
    cbf = k.sb("cbf", [128, 512], BF16)
    tcb = Trk("cbf")
    k.v("dve", "tensor_copy", cbf[:], c32[:, 0:512], R=[tc], W=[tcb])
    return dict(ident=c32[:, 0:128], iota=c32[:, 128:256], ebase=c32[:, 512:544], ones=c32[:, 384:512],
                ident_bf=cbf[:, 0:128], tri_bf=cbf[:, 256:384], ones_bf=cbf[:, 384:512], t32=tc, tbf=tcb)


def pb(ap_row):
    return ap_row.partition_broadcast(128)


def emit_ln_tok(k, x_ap, tx, G, B, tgb, st, tst):
    nc = k.nc
    for i in range(4):
        k.v("dve", "bn_stats", st[:, i * 6:(i + 1) * 6], x_ap[:, i * 512:(i + 1) * 512], R=[tx, tst], W=[tst])
    k.v("dve", "bn_aggr", st[:, 24:26], st[:, 0:24], R=[tst], W=[tst])
    k.act(st[:, 26:27], st[:, 25:26], AF.Sqrt, bias=LN_EPS, scale=1.0, R=[tst], W=[tst])
    k.v("dve", "reciprocal", st[:, 27:28], st[:, 26:27], R=[tst], W=[tst])
    k.v("dve", "scalar_tensor_tensor", st[:, 28:29], st[:, 24:25], -1.0, st[:, 27:28], ALU.mult, ALU.mult,
        R=[tst], W=[tst])
    k.v("dve", "tensor_scalar", x_ap, x_ap, st[:, 27:28], st[:, 28:29], ALU.mult, ALU.add, R=[tx, tst], W=[tx])
    k.v("dve", "tensor_tensor", x_ap, x_ap, G, ALU.mult, R=[tx, tgb], W=[tx])
    k.v("pool", "tensor_tensor", x_ap, x_ap, B, ALU.add, R=[tx, tgb], W=[tx])


def emit_moe(k, C, h_tok, th, wg_d, bg_d, we_d, be_d, wgu_d, wdn_d, ybuf, lng_d, lnb_d, nexp=NEXP, wconv=None,
             tile_cb=None):
    nc = k.nc
    rankm = k.sb("rankm", [128, TT, 32], F32)
    t_rank = [Trk(f"rank{i}") for i in range(TT)]
    dest = k.sb("dest", [128, TT, 2], I32)
    wts = k.sb("wts", [128, TT, 2], F32)
    t_dw = [Trk(f"dw{i}") for i in range(TT)]
    h_bf = k.sb("h_bf", [128, TT, D], BF16)
    t_hbf = [Trk(f"hbf{i}") for i in range(TT)]

    with k.scope():
        Wr = k.sb("Wr", [128, CT, 36], F32)
        tWr = Trk("Wr")
        k.dma("sp", Wr[:, :, 0:4], wg_d.rearrange("(ct p) n -> p ct n", p=128), tWr, W=[tWr])
        k.dma("sp", Wr[:, :, 4:36], we_d.rearrange("(ct p) n -> p ct n", p=128), tWr, W=[tWr], join=True)
        rb = k.sb("rb", [128, 36], F32)
        trb = Trk("rb")
        k.dma("sp", rb[:, 0:4], pb(bg_d), trb, W=[trb])
        k.dma("sp", rb[:, 4:36], pb(be_d), trb, W=[trb], join=True)
        hTt = [k.sb("hTt", [128, CT, 128], F32) for _ in range(2)]
        thT = [[Trk(f"hTt{a}{b}") for b in range(4)] for a in range(2)]
        psA = [k.ps("psA", [128, 512]) for _ in range(2)]
        tpsA = [Trk("psA0"), Trk("psA1")]
        psL = k.ps("psL", [128, 512])
        tpsL = Trk("psL")
        psR = k.ps("psR", [128, 512])
        tpsR = Trk("psR")
        L = k.sb("L", [128, TT, 36], F32)
        sm = k.sb("sm", [128, TT, 16], F32)
        ohg = k.sb("ohg", [128, TT, 4], F32)
        r8 = k.sb("r8", [128, TT, 5, 8], F32)
        r32 = k.sb("r32", [128, TT, 5, 32], F32)
        d12 = k.sb("d12", [128, TT, 2], F32)
        M_bf = k.sb("M_bf", [128, TT, 32], BF16)
        tM = [Trk(f"M{i}") for i in range(TT)]
        ts = [Trk(f"rs{i}") for i in range(TT)]
        nq = 0
        for tt in range(TT):
            hb = tt % 2
            k.act(h_bf[:, tt, :], h_tok[:, tt, :], AF.Copy, R=[th[tt]], W=[t_hbf[tt]])
            for q in range(4):
                pa = nq % 2
                nq += 1
                for i in range(4):
                    ct = q * 4 + i
                    k.tr(psA[pa][:, i * 128:(i + 1) * 128], h_tok[:, tt, ct * 128:(ct + 1) * 128], C["ident"],
                         R=[th[tt], C["t32"]], W=[tpsA[pa]], inc=(i == 3))
                dst = hTt[hb][:, q * 4:(q + 1) * 4, :]
                if q % 2 == 0:
                    k.act(dst, psA[pa][:, :].rearrange("p (a b) -> p a b", a=4), AF.Copy, R=[tpsA[pa]], W=[thT[hb][q]])
                else:
                    k.v("dve", "tensor_copy", dst, psA[pa][:, :].rearrange("p (a b) -> p a b", a=4),
                        R=[tpsA[pa]], W=[thT[hb][q]])
            for ct in range(CT):
                k.mm(psL[:, 0:36], hTt[hb][:, ct, :], Wr[:, ct, :], ct == 0, ct == CT - 1,
                     R=[thT[hb][ct // 4], tWr], W=[tpsL])
            S = ts[tt]
            sc = lambda j: sm[:, tt, j:j + 1]
            Lt = L[:, tt, :]
            les, eq1, le2, eq2, e12 = (r8[:, tt, j, :] for j in range(5))
            Mf, E1, E2, rk, tmp = (r32[:, tt, j, :] for j in range(5))
            dv = lambda name, *a, R=(), W=(): k.v("dve", name, *a, R=[S] + list(R), W=[S] + list(W))
            dv("tensor_tensor", Lt, psL[:, 0:36], rb[:], ALU.add, R=[tpsL, trb])
            dv("reduce_max", sc(0), Lt[:, 0:4], AX.X)
            dv("tensor_scalar", ohg[:, tt, :], Lt[:, 0:4], sc(0), None, ALU.is_equal)
            dv("tensor_scalar", sc(1), sc(0), -1.0, None, ALU.mult)
            k.act(le2[:, 0:4], Lt[:, 0:4], AF.Exp,
                  bias=sc(1), scale=1.0, accum_out=sc(2), R=[S], W=[S])
            dv("reciprocal", sc(3), sc(2))
            dv("tensor_scalar", les, Lt[:, 4:12], ohg[:, tt, 0:1], None, ALU.mult)
            for g in range(1, 4):
                dv("scalar_tensor_tensor", les, Lt[:, 4 + 8 * g:12 + 8 * g], ohg[:, tt, g:g + 1], les,
                   ALU.mult, ALU.add)
            dv("reduce_max", sc(4), les, AX.X)
            dv("tensor_scalar", eq1, les, sc(4), None, ALU.is_equal)
            dv("scalar_tensor_tensor", le2, eq1, -1e30, les, ALU.mult, ALU.add)
            dv("reduce_max", sc(5), le2, AX.X)
            dv("tensor_scalar", eq2, le2, sc(5), None, ALU.is_equal)
            dv("tensor_tensor", sc(6), sc(5), sc(4), ALU.subtract)
            k.act(sc(7), sc(6), AF.Exp, R=[S], W=[S])
            dv("tensor_scalar", sc(8), sc(7), 1.0, None, ALU.add)
            dv("reciprocal", sc(9), sc(8))
            dv("tensor_tensor", wts[:, tt, 0:1], sc(9), sc(3), ALU.mult, W=[t_dw[tt]])
            dv("tensor_tensor", wts[:, tt, 1:2], sc(3), wts[:, tt, 0:1], ALU.subtract, W=[t_dw[tt]])
            dv("tensor_tensor", e12, eq1, eq2, ALU.add)
            for g in range(4):
                gs = slice(8 * g, 8 * g + 8)
                dv("tensor_scalar", Mf[:, gs], e12, ohg[:, tt, g:g + 1], None, ALU.mult)
                dv("tensor_scalar", E1[:, gs], eq1, ohg[:, tt, g:g + 1], None, ALU.mult)
                dv("tensor_scalar", E2[:, gs], eq2, ohg[:, tt, g:g + 1], None, ALU.mult)
            dv("tensor_copy", M_bf[:, tt, :], Mf, W=[tM[tt]])
            for t2 in range(tt + 1):
                lhsT = C["ones_bf"] if t2 < tt else C["tri_bf"]
                k.mm(psR[:, 0:32], lhsT, M_bf[:, t2, :], t2 == 0, t2 == tt, R=[tM[t2], C["tbf"]], W=[tpsR])
            dv("scalar_tensor_tensor", rankm[:, tt, :], psR[:, 0:32], 1.0, Mf, ALU.add, ALU.mult,
               R=[tpsR], W=[t_rank[tt]])
            dv("tensor_scalar", rankm[:, tt, :], rankm[:, tt, :], -1.0, None, ALU.add, W=[t_rank[tt]])
            dv("tensor_tensor", rk, psR[:, 0:32], C["ebase"], ALU.add, R=[tpsR, C["t32"]])
            for j, Ej in enumerate((E1, E2)):
                dv("tensor_tensor", tmp, rk, Ej, ALU.mult)
                dv("reduce_sum", d12[:, tt, j:j + 1], tmp, AX.X)
            dv("tensor_copy", dest[:, tt, :], d12[:, tt, :], W=[t_dw[tt]])

    with k.scope():
        NS = 4
        wsl = [k.sb("wsl", [128, 8192], BF16) for _ in range(NS)]
        twsl = [Trk(f"wsl{i}") for i in range(NS)]
        selq = k.sb("selq", [128, TT, 512], BF16)
        tsel = [Trk(f"selq{i}") for i in range(TT)]
        xq = k.sb("xq", [128, CT, 512], BF16)
        txq = [Trk(f"xq{i}") for i in range(CT)]
        sg = k.sb("sg", [128, 512], F32)
        tsg = Trk("sg")
        hT = k.sb("hT", [128, 512], BF16)
        thh = Trk("hT")
        ye = k.sb("ye", [128, D], BF16)
        tye = [Trk(f"ye{i}") for i in range(4)]
        psG = [k.ps("psG", [128, 512]) for _ in range(2)]
        tpsG = [Trk("psG0"), Trk("psG1")]
        psH1 = k.ps("psH1", [128, 512])
        tH1 = Trk("psH1")
        psH2 = k.ps("psH2", [128, 512])
        tH2 = Trk("psH2")
        psY = [k.ps("psY", [128, 512]) for _ in range(4)]
        tpsY = [Trk(f"psY{i}") for i in range(4)]
        tyb = Trk("ybuf")

        chunks = [(e, c) for e in range(nexp) for c in "gud"]

        def issue(ci):
            if ci >= len(chunks):
                return
            e, c = chunks[ci]
            s = ci % NS
            if wconv is not None and e < wconv.nconv:
                if c == "d":
                    k.dma("sp", wsl[s][:, :].rearrange("p (a n) -> p a n", a=4),
                          wconv.wdn_b[e].rearrange("(a p) n -> p a n", p=128), twsl[s], R=[wconv.trk[e]], W=[twsl[s]])
                else:
                    c0 = 0 if c == "g" else 512
                    k.dma("sp", wsl[s][:, :].rearrange("p (a n) -> p a n", a=16),
                          wconv.wgu_b[e].rearrange("(a p) n -> p a n", p=128)[:, :, c0:c0 + 512], twsl[s],
                          R=[wconv.trk[e]], W=[twsl[s]])
                return
            if c == "d":
                k.dma("pool", wsl[s][:, :].rearrange("p (a n) -> p a n", a=4),
                      wdn_d[e].rearrange("(a p) n -> p a n", p=128), twsl[s], W=[twsl[s]])
            else:
                c0 = 0 if c == "g" else 512
                k.dma("pool", wsl[s][:, :].rearrange("p (a n) -> p a n", a=16),
                      wgu_d[e].rearrange("(a p) n -> p a n", p=128)[:, :, c0:c0 + 512], twsl[s], W=[twsl[s]])

        for ci in range(NS - 1):
            issue(ci)
        ci = 0
        nev = 0
        for q0 in range(0, nexp, 4):
            for tt in range(TT):
                for j in range(4):
                    e = q0 + j
                    k.v("dve", "tensor_scalar", selq[:, tt, j * 128:(j + 1) * 128], C["iota"],
                        rankm[:, tt, e:e + 1], None, ALU.is_equal, R=[t_rank[tt], C["t32"]], W=[tsel[tt]])
            for ct in range(CT):
                pg = ct % 2
                for tt in range(TT):
                    k.mm(psG[pg][:, :], h_bf[:, tt, ct * 128:(ct + 1) * 128], selq[:, tt, :], tt == 0, tt == TT - 1,
                         R=[t_hbf[tt], tsel[tt]], W=[tpsG[pg]])
                if ct % 2 == 0:
                    k.act(xq[:, ct, :], psG[pg][:, :], AF.Copy, R=[tpsG[pg]], W=[txq[ct]])
                else:
                    k.v("dve", "tensor_copy", xq[:, ct, :], psG[pg][:, :], R=[tpsG[pg]], W=[txq[ct]])
            for j in range(4):
                e = q0 + j
                for half, (psH, tH) in enumerate(((psH1, tH1), (psH2, tH2))):
                    s = ci % NS
                    issue(ci + NS - 1)
                    wv = wsl[s][:, :].rearrange("p (a n) -> p a n", a=16)
                    for ft in range(4):
                        for ct in range(CT):
                            k.mm(psH[:, ft * 128:(ft + 1) * 128], wv[:, ct, ft * 128:(ft + 1) * 128],
                                 xq[:, ct, j * 128:(j + 1) * 128], ct == 0, ct == CT - 1,
                                 R=[twsl[s], txq[ct]], W=[tH])
                    ci += 1
                k.act(sg[:], psH1[:, :], AF.Silu, R=[tH1], W=[tsg])
                k.v("dve", "tensor_tensor", hT[:], sg[:], psH2[:, :], ALU.mult, R=[tsg, tH2], W=[thh])
                s = ci % NS
                issue(ci + NS - 1)
                wv = wsl[s][:, :].rearrange("p (a n) -> p a n", a=4)
                for cc in range(4):
                    for ft in range(4):
                        k.mm(psY[cc][:, :], hT[:, ft * 128:(ft + 1) * 128], wv[:, ft, cc * 512:(cc + 1) * 512],
                             ft == 0, ft == 3, R=[thh, twsl[s]], W=[tpsY[cc]])
                    if nev % 2 == 0:
                        k.act(ye[:, cc * 512:(cc + 1) * 512], psY[cc][:, :], AF.Copy, R=[tpsY[cc]], W=[tye[cc]])
                    else:
                        k.v("dve", "tensor_copy", ye[:, cc * 512:(cc + 1) * 512], psY[cc][:, :],
                            R=[tpsY[cc]], W=[tye[cc]])
                    nev += 1
                ci += 1
                k.dma("sp", ybuf[e * 128:(e + 1) * 128, :], ye[:], tyb, R=tye, W=[tyb], join=True)

    with k.scope():
        G = k.sb("lnG", [128, D], F32)
        B = k.sb("lnB", [128, D], F32)
        tgb = Trk("lngb")
        k.dma("sp", G[:], pb(lng_d), tgb, W=[tgb])
        k.dma("sp", B[:], pb(lnb_d), tgb, W=[tgb], join=True)
        Y = [[k.sb("Yg", [128, D], BF16) for _ in range(2)] for _ in range(2)]
        tY = [[Trk(f"Y{a}{b}") for b in range(2)] for a in range(2)]
        st = k.sb("lnst", [128, TT, 40], F32)
        tst = [Trk(f"lnst{i}") for i in range(TT)]
        def gat(tt):
            p = tt % 2
            for j in range(2):
                k.gather(Y[p][j][:], ybuf[:, :], dest[:, tt, j:j + 1], tY[p][j], R=[tyb, t_dw[tt]], W=[tY[p][j]])

        cb_ctx = tile_cb[0]() if tile_cb is not None else None
        gat(0)
        for tt in range(TT):
            p = tt % 2
            if tt + 1 < TT:
                gat(tt + 1)
            x_ap = h_tok[:, tt, :]
            k.op("act", lambda: nc.scalar.mul(x_ap, x_ap, ALPHA), R=[th[tt]], W=[th[tt]])
            k.v("dve", "scalar_tensor_tensor", x_ap, Y[p][0][:], wts[:, tt, 0:1], x_ap, ALU.mult, ALU.add,
                R=[tY[p][0], t_dw[tt], th[tt]], W=[th[tt]])
            k.v("dve", "scalar_tensor_tensor", x_ap, Y[p][1][:], wts[:, tt, 1:2], x_ap, ALU.mult, ALU.add,
                R=[tY[p][1], t_dw[tt], th[tt]], W=[th[tt]])
            emit_ln_tok(k, x_ap, th[tt], G[:], B[:], tgb, st[:, tt, :], tst[tt])
            if tile_cb is not None:
                tile_cb[1](cb_ctx, tt)


def emit_proj_res_ln(k, aT, taT, W_d, h_tok, th, lng_d, lnb_d, tts=None):
    nc = k.nc
    if tts is None:
        tts = list(range(TT))
    with k.scope():
        Wc = [k.sb("Wc", [128, CT, 512], BF16) for _ in range(2)]
        tWc = [Trk("Wc0"), Trk("Wc1")]
        G = k.sb("lnG", [128, D], F32)
        B = k.sb("lnB", [128, D], F32)
        tgb = Trk("lngb")
        k.dma("sp", G[:], pb(lng_d), tgb, W=[tgb])
        k.dma("sp", B[:], pb(lnb_d), tgb, W=[tgb], join=True)
        st = k.sb("lnst", [128, TT, 40], F32)
        tst = [Trk(f"lnst{i}") for i in range(TT)]
        ps = [k.ps("psP", [128, 512]) for _ in range(4)]
        tps = [Trk(f"psP{i}") for i in range(4)]
        Wv = W_d.rearrange("(ct p) n -> p ct n", p=128)

        def issue(cc):
            if cc < 4:
                k.dma("pool", Wc[cc % 2][:], Wv[:, :, cc * 512:(cc + 1) * 512], tWc[cc % 2], W=[tWc[cc % 2]])

        issue(0)
        n = 0
        for cc in range(4):
            issue(cc + 1)
            for i, tt in enumerate(tts):
                p = n % 4
                n += 1
                for ct in range(CT):
                    k.mm(ps[p][:, :], aT[:, ct, i * 128:(i + 1) * 128], Wc[cc % 2][:, ct, :], ct == 0, ct == CT - 1,
                         R=[taT, tWc[cc % 2]], W=[tps[p]])
                dst = h_tok[:, tt, cc * 512:(cc + 1) * 512]
                k.v("dve", "scalar_tensor_tensor", dst, dst, ALPHA, ps[p][:, :], ALU.mult, ALU.add,
                    R=[tps[p], th[tt]], W=[th[tt]])
        for tt in tts:
            emit_ln_tok(k, h_tok[:, tt, :], th[tt], G[:], B[:], tgb, st[:, tt, :], tst[tt])


def load_tok(k, h_tok, th, src_d):
    hv = src_d.rearrange("(tt p) c -> p tt c", p=128)
    for tt in range(TT):
        k.dma("sp", h_tok[:, tt, :], hv[:, tt, :], th[tt], W=[th[tt]])


def store_tok(k, h_tok, th, dst_d, to):
    ov = dst_d.rearrange("(tt p) c -> p tt c", p=128)
    for tt in range(TT):
        k.dma("sp", ov[:, tt, :], h_tok[:, tt, :], to, R=[th[tt]], W=[to], join=True)


def moe_dram(nc, sfx=""):
    d = {}
    d["wg"] = nc.dram_tensor("moe_wg" + sfx, [D, 4], F32, kind="ExternalInput").ap()
    d["bg"] = nc.dram_tensor("moe_bg" + sfx, [1, 4], F32, kind="ExternalInput").ap()
    d["we"] = nc.dram_tensor("moe_we" + sfx, [D, 32], F32, kind="ExternalInput").ap()
    d["be"] = nc.dram_tensor("moe_be" + sfx, [1, 32], F32, kind="ExternalInput").ap()
    d["wgu"] = nc.dram_tensor("moe_wgu" + sfx, [NEXP, D, 2 * DFF], F32, kind="ExternalInput").ap()
    d["wdn"] = nc.dram_tensor("moe_wdn" + sfx, [NEXP, DFF, D], F32, kind="ExternalInput").ap()
    d["ln"] = nc.dram_tensor("ln_gb" + sfx, [4, D], F32, kind="ExternalInput").ap()
    d["ybuf"] = nc.dram_tensor("ybuf" + sfx, [NEXP * 128, D], BF16).ap()
    return d


def moe_inputs(inp, l, sfx=""):
    return {
        "moe_wg" + sfx: np.ascontiguousarray(inp["moe_wg"][l]),
        "moe_bg" + sfx: np.ascontiguousarray(inp["moe_bg"][l][None]),
        "moe_we" + sfx: np.ascontiguousarray(inp["moe_we"][l]),
        "moe_be" + sfx: np.ascontiguousarray(inp["moe_be"][l][None]),
        "moe_wgu" + sfx: np.ascontiguousarray(inp["moe_w_gu"][l]),
        "moe_wdn" + sfx: np.ascontiguousarray(inp["moe_w_down"][l]),
        "ln_gb" + sfx: np.ascontiguousarray(np.stack([inp["ln_g"][l, 0], inp["ln_b"][l, 0], inp["ln_g"][l, 1], inp["ln_b"][l, 1]])),
    }


class WConv:
    def __init__(self, k, d, sfx, nconv):
        nc = k.nc
        self.k, self.d, self.nconv = k, d, nconv
        self.wgu_b = nc.dram_tensor("wgu_b" + sfx, [max(nconv, 1), D, 2 * DFF], BF16).ap()
        self.wdn_b = nc.dram_tensor("wdn_b" + sfx, [max(nconv, 1), DFF, D], BF16).ap()
        self.trk = [Trk(f"wcv{sfx}_{e}") for e in range(nconv)]
        self.jobs = [(e, c) for e in range(nconv) for c in ("gu", "dn")]
        self.nxt = 0

    def pump(self, n=1):
        k = self.k
        for _ in range(n):
            if self.nxt >= len(self.jobs):
                return
            e, c = self.jobs[self.nxt]
            self.nxt += 1
            if c == "gu":
                k.dma("pool", self.wgu_b[e].rearrange("(a p) n -> p a n", p=128),
                      self.d["wgu"][e].rearrange("(a p) n -> p a n", p=128), self.trk[e], W=[self.trk[e]], join=True,
                      background=True)
            else:
                k.dma("pool", self.wdn_b[e].rearrange("(a p) n -> p a n", p=128),
                      self.d["wdn"][e].rearrange("(a p) n -> p a n", p=128), self.trk[e], W=[self.trk[e]], join=True,
                      background=True)

    def flush(self):
        self.pump(len(self.jobs))


def emit_moe_d(k, C, h_tok, th, d, wconv=None, tile_cb=None):
    emit_moe(k, C, h_tok, th, d["wg"], d["bg"], d["we"], d["be"], d["wgu"], d["wdn"], d["ybuf"],
             d["ln"][2:3, :], d["ln"][3:4, :], wconv=wconv, tile_cb=tile_cb)


def emit_moe_d_old(k, C, h_tok, th, d):
    emit_moe(k, C, h_tok, th, d["wg"], d["bg"], d["we"], d["be"], d["wgu"], d["wdn"], d["ybuf"],
             d["ln"][2:3, :], d["ln"][3:4, :])


def build_p3():
    nc = bass.Bass("TRN2", target_bir_lowering=False)
    k = K(nc)
    cst_d = nc.dram_tensor("cst", [128, NC32], F32, kind="ExternalInput").ap()
    h_d = nc.dram_tensor("h_in", [NT, D], F32, kind="ExternalInput").ap()
    oT_d = nc.dram_tensor("oT", [D, NT], F32, kind="ExternalInput").ap()
    wo_d = nc.dram_tensor("w_o", [D, D], F32, kind="ExternalInput").ap()
    md = moe_dram(nc)
    out_d = nc.dram_tensor("out", [NT, D], F32, kind="ExternalOutput").ap()
    C = load_consts(k, cst_d)
    h_tok = k.sb("h_tok", [128, TT, D], F32)
    th = [Trk(f"h{i}") for i in range(TT)]
    load_tok(k, h_tok, th, h_d)
    with k.scope():
        aT = k.sb("aT", [128, CT, NT], BF16)
        taT = Trk("aT")
        k.dma("pool", aT[:], oT_d.rearrange("(ct p) t -> p ct t", p=128), taT, W=[taT])
        emit_proj_res_ln(k, aT, taT, wo_d, h_tok, th, md["ln"][0:1, :], md["ln"][1:2, :])
    emit_moe_d(k, C, h_tok, th, md)
    to = Trk("out")
    store_tok(k, h_tok, th, out_d, to)
    k.barrier()
    k.close()
    return nc


HALO = 30
NXC = NT + HALO + 2
NCPAR = 16 * 31 + 16 * 3 + 32


def conv_params(inp):
    wdw = inp["conv_w_dw"][0]
    a = wdw.T.reshape(CT, 128, 31).transpose(1, 0, 2).reshape(128, CT * 31)
    cols = [a]
    for v in (inp["conv_b_dw"][0], inp["conv_ln_g"][0], inp["conv_ln_b"][0]):
        cols.append(v.reshape(CT, 128).T)
    cols.append(inp["conv_b_pw1"][0].reshape(32, 128).T)
    return np.ascontiguousarray(np.concatenate(cols, axis=1).astype(np.float32))


def emit_conv(k, C, xT_d, hm_d, cpar_d, w1_d, w2_d, h_tok, th, lng_d, lnb_d, dbg=None, bg=None):
    nc = k.nc
    with k.scope():
        cp = k.sb("cpar", [128, NCPAR], F32)
        tcp = Trk("cpar")
        k.dma("sp", cp[:], cpar_d, tcp, W=[tcp])
        wdw = cp[:, 0:CT * 31].rearrange("p (j t) -> p j t", j=CT)
        bdw = cp[:, 496:512]
        cg = cp[:, 512:528]
        cb = cp[:, 528:544]
        b1 = cp[:, 544:576]
        hm = k.sb("hm", [128, 1], F32)
        thm = Trk("hm")
        k.dma("sp", hm[:], pb(hm_d), thm, W=[thm])
        gluT = k.sb("gluT", [128, CT, NXC], BF16)
        tglu = [Trk(f"glu{j}") for j in range(CT)]
        with k.scope():
            xT = k.sb("xT", [128, CT, NXC], BF16)
            txT = Trk("xT")
            k.dma("pool", xT[:, :, 0:NT + HALO], xT_d.rearrange("(ct p) t -> p ct t", p=128), txT, W=[txT])
            NS = 3
            wsl = [k.sb("w1s", [128, CT, 512], BF16) for _ in range(NS)]
            twsl = [Trk(f"w1s{i}") for i in range(NS)]
            sig = [k.sb("sig", [128, 512], F32) for _ in range(2)]
            tsig = [Trk("sig0"), Trk("sig1")]
            psA = [k.ps("psA", [128, 512]) for _ in range(2)]
            tpA = [Trk("pA0"), Trk("pA1")]
            psB = [k.ps("psB", [128, 512]) for _ in range(2)]
            tpB = [Trk("pB0"), Trk("pB1")]
            W1v = w1_d.rearrange("(ct p) n -> p ct n", p=128)
            loads = []
            for jg in range(4):
                loads.append(jg * 512)
                loads.append(D + jg * 512)

            nxt = [0]

            def pump(done):
                while nxt[0] < len(loads) and (nxt[0] < NS or (nxt[0] - NS) // 2 < done):
                    i = nxt[0]
                    s_ = i % NS
                    k.dma("pool", wsl[s_][:], W1v[:, :, loads[i]:loads[i] + 512], twsl[s_], W=[twsl[s_]])
                    nxt[0] += 1

            n = 0
            for jg in range(4):
                pump(jg)
                assert nxt[0] >= 2 * jg + 2
                sa, sg_ = (2 * jg) % NS, (2 * jg + 1) % NS
                for jj in range(4):
                    j = jg * 4 + jj
                    for c0, c1 in ((0, 512), (512, 1024), (1024, NT + HALO)):
                        w = c1 - c0
                        p = n % 2
                        n += 1
                        for ct in range(CT):
                            k.mm(psA[p][:, 0:w], wsl[sa][:, ct, jj * 128:(jj + 1) * 128], xT[:, ct, c0:c1],
                                 ct == 0, ct == CT - 1, R=[twsl[sa], txT], W=[tpA[p]])
                        for ct in range(CT):
                            k.mm(psB[p][:, 0:w], wsl[sg_][:, ct, jj * 128:(jj + 1) * 128], xT[:, ct, c0:c1],
                                 ct == 0, ct == CT - 1, R=[twsl[sg_], txT], W=[tpB[p]])
                        k.act(sig[p][:, 0:w], psB[p][:, 0:w], AF.Sigmoid, bias=b1[:, 16 + j:17 + j], scale=1.0,
                              R=[tpB[p], tcp], W=[tsig[p]])
                        k.v("dve", "scalar_tensor_tensor", gluT[:, j, c0:c1], psA[p][:, 0:w], b1[:, j:j + 1],
                            sig[p][:, 0:w], ALU.add, ALU.mult, R=[tpA[p], tsig[p], tcp], W=[tglu[j]])
                    if bg is not None:
                        bg("pw1")
            for j in range(CT):
                k.v("dve", "tensor_scalar", gluT[:, j, 0:HALO], gluT[:, j, 0:HALO], hm[:, 0:1], None, ALU.mult,
                    R=[thm], W=[tglu[j]])
        if dbg is not None:
            tdb = Trk("dbg")
            k.dma("sp", dbg["glu"].rearrange("(j p) t -> p j t", p=128), gluT[:], tdb, R=tglu, W=[tdb], join=True)
        for tc in range(2):
            with k.scope():
                sT = k.sb("sT", [128, CT, 512], BF16)
                tsT = Trk("sT")
                with k.scope():
                    v = k.sb("cv", [128, CT, 512], F32)
                    tv = [Trk(f"cv{j}") for j in range(CT)]
                    dg = [k.sb("dg", [128, 31, 128], BF16) for _ in range(2)]
                    tdg = [Trk("dg0"), Trk("dg1")]
                    vsq = [k.sb("vsq", [128, 512], F32) for _ in range(2)]
                    tvsq = [Trk("vsq0"), Trk("vsq1")]
                    psC = [k.ps("psC", [128, 512]) for _ in range(2)]
                    tpC = [Trk("pC0"), Trk("pC1")]
                    psS = k.ps("psS", [128, 512])
                    tpS = Trk("pS")
                    psQ = k.ps("psQ", [128, 512])
                    tpQ = Trk("pQ")
                    for j in range(CT):
                        p = j % 2
                        for t in range(31):
                            k.v("dve", "tensor_scalar", dg[p][:, t, :], C["ident_bf"],
                                wdw[:, j, t:t + 1], None, ALU.mult, R=[C["tbf"], tcp], W=[tdg[p]])
                        for t in range(31):
                            k.mm(psC[p][:, :], dg[p][:, t, :], gluT[:, j, tc * 512 + t:tc * 512 + t + 512], t == 0, t == 30,
                                 R=[tdg[p], tglu[j]], W=[tpC[p]])
                        k.act(v[:, j, :], psC[p][:, :], AF.Identity, bias=bdw[:, j:j + 1], scale=1.0,
                              R=[tpC[p], tcp], W=[tv[j]])
                        k.act(vsq[p][:], v[:, j, :], AF.Square, R=[tv[j]], W=[tvsq[p]])
                        k.mm(psS[:, :], C["ones"], v[:, j, :], j == 0, j == CT - 1, R=[C["t32"], tv[j]], W=[tpS])
                        k.mm(psQ[:, :], C["ones"], vsq[p][:], j == 0, j == CT - 1, R=[C["t32"], tvsq[p]], W=[tpQ])
                        if bg is not None:
                            bg("conv")
                    if dbg is not None:
                        k.dma("sp", dbg["v"][tc].rearrange("(j p) t -> p j t", p=128), v[:], tdb, R=tv, W=[tdb], join=True)
                    mu = k.sb("mu", [128, 512], F32)
                    rs = k.sb("rs", [128, 512], F32)
                    m2 = k.sb("m2", [128, 512], F32)
                    tm = Trk("murs")
                    k.v("dve", "tensor_scalar", mu[:], psS[:, :], 1.0 / D, None, ALU.mult, R=[tpS], W=[tm])
                    k.v("dve", "tensor_tensor", m2[:], mu[:], mu[:], ALU.mult, R=[tm], W=[tm])
                    k.v("dve", "scalar_tensor_tensor", m2[:], psQ[:, :], 1.0 / D, m2[:], ALU.mult, ALU.subtract,
                        R=[tpQ, tm], W=[tm])
                    k.act(m2[:], m2[:], AF.Sqrt, bias=LN_EPS, scale=1.0, R=[tm], W=[tm])
                    k.v("dve", "reciprocal", rs[:], m2[:], R=[tm], W=[tm])
                    for j in range(CT):
                        k.v("dve", "tensor_tensor", v[:, j, :], v[:, j, :], mu[:], ALU.subtract, R=[tm, tv[j]], W=[tv[j]])
                        k.v("dve", "tensor_tensor", v[:, j, :], v[:, j, :], rs[:], ALU.mult, R=[tm, tv[j]], W=[tv[j]])
                        k.act(sT[:, j, :], v[:, j, :], AF.Silu, bias=cb[:, j:j + 1], scale=cg[:, j:j + 1],
                              R=[tv[j], tcp], W=[tsT])
                if dbg is not None:
                    k.dma("sp", dbg["s"][tc].rearrange("(j p) t -> p j t", p=128), sT[:], tdb, R=[tsT], W=[tdb], join=True)
                emit_proj_res_ln(k, sT, tsT, w2_d, h_tok, th, lng_d, lnb_d, tts=[tc * 4 + i for i in range(4)])


def build_p1():
    nc = bass.Bass("TRN2", target_bir_lowering=False)
    k = K(nc)
    cst_d = nc.dram_tensor("cst", [128, NC32], F32, kind="ExternalInput").ap()
    x_d = nc.dram_tensor("x_tok", [NT, D], F32, kind="ExternalInput").ap()
    xT_d = nc.dram_tensor("xT", [D, NT + HALO], F32, kind="ExternalInput").ap()
    hm_d = nc.dram_tensor("hm", [1, 1], F32, kind="ExternalInput").ap()
    cpar_d = nc.dram_tensor("cpar", [128, NCPAR], F32, kind="ExternalInput").ap()
    w1_d = nc.dram_tensor("w_pw1", [D, 2 * D], F32, kind="ExternalInput").ap()
    w2_d = nc.dram_tensor("w_pw2", [D, D], F32, kind="ExternalInput").ap()
    md = moe_dram(nc)
    out_d = nc.dram_tensor("out", [NT, D], F32, kind="ExternalOutput").ap()
    C = load_consts(k, cst_d)
    h_tok = k.sb("h_tok", [128, TT, D], F32)
    th = [Trk(f"h{i}") for i in range(TT)]
    load_tok(k, h_tok, th, x_d)
    emit_conv(k, C, xT_d, hm_d, cpar_d, w1_d, w2_d, h_tok, th, md["ln"][0:1, :], md["ln"][1:2, :])
    emit_moe_d(k, C, h_tok, th, md)
    to = Trk("out")
    store_tok(k, h_tok, th, out_d, to)
    k.barrier()
    k.close()
    return nc


def p1_inputs(inp, c):
    b, j = divmod(c, 4)
    x = inp["x"][b]
    s0 = j * NT
    xe = np.zeros((NT + HALO, D), np.float32)
    if j > 0:
        xe[:] = x[s0 - HALO:s0 + NT]
    else:
        xe[HALO:] = x[0:NT]
    d = dict(cst=make_consts(), x_tok=np.ascontiguousarray(x[s0:s0 + NT]), xT=np.ascontiguousarray(xe.T),
             hm=np.full((1, 1), 0.0 if j == 0 else 1.0, np.float32), cpar=conv_params(inp),
             w_pw1=np.ascontiguousarray(inp["conv_w_pw1"][0]), w_pw2=np.ascontiguousarray(inp["conv_w_pw2"][0]))
    d.update(moe_inputs(inp, 0))
    return d


NQB = SEQ // 128
NCMP = 255
QSCALE = 128.0 ** -0.5
NCM = 33


def _split3(v):
    import ml_dtypes
    v = v.astype(np.float32)
    a = v.astype(ml_dtypes.bfloat16).astype(np.float32)
    b = (v - a).astype(ml_dtypes.bfloat16).astype(np.float32)
    c = (v - a - b).astype(ml_dtypes.bfloat16).astype(np.float32)
    return a, b, c


def attn_consts(g):
    slopes = np.array([2.0 ** (-8.0 * (4 * g + hh + 1) / 16.0) for hh in range(4)], np.float64)
    kr = np.arange(128)
    LT = np.zeros((128, 32 * 128), np.float32)
    for d in range(32):
        for hh in range(4):
            val = slopes[hh] * (128.0 * (-d) + kr - 64.0)
            for s_, part in enumerate(_split3(val)):
                LT[hh * 3 + s_, d * 128:(d + 1) * 128] = part
    LTc = np.zeros((128, 32 * 128), np.float32)
    for dc in range(32):
        for hh in range(4):
            val = slopes[hh] * (16.0 * kr - 33.0 - 128.0 * dc)
            for s_, part in enumerate(_split3(val)):
                LTc[hh * 3 + s_, dc * 128:(dc + 1) * 128] = part
    HI = np.zeros((128, 512), np.float32)
    for hh in range(4):
        HI[hh * 3:hh * 3 + 3, hh * 128:(hh + 1) * 128] = 1.0
    E = np.zeros((128, SEQ), np.float32)
    for d in range(32):
        for half in range(2):
            r = 62 - 2 * d + half
            E[r, d * 128 + half * 64:d * 128 + half * 64 + 64] = 1.0
    E[64:76, :] = LT[0:12, :]
    qr = np.arange(128)
    Mc = np.where(kr[:, None] <= qr[None, :], 0.0, NEG).astype(np.float32)
    Mw = np.where(kr[:, None] > qr[None, :], 0.0, NEG).astype(np.float32)
    Mc4 = np.tile(Mc, (1, 4))
    Mw4 = np.tile(Mw, (1, 4))
    CM = np.zeros((128, NCM * 128), np.float32)
    for di in range(17):
        m = np.where(16 * kr[:, None] + 31 <= 128 * di + qr[None, :], 0.0, NEG)
        CM[:, di * 128:(di + 1) * 128] = m
    for di in range(16):
        m = np.where(16 * kr[:, None] + 31 <= 128 * di + qr[None, :], 0.0, NEG)
        m[127, :] = NEG
        CM[:, (17 + di) * 128:(18 + di) * 128] = m
    n = np.arange(256)[:, None]
    j = np.arange(64)[None, :]
    ov = np.maximum(np.minimum(16 * n + 32, 64 * j + 64) - np.maximum(16 * n, 64 * j), 0) / 32.0
    ov[255] = 0
    ovl = ov.reshape(2, 128, 64).transpose(1, 0, 2).reshape(128, 128)
    ident = np.eye(128, dtype=np.float32)
    t = np.arange(SEQ)
    cur = (t // 64)[:, None]
    blk = np.arange(64)[None, :]
    forced = ((blk == 0) | (blk == cur) | (blk == cur - 1)).astype(np.float32)
    cand = ((blk >= 1) & (blk <= cur - 2)).astype(np.float32)
    f3 = forced.reshape(32, 128, 64).transpose(1, 0, 2).reshape(128, 32 * 64)
    c3 = cand.reshape(32, 128, 64).transpose(1, 0, 2).reshape(128, 32 * 64)
    cb = np.concatenate([LT, LTc, HI, E, Mc4, Mw4, CM, ovl, ident, c3, f3], axis=1).astype(np.float32)
    cf = ident.copy()
    return np.ascontiguousarray(cb), np.ascontiguousarray(cf)


CB_OFF = {}
_o = 0
for _n, _w in (("LT", 32 * 128), ("LTc", 32 * 128), ("HI", 512), ("E", SEQ), ("Mc4", 512), ("Mw4", 512),
               ("CM", NCM * 128), ("ov", 128), ("ident", 128), ("cand", 2048), ("forced", 2048)):
    CB_OFF[_n] = (_o, _o + _w)
    _o += _w
NCB = _o
NCF = 128
NWF = 1024
NWT = 268


def attn_weights(inp, g):
    wqg = inp["nsa_w_qg"][0]
    kvw = inp["kv_w"]
    sl = lambda s_: kvw[:, s_ * 512 + g * 128: s_ * 512 + (g + 1) * 128]
    WF = np.concatenate([wqg[:, g * 512:(g + 1) * 512], sl(2), sl(4), sl(0), sl(1)], axis=1)
    WT = np.concatenate([sl(3), sl(5), wqg[:, 2048 + g * 12: 2048 + (g + 1) * 12]], axis=1)
    posT = np.concatenate([inp["cmp_pos"][0].T, inp["cmp_pos"][1].T], axis=1)
    return dict(WF=np.ascontiguousarray(WF), WT=np.ascontiguousarray(WT), posT=np.ascontiguousarray(posT),
                cw1=np.ascontiguousarray(inp["cmp_w1"]), cw2=np.ascontiguousarray(inp["cmp_w2"]))


def emit_attn(k, hT_d, WF_d, WT_d, posT_d, cw1_d, cw2_d, cb_d, cf_d, o_d, dbg=None, hsrc=None, ob=None, tob=None,
              chunk_order=None, after_qb=None, bg=None):
    nc = k.nc
    cbs = k.sb("cbs", [128, NCB], BF16)
    tcb = Trk("cbs")
    k.dma("pool", cbs[:], cb_d, tcb, W=[tcb])
    cfs = k.sb("cfs", [128, NCF], F32)
    tcf = Trk("cfs")
    k.dma("sp", cfs[:], cf_d, tcf, W=[tcf])
    cbv = lambda n_: cbs[:, CB_OFF[n_][0]:CB_OFF[n_][1]]
    LT, LTc, HI, E_, Mc4, Mw4, CM, ovl, identb = (cbv(n_) for n_ in ("LT", "LTc", "HI", "E", "Mc4", "Mw4", "CM", "ov", "ident"))
    cand = cbv("cand").rearrange("p (a b) -> p a b", a=32)
    forced = cbv("forced").rearrange("p (a b) -> p a b", a=32)
    identf = cfs[:, 0:128]
    QT = k.sb("QT", [128, NQB, 4, 128], BF16)
    tQ = [Trk(f"Q{i}") for i in range(16)]
    KsT = k.sb("KsT", [128, SEQ], BF16)
    KwT = k.sb("KwT", [128, SEQ], BF16)
    tK = [Trk(f"K{i}") for i in range(16)]
    Vs = k.sb("Vs", [128, NQB, 132], BF16)
    Vw = k.sb("Vw", [128, NQB, 132], BF16)
    tV = [Trk(f"V{i}") for i in range(16)]
    gat = k.sb("gat", [128, NQB, 12], F32)
    KcT = k.sb("KcT", [128, 256], BF16)
    Vc = k.sb("Vc", [128, 2, 196], BF16)
    tC = Trk("cmpkv")
    k.v("dve", "memset", Vs[:, :, 128:132], 1.0, W=tV)
    k.v("dve", "memset", Vw[:, :, 128:132], 1.0, W=tV)
    k.v("dve", "memset", Vc[:], 0.0, W=[tC])
    k.v("dve", "memset", KcT[:], 0.0, W=[tC])

    with k.scope():
        rawT = [k.sb("rawT", [128, SEQ], BF16) for _ in range(2)]
        traw = [Trk(f"raw{i}") for i in range(16)]
        with k.scope():
            WF = k.sb("WF", [128, CT, NWF], BF16)
            tWF = Trk("WF")
            WFv = WF_d.rearrange("(ct p) n -> p ct n", p=128)
            k.dma("pool", WF[:, :, 0:512], WFv[:, :, 0:512], tWF, W=[tWF])
            k.dma("pool", WF[:, :, 512:1024], WFv[:, :, 512:1024], tWF, W=[tWF], join=True)
            WT = k.sb("WT", [128, CT, NWT], BF16)
            tWT = Trk("WT")
            k.dma("pool", WT[:], WT_d.rearrange("(ct p) n -> p ct n", p=128), tWT, W=[tWT])
            hc = [k.sb("hc", [128, CT, 256], BF16) for _ in range(2)]
            thc = [Trk("hc0"), Trk("hc1")]
            psF = [k.ps("psF", [128, 512]) for _ in range(3)]
            tpF = [Trk(f"pF{i}") for i in range(3)]
            psT = [k.ps("psT", [128, 512]) for _ in range(2)]
            tpT = [Trk(f"pT{i}") for i in range(2)]
            hv = hT_d.rearrange("(ct p) t -> p ct t", p=128) if hT_d is not None else None

            def issue(c):
                if c < 16:
                    if hsrc is None:
                        k.dma("pool", hc[c % 2][:], hv[:, :, c * 256:(c + 1) * 256], thc[c % 2], W=[thc[c % 2]])
                    else:
                        ap_, Rr = hsrc(c)
                        k.dma("sp", hc[c % 2][:], ap_, thc[c % 2], R=Rr, W=[thc[c % 2]])

            order = list(range(16)) if chunk_order is None else list(chunk_order)

            def issue(i):
                if i < 16:
                    c_ = order[i]
                    if hsrc is None:
                        k.dma("pool", hc[i % 2][:], hv[:, :, c_ * 256:(c_ + 1) * 256], thc[i % 2], W=[thc[i % 2]])
                    else:
                        ap_, Rr = hsrc(c_)
                        k.dma("sp", hc[i % 2][:], ap_, thc[i % 2], R=Rr, W=[thc[i % 2]])

            issue(0)
            nf = 0
            ntk = 0
            for ci_, c in enumerate(order):
                issue(ci_ + 1)
                H = hc[ci_ % 2]
                tH = thc[ci_ % 2]
                for f in range(8):
                    p = nf % 3
                    nf += 1
                    for ct in range(CT):
                        k.mm(psF[p][:, 0:256], WF[:, ct, f * 128:(f + 1) * 128], H[:, ct, :], ct == 0, ct == CT - 1,
                             R=[tWF, tH], W=[tpF[p]])
                    if f < 4:
                        k.act(QT[:, 2 * c:2 * c + 2, f, :], psF[p][:, 0:256].rearrange("p (a b) -> p a b", a=2), AF.Copy,
                              scale=QSCALE, R=[tpF[p]], W=[tQ[c]])
                    else:
                        dst = (KsT, KwT, rawT[0], rawT[1])[f - 4]
                        trk = (tK[c], tK[c], traw[c], traw[c])[f - 4]
                        if f % 2 == 0:
                            k.v("dve", "tensor_copy", dst[:, c * 256:(c + 1) * 256], psF[p][:, 0:256], R=[tpF[p]], W=[trk])
                        else:
                            k.act(dst[:, c * 256:(c + 1) * 256], psF[p][:, 0:256], AF.Copy, R=[tpF[p]], W=[trk])
                for i in range(2):
                    tt = c * 2 + i
                    p = ntk % 2
                    ntk += 1
                    for ct in range(CT):
                        k.mm(psT[p][:, 0:NWT], H[:, ct, i * 128:(i + 1) * 128], WT[:, ct, :], ct == 0, ct == CT - 1,
                             R=[tH, tWT], W=[tpT[p]])
                    k.v("dve", "tensor_copy", Vs[:, tt, 0:128], psT[p][:, 0:128], R=[tpT[p]], W=[tV[c]])
                    k.v("dve", "tensor_copy", Vw[:, tt, 0:128], psT[p][:, 128:256], R=[tpT[p]], W=[tV[c]])
                    k.act(gat[:, tt, :], psT[p][:, 256:268], AF.Sigmoid, R=[tpT[p]], W=[tV[c]])
                if bg is not None:
                    bg("proj")
        with k.scope():
            posT = k.sb("posT", [128, 64], BF16)
            tpos = Trk("posT")
            k.dma("pool", posT[:], posT_d, tpos, W=[tpos])
            W1 = [k.sb("cW1", [128, 32, 256], BF16) for _ in range(2)]
            W2 = [k.sb("cW2", [128, 2, 128], BF16) for _ in range(2)]
            tW = Trk("cW")
            for c in range(2):
                k.dma("pool", W1[c][:], cw1_d[c].rearrange("(cb p) n -> p cb n", p=128), tW, W=[tW], join=True)
                k.dma("pool", W2[c][:], cw2_d[c].rearrange("(a p) n -> p a n", p=128), tW, W=[tW], join=True)
            psH = [k.ps("psH", [128, 512]) for _ in range(2)]
            tpH = [Trk("pH0"), Trk("pH1")]
            psB = k.ps("psB", [128, 512])
            tpB = Trk("pB")
            psO = k.ps("psO", [128, 512])
            tpO = Trk("pO")
            hb = k.sb("hbias", [128, 4], F32)
            thb = Trk("hbias")
            xg = k.sb("xg", [128, 3, 256], F32)
            txg = Trk("xg")
            hid = [[k.sb("hid", [128, 256], BF16) for _ in range(2)] for _ in range(2)]
            thid = [[Trk(f"hid{a}{b}") for b in range(2)] for a in range(2)]
            nh = 0
            for c in range(2):
                rv = rawT[c][:, :].rearrange("p (n s) -> p n s", s=16)
                for hk in range(2):
                    for cb_ in range(32):
                        k.mm(psB[:, hk:hk + 1], W1[c][:, cb_, hk * 128:(hk + 1) * 128], posT[:, c * 32 + cb_:c * 32 + cb_ + 1],
                             cb_ == 0, cb_ == 31, R=[tW, tpos], W=[tpB])
                    k.v("dve", "tensor_copy", hb[:, c * 2 + hk:c * 2 + hk + 1], psB[:, hk:hk + 1], R=[tpB], W=[thb])
                    p = nh % 2
                    nh += 1
                    for cb_ in range(32):
                        k.mm(psH[p][:, 0:NCMP], W1[c][:, cb_, hk * 128:(hk + 1) * 128],
                             rv[:, cb_ // 16:cb_ // 16 + NCMP, cb_ % 16], cb_ == 0, cb_ == 31,
                             R=[tW] + traw, W=[tpH[p]])
                    x_ = xg[:, 0, 0:NCMP]
                    u_ = xg[:, 1, 0:NCMP]
                    s_ = xg[:, 2, 0:NCMP]
                    k.act(x_, psH[p][:, 0:NCMP], AF.Identity, bias=hb[:, c * 2 + hk:c * 2 + hk + 1], scale=1.0,
                          R=[tpH[p], thb], W=[txg])
                    k.v("dve", "tensor_tensor", u_, x_, x_, ALU.mult, R=[txg], W=[txg])
                    k.v("dve", "tensor_scalar", u_, u_, 0.044715, 1.0, ALU.mult, ALU.add, R=[txg], W=[txg])
                    k.v("dve", "tensor_tensor", u_, u_, x_, ALU.mult, R=[txg], W=[txg])
                    k.act(s_, u_, AF.Sigmoid, scale=2.0 * 0.7978845608028654, R=[txg], W=[txg])
                    k.v("dve", "tensor_tensor", hid[c][hk][:, 0:NCMP], x_, s_, ALU.mult, R=[txg], W=[thid[c][hk]])
            for hk in range(2):
                k.mm(psO[:, 0:NCMP], W2[0][:, hk, :], hid[0][hk][:, 0:NCMP], hk == 0, hk == 1,
                     R=[tW, thid[0][hk]], W=[tpO])
            k.v("dve", "tensor_copy", KcT[:, 0:NCMP], psO[:, 0:NCMP], R=[tpO], W=[tC])
            for nt, (n0, n1) in enumerate(((0, 128), (128, NCMP))):
                w = n1 - n0
                for hk in range(2):
                    k.mm(psO[0:w, 256:384], hid[1][hk][:, n0:n1], W2[1][:, hk, :], hk == 0, hk == 1,
                         R=[tW, thid[1][hk]], W=[tpO])
                k.v("dve", "tensor_copy", Vc[0:w, nt, 0:128], psO[0:w, 256:384], R=[tpO], W=[tC])
                k.v("dve", "tensor_copy", Vc[0:w, nt, 128:192], ovl[0:w, nt * 64:(nt + 1) * 64], R=[tcb], W=[tC])
                k.v("dve", "memset", Vc[0:w, nt, 192:193], 1.0, W=[tC])
    if dbg is not None:
        tdb = Trk("dbg")
        k.dma("sp", dbg["KcT"], KcT[:], tdb, R=[tC], W=[tdb], join=True)
        k.dma("sp", dbg["Vc"], Vc[:], tdb, R=[tC], W=[tdb], join=True)
        k.dma("sp", dbg["QT"], QT[:], tdb, R=tQ, W=[tdb], join=True)
        k.dma("sp", dbg["KsT"], KsT[:], tdb, R=tK, W=[tdb], join=True)
        k.dma("sp", dbg["Vs"], Vs[:], tdb, R=tV, W=[tdb], join=True)
        k.dma("sp", dbg["gat"], gat[:], tdb, R=tV, W=[tdb], join=True)

    with k.scope():
        NPS = 3
        psS = [k.ps("psS", [128, 512]) for _ in range(NPS)]
        tpS = [Trk(f"pS{i}") for i in range(NPS)]
        psO = [[k.ps("psOa", [128, 512]) for _ in range(2)] for _ in range(2)]
        tpO = [[Trk(f"pO{a}{b}") for b in range(2)] for a in range(2)]
        psX = k.ps("psX", [128, 512])
        tpX = Trk("pX")
        NPT = 4
        PT = [k.sb("PT", [128, 512], BF16) for _ in range(NPT)]
        tPT = [Trk(f"PT{i}") for i in range(NPT)]
        oacc = [k.sb("oacc", [128, 512], F32) for _ in range(3)]
        toacc = [Trk("oacc0"), Trk("oacc1"), Trk("oacc2")]
        imp = [k.sb("imp", [128, 64], F32) for _ in range(2)]
        timp = [Trk("imp0"), Trk("imp1")]
        sm = k.sb("asm", [128, 6, 16], F32)
        tsm = [Trk(f"asm{i}") for i in range(6)]
        selw = k.sb("selw", [128, 2, 4, 64], F32)
        tselw = [Trk("selw0"), Trk("selw1")]
        NSB = 3
        selb4 = [k.sb("selb4", [76, 512], BF16) for _ in range(NSB)]
        tsb4 = [Trk(f"selb4{i}") for i in range(NSB)]
        for i_ in range(NSB):
            k.dma("pool", selb4[i_][64:76, :], cb_d[0:12, CB_OFF["HI"][0]:CB_OFF["HI"][1]], tsb4[i_], W=[tsb4[i_]])
        tout = Trk("o_out")
        oTt = [k.sb("oTt", [128, 4, 128], BF16) for _ in range(2)]
        toTt = [Trk("oTt0"), Trk("oTt1")]
        st = dict(ns=0, npt=0, no=0, nsm=0)
        Qv = lambda qb: QT[:, qb, :, :].rearrange("p h q -> p (h q)")
        glist = []

        def add_group(qb, lhsK, tKk, extra, Vaug, vw, tVv, first, last, ob, after=None):
            slot = {}

            def scores():
                s_ = st["ns"] % NPS
                st["ns"] += 1
                k.mm(psS[s_][:, :], lhsK, Qv(qb), True, False, R=[tKk, tQ[qb // 2]], W=[tpS[s_]], inc=False)
                for i, (l_, r_, o_, Rr) in enumerate(extra):
                    out_ap = psS[s_][:, :] if o_ is None else psS[s_][:, o_[0]:o_[1]]
                    k.mm(out_ap, l_, r_, False, i == len(extra) - 1, R=Rr, W=[tpS[s_]], inc=(i == len(extra) - 1))
                pt = st["npt"] % NPT
                st["npt"] += 1
                slot["pt"] = pt
                k.act(PT[pt][:], psS[s_][:, :], AF.Exp, R=[tpS[s_]], W=[tPT[pt]])

            def pv():
                pt = slot["pt"]
                for hh in range(4):
                    bank = psO[ob][hh // 2]
                    c0 = (hh % 2) * 256
                    k.mm(bank[:, c0:c0 + vw], PT[pt][:, hh * 128:(hh + 1) * 128], Vaug, first and hh % 2 == 0, last,
                         R=[tPT[pt], tVv], W=[tpO[ob][hh // 2]], inc=last, skip_group_check=True)

            glist.append((scores, pv, after))

        def finish(qb, ob, br, first_branch, vw, with_imp=False):
            oa = oacc[qb % 3]
            to_ = toacc[qb % 3]
            i6 = st["nsm"] % 6
            st["nsm"] += 1
            S_ = tsm[i6]
            for hh in range(4):
                bank = psO[ob][hh // 2]
                tb = tpO[ob][hh // 2]
                c0 = (hh % 2) * 256
                rs = sm[:, i6, hh * 4:hh * 4 + 1]
                sc = sm[:, i6, hh * 4 + 1:hh * 4 + 2]
                k.v("dve", "tensor_scalar", rs, bank[:, c0 + vw - 1:c0 + vw], 1e-30, None, ALU.max, R=[tb, S_], W=[S_])
                k.v("dve", "reciprocal", rs, rs, R=[S_], W=[S_])
                k.v("dve", "tensor_tensor", sc, rs, gat[:, qb, hh * 3 + br:hh * 3 + br + 1], ALU.mult,
                    R=[S_, tV[qb // 2]], W=[S_])
                dst = oa[:, hh * 128:(hh + 1) * 128]
                if first_branch:
                    k.v("dve", "tensor_scalar", dst, bank[:, c0:c0 + 128], sc, None, ALU.mult, R=[tb, S_], W=[to_])
                else:
                    k.v("dve", "scalar_tensor_tensor", dst, bank[:, c0:c0 + 128], sc, dst, ALU.mult, ALU.add,
                        R=[tb, S_], W=[to_])
                if with_imp:
                    im = imp[qb % 2]
                    if hh == 0:
                        k.v("dve", "tensor_scalar", im[:], bank[:, c0 + 128:c0 + 192], rs, None, ALU.mult,
                            R=[tb, S_], W=[timp[qb % 2]])
                    else:
                        k.v("dve", "scalar_tensor_tensor", im[:], bank[:, c0 + 128:c0 + 192], rs, im[:], ALU.mult,
                            ALU.add, R=[tb, S_], W=[timp[qb % 2]])

        deferred = []

        def defer(n, fn):
            deferred.append([n, fn])

        def select_math(qb):
            w = qb % 2
            V_ = selw[:, w, 0, :]
            V2 = selw[:, w, 1, :]
            sl_ = selw[:, w, 2, :]
            m8 = selw[:, w, 3, 0:16]
            T_ = tselw[w]
            k.v("dve", "scalar_tensor_tensor", V_, imp[w][:], 1.0, cand[:, qb, :], ALU.add, ALU.mult,
                R=[timp[w], tcb], W=[T_])
            k.v("dve", "tensor_scalar", V_, V_, -1.0, None, ALU.add, R=[T_], W=[T_])
            k.v("dve", "max", m8[:, 0:8], V_, R=[T_], W=[T_])
            k.v("dve", "match_replace", V2, m8[:, 0:8], V_, -2.0, R=[T_], W=[T_])
            k.v("dve", "max", m8[:, 8:16], V2, R=[T_], W=[T_])
            k.v("dve", "tensor_scalar", sl_, V_, m8[:, 12:13], None, ALU.is_ge, R=[T_], W=[T_])
            k.v("dve", "tensor_tensor", sl_, sl_, cand[:, qb, :], ALU.mult, R=[T_, tcb], W=[T_])
            k.v("dve", "tensor_tensor", sl_, sl_, forced[:, qb, :], ALU.max, R=[T_, tcb], W=[T_])
            k.v("dve", "tensor_scalar", sl_, sl_, -NEG, NEG, ALU.mult, ALU.add, R=[T_], W=[T_])
            rlo = max(0, 62 - 2 * qb)
            jlo = rlo - 62 + 2 * qb
            k.v("dve", "memset", V2, 0.0, R=[T_], W=[T_])
            k.v("dve", "tensor_copy", V2[:, rlo:64], sl_[:, jlo:jlo + 64 - rlo], R=[T_], W=[T_])

            def tr_part():
                k.tr(psX[0:64, 0:128], V2, identf, R=[T_, tcf], W=[tpX])
                sb_ = qb % NSB
                for hh in range(4):
                    if hh % 2 == 0:
                        k.act(selb4[sb_][0:64, hh * 128:(hh + 1) * 128], psX[0:64, 0:128], AF.Copy, R=[tpX], W=[tsb4[sb_]])
                    else:
                        k.v("dve", "tensor_copy", selb4[sb_][0:64, hh * 128:(hh + 1) * 128], psX[0:64, 0:128],
                            R=[tpX], W=[tsb4[sb_]])
            defer(2, tr_part)

        def out_part(qb):
            if ob is None:
                k.dma("sp", o_d[qb * 128:(qb + 1) * 128, :], oacc[qb % 3][:], tout, R=[toacc[qb % 3]], W=[tout], join=True)
                return

            def tr_out():
                for hh in range(4):
                    k.tr(psX[:, hh * 128:(hh + 1) * 128], oacc[qb % 3][:, hh * 128:(hh + 1) * 128], identf,
                         R=[toacc[qb % 3], tcf], W=[tpX], inc=(hh == 3))
                k.act(oTt[qb % 2][:], psX[:, :].rearrange("p (h q) -> p h q", h=4), AF.Copy, R=[tpX], W=[toTt[qb % 2]])
                j_, tq = qb // 8, (qb % 8) * 128
                k.dma("sp", ob[j_][:, tq:tq + 128].rearrange("(h p) q -> p h q", p=128), oTt[qb % 2][:],
                      tob[j_], R=[toTt[qb % 2]], W=[tob[j_]], join=True)
                if after_qb is not None:
                    after_qb(qb)
            defer(2, tr_out)

        def cmp_branch(qb):
            ob_ = st["no"] % 2
            st["no"] += 1
            nts = [0] if qb < 16 else [0, 1]
            for ii, nt in enumerate(nts):
                dc = qb - 16 * nt
                extra = [(LTc[0:12, dc * 128:(dc + 1) * 128], HI[0:12, :], None, [tcb])]
                mi = None
                if nt == 0 and qb <= 16:
                    mi = qb
                elif nt == 1:
                    mi = 17 + (qb - 16)
                if mi is not None:
                    for hh in range(4):
                        extra.append((identb, CM[:, mi * 128:(mi + 1) * 128], (hh * 128, (hh + 1) * 128), [tcb]))
                aft = None
                if ii == len(nts) - 1:
                    def aft(qb=qb, ob_=ob_):
                        finish(qb, ob_, 0, True, 193, with_imp=True)
                        select_math(qb)
                add_group(qb, KcT[:, nt * 128:(nt + 1) * 128], tC, extra, Vc[:, nt, 0:193], 193, tC,
                          ii == 0, ii == len(nts) - 1, ob_, after=aft)

        def win_branch(qb):
            ob_ = st["no"] % 2
            st["no"] += 1
            kts = list(range(max(0, qb - 4), qb + 1))
            for ii, kt in enumerate(kts):
                d = qb - kt
                extra = [(LT[0:12, d * 128:(d + 1) * 128], HI[0:12, :], None, [tcb])]
                if kt == qb:
                    extra.append((identb, Mc4, None, [tcb]))
                if kt == qb - 4:
                    extra.append((identb, Mw4, None, [tcb]))
                aft = None
                if ii == len(kts) - 1:
                    def aft(qb=qb, ob_=ob_):
                        finish(qb, ob_, 2, False, 129)
                add_group(qb, KwT[:, kt * 128:(kt + 1) * 128], tK[kt // 2], extra, Vw[:, kt, 0:129], 129, tV[kt // 2],
                          ii == 0, ii == len(kts) - 1, ob_, after=aft)

        def sel_branch(qb):
            ob_ = st["no"] % 2
            st["no"] += 1
            sb_ = qb % NSB
            for kt in range(qb + 1):
                d = qb - kt
                extra = [(E_[0:76, d * 128:(d + 1) * 128], selb4[sb_][0:76, :], None, [tcb, tsb4[sb_]])]
                if kt == qb:
                    extra.append((identb, Mc4, None, [tcb]))
                aft = None
                if kt == qb:
                    def aft(qb=qb, ob_=ob_):
                        finish(qb, ob_, 1, False, 129)
                        out_part(qb)
                        if bg is not None:
                            bg("attn")
                add_group(qb, KsT[:, kt * 128:(kt + 1) * 128], tK[kt // 2], extra, Vs[:, kt, 0:129], 129, tV[kt // 2],
                          kt == 0, kt == qb, ob_, after=aft)

        cmp_branch(0)
        for qb in range(NQB):
            if qb + 1 < NQB:
                cmp_branch(qb + 1)
            win_branch(qb)
            sel_branch(qb)

        def tick():
            for d_ in list(deferred):
                d_[0] -= 1
                if d_[0] <= 0:
                    deferred.remove(d_)
                    d_[1]()

        prev = None
        for g_ in glist:
            g_[0]()
            if prev is not None:
                prev[1]()
                if prev[2] is not None:
                    prev[2]()
                tick()
            prev = g_
        prev[1]()
        if prev[2] is not None:
            prev[2]()
        while deferred:
            tick()


def build_p2(debug=False):
    nc = bass.Bass("TRN2", target_bir_lowering=False)
    k = K(nc)
    hT_d = nc.dram_tensor("hT", [D, SEQ], F32, kind="ExternalInput").ap()
    WF_d = nc.dram_tensor("WF", [D, NWF], F32, kind="ExternalInput").ap()
    WT_d = nc.dram_tensor("WT", [D, NWT], F32, kind="ExternalInput").ap()
    posT_d = nc.dram_tensor("posT", [128, 64], F32, kind="ExternalInput").ap()
    cw1_d = nc.dram_tensor("cw1", [2, 4096, 256], F32, kind="ExternalInput").ap()
    cw2_d = nc.dram_tensor("cw2", [2, 256, 128], F32, kind="ExternalInput").ap()
    cb_d = nc.dram_tensor("cb", [128, NCB], F32, kind="ExternalInput").ap()
    cf_d = nc.dram_tensor("cf", [128, NCF], F32, kind="ExternalInput").ap()
    o_d = nc.dram_tensor("o", [SEQ, 512], F32, kind="ExternalOutput").ap()
    dbg = None
    if debug:
        dbg = dict(KcT=nc.dram_tensor("d_KcT", [128, 256], BF16, kind="ExternalOutput").ap(),
                   Vc=nc.dram_tensor("d_Vc", [128, 2, 196], BF16, kind="ExternalOutput").ap(),
                   QT=nc.dram_tensor("d_QT", [128, NQB, 4, 128], BF16, kind="ExternalOutput").ap(),
                   KsT=nc.dram_tensor("d_KsT", [128, SEQ], BF16, kind="ExternalOutput").ap(),
                   Vs=nc.dram_tensor("d_Vs", [128, NQB, 132], BF16, kind="ExternalOutput").ap(),
                   gat=nc.dram_tensor("d_gat", [128, NQB, 12], F32, kind="ExternalOutput").ap())
    emit_attn(k, hT_d, WF_d, WT_d, posT_d, cw1_d, cw2_d, cb_d, cf_d, o_d, dbg=dbg)
    k.barrier()
    k.close()
    return nc


def p2_inputs(inp, h2T_b, g):
    cb, cf = attn_consts(g)
    d = dict(hT=h2T_b, cb=cb, cf=cf)
    d.update(attn_weights(inp, g))
    return d


def p3_inputs(inp, h2_c, oT_c):
    d = dict(cst=make_consts(), h_in=h2_c, oT=oT_c, w_o=np.ascontiguousarray(inp["nsa_w_o"][0]))
    d.update(moe_inputs(inp, 1))
    return d


_PROGS = {}


def _prog(name, fn):
    if name not in _PROGS:
        _PROGS[name] = fn()
    return _PROGS[name]


def _kernel3(**inputs):
    inp = {k_: np.asarray(v_) for k_, v_ in inputs.items()}
    cores = list(range(NCORE))
    r1 = run_bass_kernel_spmd(_prog("p1", build_p1), [p1_inputs(inp, c) for c in cores], core_ids=cores)
    h2 = np.stack([r1.results[c]["out"] for c in cores]).reshape(2, SEQ, D)
    h2T = [np.ascontiguousarray(h2[b].T) for b in range(2)]
    r2 = run_bass_kernel_spmd(_prog("p2", build_p2), [p2_inputs(inp, h2T[c // 4], c % 4) for c in cores], core_ids=cores)
    o = np.zeros((2, SEQ, D), np.float32)
    for c in cores:
        o[c // 4, :, (c % 4) * 512:(c % 4 + 1) * 512] = r2.results[c]["o"]
    ins3 = []
    for c in cores:
        b, j = divmod(c, 4)
        ins3.append(p3_inputs(inp, np.ascontiguousarray(h2[b, j * NT:(j + 1) * NT]),
                              np.ascontiguousarray(o[b, j * NT:(j + 1) * NT].T)))
    r3 = run_bass_kernel_spmd(_prog("p3", build_p3), ins3, core_ids=cores)
    out = np.stack([r3.results[c]["out"] for c in cores]).reshape(2, SEQ, D)
    return out.astype(np.float32)


RG4 = [[0, 1, 2, 3], [4, 5, 6, 7]]
NCONV0 = 8
NCONV1 = 16


def build_fused():
    nc = bass.Bass("TRN2", target_bir_lowering=False)
    k = K(nc)
    cst_d = nc.dram_tensor("cst", [128, NC32], F32, kind="ExternalInput").ap()
    x_d = nc.dram_tensor("x_tok", [NT, D], F32, kind="ExternalInput").ap()
    xT_d = nc.dram_tensor("xT", [D, NT + HALO], F32, kind="ExternalInput").ap()
    hm_d = nc.dram_tensor("hm", [1, 1], F32, kind="ExternalInput").ap()
    cpar_d = nc.dram_tensor("cpar", [128, NCPAR], F32, kind="ExternalInput").ap()
    w1_d = nc.dram_tensor("w_pw1", [D, 2 * D], F32, kind="ExternalInput").ap()
    w2_d = nc.dram_tensor("w_pw2", [D, D], F32, kind="ExternalInput").ap()
    md0 = moe_dram(nc, "0")
    WF_d = nc.dram_tensor("WF", [D, NWF], F32, kind="ExternalInput").ap()
    WT_d = nc.dram_tensor("WT", [D, NWT], F32, kind="ExternalInput").ap()
    posT_d = nc.dram_tensor("posT", [128, 64], F32, kind="ExternalInput").ap()
    cw1_d = nc.dram_tensor("cw1", [2, 4096, 256], F32, kind="ExternalInput").ap()
    cw2_d = nc.dram_tensor("cw2", [2, 256, 128], F32, kind="ExternalInput").ap()
    cb_d = nc.dram_tensor("cb", [128, NCB], F32, kind="ExternalInput").ap()
    cf_d = nc.dram_tensor("cf", [128, NCF], F32, kind="ExternalInput").ap()
    idx3_d = nc.dram_tensor("idx3", [128, CT], I32, kind="ExternalInput").ap()
    wo_d = nc.dram_tensor("w_o", [D, D], F32, kind="ExternalInput").ap()
    md1 = moe_dram(nc, "1")
    out_d = nc.dram_tensor("out", [NT, D], F32, kind="ExternalOutput").ap()
    hsp = nc.dram_tensor("hsp", [NT, D], F32).ap()
    xb1 = [nc.dram_tensor(f"xb1_{q}", [D, 256], BF16).ap() for q in range(4)]
    xg1 = [nc.dram_tensor(f"xg1_{q}", [4 * D, 256], BF16).ap() for q in range(4)]
    ob = [nc.dram_tensor(f"ob_{j}", [512, NT], BF16).ap() for j in range(4)]
    og = nc.dram_tensor("og", [4 * D, NT], BF16).ap()
    thsp = Trk("hsp")
    txb1 = [Trk(f"xb1{q}") for q in range(4)]
    txg1 = [Trk(f"xg1{q}") for q in range(4)]
    tob = [Trk(f"ob{j}") for j in range(4)]
    tog = [Trk(f"og{j}") for j in range(4)]

    wc0 = WConv(k, md0, "0", NCONV0)
    wc1 = WConv(k, md1, "1", NCONV1)

    cnt = dict(pw1=0, conv=0, attn=0, proj=0)

    def bg0(where):
        cnt[where] += 1
        if where == "conv" and cnt[where] % 2 == 0:
            wc0.pump(1)

    def bg1(where):
        cnt[where] += 1
        if where == "attn":
            wc1.pump(1)

    C = load_consts(k, cst_d)
    with k.scope():
        h_tok = k.sb("h_tok", [128, TT, D], F32)
        th = [Trk(f"h{i}") for i in range(TT)]
        load_tok(k, h_tok, th, x_d)
        emit_conv(k, C, xT_d, hm_d, cpar_d, w1_d, w2_d, h_tok, th, md0["ln"][0:1, :], md0["ln"][1:2, :], bg=bg0)
        wc0.flush()
        def cb_setup():
            ctx = dict(hTb=k.sb("hTb", [128, CT, NT], BF16), thTb=[Trk(f"hTb{i}") for i in range(TT)],
                       psA=[k.ps("psTr", [128, 512]) for _ in range(4)], tpsA=[Trk(f"psTr{i}") for i in range(4)], n=0)
            return ctx

        def cb_tile(ctx, tt):
            hvs = hsp.rearrange("(tt p) c -> p tt c", p=128)
            k.dma("sp", hvs[:, tt, :], h_tok[:, tt, :], thsp, R=[th[tt]], W=[thsp], join=True)
            hTb, psA, tpsA = ctx["hTb"], ctx["psA"], ctx["tpsA"]
            for q in range(4):
                p = ctx["n"] % 4
                ctx["n"] += 1
                for i in range(4):
                    ct = q * 4 + i
                    k.tr(psA[p][:, i * 128:(i + 1) * 128], h_tok[:, tt, ct * 128:(ct + 1) * 128], C["ident"],
                         R=[th[tt], C["t32"]], W=[tpsA[p]], inc=(i == 3))
                dst = hTb[:, q * 4:(q + 1) * 4, tt * 128:(tt + 1) * 128]
                src = psA[p][:, :].rearrange("p (a b) -> p a b", a=4)
                if q % 2 == 0:
                    k.act(dst, src, AF.Copy, R=[tpsA[p]], W=[ctx["thTb"][tt]])
                else:
                    k.v("dve", "tensor_copy", dst, src, R=[tpsA[p]], W=[ctx["thTb"][tt]])
            if tt % 2 == 1:
                q_ = tt // 2
                k.dma("sp", xb1[q_].rearrange("(ct p) t -> p ct t", p=128), hTb[:, :, q_ * 256:(q_ + 1) * 256],
                      txb1[q_], R=[ctx["thTb"][tt - 1], ctx["thTb"][tt]], W=[txb1[q_]])
                k.collective("AllGather", RG4, xb1[q_], xg1[q_], txg1[q_], R=[txb1[q_]], W=[txg1[q_]])

        emit_moe_d(k, C, h_tok, th, md0, wconv=wc0, tile_cb=(cb_setup, cb_tile))
    k.release(wc0.trk)

    def hsrc(c):
        r, q = divmod(c, 4)
        return xg1[q][r * D:(r + 1) * D, :].rearrange("(ct p) t -> p ct t", p=128), [txg1[q]]

    def after_qb(qb):
        if qb % 8 == 7:
            j_ = qb // 8
            k.collective("AllGather", RG4, ob[j_], og[j_ * D:(j_ + 1) * D, :], tog[j_], R=[tob[j_]], W=[tog[j_]])

    with k.scope():
        emit_attn(k, None, WF_d, WT_d, posT_d, cw1_d, cw2_d, cb_d, cf_d, None, hsrc=hsrc, ob=ob, tob=tob,
                  chunk_order=[r * 4 + q for q in range(4) for r in range(4)], after_qb=after_qb, bg=bg1)
        wc1.flush()

    with k.scope():
        h_tok = k.sb("h_tok", [128, TT, D], F32)
        th = [Trk(f"h{i}") for i in range(TT)]
        hv = hsp.rearrange("(tt p) c -> p tt c", p=128)
        for tt in range(TT):
            k.dma("sp", h_tok[:, tt, :], hv[:, tt, :], th[tt], R=[thsp], W=[th[tt]])
        with k.scope():
            idx3 = k.sb("idx3", [128, CT], I32)
            tidx = Trk("idx3")
            k.dma("sp", idx3[:], idx3_d, tidx, W=[tidx])
            aT = k.sb("aT", [128, CT, NT], BF16)
            taT = Trk("aT")
            tg_ = [Trk(f"aTg{i}") for i in range(CT)]
            for ct in range(CT):
                k.gather(aT[:, ct, :], og[:, :], idx3[:, ct:ct + 1], tg_[ct], R=tog + [tidx], W=[taT] if ct == 0 else [])
            taT.w = [m for t_ in tg_ for m in [(t_.dsem, t_.dcnt)]]
            emit_proj_res_ln(k, aT, taT, wo_d, h_tok, th, md1["ln"][0:1, :], md1["ln"][1:2, :])
        emit_moe_d(k, C, h_tok, th, md1, wconv=wc1)
        to = Trk("out")
        store_tok(k, h_tok, th, out_d, to)
    k.barrier()
    k.close()
    return nc


def fused_inputs(inp, c):
    b, j = divmod(c, 4)
    d = p1_inputs(inp, c)
    m0 = moe_inputs(inp, 0)
    for kk in list(m0):
        del d[kk]
    d.update(moe_inputs(inp, 0, "0"))
    g = j
    cb, cf = attn_consts(g)
    d.update(cb=cb, cf=cf)
    d.update(attn_weights(inp, g))
    ct = np.arange(CT)[None, :]
    p = np.arange(128)[:, None]
    d["idx3"] = np.ascontiguousarray((j * 2048 + ct * 128 + p).astype(np.int32))
    d["w_o"] = np.ascontiguousarray(inp["nsa_w_o"][0])
    d.update(moe_inputs(inp, 1, "1"))
    return d


def kernel_unfused(**inputs):
    return _kernel3(**inputs)


def kernel(**inputs):
    inp = {k_: np.asarray(v_) for k_, v_ in inputs.items()}
    cores = list(range(NCORE))
    r = run_bass_kernel_spmd(_prog("fused", build_fused), [fused_inputs(inp, c) for c in cores], core_ids=cores)
    out = np.stack([r.results[c]["out"] for c in cores]).reshape(2, SEQ, D)
    return out.astype(np.float32)
```

```python
import numpy as np
from contextlib import ExitStack, contextmanager
import concourse.bass as bass
import concourse.mybir as mybir
from concourse.bass_utils import run_bass_kernel_spmd

F32, BF16, I32 = mybir.dt.float32, mybir.dt.bfloat16, mybir.dt.int32
AF = mybir.ActivationFunctionType
ALU = mybir.AluOpType
AX = mybir.AxisListType

D = 2048
NCORE = 8
SEQ = 4096
NT = 1024
TT = NT // 128
CT = D // 128
ALPHA = 4.0 ** 0.25
LN_EPS = 1e-5
NEXP = 32
DFF = 512
NEG = -30000.0


class Trk:
    __slots__ = ("w", "r", "dsem", "dcnt", "name")

    def __init__(self, name=""):
        self.w = []
        self.r = {}
        self.dsem = None
        self.dcnt = 0
        self.name = name


class Eng:
    def __init__(self, name, h):
        self.name, self.h = name, h
        self.sem = None
        self.cnt = 0
        self.known = {}
        self.pending = False


class K:
    SEM_ROLL = 30000

    def __init__(self, nc):
        self.nc = nc
        self.es = ExitStack()
        self.stacks = [self.es]
        self.eng = {}
        self.nsem = 0
        for name, h in (("pe", nc.tensor), ("act", nc.scalar), ("dve", nc.vector),
                        ("pool", nc.gpsimd), ("sp", nc.sync)):
            e = Eng(name, h)
            e.sem = self.sem("e_" + name)
            self.eng[name] = e
        self.dmas = []
        self.uid = 0
        self.sem_free = []
        self.scope_trks = [[]]

    def sem(self, name):
        self.nsem += 1
        return self.es.enter_context(self.nc.semaphore(f"{name}_{self.nsem}"))

    def sb(self, name, shape, dt):
        self.uid += 1
        return self.stacks[-1].enter_context(self.nc.sbuf_tensor(f"{name}_{self.uid}", list(shape), dt))

    def ps(self, name, shape, dt=F32):
        self.uid += 1
        return self.stacks[-1].enter_context(self.nc.psum_tensor(f"{name}_{self.uid}", list(shape), dt))

    @contextmanager
    def scope(self):
        st = ExitStack()
        self.stacks.append(st)
        self.scope_trks.append([])
        try:
            yield
        finally:
            self.barrier()
            self.stacks.pop()
            st.close()
            for S in self.scope_trks.pop():
                self.sem_free.append((S.dsem, S.dcnt))
                S.dsem = None
                S.w = []
                S.r = {}

    def _dsem(self, S):
        if S.dsem is None:
            if self.sem_free:
                S.dsem, S.dcnt = self.sem_free.pop()
            else:
                S.dsem = self.sem("d_" + S.name)
                S.dcnt = 0
            self.scope_trks[-1].append(S)

    def close(self):
        self.es.close()

    def _emit_waits(self, E, waits, skip_own=False):
        for sem, val in waits.values():
            if sem is E.sem:
                if skip_own or E.name == "pe":
                    continue
                assert val <= E.cnt, (E.name, val, E.cnt)
            if E.known.get(id(sem), 0) >= val:
                continue
            E.h.wait_ge(sem, val)
            E.known[id(sem)] = val

    @staticmethod
    def _need(waits, mark):
        sem, val = mark
        cur = waits.get(id(sem))
        if cur is None or cur[1] < val:
            waits[id(sem)] = (sem, val)

    def _collect(self, E, R, W):
        waits = {}
        for t in R:
            for m in t.w:
                self._need(waits, m)
        for t in W:
            for m in t.w:
                self._need(waits, m)
            for m in t.r.values():
                if m[0] is E.sem:
                    continue
                self._need(waits, m)
        return waits

    def _roll(self, E):
        if E.cnt >= self.SEM_ROLL and not E.pending:
            E.sem = self.sem("e_" + E.name)
            E.cnt = 0

    def op(self, en, fn, R=(), W=(), inc=True):
        E = self.eng[en]
        self._roll(E)
        self._emit_waits(E, self._collect(E, R, W))
        ins = fn()
        if inc:
            E.cnt += 1
            ins.then_inc(E.sem, 1)
            mark = (E.sem, E.cnt)
            E.pending = False
        else:
            mark = (E.sem, E.cnt + 1)
            E.pending = True
        for t in R:
            cur = t.r.get(id(mark[0]))
            if cur is None or cur[1] < mark[1]:
                t.r[id(mark[0])] = mark
        for t in W:
            t.w = [mark]
            t.r = {}
        return ins

    def dma(self, q, out, in_, S, R=(), W=(), join=False, background=False, **kw):
        E = self.eng[q]
        had = S.dsem is not None
        self._dsem(S)
        if background and not had:
            self.scope_trks[-1].remove(S)
        waits = {}
        for t in R:
            for m in t.w:
                self._need(waits, m)
        for t in W:
            if not join:
                for m in t.w:
                    self._need(waits, m)
            for m in t.r.values():
                self._need(waits, m)
        self._emit_waits(E, waits, skip_own=True)
        ins = E.h.dma_start(out=out, in_=in_, **kw)
        S.dcnt += 16
        ins.then_inc(S.dsem, 16)
        mark = (S.dsem, S.dcnt)
        for t in R:
            t.r[id(mark[0])] = mark
        for t in W:
            if join:
                t.w = [m for m in t.w if m[0] is not S.dsem] + [mark]
            else:
                t.w = [mark]
            t.r = {}
        if not background:
            self.dmas.append(mark)
        return ins

    def release(self, trks):
        for S in trks:
            if S.dsem is not None:
                self.sem_free.append((S.dsem, S.dcnt))
                S.dsem = None
                S.w = []
                S.r = {}

    def gather(self, out, in_, idx, S, R=(), W=(), bounds=None):
        E = self.eng["pool"]
        self._dsem(S)
        waits = {}
        for t in R:
            for m in t.w:
                self._need(waits, m)
        for t in W:
            for m in t.w:
                self._need(waits, m)
            for m in t.r.values():
                self._need(waits, m)
        self._emit_waits(E, waits, skip_own=True)
        kw = {}
        if bounds is not None:
            kw = dict(bounds_check=bounds, oob_is_err=False)
        ins = E.h.indirect_dma_start(out=out, out_offset=None, in_=in_,
                                     in_offset=bass.IndirectOffsetOnAxis(ap=idx, axis=0), **kw)
        S.dcnt += 16
        ins.then_inc(S.dsem, 16)
        mark = (S.dsem, S.dcnt)
        for t in R:
            t.r[id(mark[0])] = mark
        for t in W:
            t.w = [mark]
            t.r = {}
        self.dmas.append(mark)
        return ins

    def collective(self, kind, groups, in_ap, out_ap, S, R=(), W=()):
        E = self.eng["pool"]
        self._dsem(S)
        waits = {}
        for t in R:
            for m in t.w:
                self._need(waits, m)
        for t in W:
            for m in t.w:
                self._need(waits, m)
            for m in t.r.values():
                self._need(waits, m)
        self._emit_waits(E, waits, skip_own=True)
        ins = E.h.collective_compute(kind, ALU.bypass, replica_groups=groups, ins=[in_ap.opt()], outs=[out_ap.opt()])
        S.dcnt += 1
        ins.then_inc(S.dsem, 1)
        mark = (S.dsem, S.dcnt)
        for t in R:
            t.r[id(mark[0])] = mark
        for t in W:
            t.w = [mark]
            t.r = {}
        self.dmas.append(mark)
        return ins

    def barrier(self):
        marks = []
        for F in self.eng.values():
            assert not F.pending, F.name
            if F.cnt:
                marks.append((F.sem, F.cnt))
        seen = {}
        for m in self.dmas:
            self._need(seen, m)
        marks += list(seen.values())
        self.dmas = []
        for E in self.eng.values():
            for sem, val in marks:
                if sem is E.sem:
                    continue
                if E.known.get(id(sem), 0) >= val:
                    continue
                E.h.wait_ge(sem, val)
                E.known[id(sem)] = val

    def mm(self, out, lhsT, rhs, start, stop, R=(), W=(), inc=None, **kw):
        if inc is None:
            inc = stop
        return self.op("pe", lambda: self.nc.tensor.matmul(out, lhsT, rhs, start=start, stop=stop, **kw),
                       R=R, W=W, inc=inc)

    def tr(self, out, in_, ident, R=(), W=(), inc=True):
        return self.op("pe", lambda: self.nc.tensor.transpose(out, in_, ident), R=R, W=W, inc=inc)

    def act(self, out, in_, func, R=(), W=(), **kw):
        return self.op("act", lambda: self.nc.scalar.activation(out=out, in_=in_, func=func, **kw), R=R, W=W)

    def v(self, en, name, *a, R=(), W=(), **kw):
        h = self.eng[en].h
        return self.op(en, lambda: getattr(h, name)(*a, **kw), R=R, W=W)


NC32 = 544


def make_consts():
    c = np.zeros((128, NC32), np.float32)
    c[:, 0:128] = np.eye(128)
    c[:, 128:256] = np.arange(128)[None, :]
    c[:, 256:384] = (np.arange(128)[:, None] < np.arange(128)[None, :])
    c[:, 384:512] = 1.0
    c[:, 512:544] = (np.arange(32) * 128)[None, :]
    return c


def load_consts(k, cst_d):
    c32 = k.sb("c32", [128, NC32], F32)
    tc = Trk("c32")
    k.dma("sp", c32[:], cst_d, tc, W=[tc])
    cbf = k.sb("cbf", [128, 512], BF16)
    tcb = Trk("cbf")
    k.v("dve", "tensor_copy", cbf[:], c32[:, 0:512], R=[tc], W=[tcb])
    return dict(ident=c32[:, 0:128], iota=c32[:, 128:256], ebase=c32[:, 512:544], ones=c32[:, 384:512],
                ident_bf=cbf[:, 0:128], tri_bf=cbf[:, 256:384], ones_bf=cbf[:, 384:512], t32=tc, tbf=tcb)


def pb(ap_row):
    return ap_row.partition_broadcast(128)


def emit_ln_tok(k, x_ap, tx, G, B, tgb, st, tst):
    nc = k.nc
    for i in range(4):
        k.v("dve", "bn_stats", st[:, i * 6:(i + 1) * 6], x_ap[:, i * 512:(i + 1) * 512], R=[tx, tst], W=[tst])
    k.v("dve", "bn_aggr", st[:, 24:26], st[:, 0:24], R=[tst], W=[tst])
    k.act(st[:, 26:27], st[:, 25:26], AF.Sqrt, bias=LN_EPS, scale=1.0, R=[tst], W=[tst])
    k.v("dve", "reciprocal", st[:, 27:28], st[:, 26:27], R=[tst], W=[tst])
    k.v("dve", "scalar_tensor_tensor", st[:, 28:29], st[:, 24:25], -1.0, st[:, 27:28], ALU.mult, ALU.mult,
        R=[tst], W=[tst])
    k.v("dve", "tensor_scalar", x_ap, x_ap, st[:, 27:28], st[:, 28:29], ALU.mult, ALU.add, R=[tx, tst], W=[tx])
    k.v("dve", "tensor_tensor", x_ap, x_ap, G, ALU.mult, R=[tx, tgb], W=[tx])
    k.v("pool", "tensor_tensor", x_ap, x_ap, B, ALU.add, R=[tx, tgb], W=[tx])


def emit_moe(k, C, h_tok, th, wg_d, bg_d, we_d, be_d, wgu_d, wdn_d, ybuf, lng_d, lnb_d, nexp=NEXP, wconv=None,
             tile_cb=None):
    nc = k.nc
    rankm = k.sb("rankm", [128, TT, 32], F32)
    t_rank = [Trk(f"rank{i}") for i in range(TT)]
    dest = k.sb("dest", [128, TT, 2], I32)
    wts = k.sb("wts", [128, TT, 2], F32)
    t_dw = [Trk(f"dw{i}") for i in range(TT)]
    h_bf = k.sb("h_bf", [128, TT, D], BF16)
    t_hbf = [Trk(f"hbf{i}") for i in range(TT)]

    with k.scope():
        Wr = k.sb("Wr", [128, CT, 36], F32)
        tWr = Trk("Wr")
        k.dma("sp", Wr[:, :, 0:4], wg_d.rearrange("(ct p) n -> p ct n", p=128), tWr, W=[tWr])
        k.dma("sp", Wr[:, :, 4:36], we_d.rearrange("(ct p) n -> p ct n", p=128), tWr, W=[tWr], join=True)
        rb = k.sb("rb", [128, 36], F32)
        trb = Trk("rb")
        k.dma("sp", rb[:, 0:4], pb(bg_d), trb, W=[trb])
        k.dma("sp", rb[:, 4:36], pb(be_d), trb, W=[trb], join=True)
        hTt = [k.sb("hTt", [128, CT, 128], F32) for _ in range(2)]
        thT = [[Trk(f"hTt{a}{b}") for b in range(4)] for a in range(2)]
        psA = [k.ps("psA", [128, 512]) for _ in range(2)]
        tpsA = [Trk("psA0"), Trk("psA1")]
        psL = k.ps("psL", [128, 512])
        tpsL = Trk("psL")
        psR = k.ps("psR", [128, 512])
        tpsR = Trk("psR")
        L = k.sb("L", [128, TT, 36], F32)
        sm = k.sb("sm", [128, TT, 16], F32)
        ohg = k.sb("ohg", [128, TT, 4], F32)
        r8 = k.sb("r8", [128, TT, 5, 8], F32)
        r32 = k.sb("r32", [128, TT, 5, 32], F32)
        d12 = k.sb("d12", [128, TT, 2], F32)
        M_bf = k.sb("M_bf", [128, TT, 32], BF16)
        tM = [Trk(f"M{i}") for i in range(TT)]
        ts = [Trk(f"rs{i}") for i in range(TT)]
        nq = 0
        for tt in range(TT):
            hb = tt % 2
            k.act(h_bf[:, tt, :], h_tok[:, tt, :], AF.Copy, R=[th[tt]], W=[t_hbf[tt]])
            for q in range(4):
                pa = nq % 2
                nq += 1
                for i in range(4):
                    ct = q * 4 + i
                    k.tr(psA[pa][:, i * 128:(i + 1) * 128], h_tok[:, tt, ct * 128:(ct + 1) * 128], C["ident"],
                         R=[th[tt], C["t32"]], W=[tpsA[pa]], inc=(i == 3))
                dst = hTt[hb][:, q * 4:(q + 1) * 4, :]
                if q % 2 == 0:
                    k.act(dst, psA[pa][:, :].rearrange("p (a b) -> p a b", a=4), AF.Copy, R=[tpsA[pa]], W=[thT[hb][q]])
                else:
                    k.v("dve", "tensor_copy", dst, psA[pa][:, :].rearrange("p (a b) -> p a b", a=4),
                        R=[tpsA[pa]], W=[thT[hb][q]])
            for ct in range(CT):
                k.mm(psL[:, 0:36], hTt[hb][:, ct, :], Wr[:, ct, :], ct == 0, ct == CT - 1,
                     R=[thT[hb][ct // 4], tWr], W=[tpsL])
            S = ts[tt]
            sc = lambda j: sm[:, tt, j:j + 1]
            Lt = L[:, tt, :]
            les, eq1, le2, eq2, e12 = (r8[:, tt, j, :] for j in range(5))
            Mf, E1, E2, rk, tmp = (r32[:, tt, j, :] for j in range(5))
            dv = lambda name, *a, R=(), W=(): k.v("dve", name, *a, R=[S] + list(R), W=[S] + list(W))
            dv("tensor_tensor", Lt, psL[:, 0:36], rb[:], ALU.add, R=[tpsL, trb])
            dv("reduce_max", sc(0), Lt[:, 0:4], AX.X)
            dv("tensor_scalar", ohg[:, tt, :], Lt[:, 0:4], sc(0), None, ALU.is_equal)
            dv("tensor_scalar", sc(1), sc(0), -1.0, None, ALU.mult)
            k.act(le2[:, 0:4], Lt[:, 0:4], AF.Exp,
                  bias=sc(1), scale=1.0, accum_out=sc(2), R=[S], W=[S])
            dv("reciprocal", sc(3), sc(2))
            dv("tensor_scalar", les, Lt[:, 4:12], ohg[:, tt, 0:1], None, ALU.mult)
            for g in range(1, 4):
                dv("scalar_tensor_tensor", les, Lt[:, 4 + 8 * g:12 + 8 * g], ohg[:, tt, g:g + 1], les,
                   ALU.mult, ALU.add)
            dv("reduce_max", sc(4), les, AX.X)
            dv("tensor_scalar", eq1, les, sc(4), None, ALU.is_equal)
            dv("scalar_tensor_tensor", le2, eq1, -1e30, les, ALU.mult, ALU.add)
            dv("reduce_max", sc(5), le2, AX.X)
            dv("tensor_scalar", eq2, le2, sc(5), None, ALU.is_equal)
            dv("tensor_tensor", sc(6), sc(5), sc(4), ALU.subtract)
            k.act(sc(7), sc(6), AF.Exp, R=[S], W=[S])
            dv("tensor_scalar", sc(8), sc(7), 1.0, None, ALU.add)
            dv("reciprocal", sc(9), sc(8))
            dv("tensor_tensor", wts[:, tt, 0:1], sc(9), sc(3), ALU.mult, W=[t_dw[tt]])
            dv("tensor_tensor", wts[:, tt, 1:2], sc(3), wts[:, tt, 0:1], ALU.subtract, W=[t_dw[tt]])
            dv("tensor_tensor", e12, eq1, eq2, ALU.add)
            for g in range(4):
                gs = slice(8 * g, 8 * g + 8)
                dv("tensor_scalar", Mf[:, gs], e12, ohg[:, tt, g:g + 1], None, ALU.mult)
                dv("tensor_scalar", E1[:, gs], eq1, ohg[:, tt, g:g + 1], None, ALU.mult)
                dv("tensor_scalar", E2[:, gs], eq2, ohg[:, tt, g:g + 1], None, ALU.mult)
            dv("tensor_copy", M_bf[:, tt, :], Mf, W=[tM[tt]])
            for t2 in range(tt + 1):
                lhsT = C["ones_bf"] if t2 < tt else C["tri_bf"]
                k.mm(psR[:, 0:32], lhsT, M_bf[:, t2, :], t2 == 0, t2 == tt, R=[tM[t2], C["tbf"]], W=[tpsR])
            dv("scalar_tensor_tensor", rankm[:, tt, :], psR[:, 0:32], 1.0, Mf, ALU.add, ALU.mult,
               R=[tpsR], W=[t_rank[tt]])
            dv("tensor_scalar", rankm[:, tt, :], rankm[:, tt, :], -1.0, None, ALU.add, W=[t_rank[tt]])
            dv("tensor_tensor", rk, psR[:, 0:32], C["ebase"], ALU.add, R=[tpsR, C["t32"]])
            for j, Ej in enumerate((E1, E2)):
                dv("tensor_tensor", tmp, rk, Ej, ALU.mult)
                dv("reduce_sum", d12[:, tt, j:j + 1], tmp, AX.X)
            dv("tensor_copy", dest[:, tt, :], d12[:, tt, :], W=[t_dw[tt]])

    with k.scope():
        NS = 4
        wsl = [k.sb("wsl", [128, 8192], BF16) for _ in range(NS)]
        twsl = [Trk(f"wsl{i}") for i in range(NS)]
        selq = k.sb("selq", [128, TT, 512], BF16)
        tsel = [Trk(f"selq{i}") for i in range(TT)]
        xq = k.sb("xq", [128, CT, 512], BF16)
        txq = [Trk(f"xq{i}") for i in range(CT)]
        sg = k.sb("sg", [128, 512], F32)
        tsg = Trk("sg")
        hT = k.sb("hT", [128, 512], BF16)
        thh = Trk("hT")
        ye = k.sb("ye", [128, D], BF16)
        tye = [Trk(f"ye{i}") for i in range(4)]
        psG = [k.ps("psG", [128, 512]) for _ in range(2)]
        tpsG = [Trk("psG0"), Trk("psG1")]
        psH1 = k.ps("psH1", [128, 512])
        tH1 = Trk("psH1")
        psH2 = k.ps("psH2", [128, 512])
        tH2 = Trk("psH2")
        psY = [k.ps("psY", [128, 512]) for _ in range(4)]
        tpsY = [Trk(f"psY{i}") for i in range(4)]
        tyb = Trk("ybuf")

        chunks = [(e, c) for e in range(nexp) for c in "gud"]

        def issue(ci):
            if ci >= len(chunks):
                return
            e, c = chunks[ci]
            s = ci % NS
            if wconv is not None and e < wconv.nconv:
                if c == "d":
                    k.dma("sp", wsl[s][:, :].rearrange("p (a n) -> p a n", a=4),
                          wconv.wdn_b[e].rearrange("(a p) n -> p a n", p=128), twsl[s], R=[wconv.trk[e]], W=[twsl[s]])
                else:
                    c0 = 0 if c == "g" else 512
                    k.dma("sp", wsl[s][:, :].rearrange("p (a n) -> p a n", a=16),
                          wconv.wgu_b[e].rearrange("(a p) n -> p a n", p=128)[:, :, c0:c0 + 512], twsl[s],
                          R=[wconv.trk[e]], W=[twsl[s]])
                return
            if c == "d":
                k.dma("pool", wsl[s][:, :].rearrange("p (a n) -> p a n", a=4),
                      wdn_d[e].rearrange("(a p) n -> p a n", p=128), twsl[s], W=[twsl[s]])
            else:
                c0 = 0 if c == "g" else 512
                k.dma("pool", wsl[s][:, :].rearrange("p (a n) -> p a n", a=16),
                      wgu_d[e].rearrange("(a p) n -> p a n", p=128)[:, :, c0:c0 + 512], twsl[s], W=[twsl[s]])

        for ci in range(NS - 1):
            issue(ci)
        ci = 0
        nev = 0
        for q0 in range(0, nexp, 4):
            for tt in range(TT):
                for j in range(4):
                    e = q0 + j
                    k.v("dve", "tensor_scalar", selq[:, tt, j * 128:(j + 1) * 128], C["iota"],
                        rankm[:, tt, e:e + 1], None, ALU.is_equal, R=[t_rank[tt], C["t32"]], W=[tsel[tt]])
            for ct in range(CT):
                pg = ct % 2
                for tt in range(TT):
                    k.mm(psG[pg][:, :], h_bf[:, tt, ct * 128:(ct + 1) * 128], selq[:, tt, :], tt == 0, tt == TT - 1,
                         R=[t_hbf[tt], tsel[tt]], W=[tpsG[pg]])
                if ct % 2 == 0:
                    k.act(xq[:, ct, :], psG[pg][:, :], AF.Copy, R=[tpsG[pg]], W=[txq[ct]])
                else:
                    k.v("dve", "tensor_copy", xq[:, ct, :], psG[pg][:, :], R=[tpsG[pg]], W=[txq[ct]])
            for j in range(4):
                e = q0 + j
                for half, (psH, tH) in enumerate(((psH1, tH1), (psH2, tH2))):
                    s = ci % NS
                    issue(ci + NS - 1)
                    wv = wsl[s][:, :].rearrange("p (a n) -> p a n", a=16)
                    for ft in range(4):
                        for ct in range(CT):
                            k.mm(psH[:, ft * 128:(ft + 1) * 128], wv[:, ct, ft * 128:(ft + 1) * 128],
                                 xq[:, ct, j * 128:(j + 1) * 128], ct == 0, ct == CT - 1,
                                 R=[twsl[s], txq[ct]], W=[tH])
                    ci += 1
                k.act(sg[:], psH1[:, :], AF.Silu, R=[tH1], W=[tsg])
                k.v("dve", "tensor_tensor", hT[:], sg[:], psH2[:, :], ALU.mult, R=[tsg, tH2], W=[thh])
                s = ci % NS
                issue(ci + NS - 1)
                wv = wsl[s][:, :].rearrange("p (a n) -> p a n", a=4)
                for cc in range(4):
                    for ft in range(4):
                        k.mm(psY[cc][:, :], hT[:, ft * 128:(ft + 1) * 128], wv[:, ft, cc * 512:(cc + 1) * 512],
                             ft == 0, ft == 3, R=[thh, twsl[s]], W=[tpsY[cc]])
                    if nev % 2 == 0:
                        k.act(ye[:, cc * 512:(cc + 1) * 512], psY[cc][:, :], AF.Copy, R=[tpsY[cc]], W=[tye[cc]])
                    else:
                        k.v("dve", "tensor_copy", ye[:, cc * 512:(cc + 1) * 512], psY[cc][:, :],
                            R=[tpsY[cc]], W=[tye[cc]])
                    nev += 1
                ci += 1
                k.dma("sp", ybuf[e * 128:(e + 1) * 128, :], ye[:], tyb, R=tye, W=[tyb], join=True)

    with k.scope():
        G = k.sb("lnG", [128, D], F32)
        B = k.sb("lnB", [128, D], F32)
        tgb = Trk("lngb")
        k.dma("sp", G[:], pb(lng_d), tgb, W=[tgb])
        k.dma("sp", B[:], pb(lnb_d), tgb, W=[tgb], join=True)
        Y = [[k.sb("Yg", [128, D], BF16) for _ in range(2)] for _ in range(2)]
        tY = [[Trk(f"Y{a}{b}") for b in range(2)] for a in range(2)]
        st = k.sb("lnst", [128, TT, 40], F32)
        tst = [Trk(f"lnst{i}") for i in range(TT)]
        def gat(tt):
            p = tt % 2
            for j in range(2):
                k.gather(Y[p][j][:], ybuf[:, :], dest[:, tt, j:j + 1], tY[p][j], R=[tyb, t_dw[tt]], W=[tY[p][j]])

        cb_ctx = tile_cb[0]() if tile_cb is not None else None
        gat(0)
        for tt in range(TT):
            p = tt % 2
            if tt + 1 < TT:
                gat(tt + 1)
            x_ap = h_tok[:, tt, :]
            k.op("act", lambda: nc.scalar.mul(x_ap, x_ap, ALPHA), R=[th[tt]], W=[th[tt]])
            k.v("dve", "scalar_tensor_tensor", x_ap, Y[p][0][:], wts[:, tt, 0:1], x_ap, ALU.mult, ALU.add,
                R=[tY[p][0], t_dw[tt], th[tt]], W=[th[tt]])
            k.v("dve", "scalar_tensor_tensor", x_ap, Y[p][1][:], wts[:, tt, 1:2], x_ap, ALU.mult, ALU.add,
                R=[tY[p][1], t_dw[tt], th[tt]], W=[th[tt]])
            emit_ln_tok(k, x_ap, th[tt], G[:], B[:], tgb, st[:, tt, :], tst[tt])
            if tile_cb is not None:
                tile_cb[1](cb_ctx, tt)


def emit_proj_res_ln(k, aT, taT, W_d, h_tok, th, lng_d, lnb_d, tts=None):
    nc = k.nc
    if tts is None:
        tts = list(range(TT))
    with k.scope():
        Wc = [k.sb("Wc", [128, CT, 512], BF16) for _ in range(4)]
        tWc = [Trk(f"Wc{i}") for i in range(4)]
        G = k.sb("lnG", [128, D], F32)
        B = k.sb("lnB", [128, D], F32)
        tgb = Trk("lngb")
        k.dma("sp", G[:], pb(lng_d), tgb, W=[tgb])
        k.dma("sp", B[:], pb(lnb_d), tgb, W=[tgb], join=True)
        st = k.sb("lnst", [128, TT, 40], F32)
        tst = [Trk(f"lnst{i}") for i in range(TT)]
        ps = [k.ps("psP", [128, 512]) for _ in range(4)]
        tps = [Trk(f"psP{i}") for i in range(4)]
        Wv = W_d.rearrange("(ct p) n -> p ct n", p=128)
        for cc in range(4):
            k.dma("pool", Wc[cc][:], Wv[:, :, cc * 512:(cc + 1) * 512], tWc[cc], W=[tWc[cc]])
        n = 0
        for i, tt in enumerate(tts):
            for cc in range(4):
                p = n % 4
                n += 1
                for ct in range(CT):
                    k.mm(ps[p][:, :], aT[:, ct, i * 128:(i + 1) * 128], Wc[cc][:, ct, :], ct == 0, ct == CT - 1,
                         R=[taT, tWc[cc]], W=[tps[p]])
                dst = h_tok[:, tt, cc * 512:(cc + 1) * 512]
                k.v("dve", "scalar_tensor_tensor", dst, dst, ALPHA, ps[p][:, :], ALU.mult, ALU.add,
                    R=[tps[p], th[tt]], W=[th[tt]])
            emit_ln_tok(k, h_tok[:, tt, :], th[tt], G[:], B[:], tgb, st[:, tt, :], tst[tt])


def load_tok(k, h_tok, th, src_d):
    hv = src_d.rearrange("(tt p) c -> p tt c", p=128)
    for tt in range(TT):
        k.dma("sp", h_tok[:, tt, :], hv[:, tt, :], th[tt], W=[th[tt]])


def store_tok(k, h_tok, th, dst_d, to):
    ov = dst_d.rearrange("(tt p) c -> p tt c", p=128)
    for tt in range(TT):
        k.dma("sp", ov[:, tt, :], h_tok[:, tt, :], to, R=[th[tt]], W=[to], join=True)


def moe_dram(nc, sfx=""):
    d = {}
    d["wg"] = nc.dram_tensor("moe_wg" + sfx, [D, 4], F32, kind="ExternalInput").ap()
    d["bg"] = nc.dram_tensor("moe_bg" + sfx, [1, 4], F32, kind="ExternalInput").ap()
    d["we"] = nc.dram_tensor("moe_we" + sfx, [D, 32], F32, kind="ExternalInput").ap()
    d["be"] = nc.dram_tensor("moe_be" + sfx, [1, 32], F32, kind="ExternalInput").ap()
    d["wgu"] = nc.dram_tensor("moe_wgu" + sfx, [NEXP, D, 2 * DFF], F32, kind="ExternalInput").ap()
    d["wdn"] = nc.dram_tensor("moe_wdn" + sfx, [NEXP, DFF, D], F32, kind="ExternalInput").ap()
    d["ln"] = nc.dram_tensor("ln_gb" + sfx, [4, D], F32, kind="ExternalInput").ap()
    d["ybuf"] = nc.dram_tensor("ybuf" + sfx, [NEXP * 128, D], BF16).ap()
    return d


def moe_inputs(inp, l, sfx=""):
    return {
        "moe_wg" + sfx: np.ascontiguousarray(inp["moe_wg"][l]),
        "moe_bg" + sfx: np.ascontiguousarray(inp["moe_bg"][l][None]),
        "moe_we" + sfx: np.ascontiguousarray(inp["moe_we"][l]),
        "moe_be" + sfx: np.ascontiguousarray(inp["moe_be"][l][None]),
        "moe_wgu" + sfx: np.ascontiguousarray(inp["moe_w_gu"][l]),
        "moe_wdn" + sfx: np.ascontiguousarray(inp["moe_w_down"][l]),
        "ln_gb" + sfx: np.ascontiguousarray(np.stack([inp["ln_g"][l, 0], inp["ln_b"][l, 0], inp["ln_g"][l, 1], inp["ln_b"][l, 1]])),
    }


class WConv:
    def __init__(self, k, d, sfx, nconv):
        nc = k.nc
        self.k, self.d, self.nconv = k, d, nconv
        self.wgu_b = nc.dram_tensor("wgu_b" + sfx, [max(nconv, 1), D, 2 * DFF], BF16).ap()
        self.wdn_b = nc.dram_tensor("wdn_b" + sfx, [max(nconv, 1), DFF, D], BF16).ap()
        self.trk = [Trk(f"wcv{sfx}_{e}") for e in range(nconv)]
        self.jobs = [(e, c) for e in range(nconv) for c in ("gu", "dn")]
        self.nxt = 0

    def pump(self, n=1):
        k = self.k
        for _ in range(n):
            if self.nxt >= len(self.jobs):
                return
            e, c = self.jobs[self.nxt]
            self.nxt += 1
            if c == "gu":
                k.dma("pool", self.wgu_b[e].rearrange("(a p) n -> p a n", p=128),
                      self.d["wgu"][e].rearrange("(a p) n -> p a n", p=128), self.trk[e], W=[self.trk[e]], join=True,
                      background=True)
            else:
                k.dma("pool", self.wdn_b[e].rearrange("(a p) n -> p a n", p=128),
                      self.d["wdn"][e].rearrange("(a p) n -> p a n", p=128), self.trk[e], W=[self.trk[e]], join=True,
                      background=True)

    def flush(self):
        self.pump(len(self.jobs))


def emit_moe_d(k, C, h_tok, th, d, wconv=None, tile_cb=None):
    emit_moe(k, C, h_tok, th, d["wg"], d["bg"], d["we"], d["be"], d["wgu"], d["wdn"], d["ybuf"],
             d["ln"][2:3, :], d["ln"][3:4, :], wconv=wconv, tile_cb=tile_cb)


def emit_moe_d_old(k, C, h_tok, th, d):
    emit_moe(k, C, h_tok, th, d["wg"], d["bg"], d["we"], d["be"], d["wgu"], d["wdn"], d["ybuf"],
             d["ln"][2:3, :], d["ln"][3:4, :])


def build_p3():
    nc = bass.Bass("TRN2", target_bir_lowering=False)
    k = K(nc)
    cst_d = nc.dram_tensor("cst", [128, NC32], F32, kind="ExternalInput").ap()
    h_d = nc.dram_tensor("h_in", [NT, D], F32, kind="ExternalInput").ap()
    oT_d = nc.dram_tensor("oT", [D, NT], F32, kind="ExternalInput").ap()
    wo_d = nc.dram_tensor("w_o", [D, D], F32, kind="ExternalInput").ap()
    md = moe_dram(nc)
    out_d = nc.dram_tensor("out", [NT, D], F32, kind="ExternalOutput").ap()
    C = load_consts(k, cst_d)
    h_tok = k.sb("h_tok", [128, TT, D], F32)
    th = [Trk(f"h{i}") for i in range(TT)]
    load_tok(k, h_tok, th, h_d)
    with k.scope():
        aT = k.sb("aT", [128, CT, NT], BF16)
        taT = Trk("aT")
        k.dma("pool", aT[:], oT_d.rearrange("(ct p) t -> p ct t", p=128), taT, W=[taT])
        emit_proj_res_ln(k, aT, taT, wo_d, h_tok, th, md["ln"][0:1, :], md["ln"][1:2, :])
    emit_moe_d(k, C, h_tok, th, md)
    to = Trk("out")
    store_tok(k, h_tok, th, out_d, to)
    k.barrier()
    k.close()
    return nc


HALO = 30
NXC = NT + HALO + 2
NCPAR = 16 * 31 + 16 * 3 + 32


def conv_params(inp):
    wdw = inp["conv_w_dw"][0]
    a = wdw.T.reshape(CT, 128, 31).transpose(1, 0, 2).reshape(128, CT * 31)
    cols = [a]
    for v in (inp["conv_b_dw"][0], inp["conv_ln_g"][0], inp["conv_ln_b"][0]):
        cols.append(v.reshape(CT, 128).T)
    cols.append(inp["conv_b_pw1"][0].reshape(32, 128).T)
    return np.ascontiguousarray(np.concatenate(cols, axis=1).astype(np.float32))


def emit_conv(k, C, xT_d, hm_d, cpar_d, w1_d, w2_d, h_tok, th, lng_d, lnb_d, dbg=None, bg=None):
    nc = k.nc
    with k.scope():
        cp = k.sb("cpar", [128, NCPAR], F32)
        tcp = Trk("cpar")
        k.dma("sp", cp[:], cpar_d, tcp, W=[tcp])
        wdw = cp[:, 0:CT * 31].rearrange("p (j t) -> p j t", j=CT)
        bdw = cp[:, 496:512]
        cg = cp[:, 512:528]
        cb = cp[:, 528:544]
        b1 = cp[:, 544:576]
        hm = k.sb("hm", [128, 1], F32)
        thm = Trk("hm")
        k.dma("sp", hm[:], pb(hm_d), thm, W=[thm])
        gluT = k.sb("gluT", [128, CT, NXC], BF16)
        tglu = [Trk(f"glu{j}") for j in range(CT)]
        with k.scope():
            xT = k.sb("xT", [128, CT, NXC], BF16)
            txT = Trk("xT")
            k.dma("pool", xT[:, :, 0:NT + HALO], xT_d.rearrange("(ct p) t -> p ct t", p=128), txT, W=[txT])
            NS = 3
            wsl = [k.sb("w1s", [128, CT, 512], BF16) for _ in range(NS)]
            twsl = [Trk(f"w1s{i}") for i in range(NS)]
            sig = [k.sb("sig", [128, 512], F32) for _ in range(2)]
            tsig = [Trk("sig0"), Trk("sig1")]
            psA = [k.ps("psA", [128, 512]) for _ in range(2)]
            tpA = [Trk("pA0"), Trk("pA1")]
            psB = [k.ps("psB", [128, 512]) for _ in range(2)]
            tpB = [Trk("pB0"), Trk("pB1")]
            W1v = w1_d.rearrange("(ct p) n -> p ct n", p=128)
            loads = []
            for jg in range(4):
                loads.append(jg * 512)
                loads.append(D + jg * 512)

            nxt = [0]

            def pump(done):
                while nxt[0] < len(loads) and (nxt[0] < NS or (nxt[0] - NS) // 2 < done):
                    i = nxt[0]
                    s_ = i % NS
                    k.dma("pool", wsl[s_][:], W1v[:, :, loads[i]:loads[i] + 512], twsl[s_], W=[twsl[s_]])
                    nxt[0] += 1

            n = 0
            for jg in range(4):
                pump(jg)
                assert nxt[0] >= 2 * jg + 2
                sa, sg_ = (2 * jg) % NS, (2 * jg + 1) % NS
                for jj in range(4):
                    j = jg * 4 + jj
                    for c0, c1 in ((0, 512), (512, 1024), (1024, NT + HALO)):
                        w = c1 - c0
                        p = n % 2
                        n += 1
                        for ct in range(CT):
                            k.mm(psA[p][:, 0:w], wsl[sa][:, ct, jj * 128:(jj + 1) * 128], xT[:, ct, c0:c1],
                                 ct == 0, ct == CT - 1, R=[twsl[sa], txT], W=[tpA[p]])
                        for ct in range(CT):
                            k.mm(psB[p][:, 0:w], wsl[sg_][:, ct, jj * 128:(jj + 1) * 128], xT[:, ct, c0:c1],
                                 ct == 0, ct == CT - 1, R=[twsl[sg_], txT], W=[tpB[p]])
                        k.act(sig[p][:, 0:w], psB[p][:, 0:w], AF.Sigmoid, bias=b1[:, 16 + j:17 + j], scale=1.0,
                              R=[tpB[p], tcp], W=[tsig[p]])
                        k.v("dve", "scalar_tensor_tensor", gluT[:, j, c0:c1], psA[p][:, 0:w], b1[:, j:j + 1],
                            sig[p][:, 0:w], ALU.add, ALU.mult, R=[tpA[p], tsig[p], tcp], W=[tglu[j]])
                    if bg is not None:
                        bg("pw1")
            for j in range(CT):
                k.v("dve", "tensor_scalar", gluT[:, j, 0:HALO], gluT[:, j, 0:HALO], hm[:, 0:1], None, ALU.mult,
                    R=[thm], W=[tglu[j]])
        if dbg is not None:
            tdb = Trk("dbg")
            k.dma("sp", dbg["glu"].rearrange("(j p) t -> p j t", p=128), gluT[:], tdb, R=tglu, W=[tdb], join=True)
        for tc in range(2):
            with k.scope():
                sT = k.sb("sT", [128, CT, 512], BF16)
                tsT = Trk("sT")
                with k.scope():
                    v = k.sb("cv", [128, CT, 512], F32)
                    tv = [Trk(f"cv{j}") for j in range(CT)]
                    dg = [k.sb("dg", [128, 31, 128], BF16) for _ in range(2)]
                    tdg = [Trk("dg0"), Trk("dg1")]
                    vsq = [k.sb("vsq", [128, 512], F32) for _ in range(2)]
                    tvsq = [Trk("vsq0"), Trk("vsq1")]
                    psC = [k.ps("psC", [128, 512]) for _ in range(2)]
                    tpC = [Trk("pC0"), Trk("pC1")]
                    psS = k.ps("psS", [128, 512])
                    tpS = Trk("pS")
                    psQ = k.ps("psQ", [128, 512])
                    tpQ = Trk("pQ")
                    for j in range(CT):
                        p = j % 2
                        for t in range(31):
                            k.v("dve", "tensor_scalar", dg[p][:, t, :], C["ident_bf"],
                                wdw[:, j, t:t + 1], None, ALU.mult, R=[C["tbf"], tcp], W=[tdg[p]])
                        for t in range(31):
                            k.mm(psC[p][:, :], dg[p][:, t, :], gluT[:, j, tc * 512 + t:tc * 512 + t + 512], t == 0, t == 30,
                                 R=[tdg[p], tglu[j]], W=[tpC[p]])
                        k.act(v[:, j, :], psC[p][:, :], AF.Identity, bias=bdw[:, j:j + 1], scale=1.0,
                              R=[tpC[p], tcp], W=[tv[j]])
                        k.act(vsq[p][:], v[:, j, :], AF.Square, R=[tv[j]], W=[tvsq[p]])
                        k.mm(psS[:, :], C["ones"], v[:, j, :], j == 0, j == CT - 1, R=[C["t32"], tv[j]], W=[tpS])
                        k.mm(psQ[:, :], C["ones"], vsq[p][:], j == 0, j == CT - 1, R=[C["t32"], tvsq[p]], W=[tpQ])
                        if bg is not None:
                            bg("conv")
                    if dbg is not None:
                        k.dma("sp", dbg["v"][tc].rearrange("(j p) t -> p j t", p=128), v[:], tdb, R=tv, W=[tdb], join=True)
                    mu = k.sb("mu", [128, 512], F32)
                    rs = k.sb("rs", [128, 512], F32)
                    m2 = k.sb("m2", [128, 512], F32)
                    tm = Trk("murs")
                    k.v("dve", "tensor_scalar", mu[:], psS[:, :], 1.0 / D, None, ALU.mult, R=[tpS], W=[tm])
                    k.v("dve", "tensor_tensor", m2[:], mu[:], mu[:], ALU.mult, R=[tm], W=[tm])
                    k.v("dve", "scalar_tensor_tensor", m2[:], psQ[:, :], 1.0 / D, m2[:], ALU.mult, ALU.subtract,
                        R=[tpQ, tm], W=[tm])
                    k.act(m2[:], m2[:], AF.Sqrt, bias=LN_EPS, scale=1.0, R=[tm], W=[tm])
                    k.v("dve", "reciprocal", rs[:], m2[:], R=[tm], W=[tm])
                    for j in range(CT):
                        k.v("dve", "tensor_tensor", v[:, j, :], v[:, j, :], mu[:], ALU.subtract, R=[tm, tv[j]], W=[tv[j]])
                        k.v("dve", "tensor_tensor", v[:, j, :], v[:, j, :], rs[:], ALU.mult, R=[tm, tv[j]], W=[tv[j]])
                        k.act(sT[:, j, :], v[:, j, :], AF.Silu, bias=cb[:, j:j + 1], scale=cg[:, j:j + 1],
                              R=[tv[j], tcp], W=[tsT])
                if dbg is not None:
                    k.dma("sp", dbg["s"][tc].rearrange("(j p) t -> p j t", p=128), sT[:], tdb, R=[tsT], W=[tdb], join=True)
                emit_proj_res_ln(k, sT, tsT, w2_d, h_tok, th, lng_d, lnb_d, tts=[tc * 4 + i for i in range(4)])


def build_p1():
    nc = bass.Bass("TRN2", target_bir_lowering=False)
    k = K(nc)
    cst_d = nc.dram_tensor("cst", [128, NC32], F32, kind="ExternalInput").ap()
    x_d = nc.dram_tensor("x_tok", [NT, D], F32, kind="ExternalInput").ap()
    xT_d = nc.dram_tensor("xT", [D, NT + HALO], F32, kind="ExternalInput").ap()
    hm_d = nc.dram_tensor("hm", [1, 1], F32, kind="ExternalInput").ap()
    cpar_d = nc.dram_tensor("cpar", [128, NCPAR], F32, kind="ExternalInput").ap()
    w1_d = nc.dram_tensor("w_pw1", [D, 2 * D], F32, kind="ExternalInput").ap()
    w2_d = nc.dram_tensor("w_pw2", [D, D], F32, kind="ExternalInput").ap()
    md = moe_dram(nc)
    out_d = nc.dram_tensor("out", [NT, D], F32, kind="ExternalOutput").ap()
    C = load_consts(k, cst_d)
    h_tok = k.sb("h_tok", [128, TT, D], F32)
    th = [Trk(f"h{i}") for i in range(TT)]
    load_tok(k, h_tok, th, x_d)
    emit_conv(k, C, xT_d, hm_d, cpar_d, w1_d, w2_d, h_tok, th, md["ln"][0:1, :], md["ln"][1:2, :])
    emit_moe_d(k, C, h_tok, th, md)
    to = Trk("out")
    store_tok(k, h_tok, th, out_d, to)
    k.barrier()
    k.close()
    return nc


def p1_inputs(inp, c):
    b, j = divmod(c, 4)
    x = inp["x"][b]
    s0 = j * NT
    xe = np.zeros((NT + HALO, D), np.float32)
    if j > 0:
        xe[:] = x[s0 - HALO:s0 + NT]
    else:
        xe[HALO:] = x[0:NT]
    d = dict(cst=make_consts(), x_tok=np.ascontiguousarray(x[s0:s0 + NT]), xT=np.ascontiguousarray(xe.T),
             hm=np.full((1, 1), 0.0 if j == 0 else 1.0, np.float32), cpar=conv_params(inp),
             w_pw1=np.ascontiguousarray(inp["conv_w_pw1"][0]), w_pw2=np.ascontiguousarray(inp["conv_w_pw2"][0]))
    d.update(moe_inputs(inp, 0))
    return d


NQB = SEQ // 128
NCMP = 255
QSCALE = 128.0 ** -0.5
NCM = 33


def _split3(v):
    import ml_dtypes
    v = v.astype(np.float32)
    a = v.astype(ml_dtypes.bfloat16).astype(np.float32)
    b = (v - a).astype(ml_dtypes.bfloat16).astype(np.float32)
    c = (v - a - b).astype(ml_dtypes.bfloat16).astype(np.float32)
    return a, b, c


def attn_consts(g):
    slopes = np.array([2.0 ** (-8.0 * (4 * g + hh + 1) / 16.0) for hh in range(4)], np.float64)
    kr = np.arange(128)
    LT = np.zeros((128, 32 * 128), np.float32)
    for d in range(32):
        for hh in range(4):
            val = slopes[hh] * (128.0 * (-d) + kr - 64.0)
            for s_, part in enumerate(_split3(val)):
                LT[hh * 3 + s_, d * 128:(d + 1) * 128] = part
    LTc = np.zeros((128, 32 * 128), np.float32)
    for dc in range(32):
        for hh in range(4):
            val = slopes[hh] * (16.0 * kr - 33.0 - 128.0 * dc)
            for s_, part in enumerate(_split3(val)):
                LTc[hh * 3 + s_, dc * 128:(dc + 1) * 128] = part
    HI = np.zeros((128, 512), np.float32)
    for hh in range(4):
        HI[hh * 3:hh * 3 + 3, hh * 128:(hh + 1) * 128] = 1.0
    E = np.zeros((128, SEQ), np.float32)
    for d in range(32):
        for half in range(2):
            r = 62 - 2 * d + half
            E[r, d * 128 + half * 64:d * 128 + half * 64 + 64] = 1.0
    E[64:76, :] = LT[0:12, :]
    qr = np.arange(128)
    Mc = np.where(kr[:, None] <= qr[None, :], 0.0, NEG).astype(np.float32)
    Mw = np.where(kr[:, None] > qr[None, :], 0.0, NEG).astype(np.float32)
    Mc4 = np.tile(Mc, (1, 4))
    Mw4 = np.tile(Mw, (1, 4))
    CM = np.zeros((128, NCM * 128), np.float32)
    for di in range(17):
        m = np.where(16 * kr[:, None] + 31 <= 128 * di + qr[None, :], 0.0, NEG)
        CM[:, di * 128:(di + 1) * 128] = m
    for di in range(16):
        m = np.where(16 * kr[:, None] + 31 <= 128 * di + qr[None, :], 0.0, NEG)
        m[127, :] = NEG
        CM[:, (17 + di) * 128:(18 + di) * 128] = m
    n = np.arange(256)[:, None]
    j = np.arange(64)[None, :]
    ov = np.maximum(np.minimum(16 * n + 32, 64 * j + 64) - np.maximum(16 * n, 64 * j), 0) / 32.0
    ov[255] = 0
    ovl = ov.reshape(2, 128, 64).transpose(1, 0, 2).reshape(128, 128)
    ident = np.eye(128, dtype=np.float32)
    t = np.arange(SEQ)
    cur = (t // 64)[:, None]
    blk = np.arange(64)[None, :]
    forced = ((blk == 0) | (blk == cur) | (blk == cur - 1)).astype(np.float32)
    cand = ((blk >= 1) & (blk <= cur - 2)).astype(np.float32)
    f3 = forced.reshape(32, 128, 64).transpose(1, 0, 2).reshape(128, 32 * 64)
    c3 = cand.reshape(32, 128, 64).transpose(1, 0, 2).reshape(128, 32 * 64)
    cb = np.concatenate([LT, LTc, HI, E, Mc4, Mw4, CM, ovl, ident, c3, f3], axis=1).astype(np.float32)
    cf = ident.copy()
    return np.ascontiguousarray(cb), np.ascontiguousarray(cf)


CB_OFF = {}
_o = 0
for _n, _w in (("LT", 32 * 128), ("LTc", 32 * 128), ("HI", 512), ("E", SEQ), ("Mc4", 512), ("Mw4", 512),
               ("CM", NCM * 128), ("ov", 128), ("ident", 128), ("cand", 2048), ("forced", 2048)):
    CB_OFF[_n] = (_o, _o + _w)
    _o += _w
NCB = _o
NCF = 128
NWF = 1024
NWT = 268


def attn_weights(inp, g):
    wqg = inp["nsa_w_qg"][0]
    kvw = inp["kv_w"]
    sl = lambda s_: kvw[:, s_ * 512 + g * 128: s_ * 512 + (g + 1) * 128]
    WF = np.concatenate([wqg[:, g * 512:(g + 1) * 512], sl(2), sl(4), sl(0), sl(1)], axis=1)
    WT = np.concatenate([sl(3), sl(5), wqg[:, 2048 + g * 12: 2048 + (g + 1) * 12]], axis=1)
    posT = np.concatenate([inp["cmp_pos"][0].T, inp["cmp_pos"][1].T], axis=1)
    return dict(WF=np.ascontiguousarray(WF), WT=np.ascontiguousarray(WT), posT=np.ascontiguousarray(posT),
                cw1=np.ascontiguousarray(inp["cmp_w1"]), cw2=np.ascontiguousarray(inp["cmp_w2"]))


def emit_attn(k, hT_d, WF_d, WT_d, posT_d, cw1_d, cw2_d, cb_d, cf_d, o_d, dbg=None, hsrc=None, ob=None, tob=None,
              chunk_order=None, after_qb=None, bg=None):
    nc = k.nc
    cbs = k.sb("cbs", [128, NCB], BF16)
    tcb = Trk("cbs")
    k.dma("pool", cbs[:], cb_d, tcb, W=[tcb])
    cfs = k.sb("cfs", [128, NCF], F32)
    tcf = Trk("cfs")
    k.dma("sp", cfs[:], cf_d, tcf, W=[tcf])
    cbv = lambda n_: cbs[:, CB_OFF[n_][0]:CB_OFF[n_][1]]
    LT, LTc, HI, E_, Mc4, Mw4, CM, ovl, identb = (cbv(n_) for n_ in ("LT", "LTc", "HI", "E", "Mc4", "Mw4", "CM", "ov", "ident"))
    cand = cbv("cand").rearrange("p (a b) -> p a b", a=32)
    forced = cbv("forced").rearrange("p (a b) -> p a b", a=32)
    identf = cfs[:, 0:128]
    QT = k.sb("QT", [128, NQB, 4, 128], BF16)
    tQ = [Trk(f"Q{i}") for i in range(16)]
    KsT = k.sb("KsT", [128, SEQ], BF16)
    KwT = k.sb("KwT", [128, SEQ], BF16)
    tK = [Trk(f"K{i}") for i in range(16)]
    Vs = k.sb("Vs", [128, NQB, 132], BF16)
    Vw = k.sb("Vw", [128, NQB, 132], BF16)
    tV = [Trk(f"V{i}") for i in range(16)]
    gat = k.sb("gat", [128, NQB, 12], F32)
    KcT = k.sb("KcT", [128, 256], BF16)
    Vc = k.sb("Vc", [128, 2, 196], BF16)
    tC = Trk("cmpkv")
    k.v("dve", "memset", Vs[:, :, 128:132], 1.0, W=tV)
    k.v("dve", "memset", Vw[:, :, 128:132], 1.0, W=tV)
    k.v("dve", "memset", Vc[:], 0.0, W=[tC])
    k.v("dve", "memset", KcT[:], 0.0, W=[tC])

    with k.scope():
        rawT = [k.sb("rawT", [128, SEQ], BF16) for _ in range(2)]
        traw = [Trk(f"raw{i}") for i in range(16)]
        with k.scope():
            WF = k.sb("WF", [128, CT, NWF], BF16)
            tWF = Trk("WF")
            WFv = WF_d.rearrange("(ct p) n -> p ct n", p=128)
            k.dma("pool", WF[:, :, 0:512], WFv[:, :, 0:512], tWF, W=[tWF])
            k.dma("pool", WF[:, :, 512:1024], WFv[:, :, 512:1024], tWF, W=[tWF], join=True)
            WT = k.sb("WT", [128, CT, NWT], BF16)
            tWT = Trk("WT")
            k.dma("pool", WT[:], WT_d.rearrange("(ct p) n -> p ct n", p=128), tWT, W=[tWT])
            hc = [k.sb("hc", [128, CT, 256], BF16) for _ in range(2)]
            thc = [Trk("hc0"), Trk("hc1")]
            psF = [k.ps("psF", [128, 512]) for _ in range(3)]
            tpF = [Trk(f"pF{i}") for i in range(3)]
            psT = [k.ps("psT", [128, 512]) for _ in range(2)]
            tpT = [Trk(f"pT{i}") for i in range(2)]
            hv = hT_d.rearrange("(ct p) t -> p ct t", p=128) if hT_d is not None else None

            def issue(c):
                if c < 16:
                    if hsrc is None:
                        k.dma("pool", hc[c % 2][:], hv[:, :, c * 256:(c + 1) * 256], thc[c % 2], W=[thc[c % 2]])
                    else:
                        ap_, Rr = hsrc(c)
                        k.dma("sp", hc[c % 2][:], ap_, thc[c % 2], R=Rr, W=[thc[c % 2]])

            order = list(range(16)) if chunk_order is None else list(chunk_order)

            def issue(i):
                if i < 16:
                    c_ = order[i]
                    if hsrc is None:
                        k.dma("pool", hc[i % 2][:], hv[:, :, c_ * 256:(c_ + 1) * 256], thc[i % 2], W=[thc[i % 2]])
                    else:
                        ap_, Rr = hsrc(c_)
                        k.dma("sp", hc[i % 2][:], ap_, thc[i % 2], R=Rr, W=[thc[i % 2]])

            issue(0)
            nf = 0
            ntk = 0
            for ci_, c in enumerate(order):
                issue(ci_ + 1)
                H = hc[ci_ % 2]
                tH = thc[ci_ % 2]
                for f in range(8):
                    p = nf % 3
                    nf += 1
                    for ct in range(CT):
                        k.mm(psF[p][:, 0:256], WF[:, ct, f * 128:(f + 1) * 128], H[:, ct, :], ct == 0, ct == CT - 1,
                             R=[tWF, tH], W=[tpF[p]])
                    if f < 4:
                        k.act(QT[:, 2 * c:2 * c + 2, f, :], psF[p][:, 0:256].rearrange("p (a b) -> p a b", a=2), AF.Copy,
                              scale=QSCALE, R=[tpF[p]], W=[tQ[c]])
                    else:
                        dst = (KsT, KwT, rawT[0], rawT[1])[f - 4]
                        trk = (tK[c], tK[c], traw[c], traw[c])[f - 4]
                        if f % 2 == 0:
                            k.v("dve", "tensor_copy", dst[:, c * 256:(c + 1) * 256], psF[p][:, 0:256], R=[tpF[p]], W=[trk])
                        else:
                            k.act(dst[:, c * 256:(c + 1) * 256], psF[p][:, 0:256], AF.Copy, R=[tpF[p]], W=[trk])
                for i in range(2):
                    tt = c * 2 + i
                    p = ntk % 2
                    ntk += 1
                    for ct in range(CT):
                        k.mm(psT[p][:, 0:NWT], H[:, ct, i * 128:(i + 1) * 128], WT[:, ct, :], ct == 0, ct == CT - 1,
                             R=[tH, tWT], W=[tpT[p]])
                    k.v("dve", "tensor_copy", Vs[:, tt, 0:128], psT[p][:, 0:128], R=[tpT[p]], W=[tV[c]])
                    k.v("dve", "tensor_copy", Vw[:, tt, 0:128], psT[p][:, 128:256], R=[tpT[p]], W=[tV[c]])
                    k.act(gat[:, tt, :], psT[p][:, 256:268], AF.Sigmoid, R=[tpT[p]], W=[tV[c]])
                if bg is not None:
                    bg("proj")
        with k.scope():
            posT = k.sb("posT", [128, 64], BF16)
            tpos = Trk("posT")
            k.dma("pool", posT[:], posT_d, tpos, W=[tpos])
            W1 = [k.sb("cW1", [128, 32, 256], BF16) for _ in range(2)]
            W2 = [k.sb("cW2", [128, 2, 128], BF16) for _ in range(2)]
            tW = Trk("cW")
            for c in range(2):
                k.dma("pool", W1[c][:], cw1_d[c].rearrange("(cb p) n -> p cb n", p=128), tW, W=[tW], join=True)
                k.dma("pool", W2[c][:], cw2_d[c].rearrange("(a p) n -> p a n", p=128), tW, W=[tW], join=True)
            psH = [k.ps("psH", [128, 512]) for _ in range(2)]
            tpH = [Trk("pH0"), Trk("pH1")]
            psB = k.ps("psB", [128, 512])
            tpB = Trk("pB")
            psO = k.ps("psO", [128, 512])
            tpO = Trk("pO")
            hb = k.sb("hbias", [128, 4], F32)
            thb = Trk("hbias")
            xg = k.sb("xg", [128, 3, 256], F32)
            txg = Trk("xg")
            hid = [[k.sb("hid", [128, 256], BF16) for _ in range(2)] for _ in range(2)]
            thid = [[Trk(f"hid{a}{b}") for b in range(2)] for a in range(2)]
            nh = 0
            for c in range(2):
                rv = rawT[c][:, :].rearrange("p (n s) -> p n s", s=16)
                for hk in range(2):
                    for cb_ in range(32):
                        k.mm(psB[:, hk:hk + 1], W1[c][:, cb_, hk * 128:(hk + 1) * 128], posT[:, c * 32 + cb_:c * 32 + cb_ + 1],
                             cb_ == 0, cb_ == 31, R=[tW, tpos], W=[tpB])
                    k.v("dve", "tensor_copy", hb[:, c * 2 + hk:c * 2 + hk + 1], psB[:, hk:hk + 1], R=[tpB], W=[thb])
                    p = nh % 2
                    nh += 1
                    for cb_ in range(32):
                        k.mm(psH[p][:, 0:NCMP], W1[c][:, cb_, hk * 128:(hk + 1) * 128],
                             rv[:, cb_ // 16:cb_ // 16 + NCMP, cb_ % 16], cb_ == 0, cb_ == 31,
                             R=[tW] + traw, W=[tpH[p]])
                    x_ = xg[:, 0, 0:NCMP]
                    u_ = xg[:, 1, 0:NCMP]
                    s_ = xg[:, 2, 0:NCMP]
                    k.act(x_, psH[p][:, 0:NCMP], AF.Identity, bias=hb[:, c * 2 + hk:c * 2 + hk + 1], scale=1.0,
                          R=[tpH[p], thb], W=[txg])
                    k.v("dve", "tensor_tensor", u_, x_, x_, ALU.mult, R=[txg], W=[txg])
                    k.v("dve", "tensor_scalar", u_, u_, 0.044715, 1.0, ALU.mult, ALU.add, R=[txg], W=[txg])
                    k.v("dve", "tensor_tensor", u_, u_, x_, ALU.mult, R=[txg], W=[txg])
                    k.act(s_, u_, AF.Sigmoid, scale=2.0 * 0.7978845608028654, R=[txg], W=[txg])
                    k.v("dve", "tensor_tensor", hid[c][hk][:, 0:NCMP], x_, s_, ALU.mult, R=[txg], W=[thid[c][hk]])
            for hk in range(2):
                k.mm(psO[:, 0:NCMP], W2[0][:, hk, :], hid[0][hk][:, 0:NCMP], hk == 0, hk == 1,
                     R=[tW, thid[0][hk]], W=[tpO])
            k.v("dve", "tensor_copy", KcT[:, 0:NCMP], psO[:, 0:NCMP], R=[tpO], W=[tC])
            for nt, (n0, n1) in enumerate(((0, 128), (128, NCMP))):
                w = n1 - n0
                for hk in range(2):
                    k.mm(psO[0:w, 256:384], hid[1][hk][:, n0:n1], W2[1][:, hk, :], hk == 0, hk == 1,
                         R=[tW, thid[1][hk]], W=[tpO])
                k.v("dve", "tensor_copy", Vc[0:w, nt, 0:128], psO[0:w, 256:384], R=[tpO], W=[tC])
                k.v("dve", "tensor_copy", Vc[0:w, nt, 128:192], ovl[0:w, nt * 64:(nt + 1) * 64], R=[tcb], W=[tC])
                k.v("dve", "memset", Vc[0:w, nt, 192:193], 1.0, W=[tC])
    if dbg is not None:
        tdb = Trk("dbg")
        k.dma("sp", dbg["KcT"], KcT[:], tdb, R=[tC], W=[tdb], join=True)
        k.dma("sp", dbg["Vc"], Vc[:], tdb, R=[tC], W=[tdb], join=True)
        k.dma("sp", dbg["QT"], QT[:], tdb, R=tQ, W=[tdb], join=True)
        k.dma("sp", dbg["KsT"], KsT[:], tdb, R=tK, W=[tdb], join=True)
        k.dma("sp", dbg["Vs"], Vs[:], tdb, R=tV, W=[tdb], join=True)
        k.dma("sp", dbg["gat"], gat[:], tdb, R=tV, W=[tdb], join=True)

    with k.scope():
        NPS = 3
        psS = [k.ps("psS", [128, 512]) for _ in range(NPS)]
        tpS = [Trk(f"pS{i}") for i in range(NPS)]
        psO = [[k.ps("psOa", [128, 512]) for _ in range(2)] for _ in range(2)]
        tpO = [[Trk(f"pO{a}{b}") for b in range(2)] for a in range(2)]
        psX = k.ps("psX", [128, 512])
        tpX = Trk("pX")
        NPT = 4
        PT = [k.sb("PT", [128, 512], BF16) for _ in range(NPT)]
        tPT = [Trk(f"PT{i}") for i in range(NPT)]
        oacc = [k.sb("oacc", [128, 512], F32) for _ in range(3)]
        toacc = [Trk("oacc0"), Trk("oacc1"), Trk("oacc2")]
        imp = [k.sb("imp", [128, 64], F32) for _ in range(2)]
        timp = [Trk("imp0"), Trk("imp1")]
        sm = k.sb("asm", [128, 6, 16], F32)
        tsm = [Trk(f"asm{i}") for i in range(6)]
        selw = k.sb("selw", [128, 2, 4, 64], F32)
        tselw = [Trk("selw0"), Trk("selw1")]
        NSB = 3
        selb4 = [k.sb("selb4", [76, 512], BF16) for _ in range(NSB)]
        tsb4 = [Trk(f"selb4{i}") for i in range(NSB)]
        for i_ in range(NSB):
            k.dma("pool", selb4[i_][64:76, :], cb_d[0:12, CB_OFF["HI"][0]:CB_OFF["HI"][1]], tsb4[i_], W=[tsb4[i_]])
        tout = Trk("o_out")
        oTt = [k.sb("oTt", [128, 4, 128], BF16) for _ in range(2)]
        toTt = [Trk("oTt0"), Trk("oTt1")]
        st = dict(ns=0, npt=0, no=0, nsm=0)
        Qv = lambda qb: QT[:, qb, :, :].rearrange("p h q -> p (h q)")
        glist = []

        def add_group(qb, lhsK, tKk, extra, Vaug, vw, tVv, first, last, ob, after=None):
            slot = {}

            def scores():
                s_ = st["ns"] % NPS
                st["ns"] += 1
                k.mm(psS[s_][:, :], lhsK, Qv(qb), True, False, R=[tKk, tQ[qb // 2]], W=[tpS[s_]], inc=False)
                for i, (l_, r_, o_, Rr) in enumerate(extra):
                    out_ap = psS[s_][:, :] if o_ is None else psS[s_][:, o_[0]:o_[1]]
                    k.mm(out_ap, l_, r_, False, i == len(extra) - 1, R=Rr, W=[tpS[s_]], inc=(i == len(extra) - 1))
                pt = st["npt"] % NPT
                st["npt"] += 1
                slot["pt"] = pt
                k.act(PT[pt][:], psS[s_][:, :], AF.Exp, R=[tpS[s_]], W=[tPT[pt]])

            def pv():
                pt = slot["pt"]
                for hh in range(4):
                    bank = psO[ob][hh // 2]
                    c0 = (hh % 2) * 256
                    k.mm(bank[:, c0:c0 + vw], PT[pt][:, hh * 128:(hh + 1) * 128], Vaug, first and hh % 2 == 0, last,
                         R=[tPT[pt], tVv], W=[tpO[ob][hh // 2]], inc=last, skip_group_check=True)

            glist.append((scores, pv, after))

        def finish(qb, ob, br, first_branch, vw, with_imp=False):
            oa = oacc[qb % 3]
            to_ = toacc[qb % 3]
            i6 = st["nsm"] % 6
            st["nsm"] += 1
            S_ = tsm[i6]
            for hh in range(4):
                bank = psO[ob][hh // 2]
                tb = tpO[ob][hh // 2]
                c0 = (hh % 2) * 256
                rs = sm[:, i6, hh * 4:hh * 4 + 1]
                sc = sm[:, i6, hh * 4 + 1:hh * 4 + 2]
                k.v("dve", "tensor_scalar", rs, bank[:, c0 + vw - 1:c0 + vw], 1e-30, None, ALU.max, R=[tb, S_], W=[S_])
                k.v("dve", "reciprocal", rs, rs, R=[S_], W=[S_])
                k.v("dve", "tensor_tensor", sc, rs, gat[:, qb, hh * 3 + br:hh * 3 + br + 1], ALU.mult,
                    R=[S_, tV[qb // 2]], W=[S_])
                dst = oa[:, hh * 128:(hh + 1) * 128]
                if first_branch:
                    k.v("dve", "tensor_scalar", dst, bank[:, c0:c0 + 128], sc, None, ALU.mult, R=[tb, S_], W=[to_])
                else:
                    k.v("dve", "scalar_tensor_tensor", dst, bank[:, c0:c0 + 128], sc, dst, ALU.mult, ALU.add,
                        R=[tb, S_], W=[to_])
                if with_imp:
                    im = imp[qb % 2]
                    if hh == 0:
                        k.v("dve", "tensor_scalar", im[:], bank[:, c0 + 128:c0 + 192], rs, None, ALU.mult,
                            R=[tb, S_], W=[timp[qb % 2]])
                    else:
                        k.v("dve", "scalar_tensor_tensor", im[:], bank[:, c0 + 128:c0 + 192], rs, im[:], ALU.mult,
                            ALU.add, R=[tb, S_], W=[timp[qb % 2]])

        deferred = []

        def defer(n, fn):
            deferred.append([n, fn])

        def select_math(qb):
            w = qb % 2
            V_ = selw[:, w, 0, :]
            V2 = selw[:, w, 1, :]
            sl_ = selw[:, w, 2, :]
            m8 = selw[:, w, 3, 0:16]
            T_ = tselw[w]
            k.v("dve", "scalar_tensor_tensor", V_, imp[w][:], 1.0, cand[:, qb, :], ALU.add, ALU.mult,
                R=[timp[w], tcb], W=[T_])
            k.v("dve", "tensor_scalar", V_, V_, -1.0, None, ALU.add, R=[T_], W=[T_])
            k.v("dve", "max", m8[:, 0:8], V_, R=[T_], W=[T_])
            k.v("dve", "match_replace", V2, m8[:, 0:8], V_, -2.0, R=[T_], W=[T_])
            k.v("dve", "max", m8[:, 8:16], V2, R=[T_], W=[T_])
            k.v("dve", "tensor_scalar", sl_, V_, m8[:, 12:13], None, ALU.is_ge, R=[T_], W=[T_])
            k.v("dve", "tensor_tensor", sl_, sl_, cand[:, qb, :], ALU.mult, R=[T_, tcb], W=[T_])
            k.v("dve", "tensor_tensor", sl_, sl_, forced[:, qb, :], ALU.max, R=[T_, tcb], W=[T_])
            k.v("dve", "tensor_scalar", sl_, sl_, -NEG, NEG, ALU.mult, ALU.add, R=[T_], W=[T_])
            rlo = max(0, 62 - 2 * qb)
            jlo = rlo - 62 + 2 * qb
            k.v("dve", "memset", V2, 0.0, R=[T_], W=[T_])
            k.v("dve", "tensor_copy", V2[:, rlo:64], sl_[:, jlo:jlo + 64 - rlo], R=[T_], W=[T_])

            def tr_part():
                k.tr(psX[0:64, 0:128], V2, identf, R=[T_, tcf], W=[tpX])
                sb_ = qb % NSB
                for hh in range(4):
                    if hh % 2 == 0:
                        k.act(selb4[sb_][0:64, hh * 128:(hh + 1) * 128], psX[0:64, 0:128], AF.Copy, R=[tpX], W=[tsb4[sb_]])
                    else:
                        k.v("dve", "tensor_copy", selb4[sb_][0:64, hh * 128:(hh + 1) * 128], psX[0:64, 0:128],
                            R=[tpX], W=[tsb4[sb_]])
            defer(2, tr_part)

        def out_part(qb):
            if ob is None:
                k.dma("sp", o_d[qb * 128:(qb + 1) * 128, :], oacc[qb % 3][:], tout, R=[toacc[qb % 3]], W=[tout], join=True)
                return

            def tr_out():
                for hh in range(4):
                    k.tr(psX[:, hh * 128:(hh + 1) * 128], oacc[qb % 3][:, hh * 128:(hh + 1) * 128], identf,
                         R=[toacc[qb % 3], tcf], W=[tpX], inc=(hh == 3))
                k.act(oTt[qb % 2][:], psX[:, :].rearrange("p (h q) -> p h q", h=4), AF.Copy, R=[tpX], W=[toTt[qb % 2]])
                j_, tq = qb // 8, (qb % 8) * 128
                k.dma("sp", ob[j_][:, tq:tq + 128].rearrange("(h p) q -> p h q", p=128), oTt[qb % 2][:],
                      tob[j_], R=[toTt[qb % 2]], W=[tob[j_]], join=True)
                if after_qb is not None:
                    after_qb(qb)
            defer(2, tr_out)

        def cmp_branch(qb):
            ob_ = st["no"] % 2
            st["no"] += 1
            nts = [0] if qb < 16 else [0, 1]
            for ii, nt in enumerate(nts):
                dc = qb - 16 * nt
                extra = [(LTc[0:12, dc * 128:(dc + 1) * 128], HI[0:12, :], None, [tcb])]
                mi = None
                if nt == 0 and qb <= 16:
                    mi = qb
                elif nt == 1:
                    mi = 17 + (qb - 16)
                if mi is not None:
                    for hh in range(4):
                        extra.append((identb, CM[:, mi * 128:(mi + 1) * 128], (hh * 128, (hh + 1) * 128), [tcb]))
                aft = None
                if ii == len(nts) - 1:
                    def aft(qb=qb, ob_=ob_):
                        finish(qb, ob_, 0, True, 193, with_imp=True)
                        select_math(qb)
                add_group(qb, KcT[:, nt * 128:(nt + 1) * 128], tC, extra, Vc[:, nt, 0:193], 193, tC,
                          ii == 0, ii == len(nts) - 1, ob_, after=aft)

        def win_branch(qb):
            ob_ = st["no"] % 2
            st["no"] += 1
            kts = list(range(max(0, qb - 4), qb + 1))
            for ii, kt in enumerate(kts):
                d = qb - kt
                extra = [(LT[0:12, d * 128:(d + 1) * 128], HI[0:12, :], None, [tcb])]
                if kt == qb:
                    extra.append((identb, Mc4, None, [tcb]))
                if kt == qb - 4:
                    extra.append((identb, Mw4, None, [tcb]))
                aft = None
                if ii == len(kts) - 1:
                    def aft(qb=qb, ob_=ob_):
                        finish(qb, ob_, 2, False, 129)
                add_group(qb, KwT[:, kt * 128:(kt + 1) * 128], tK[kt // 2], extra, Vw[:, kt, 0:129], 129, tV[kt // 2],
                          ii == 0, ii == len(kts) - 1, ob_, after=aft)

        def sel_branch(qb):
            ob_ = st["no"] % 2
            st["no"] += 1
            sb_ = qb % NSB
            for kt in range(qb + 1):
                d = qb - kt
                extra = [(E_[0:76, d * 128:(d + 1) * 128], selb4[sb_][0:76, :], None, [tcb, tsb4[sb_]])]
                if kt == qb:
                    extra.append((identb, Mc4, None, [tcb]))
                aft = None
                if kt == qb:
                    def aft(qb=qb, ob_=ob_):
                        finish(qb, ob_, 1, False, 129)
                        out_part(qb)
                        if bg is not None:
                            bg("attn")
                add_group(qb, KsT[:, kt * 128:(kt + 1) * 128], tK[kt // 2], extra, Vs[:, kt, 0:129], 129, tV[kt // 2],
                          kt == 0, kt == qb, ob_, after=aft)

        cmp_branch(0)
        for qb in range(NQB):
            if qb + 1 < NQB:
                cmp_branch(qb + 1)
            win_branch(qb)
            sel_branch(qb)

        def tick():
            for d_ in list(deferred):
                d_[0] -= 1
                if d_[0] <= 0:
                    deferred.remove(d_)
                    d_[1]()

        prev = None
        for g_ in glist:
            g_[0]()
            if prev is not None:
                prev[1]()
                if prev[2] is not None:
                    prev[2]()
                tick()
            prev = g_
        prev[1]()
        if prev[2] is not None:
            prev[2]()
        while deferred:
            tick()


def build_p2(debug=False):
    nc = bass.Bass("TRN2", target_bir_lowering=False)
    k = K(nc)
    hT_d = nc.dram_tensor("hT", [D, SEQ], F32, kind="ExternalInput").ap()
    WF_d = nc.dram_tensor("WF", [D, NWF], F32, kind="ExternalInput").ap()
    WT_d = nc.dram_tensor("WT", [D, NWT], F32, kind="ExternalInput").ap()
    posT_d = nc.dram_tensor("posT", [128, 64], F32, kind="ExternalInput").ap()
    cw1_d = nc.dram_tensor("cw1", [2, 4096, 256], F32, kind="ExternalInput").ap()
    cw2_d = nc.dram_tensor("cw2", [2, 256, 128], F32, kind="ExternalInput").ap()
    cb_d = nc.dram_tensor("cb", [128, NCB], F32, kind="ExternalInput").ap()
    cf_d = nc.dram_tensor("cf", [128, NCF], F32, kind="ExternalInput").ap()
    o_d = nc.dram_tensor("o", [SEQ, 512], F32, kind="ExternalOutput").ap()
    dbg = None
    if debug:
        dbg = dict(KcT=nc.dram_tensor("d_KcT", [128, 256], BF16, kind="ExternalOutput").ap(),
                   Vc=nc.dram_tensor("d_Vc", [128, 2, 196], BF16, kind="ExternalOutput").ap(),
                   QT=nc.dram_tensor("d_QT", [128, NQB, 4, 128], BF16, kind="ExternalOutput").ap(),
                   KsT=nc.dram_tensor("d_KsT", [128, SEQ], BF16, kind="ExternalOutput").ap(),
                   Vs=nc.dram_tensor("d_Vs", [128, NQB, 132], BF16, kind="ExternalOutput").ap(),
                   gat=nc.dram_tensor("d_gat", [128, NQB, 12], F32, kind="ExternalOutput").ap())
    emit_attn(k, hT_d, WF_d, WT_d, posT_d, cw1_d, cw2_d, cb_d, cf_d, o_d, dbg=dbg)
    k.barrier()
    k.close()
    return nc


def p2_inputs(inp, h2T_b, g):
    cb, cf = attn_consts(g)
    d = dict(hT=h2T_b, cb=cb, cf=cf)
    d.update(attn_weights(inp, g))
    return d


def p3_inputs(inp, h2_c, oT_c):
    d = dict(cst=make_consts(), h_in=h2_c, oT=oT_c, w_o=np.ascontiguousarray(inp["nsa_w_o"][0]))
    d.update(moe_inputs(inp, 1))
    return d


_PROGS = {}


def _prog(name, fn):
    if name not in _PROGS:
        _PROGS[name] = fn()
    return _PROGS[name]


def _kernel3(**inputs):
    inp = {k_: np.asarray(v_) for k_, v_ in inputs.items()}
    cores = list(range(NCORE))
    r1 = run_bass_kernel_spmd(_prog("p1", build_p1), [p1_inputs(inp, c) for c in cores], core_ids=cores)
    h2 = np.stack([r1.results[c]["out"] for c in cores]).reshape(2, SEQ, D)
    h2T = [np.ascontiguousarray(h2[b].T) for b in range(2)]
    r2 = run_bass_kernel_spmd(_prog("p2", build_p2), [p2_inputs(inp, h2T[c // 4], c % 4) for c in cores], core_ids=cores)
    o = np.zeros((2, SEQ, D), np.float32)
    for c in cores:
        o[c // 4, :, (c % 4) * 512:(c % 4 + 1) * 512] = r2.results[c]["o"]
    ins3 = []
    for c in cores:
        b, j = divmod(c, 4)
        ins3.append(p3_inputs(inp, np.ascontiguousarray(h2[b, j * NT:(j + 1) * NT]),
                              np.ascontiguousarray(o[b, j * NT:(j + 1) * NT].T)))
    r3 = run_bass_kernel_spmd(_prog("p3", build_p3), ins3, core_ids=cores)
    out = np.stack([r3.results[c]["out"] for c in cores]).reshape(2, SEQ, D)
    return out.astype(np.float32)


RG4 = [[0, 1, 2, 3], [4, 5, 6, 7]]
NCONV0 = 8
NCONV1 = 16


def build_fused():
    nc = bass.Bass("TRN2", target_bir_lowering=False)
    k = K(nc)
    cst_d = nc.dram_tensor("cst", [128, NC32], F32, kind="ExternalInput").ap()
    x_d = nc.dram_tensor("x_tok", [NT, D], F32, kind="ExternalInput").ap()
    xT_d = nc.dram_tensor("xT", [D, NT + HALO], F32, kind="ExternalInput").ap()
    hm_d = nc.dram_tensor("hm", [1, 1], F32, kind="ExternalInput").ap()
    cpar_d = nc.dram_tensor("cpar", [128, NCPAR], F32, kind="ExternalInput").ap()
    w1_d = nc.dram_tensor("w_pw1", [D, 2 * D], F32, kind="ExternalInput").ap()
    w2_d = nc.dram_tensor("w_pw2", [D, D], F32, kind="ExternalInput").ap()
    md0 = moe_dram(nc, "0")
    WF_d = nc.dram_tensor("WF", [D, NWF], F32, kind="ExternalInput").ap()
    WT_d = nc.dram_tensor("WT", [D, NWT], F32, kind="ExternalInput").ap()
    posT_d = nc.dram_tensor("posT", [128, 64], F32, kind="ExternalInput").ap()
    cw1_d = nc.dram_tensor("cw1", [2, 4096, 256], F32, kind="ExternalInput").ap()
    cw2_d = nc.dram_tensor("cw2", [2, 256, 128], F32, kind="ExternalInput").ap()
    cb_d = nc.dram_tensor("cb", [128, NCB], F32, kind="ExternalInput").ap()
    cf_d = nc.dram_tensor("cf", [128, NCF], F32, kind="ExternalInput").ap()
    idx3_d = nc.dram_tensor("idx3", [128, CT], I32, kind="ExternalInput").ap()
    wo_d = nc.dram_tensor("w_o", [D, D], F32, kind="ExternalInput").ap()
    md1 = moe_dram(nc, "1")
    out_d = nc.dram_tensor("out", [NT, D], F32, kind="ExternalOutput").ap()
    hsp = nc.dram_tensor("hsp", [NT, D], F32).ap()
    xb1 = [nc.dram_tensor(f"xb1_{q}", [D, 256], BF16).ap() for q in range(4)]
    xg1 = [nc.dram_tensor(f"xg1_{q}", [4 * D, 256], BF16).ap() for q in range(4)]
    ob = [nc.dram_tensor(f"ob_{j}", [512, NT], BF16).ap() for j in range(4)]
    og = nc.dram_tensor("og", [4 * D, NT], BF16).ap()
    thsp = Trk("hsp")
    txb1 = [Trk(f"xb1{q}") for q in range(4)]
    txg1 = [Trk(f"xg1{q}") for q in range(4)]
    tob = [Trk(f"ob{j}") for j in range(4)]
    tog = [Trk(f"og{j}") for j in range(4)]

    wc0 = WConv(k, md0, "0", NCONV0)
    wc1 = WConv(k, md1, "1", NCONV1)

    cnt = dict(pw1=0, conv=0, attn=0, proj=0)

    def bg0(where):
        cnt[where] += 1
        if where == "conv" and cnt[where] % 2 == 0:
            wc0.pump(1)

    def bg1(where):
        cnt[where] += 1
        if where == "attn":
            wc1.pump(1)

    C = load_consts(k, cst_d)
    with k.scope():
        h_tok = k.sb("h_tok", [128, TT, D], F32)
        th = [Trk(f"h{i}") for i in range(TT)]
        load_tok(k, h_tok, th, x_d)
        emit_conv(k, C, xT_d, hm_d, cpar_d, w1_d, w2_d, h_tok, th, md0["ln"][0:1, :], md0["ln"][1:2, :], bg=bg0)
        wc0.flush()
        def cb_setup():
            ctx = dict(hTb=k.sb("hTb", [128, CT, NT], BF16), thTb=[Trk(f"hTb{i}") for i in range(TT)],
                       psA=[k.ps("psTr", [128, 512]) for _ in range(4)], tpsA=[Trk(f"psTr{i}") for i in range(4)], n=0)
            return ctx

        def cb_tile(ctx, tt):
            hvs = hsp.rearrange("(tt p) c -> p tt c", p=128)
            k.dma("sp", hvs[:, tt, :], h_tok[:, tt, :], thsp, R=[th[tt]], W=[thsp], join=True)
            hTb, psA, tpsA = ctx["hTb"], ctx["psA"], ctx["tpsA"]
            for q in range(4):
                p = ctx["n"] % 4
                ctx["n"] += 1
                for i in range(4):
                    ct = q * 4 + i
                    k.tr(psA[p][:, i * 128:(i + 1) * 128], h_tok[:, tt, ct * 128:(ct + 1) * 128], C["ident"],
                         R=[th[tt], C["t32"]], W=[tpsA[p]], inc=(i == 3))
                dst = hTb[:, q * 4:(q + 1) * 4, tt * 128:(tt + 1) * 128]
                src = psA[p][:, :].rearrange("p (a b) -> p a b", a=4)
                if q % 2 == 0:
                    k.act(dst, src, AF.Copy, R=[tpsA[p]], W=[ctx["thTb"][tt]])
                else:
                    k.v("dve", "tensor_copy", dst, src, R=[tpsA[p]], W=[ctx["thTb"][tt]])
            if tt % 2 == 1:
                q_ = tt // 2
                k.dma("sp", xb1[q_].rearrange("(ct p) t -> p ct t", p=128), hTb[:, :, q_ * 256:(q_ + 1) * 256],
                      txb1[q_], R=[ctx["thTb"][tt - 1], ctx["thTb"][tt]], W=[txb1[q_]])
                k.collective("AllGather", RG4, xb1[q_], xg1[q_], txg1[q_], R=[txb1[q_]], W=[txg1[q_]])

        emit_moe_d(k, C, h_tok, th, md0, wconv=wc0, tile_cb=(cb_setup, cb_tile))
    k.release(wc0.trk)

    def hsrc(c):
        r, q = divmod(c, 4)
        return xg1[q][r * D:(r + 1) * D, :].rearrange("(ct p) t -> p ct t", p=128), [txg1[q]]

    def after_qb(qb):
        if qb % 8 == 7:
            j_ = qb // 8
            k.collective("AllGather", RG4, ob[j_], og[j_ * D:(j_ + 1) * D, :], tog[j_], R=[tob[j_]], W=[tog[j_]])

    with k.scope():
        emit_attn(k, None, WF_d, WT_d, posT_d, cw1_d, cw2_d, cb_d, cf_d, None, hsrc=hsrc, ob=ob, tob=tob,
                  chunk_order=[r * 4 + q for q in range(4) for r in range(4)], after_qb=after_qb, bg=bg1)
        wc1.flush()

    with k.scope():
        h_tok = k.sb("h_tok", [128, TT, D], F32)
        th = [Trk(f"h{i}") for i in range(TT)]
        hv = hsp.rearrange("(tt p) c -> p tt c", p=128)
        for tt in range(TT):
            k.dma("sp", h_tok[:, tt, :], hv[:, tt, :], th[tt], R=[thsp], W=[th[tt]])
        with k.scope():
            idx3 = k.sb("idx3", [128, CT], I32)
            tidx = Trk("idx3")
            k.dma("sp", idx3[:], idx3_d, tidx, W=[tidx])
            aT = k.sb("aT", [128, CT, NT], BF16)
            taT = Trk("aT")
            tg_ = [Trk(f"aTg{i}") for i in range(CT)]
            for ct in range(CT):
                k.gather(aT[:, ct, :], og[:, :], idx3[:, ct:ct + 1], tg_[ct], R=tog + [tidx], W=[taT] if ct == 0 else [])
            taT.w = [m for t_ in tg_ for m in [(t_.dsem, t_.dcnt)]]
            emit_proj_res_ln(k, aT, taT, wo_d, h_tok, th, md1["ln"][0:1, :], md1["ln"][1:2, :])
        emit_moe_d(k, C, h_tok, th, md1, wconv=wc1)
        to = Trk("out")
        store_tok(k, h_tok, th, out_d, to)
    k.barrier()
    k.close()
    return nc


def fused_inputs(inp, c):
    b, j = divmod(c, 4)
    d = p1_inputs(inp, c)
    m0 = moe_inputs(inp, 0)
    for kk in list(m0):
        del d[kk]
    d.update(moe_inputs(inp, 0, "0"))
    g = j
    cb, cf = attn_consts(g)
    d.update(cb=cb, cf=cf)
    d.update(attn_weights(inp, g))
    ct = np.arange(CT)[None, :]
    p = np.arange(128)[:, None]
    d["idx3"] = np.ascontiguousarray((j * 2048 + ct * 128 + p).astype(np.int32))
    d["w_o"] = np.ascontiguousarray(inp["nsa_w_o"][0])
    d.update(moe_inputs(inp, 1, "1"))
    return d


def kernel_unfused(**inputs):
    return _kernel3(**inputs)


def kernel(**inputs):
    inp = {k_: np.asarray(v_) for k_, v_ in inputs.items()}
    cores = list(range(NCORE))
    r = run_bass_kernel_spmd(_prog("fused", build_fused), [fused_inputs(inp, c) for c in cores], core_ids=cores)
    out = np.stack([r.results[c]["out"] for c in cores]).reshape(2, SEQ, D)
    return out.astype(np.float32)
```
